# Optimizing a Trainium2 kernel written in Bass

```python
import math
import jax, jax.numpy as jnp
from jax import lax
import numpy as np

D_MODEL = 1024
BATCH = 8
SEQ = 4096
DEPTH = 2

CONV_CH = 512
CONV_WIDTH = 31
DSA_HEADS = 8
DSA_HEAD_DIM = 64
IDX_HEADS = 8
IDX_DIM = 64
DSA_TOPK_MAX = 256
Q_BLOCK = 128
DIL_GROUPS = ((128, 1), (512, 4), (2048, 16))
DIL_HEADS = 8
DIL_HEAD_DIM = 128
BAND_BLOCK = 128
NUM_BUCKETS = 32
MAX_DISTANCE = 2048
N_BIAS_HEADS = 8
D_FF_DENSE = 2816
N_EXPERTS = 8
TOP_K = 2
D_FF_EXPERT = 3584
LN_EPS = 1e-5
NEG = -1e30
N_EVEN = (DEPTH + 1) // 2
N_ODD = DEPTH // 2
DEEPNORM_ALPHA = (2 * DEPTH) ** 0.25
DEEPNORM_BETA = (8 * DEPTH) ** -0.25
EVEN_IN_PARTS = (CONV_CH, CONV_CH, DSA_HEADS * DSA_HEAD_DIM, DSA_HEADS * DSA_HEAD_DIM,
                 DSA_HEADS * DSA_HEAD_DIM, IDX_HEADS * IDX_DIM, IDX_DIM, IDX_HEADS)
EVEN_IN_WIDTH = sum(EVEN_IN_PARTS)
EVEN_SPLITS = [int(s) for s in np.cumsum(EVEN_IN_PARTS)[:-1]]
EVEN_OUT_WIDTH = CONV_CH + DSA_HEADS * DSA_HEAD_DIM
ODD_IN_WIDTH = len(DIL_GROUPS) * 3 * DIL_HEADS * DIL_HEAD_DIM
ODD_OUT_WIDTH = DIL_HEADS * DIL_HEAD_DIM

kernel_name = 'hybrid_conv_dsa_dilated_moe_deepnorm'


def layer_norm(x, g, b):
    xf = x.astype(jnp.float32)
    mu = xf.mean(-1, keepdims=True)
    var = jnp.square(xf - mu).mean(-1, keepdims=True)
    y = (xf - mu) * lax.rsqrt(var + LN_EPS) * g.astype(jnp.float32) + b.astype(jnp.float32)
    return y.astype(x.dtype)


def rel_bucket(dist):
    max_exact = NUM_BUCKETS // 2
    n = dist.astype(jnp.int32)
    nf = jnp.maximum(n, 1).astype(jnp.float32)
    large = max_exact + (jnp.log(nf / max_exact) / math.log(MAX_DISTANCE / max_exact)
                         * (NUM_BUCKETS - max_exact)).astype(jnp.int32)
    large = jnp.minimum(large, NUM_BUCKETS - 1)
    return jnp.where(n < max_exact, n, large)


def conformer_conv(a_val, a_gate, conv_w, conv_b, ln_g, ln_b):
    u = a_val * jax.nn.sigmoid(a_gate)
    y = lax.conv_general_dilated(u, conv_w.astype(u.dtype)[:, None, :], (1,), [(CONV_WIDTH - 1, 0)],
                                 dimension_numbers=('NWC', 'WIO', 'NWC'), feature_group_count=CONV_CH)
    y = layer_norm(y + conv_b, ln_g, ln_b)
    return jax.nn.silu(y)


def dsa_attention(q, k, v, q_idx, k_idx, w_idx, rel_bias):
    B, S, H, Dh = q.shape
    topk = min(DSA_TOPK_MAX, S // 4)
    nqb = S // Q_BLOCK
    key_pos = jnp.arange(S, dtype=jnp.int32)
    w_scaled = w_idx.astype(jnp.float32) * (IDX_HEADS ** -0.5 * IDX_DIM ** -0.5)

    def blocks(t):
        return t.reshape((B, nqb, Q_BLOCK) + t.shape[2:]).swapaxes(0, 1)

    starts = jnp.arange(nqb, dtype=jnp.int32) * Q_BLOCK

    def one_block(args):
        qb, qib, wb, start = args
        q_pos = start + jnp.arange(Q_BLOCK, dtype=jnp.int32)
        causal = key_pos[None, :] <= q_pos[:, None]
        dots = jnp.einsum('bqhe,bse->bqhs', qib, k_idx).astype(jnp.float32)
        score = jnp.einsum('bqh,bqhs->bqs', wb, jax.nn.relu(dots))
        score = jnp.where(causal[None], score, -jnp.inf)
        _, sel = lax.top_k(score, topk)
        k_sel = jax.vmap(lambda kk, ii: kk[ii])(k, sel)
        v_sel = jax.vmap(lambda vv, ii: vv[ii])(v, sel)
        dist = q_pos[None, :, None] - sel
        bias = rel_bias[rel_bucket(jnp.maximum(dist, 0))].astype(jnp.float32)
        logits = jnp.einsum('bqhd,bqkhd->bqhk', qb, k_sel).astype(jnp.float32) * (Dh ** -0.5)
        logits = logits + bias.swapaxes(-1, -2)
        logits = jnp.where((dist >= 0)[:, :, None, :], logits, NEG)
        p = jax.nn.softmax(logits, axis=-1)
        return jnp.einsum('bqhk,bqkhd->bqhd', p.astype(v.dtype), v_sel)

    out = lax.map(one_block, (blocks(q), blocks(q_idx), blocks(w_scaled), starts))
    return out.swapaxes(0, 1).reshape(B, S, H, Dh)


def dilated_branch(q, k, v, dil, span, rel_bias):
    B, S, H, Dh = q.shape
    L = S // dil
    nb = -(-L // BAND_BLOCK)
    Lp = nb * BAND_BLOCK

    def to_sub(t):
        t = t.reshape(B, L, dil, H, Dh).transpose(0, 2, 3, 1, 4)
        t = jnp.pad(t, ((0, 0), (0, 0), (0, 0), (0, Lp - L), (0, 0)))
        return t.reshape(B, dil, H, nb, BAND_BLOCK, Dh)

    def with_prev(t):
        prev = jnp.pad(t, ((0, 0), (0, 0), (0, 0), (1, 0), (0, 0), (0, 0)))[:, :, :, :nb]
        return jnp.concatenate([prev, t], axis=4)

    qs = to_sub(q)
    kk = with_prev(to_sub(k))
    vv = with_prev(to_sub(v))
    logits = jnp.einsum('bghnqe,bghnke->bghnqk', qs, kk).astype(jnp.float32) * (Dh ** -0.5)
    i = jnp.arange(BAND_BLOCK, dtype=jnp.int32)[:, None]
    j = jnp.arange(2 * BAND_BLOCK, dtype=jnp.int32)[None, :]
    dist = BAND_BLOCK + i - j
    bias = rel_bias[rel_bucket(jnp.maximum(dist, 0) * dil)]
    bias = jnp.moveaxis(bias, -1, 0).astype(jnp.float32)
    blk = jnp.arange(nb, dtype=jnp.int32)[:, None, None]
    valid = (dist >= 0) & (dist <= span) & ((blk > 0) | (j >= BAND_BLOCK))
    logits = jnp.where(valid, logits + bias[:, None], NEG)
    m = logits.max(-1, keepdims=True)
    p = jnp.exp(logits - m)
    s = p.sum(-1, keepdims=True)
    o = jnp.einsum('bghnqk,bghnke->bghnqe', p.astype(vv.dtype), vv).astype(jnp.float32) / s
    lse = (m + jnp.log(s))[..., 0]

    def from_sub(t):
        t = t.reshape((B, dil, H, Lp) + t.shape[5:])[:, :, :, :L]
        t = jnp.moveaxis(t, 3, 1)
        return t.reshape((B, S, H) + t.shape[4:])

    return from_sub(o), from_sub(lse)


def swiglu(x, wg, wu, wd):
    return (jax.nn.silu(x @ wg) * (x @ wu)) @ wd


def moe_swiglu(x, router, wg, wu, wd):
    B, S, D = x.shape
    xt = x.reshape(-1, D)
    logits = (xt @ router).astype(jnp.float32)
    top_val, top_idx = lax.top_k(logits, TOP_K)
    gate_w = jax.nn.softmax(top_val, axis=-1)
    gates = jnp.sum(jax.nn.one_hot(top_idx, N_EXPERTS, dtype=jnp.float32) * gate_w[..., None], axis=1)
    y = jnp.zeros(xt.shape, jnp.float32)
    for e in range(N_EXPERTS):
        h = jax.nn.silu(xt @ wg[e]) * (xt @ wu[e])
        y = y + gates[:, e:e + 1] * (h @ wd[e]).astype(jnp.float32)
    return y.astype(x.dtype).reshape(B, S, D)


def even_layer(x, rel_bias, w_in, conv_w, conv_b, conv_ln_g, conv_ln_b, w_out, ln1_g, ln1_b,
               ffn_wg, ffn_wu, ffn_wd, ln2_g, ln2_b):
    B, S, _ = x.shape
    h = x @ w_in
    a_val, a_gate, q, k, v, q_idx, k_idx, w_idx = jnp.split(h, EVEN_SPLITS, axis=-1)
    a_out = conformer_conv(a_val, a_gate, conv_w, conv_b, conv_ln_g, conv_ln_b)
    hd = (B, S, DSA_HEADS, DSA_HEAD_DIM)
    att = dsa_attention(q.reshape(hd), k.reshape(hd), v.reshape(hd),
                        q_idx.reshape(B, S, IDX_HEADS, IDX_DIM), k_idx, w_idx, rel_bias)
    mix = jnp.concatenate([a_out, att.reshape(B, S, -1).astype(a_out.dtype)], axis=-1) @ w_out
    x = layer_norm(DEEPNORM_ALPHA * x + mix, ln1_g, ln1_b)
    return layer_norm(DEEPNORM_ALPHA * x + swiglu(x, ffn_wg, ffn_wu, ffn_wd), ln2_g, ln2_b)


def odd_layer(x, rel_bias, w_in, w_out, ln1_g, ln1_b, router, moe_wg, moe_wu, moe_wd, ln2_g, ln2_b):
    B, S, _ = x.shape
    h = (x @ w_in).reshape(B, S, len(DIL_GROUPS), 3, DIL_HEADS, DIL_HEAD_DIM)
    outs, lses = [], []
    for g, (window, dil) in enumerate(DIL_GROUPS):
        o, lse = dilated_branch(h[:, :, g, 0], h[:, :, g, 1], h[:, :, g, 2], dil, window // dil, rel_bias)
        outs.append(o)
        lses.append(lse)
    wts = jax.nn.softmax(jnp.stack(lses), axis=0)
    o = jnp.sum(wts[..., None] * jnp.stack(outs), axis=0)
    mix = o.astype(x.dtype).reshape(B, S, ODD_OUT_WIDTH) @ w_out
    x = layer_norm(DEEPNORM_ALPHA * x + mix, ln1_g, ln1_b)
    return layer_norm(DEEPNORM_ALPHA * x + moe_swiglu(x, router, moe_wg, moe_wu, moe_wd), ln2_g, ln2_b)


def setup_inputs(seed: int = 0) -> dict:
    key = jax.random.key(seed)
    ks = jax.random.split(key, 32)
    nrm = lambda k, shape, scale: jax.random.normal(k, shape, jnp.float32) * scale
    D = D_MODEL
    return {
        'x': nrm(ks[0], (BATCH, SEQ, D), 1.0),
        'rel_bias': nrm(ks[1], (NUM_BUCKETS, N_BIAS_HEADS), 0.2),
        'even_w_in': nrm(ks[2], (N_EVEN, D, EVEN_IN_WIDTH), D ** -0.5),
        'even_conv_w': nrm(ks[3], (N_EVEN, CONV_WIDTH, CONV_CH), CONV_WIDTH ** -0.5),
        'even_conv_b': nrm(ks[4], (N_EVEN, CONV_CH), 0.02),
        'even_conv_ln_g': 1.0 + nrm(ks[5], (N_EVEN, CONV_CH), 0.02),
        'even_conv_ln_b': nrm(ks[6], (N_EVEN, CONV_CH), 0.02),
        'even_w_out': nrm(ks[7], (N_EVEN, EVEN_OUT_WIDTH, D), EVEN_OUT_WIDTH ** -0.5 * DEEPNORM_BETA),
        'even_ln1_g': 1.0 + nrm(ks[8], (N_EVEN, D), 0.02),
        'even_ln1_b': nrm(ks[9], (N_EVEN, D), 0.02),
        'even_ffn_wg': nrm(ks[10], (N_EVEN, D, D_FF_DENSE), D ** -0.5),
        'even_ffn_wu': nrm(ks[11], (N_EVEN, D, D_FF_DENSE), D ** -0.5),
        'even_ffn_wd': nrm(ks[12], (N_EVEN, D_FF_DENSE, D), D_FF_DENSE ** -0.5 * DEEPNORM_BETA),
        'even_ln2_g': 1.0 + nrm(ks[13], (N_EVEN, D), 0.02),
        'even_ln2_b': nrm(ks[14], (N_EVEN, D), 0.02),
        'odd_w_in': nrm(ks[15], (N_ODD, D, ODD_IN_WIDTH), D ** -0.5),
        'odd_w_out': nrm(ks[16], (N_ODD, ODD_OUT_WIDTH, D), ODD_OUT_WIDTH ** -0.5 * DEEPNORM_BETA),
        'odd_ln1_g': 1.0 + nrm(ks[17], (N_ODD, D), 0.02),
        'odd_ln1_b': nrm(ks[18], (N_ODD, D), 0.02),
        'odd_router': nrm(ks[19], (N_ODD, D, N_EXPERTS), D ** -0.5),
        'odd_moe_wg': nrm(ks[20], (N_ODD, N_EXPERTS, D, D_FF_EXPERT), D ** -0.5),
        'odd_moe_wu': nrm(ks[21], (N_ODD, N_EXPERTS, D, D_FF_EXPERT), D ** -0.5),
        'odd_moe_wd': nrm(ks[22], (N_ODD, N_EXPERTS, D_FF_EXPERT, D), D_FF_EXPERT ** -0.5 * DEEPNORM_BETA),
        'odd_ln2_g': 1.0 + nrm(ks[23], (N_ODD, D), 0.02),
        'odd_ln2_b': nrm(ks[24], (N_ODD, D), 0.02),
    }


def reference(x, rel_bias, even_w_in, even_conv_w, even_conv_b, even_conv_ln_g, even_conv_ln_b,
              even_w_out, even_ln1_g, even_ln1_b, even_ffn_wg, even_ffn_wu, even_ffn_wd,
              even_ln2_g, even_ln2_b, odd_w_in, odd_w_out, odd_ln1_g, odd_ln1_b, odd_router,
              odd_moe_wg, odd_moe_wu, odd_moe_wd, odd_ln2_g, odd_ln2_b):
    for layer in range(DEPTH):
        i = layer // 2
        if layer % 2 == 0:
            x = even_layer(x, rel_bias, even_w_in[i], even_conv_w[i], even_conv_b[i], even_conv_ln_g[i],
                           even_conv_ln_b[i], even_w_out[i], even_ln1_g[i], even_ln1_b[i],
                           even_ffn_wg[i], even_ffn_wu[i], even_ffn_wd[i], even_ln2_g[i], even_ln2_b[i])
        else:
            x = odd_layer(x, rel_bias, odd_w_in[i], odd_w_out[i], odd_ln1_g[i], odd_ln1_b[i], odd_router[i],
                          odd_moe_wg[i], odd_moe_wu[i], odd_moe_wd[i], odd_ln2_g[i], odd_ln2_b[i])
    return x
```

```python
import math
import bisect
from contextlib import ExitStack

import numpy as np
import concourse.bass as bass
import concourse.mybir as mybir
from concourse.bass_utils import run_bass_kernel_spmd

F32 = mybir.dt.float32
BF16 = mybir.dt.bfloat16
AF = mybir.ActivationFunctionType
ALU = mybir.AluOpType
AX = mybir.AxisListType

S = 4096
D = 1024
NT = S // 128
ALPHA = 4 ** 0.25
EPS = 1e-5
NEGM = -30000.0
NIT = 14
EPOCH = 4000
KDMA = 12


class Buf:
    __slots__ = ("name", "w", "rs")

    def __init__(self, name=""):
        self.name = name
        self.w = None
        self.rs = []


class Stream:
    def __init__(self, name, issuer, is_dma):
        self.name = name
        self.issuer = issuer
        self.is_dma = is_dma
        self.ops = []
        self.n_total = 0
        self.inc_idx = []
        self.n_inc = 0
        self.sems = []


class Op:
    __slots__ = ("stream", "fn", "deps", "idx", "inc")


class _Rec:
    __slots__ = ("call",)

    def __init__(self):
        self.call = None

    def __getattr__(self, name):
        def f(*a, **k):
            self.call = (name, a, k)
        return f


class Prog:
    def __init__(self, nc):
        self.nc = nc
        self.es = ExitStack()
        self.eng = {"pe": nc.tensor, "act": nc.scalar, "dve": nc.vector, "pool": nc.gpsimd, "sp": nc.sync}
        self.streams = {}
        for n in ("pe", "act", "dve", "pool"):
            self.streams[n] = Stream(n, n, False)
        for n, iss in (("ld", "sp"), ("st", "sp"), ("gld", "pool"), ("gst", "pool")):
            self.streams[n] = Stream(n, iss, True)
        self.order = []
        self.waited = {}

    def op(self, sname, fn, r=(), w=(), lazy=False):
        st = self.streams[sname]
        o = Op()
        o.stream = st
        if lazy:
            o.fn = fn
        else:
            rec = _Rec()
            fn(rec)
            assert rec.call is not None
            o.fn = rec.call
        o.idx = st.n_total
        o.inc = False
        st.n_total += 1
        deps = set()
        me = (sname, o.idx)
        for b in r:
            if b.w is not None:
                deps.add(b.w + ("raw",))
        for b in w:
            if b.w is not None:
                deps.add(b.w + ("waw",))
            for rd in b.rs:
                deps.add(rd + ("war",))
        fd = set()
        best = {}
        for (s, i, kind) in deps:
            if s == sname:
                if sname == "pe":
                    continue
                if kind == "waw":
                    continue
                if not st.is_dma and kind != "raw":
                    continue
            if self.streams[s].is_dma:
                fd.add((s, i))
            else:
                if s not in best or best[s] < i:
                    best[s] = i
        for s, i in best.items():
            fd.add((s, i))
        o.deps = fd
        for b in r:
            b.rs.append(me)
        for b in w:
            b.w = me
            b.rs = []
        st.ops.append(o)
        self.order.append(o)
        return o

    def _sem_for(self, st, ordinal):
        ep = (ordinal - 1) // EPOCH
        while len(st.sems) <= ep:
            st.sems.append(self.es.enter_context(self.nc.semaphore("s_%s_%d" % (st.name, len(st.sems)))))
        return st.sems[ep], (ordinal - 1) % EPOCH + 1

    def _dma_sem(self, st, idx):
        k = idx % KDMA
        while len(st.sems) <= k:
            st.sems.append(self.es.enter_context(self.nc.semaphore("d_%s_%d" % (st.name, len(st.sems)))))
        return k, st.sems[k], 16 * (idx // KDMA + 1)

    def _wait_dma(self, issuer, st, idx):
        k, sem, val = self._dma_sem(st, idx)
        key = (issuer, st.name, k)
        if self.waited.get(key, 0) >= val:
            return
        self.waited[key] = val
        self.eng[issuer].wait_ge(sem, val)

    def _wait(self, issuer, st, ordinal):
        sem, val = self._sem_for(st, ordinal)
        key = (issuer, st.name)
        if self.waited.get(key, 0) >= ordinal:
            return
        self.waited[key] = ordinal
        self.eng[issuer].wait_ge(sem, val)

    def flush(self, barrier=True):
        need = {}
        for o in self.order:
            for s, i in o.deps:
                need.setdefault(s, set()).add(i)
        for sname, st in self.streams.items():
            if not st.ops or st.is_dma:
                continue
            tg = need.get(sname, set())
            for o in st.ops:
                if o.idx in tg:
                    o.inc = True
            st.ops[-1].inc = True
        ordinal_of = {}
        for sname, st in self.streams.items():
            if st.is_dma:
                continue
            for o in st.ops:
                if o.inc:
                    st.n_inc += 1
                    st.inc_idx.append(o.idx)
                    ordinal_of[(sname, o.idx)] = st.n_inc
        for o in self.order:
            st = o.stream
            issuer = st.issuer
            for s, i in sorted(o.deps):
                ps = self.streams[s]
                if ps.is_dma:
                    self._wait_dma(issuer, ps, i)
                else:
                    k = bisect.bisect_left(ps.inc_idx, i)
                    assert k < len(ps.inc_idx), (s, i)
                    self._wait(issuer, ps, k + 1)
            if st.is_dma and o.idx >= KDMA:
                self._wait_dma(issuer, st, o.idx - KDMA)
            if callable(o.fn):
                inst = o.fn(self.eng[issuer])
            else:
                nm, a_, k_ = o.fn
                inst = getattr(self.eng[issuer], nm)(*a_, **k_)
            if st.is_dma:
                k, sem, val = self._dma_sem(st, o.idx)
                inst.then_inc(sem, 16)
            elif o.inc:
                sem, val = self._sem_for(st, ordinal_of[(st.name, o.idx)])
                inst.then_inc(sem, 1)
        self.order = []
        for st in self.streams.values():
            st.ops = []
        if barrier:
            self.barrier()

    def barrier(self, issuers=("pe", "act", "dve", "pool", "sp")):
        for iss in issuers:
            for st in self.streams.values():
                if st.is_dma:
                    for i in range(max(0, st.n_total - KDMA), st.n_total):
                        self._wait_dma(iss, st, i)
                elif st.n_inc > 0:
                    self._wait(iss, st, st.n_inc)


class T:
    __slots__ = ("t", "b")

    def __init__(self, t, name=""):
        self.t = t
        self.b = Buf(name)


class Phase:
    cnt = 0

    def __init__(self, P):
        self.P = P
        self.nc = P.nc
        self.es = ExitStack()
        self.n = 0

    def sb(self, shape, dt, name="t"):
        Phase.cnt += 1
        nm = "%s_%d" % (name, Phase.cnt)
        return T(self.es.enter_context(self.nc.sbuf_tensor(nm, list(shape), dt)), nm)

    def ps(self, shape, dt, name="p"):
        Phase.cnt += 1
        nm = "%s_%d" % (name, Phase.cnt)
        return T(self.es.enter_context(self.nc.psum_tensor(nm, list(shape), dt)), nm)

    def close(self):
        self.P.flush(barrier=True)
        self.es.close()


def _bucket(n):
    n = np.asarray(n).astype(np.int64)
    nf = np.maximum(n, 1).astype(np.float32)
    large = 16 + (np.log(nf / np.float32(16)) / np.float32(math.log(2048 / 16)) * 16).astype(np.int32)
    large = np.minimum(large, 31)
    return np.where(n < 16, n, large)


L0 = 2688
L1 = 384


def host_consts():
    c = {}
    c["ident"] = np.eye(128, dtype=np.float32)
    c["anti"] = np.eye(128, dtype=np.float32)[::-1].copy()
    qi = np.arange(128)[:, None]
    ki = np.arange(128)[None, :]
    c["causneg"] = np.where(ki <= qi, 0.0, -1e30).astype(np.float32)
    n = np.arange(L0)
    b0 = _bucket(np.maximum(n - 511, 0))
    oh0 = np.zeros((32, L0), np.float32)
    oh0[b0, n] = 1.0
    c["oh0"] = oh0
    oh1 = np.zeros((3, 33, L1), np.float32)
    n1 = np.arange(L1)
    dist = n1 - 127
    valid = (dist >= 0) & (dist <= 128)
    for g, r in enumerate((1, 4, 16)):
        bb = _bucket(np.maximum(dist, 0) * r)
        oh1[g, bb[valid], n1[valid]] = 1.0
        oh1[g, 32, :] = np.where(valid, 0.0, NEGM)
    c["oh1"] = oh1
    c["pow2"] = np.tile((0.5 ** np.arange(NIT + 2)).astype(np.float32)[None, :], (128, 1))
    pp = np.arange(128)
    c["ustr"] = (pp[:, None] < pp[None, :]).astype(np.float32)
    c["thr8"] = np.tile((512.0 * np.arange(8)).astype(np.float32)[None, :], (128, 1))
    c["iota23"] = np.tile(np.arange(23, dtype=np.float32)[None, :], (128, 1))
    c["cwg"] = (np.arange(7)[None, :] * 128 + pp[:, None]).astype(np.float32)
    c["cwd"] = (np.arange(8)[None, :] * 128 + pp[:, None]).astype(np.float32)
    return c


def build(stop_after=None, debug=(), hcfg=(4, 8), skip=(), feed=()):
    nc = bass.Bass("TRN2", target_bir_lowering=False)
    P = Prog(nc)

    def din(name, shape, dt=F32):
        return nc.dram_tensor(name, list(shape), dt, kind="ExternalInput")

    def dscr(name, shape, dt):
        kind = "ExternalOutput" if name in debug else ("ExternalInput" if name in feed else "Internal")
        return nc.dram_tensor(name, list(shape), dt, kind=kind)

    x_d = din("x", [S, D])
    rb_d = din("rel_bias", [32, 8])
    ewin = din("even_w_in", [D, 3144])
    ecw = din("even_conv_w", [31, 512])
    ecb = din("even_conv_b", [1, 512])
    ecg = din("even_conv_ln_g", [1, 512])
    ecbb = din("even_conv_ln_b", [1, 512])
    ewout = din("even_w_out", [D, D])
    eln1g = din("even_ln1_g", [1, D])
    eln1b = din("even_ln1_b", [1, D])
    ewg = din("even_ffn_wg", [D, 2816])
    ewu = din("even_ffn_wu", [D, 2816])
    ewd = din("even_ffn_wd", [2816, D])
    eln2g = din("even_ln2_g", [1, D])
    eln2b = din("even_ln2_b", [1, D])
    owin = din("odd_w_in", [D, 9216])
    owout = din("odd_w_out", [D, D])
    oln1g = din("odd_ln1_g", [1, D])
    oln1b = din("odd_ln1_b", [1, D])
    orouter = din("odd_router", [D, 8])
    omwg = din("odd_moe_wg", [8, D, 3584])
    omwu = din("odd_moe_wu", [8, D, 3584])
    omwd = din("odd_moe_wd", [8, 3584, D])
    oln2g = din("odd_ln2_g", [1, D])
    oln2b = din("odd_ln2_b", [1, D])
    c_ident = din("c_ident", [128, 128])
    c_anti = din("c_anti", [128, 128])
    c_caus = din("c_causneg", [128, 128])
    c_oh0 = din("c_oh0", [32, L0])
    c_oh1 = din("c_oh1", [3, 33, L1])
    c_pow2 = din("c_pow2", [128, NIT + 2])
    c_ustr = din("c_ustr", [128, 128])
    c_thr8 = din("c_thr8", [128, 8])
    c_iota23 = din("c_iota23", [128, 23])
    c_cwg = din("c_cwg", [128, 7])
    c_cwd = din("c_cwd", [128, 8])

    out_d = nc.dram_tensor("out", [S, D], F32, kind="ExternalOutput")

    qT_d = dscr("qT", [512, S], BF16)
    kT_d = dscr("kT", [512, S], BF16)
    qiT_d = dscr("qiT", [512, S], BF16)
    kiT_d = dscr("kiT", [128, S], BF16)
    v_d = dscr("vaug", [S, 520], BF16)
    w_d = dscr("widx", [S, 8], F32)
    catT_d = dscr("catT", [D, S], BF16)
    mask_d = dscr("maskneg", [S, S], BF16)
    f0_d = dscr("f0tab", [8, L0], BF16)
    f1_d = dscr("f1tab", [3, 8, L1], BF16)
    x1_d = dscr("x1", [S, D], F32)
    x2_d = dscr("x2", [S, D], F32)
    u_d = dscr("ug", [3, S, 8, 132], F32)
    x3_d = dscr("x3", [S, D], F32)
    wgs_d = dscr("wgs", [56 * 128, 4096], BF16)
    wus_d = dscr("wus", [56 * 128, 4096], BF16)
    wds_d = dscr("wds", [64 * 128, 3584], BF16)
    xs_d = dscr("xs", [23 * 512, D], BF16)
    ys_d = dscr("ys", [23 * 512, D], F32)

    G = Phase(P)
    ident_f = G.sb([128, 128], F32, "identf")
    ident_b = G.sb([128, 128], BF16, "identb")
    anti_b = G.sb([128, 128], BF16, "antib")
    ones_f = G.sb([128, 128], F32, "onesf")
    P.op("ld", lambda e: e.dma_start(out=ident_f.t[:], in_=c_ident.ap()), w=[ident_f.b])
    P.op("gld", lambda e: e.dma_start(out=ident_b.t[:], in_=c_ident.ap()), w=[ident_b.b])
    P.op("gld", lambda e: e.dma_start(out=anti_b.t[:], in_=c_anti.ap()), w=[anti_b.b])
    P.op("dve", lambda e: e.memset(ones_f.t[:], 1.0), w=[ones_f.b])
    GA = G.sb([128, NT], F32, "GA")
    GB = G.sb([128, NT], F32, "GB")
    SLI = G.sb([128, 2, NT], mybir.dt.int32, "SLI")
    IWG = G.sb([128, 23, 7], mybir.dt.int32, "IWG")
    IWD = G.sb([128, 23, 8], mybir.dt.int32, "IWD")
    zer_b = G.sb([128, 512], BF16, "zerb")
    P.op("dve", lambda e: e.memset(zer_b.t[:], 0.0), w=[zer_b.b])

    def evac(i, fn_act, fn_dve):
        return ("act", fn_act) if i % 2 == 0 else ("dve", fn_dve)

    def load_tok_T(ph, src_rows_ap_fn, ntiles, xtok, xT, pts, tag, f32_copy=None, queue="gld"):
        for j in range(ntiles):
            P.op(queue, lambda e, j=j: e.dma_start(out=xtok[j].t[:], in_=src_rows_ap_fn(j)), w=[xtok[j].b])
        k = 0
        for j0 in range(0, ntiles, 4):
            nj = min(4, ntiles - j0)
            for c in range(8):
                pt = pts[k % len(pts)]
                k += 1
                for jj in range(nj):
                    P.op("pe", lambda e, pt=pt, jj=jj, c=c, j0=j0: e.transpose(
                        out=pt.t[:, jj * 128:(jj + 1) * 128], in_=xtok[j0 + jj].t[:, c * 128:(c + 1) * 128],
                        identity=ident_b.t[:]), r=[xtok[j0 + jj].b, ident_b.b], w=[pt.b])
                if k % 2 == 0:
                    P.op("act", lambda e, pt=pt, c=c, j0=j0, nj=nj: e.copy(
                        out=xT.t[:, c, j0 * 128:(j0 + nj) * 128], in_=pt.t[:, 0:nj * 128]), r=[pt.b], w=[xT.b])
                else:
                    P.op("dve", lambda e, pt=pt, c=c, j0=j0, nj=nj: e.tensor_copy(
                        out=xT.t[:, c, j0 * 128:(j0 + nj) * 128], in_=pt.t[:, 0:nj * 128]), r=[pt.b], w=[xT.b])

    def layernorm_rows(ph, z, gB, bB, outt, st6, mv, rstd, nb):
        for hh in range(2):
            P.op("dve", lambda e, hh=hh: e.bn_stats(out=st6.t[:, hh, :], in_=z.t[:, hh * 512:(hh + 1) * 512]),
                 r=[z.b], w=[st6.b])
        P.op("dve", lambda e: e.bn_aggr(out=mv.t[:], in_=st6.t[:]), r=[st6.b], w=[mv.b])
        P.op("dve", lambda e: e.tensor_scalar(out=rstd.t[:], in0=mv.t[:, 1:2], scalar1=EPS, scalar2=None,
                                              op0=ALU.add), r=[mv.b], w=[rstd.b])
        P.op("act", lambda e: e.activation(out=rstd.t[:], in_=rstd.t[:], func=AF.Sqrt), r=[rstd.b], w=[rstd.b])
        P.op("dve", lambda e: e.reciprocal(out=rstd.t[:], in_=rstd.t[:]), r=[rstd.b], w=[rstd.b])
        P.op("dve", lambda e: e.tensor_scalar(out=z.t[:], in0=z.t[:], scalar1=mv.t[:, 0:1], scalar2=rstd.t[:, 0:1],
                                              op0=ALU.subtract, op1=ALU.mult), r=[z.b, mv.b, rstd.b], w=[z.b])
        P.op("dve", lambda e: e.tensor_tensor(out=z.t[:], in0=z.t[:], in1=gB.t[:], op=ALU.mult),
             r=[z.b, gB.b], w=[z.b])
        P.op("dve", lambda e: e.tensor_tensor(out=outt.t[:], in0=z.t[:], in1=bB.t[:], op=ALU.add),
             r=[z.b, bB.b], w=[outt.b])

    def bcast_row(dr):
        a = dr.ap()
        n = a.shape[-1]
        return bass.AP(tensor=a.tensor, offset=0, ap=[[0, 128], [1, n]])

    def phase_tables():
        ph = Phase(P)
        rb33 = ph.sb([33, 8], BF16, "rb33")
        rb33f = ph.sb([33, 8], F32, "rb33f")
        oh0 = ph.sb([32, L0], BF16, "oh0")
        oh1 = ph.sb([33, 3, L1], BF16, "oh1")
        f0 = ph.sb([8, L0], BF16, "f0")
        f1 = ph.sb([8, 3, L1], BF16, "f1")
        pp = [ph.ps([128, 512], F32, "pp") for _ in range(2)]
        P.op("dve", lambda e: e.memset(rb33f.t[:], 1.0), w=[rb33f.b])
        P.op("ld", lambda e: e.dma_start(out=rb33f.t[0:32, :], in_=rb_d.ap()), r=[], w=[rb33f.b])
        P.op("dve", lambda e: e.tensor_copy(out=rb33.t[:], in_=rb33f.t[:]), r=[rb33f.b], w=[rb33.b])
        P.op("gld", lambda e: e.dma_start(out=oh0.t[:], in_=c_oh0.ap()), w=[oh0.b])
        P.op("gld", lambda e: e.dma_start(out=oh1.t[:], in_=c_oh1.ap().rearrange("g k n -> k g n")), w=[oh1.b])
        k = 0
        for c0 in range(0, L0, 512):
            n = min(512, L0 - c0)
            p = pp[k % 2]
            k += 1
            P.op("pe", lambda e, p=p, c0=c0, n=n: e.matmul(p.t[0:8, 0:n], rb33.t[0:32, :], oh0.t[:, c0:c0 + n],
                                                          start=True, stop=True), r=[rb33.b, oh0.b], w=[p.b])
            P.op("dve", lambda e, p=p, c0=c0, n=n: e.tensor_copy(out=f0.t[:, c0:c0 + n], in_=p.t[0:8, 0:n]),
                 r=[p.b], w=[f0.b])
        for g in range(3):
            p = pp[k % 2]
            k += 1
            P.op("pe", lambda e, p=p, g=g: e.matmul(p.t[0:8, 0:L1], rb33.t[:, :], oh1.t[:, g, :],
                                                    start=True, stop=True), r=[rb33.b, oh1.b], w=[p.b])
            P.op("dve", lambda e, p=p, g=g: e.tensor_copy(out=f1.t[:, g, :], in_=p.t[0:8, 0:L1]),
                 r=[p.b], w=[f1.b])
        P.op("st", lambda e: e.dma_start(out=f0_d.ap(), in_=f0.t[:]), r=[f0.b])
        P.op("st", lambda e: e.dma_start(out=f1_d.ap().rearrange("g h n -> h g n"), in_=f1.t[:]), r=[f1.b])
        ph.close()

    def phase_A():
        ph = Phase(P)
        win = ph.sb([128, 8, 3144], BF16, "win")
        wki2 = ph.sb([128, 8, 128], BF16, "wki2")
        wv_ap = ewin.ap().rearrange("(c p) n -> p c n", p=128)
        for c in range(8):
            P.op("gld", lambda e, c=c: e.dma_start(out=win.t[:, c, :], in_=wv_ap[:, c, :]), w=[win.b])
        P.op("gld", lambda e: e.dma_start(out=wki2.t[:, :, 0:64], in_=wv_ap[:, :, 3072:3136]), w=[wki2.b])
        P.op("gld", lambda e: e.dma_start(out=wki2.t[:, :, 64:128], in_=wv_ap[:, :, 3072:3136]), w=[wki2.b])
        cw_sb = ph.sb([34, 512], F32, "cwsb")
        cwT = ph.sb([128, 4, 34], F32, "cwT")
        P.op("ld", lambda e: e.dma_start(out=cw_sb.t[0:31, :], in_=ecw.ap()), w=[cw_sb.b])
        P.op("ld", lambda e: e.dma_start(out=cw_sb.t[31:32, :], in_=ecb.ap()), w=[cw_sb.b])
        P.op("ld", lambda e: e.dma_start(out=cw_sb.t[32:33, :], in_=ecg.ap()), w=[cw_sb.b])
        P.op("ld", lambda e: e.dma_start(out=cw_sb.t[33:34, :], in_=ecbb.ap()), w=[cw_sb.b])
        pm = [ph.ps([128, 512], F32, "pm") for _ in range(4)]
        pst = [ph.ps([128, 512], F32, "pst") for _ in range(2)]
        ptr = [ph.ps([128, 1024], BF16, "ptr") for _ in range(2)]
        for c in range(4):
            P.op("pe", lambda e, c=c: e.transpose(out=pm[0].t[:, c * 34:(c + 1) * 34], in_=cw_sb.t[0:34, c * 128:(c + 1) * 128],
                                                  identity=ident_f.t[0:34, 0:34]), r=[cw_sb.b, ident_f.b], w=[pm[0].b])
        P.op("dve", lambda e: e.tensor_copy(out=cwT.t[:].rearrange("p c j -> p (c j)"), in_=pm[0].t[:, 0:136]),
             r=[pm[0].b], w=[cwT.b])
        diag = ph.sb([128, 4, 31, 128], BF16, "diag")
        for c in range(4):
            for j in range(31):
                eng = "dve" if (c * 31 + j) % 2 == 0 else "pool"
                P.op(eng, lambda e, c=c, j=j: e.tensor_scalar(out=diag.t[:, c, j, :], in0=ident_f.t[:],
                                                              scalar1=cwT.t[:, c, j:j + 1], scalar2=None, op0=ALU.mult),
                     r=[ident_f.b, cwT.b], w=[diag.b])
        xtok = [ph.sb([128, 1024], BF16, "xtok") for _ in range(4)]
        xT = [ph.sb([128, 8, 512], BF16, "xT") for _ in range(2)]
        ub = [ph.sb([128, 4, 542], BF16, "ub") for _ in range(2)]
        stg = ph.sb([128, 13, 512], BF16, "stg")
        aout = ph.sb([128, 4, 512], BF16, "aout")
        sg = [ph.sb([128, 512], F32, "sg") for _ in range(2)]
        yv = ph.sb([128, 4, 512], F32, "yv")
        ysq = ph.sb([128, 4, 512], F32, "ysq")
        mean = ph.sb([128, 512], F32, "mean")
        rstd = ph.sb([128, 512], F32, "rstd")
        tmp = ph.sb([128, 512], F32, "tmp")
        vst = ph.sb([128, 4, 8, 65], BF16, "vst")
        wst = ph.sb([128, 4, 8], F32, "wst")
        P.op("dve", lambda e: e.memset(ub[0].t[:, :, 0:30], 0.0), w=[ub[0].b])
        P.op("dve", lambda e: e.memset(vst.t[:], 1.0), w=[vst.b])
        xrows = x_d.ap()
        qTv = qT_d.ap().rearrange("(c p) t -> p c t", p=128)
        kTv = kT_d.ap().rearrange("(c p) t -> p c t", p=128)
        qiTv = qiT_d.ap().rearrange("(c p) t -> p c t", p=128)
        catTv = catT_d.ap().rearrange("(c p) t -> p c t", p=128)
        WSCALE = float(8 ** -0.5 * 64 ** -0.5)
        pmi = 0
        for sbk in range(8):
            t0 = sbk * 512
            xt = xT[sbk % 2]
            u_cur = ub[sbk % 2]
            u_prev = ub[(sbk + 1) % 2]
            load_tok_T(ph, lambda j, t0=t0: xrows[t0 + j * 128:t0 + (j + 1) * 128, :], 4, xtok, xt, ptr, "x")
            if sbk > 0:
                P.op("dve", lambda e, u_cur=u_cur, u_prev=u_prev: e.tensor_copy(out=u_cur.t[:, :, 0:30], in_=u_prev.t[:, :, 512:542]),
                     r=[u_prev.b], w=[u_cur.b])

            def proj_fm(col0, wt, pmt):
                for c in range(8):
                    P.op("pe", lambda e, c=c: e.matmul(pmt.t[:], wt.t[:, c, col0:col0 + 128], xt.t[:, c, :],
                                                       start=(c == 0), stop=(c == 7)), r=[wt.b, xt.b], w=[pmt.b])
            for c in range(4):
                pg = pm[pmi % 4]; pmi += 1
                pv = pm[pmi % 4]; pmi += 1
                sgt = sg[c % 2]
                proj_fm(512 + c * 128, win, pg)
                P.op("act", lambda e, pg=pg, sgt=sgt: e.activation(out=sgt.t[:], in_=pg.t[:], func=AF.Sigmoid),
                     r=[pg.b], w=[sgt.b])
                proj_fm(c * 128, win, pv)
                P.op("dve", lambda e, pv=pv, sgt=sgt, c=c, u_cur=u_cur: e.tensor_tensor(
                    out=u_cur.t[:, c, 30:542], in0=pv.t[:], in1=sgt.t[:], op=ALU.mult), r=[pv.b, sgt.b], w=[u_cur.b])
            for i in range(13):
                if i < 4:
                    col0, wt, scale = 1024 + i * 128, win, 0.125
                elif i < 8:
                    col0, wt, scale = 1536 + (i - 4) * 128, win, 1.0
                elif i < 12:
                    col0, wt, scale = 2560 + (i - 8) * 128, win, 1.0
                else:
                    col0, wt, scale = 0, wki2, 1.0
                pq = pm[pmi % 4]; pmi += 1
                proj_fm(col0, wt, pq)
                if i % 2 == 0:
                    P.op("act", lambda e, pq=pq, i=i, scale=scale: e.activation(out=stg.t[:, i, :], in_=pq.t[:], func=AF.Copy, scale=scale),
                         r=[pq.b], w=[stg.b])
                else:
                    P.op("dve", lambda e, pq=pq, i=i, scale=scale: e.tensor_scalar(out=stg.t[:, i, :], in0=pq.t[:], scalar1=scale,
                                                                               scalar2=None, op0=ALU.mult), r=[pq.b], w=[stg.b])
            P.op("st", lambda e, t0=t0: e.dma_start(out=qTv[:, :, t0:t0 + 512], in_=stg.t[:, 0:4, :]), r=[stg.b])
            P.op("st", lambda e, t0=t0: e.dma_start(out=kTv[:, :, t0:t0 + 512], in_=stg.t[:, 4:8, :]), r=[stg.b])
            P.op("st", lambda e, t0=t0: e.dma_start(out=qiTv[:, :, t0:t0 + 512], in_=stg.t[:, 8:12, :]), r=[stg.b])
            P.op("st", lambda e, t0=t0: e.dma_start(out=kiT_d.ap()[:, t0:t0 + 512], in_=stg.t[:, 12, :]), r=[stg.b])
            for j in range(4):
                pv = pm[pmi % 4]; pmi += 1
                for c in range(8):
                    P.op("pe", lambda e, c=c, j=j, pv=pv: e.matmul(pv.t[:], xt.t[:, c, j * 128:(j + 1) * 128], win.t[:, c, 2048:2560],
                                                                 start=(c == 0), stop=(c == 7)), r=[win.b, xt.b], w=[pv.b])
                P.op("act", lambda e, j=j, pv=pv: e.copy(out=vst.t[:, j, :, 0:64], in_=pv.t[:].rearrange("p (h d) -> p h d", h=8)),
                     r=[pv.b], w=[vst.b])
                pw = pm[pmi % 4]; pmi += 1
                for c in range(8):
                    P.op("pe", lambda e, c=c, j=j, pw=pw: e.matmul(pw.t[:, 0:8], xt.t[:, c, j * 128:(j + 1) * 128], win.t[:, c, 3136:3144],
                                                                 start=(c == 0), stop=(c == 7)), r=[win.b, xt.b], w=[pw.b])
                P.op("dve", lambda e, j=j, pw=pw: e.tensor_scalar(out=wst.t[:, j, :], in0=pw.t[:, 0:8], scalar1=WSCALE, scalar2=None,
                                                               op0=ALU.mult), r=[pw.b], w=[wst.b])
            P.op("st", lambda e, t0=t0: e.dma_start(out=v_d.ap()[t0:t0 + 512, :].rearrange("(j p) n -> p j n", p=128),
                                                    in_=vst.t[:].rearrange("p j h d -> p j (h d)")), r=[vst.b])
            P.op("st", lambda e, t0=t0: e.dma_start(out=w_d.ap()[t0:t0 + 512, :].rearrange("(j p) n -> p j n", p=128),
                                                    in_=wst.t[:]), r=[wst.b])
            for c in range(4):
                pc = pm[pmi % 4]; pmi += 1
                for j in range(31):
                    P.op("pe", lambda e, c=c, j=j, pc=pc, u_cur=u_cur: e.matmul(pc.t[:], diag.t[:, c, j, :], u_cur.t[:, c, j:j + 512],
                                                                             start=(j == 0), stop=(j == 30)), r=[diag.b, u_cur.b], w=[pc.b])
                P.op("act", lambda e, c=c, pc=pc: e.activation(out=yv.t[:, c, :], in_=pc.t[:], func=AF.Identity,
                                                              bias=cwT.t[:, c, 31:32], scale=1.0), r=[pc.b, cwT.b], w=[yv.b])
                P.op("act", lambda e, c=c, pc=pc: e.activation(out=ysq.t[:, c, :], in_=pc.t[:], func=AF.Square,
                                                              bias=cwT.t[:, c, 31:32], scale=1.0), r=[pc.b, cwT.b], w=[ysq.b])
            for c in range(4):
                P.op("pe", lambda e, c=c: e.matmul(pst[0].t[:], ones_f.t[:], yv.t[:, c, :], start=(c == 0), stop=(c == 3)),
                     r=[ones_f.b, yv.b], w=[pst[0].b])
            for c in range(4):
                P.op("pe", lambda e, c=c: e.matmul(pst[1].t[:], ones_f.t[:], ysq.t[:, c, :], start=(c == 0), stop=(c == 3)),
                     r=[ones_f.b, ysq.b], w=[pst[1].b])
            P.op("dve", lambda e: e.tensor_scalar(out=mean.t[:], in0=pst[0].t[:], scalar1=1.0 / 512, scalar2=None, op0=ALU.mult),
                 r=[pst[0].b], w=[mean.b])
            P.op("dve", lambda e: e.tensor_tensor(out=tmp.t[:], in0=mean.t[:], in1=mean.t[:], op=ALU.mult), r=[mean.b], w=[tmp.b])
            P.op("dve", lambda e: e.scalar_tensor_tensor(out=rstd.t[:], in0=pst[1].t[:], scalar=1.0 / 512, in1=tmp.t[:],
                                                         op0=ALU.mult, op1=ALU.subtract), r=[pst[1].b, tmp.b], w=[rstd.b])
            P.op("dve", lambda e: e.tensor_scalar(out=rstd.t[:], in0=rstd.t[:], scalar1=EPS, scalar2=None, op0=ALU.add),
                 r=[rstd.b], w=[rstd.b])
            P.op("act", lambda e: e.activation(out=rstd.t[:], in_=rstd.t[:], func=AF.Sqrt), r=[rstd.b], w=[rstd.b])
            P.op("dve", lambda e: e.reciprocal(out=rstd.t[:], in_=rstd.t[:]), r=[rstd.b], w=[rstd.b])
            for c in range(4):
                P.op("dve", lambda e, c=c: e.tensor_tensor(out=yv.t[:, c, :], in0=yv.t[:, c, :], in1=mean.t[:], op=ALU.subtract),
                     r=[yv.b, mean.b], w=[yv.b])
                P.op("dve", lambda e, c=c: e.tensor_tensor(out=yv.t[:, c, :], in0=yv.t[:, c, :], in1=rstd.t[:], op=ALU.mult),
                     r=[yv.b, rstd.b], w=[yv.b])
                P.op("act", lambda e, c=c: e.activation(out=aout.t[:, c, :], in_=yv.t[:, c, :], func=AF.Silu,
                                                       bias=cwT.t[:, c, 33:34], scale=cwT.t[:, c, 32:33]), r=[yv.b, cwT.b], w=[aout.b])
            P.op("st", lambda e, t0=t0: e.dma_start(out=catTv[:, 0:4, t0:t0 + 512], in_=aout.t[:]), r=[aout.b])
        ph.close()

    def phase_B():
        ph = Phase(P)
        qiT = ph.sb([128, 4, S], BF16, "qiT")
        kiT = ph.sb([128, S], BF16, "kiT")
        wtok = ph.sb([128, NT, 8], F32, "wtok")
        caus = ph.sb([128, 128], F32, "caus")
        pow2 = ph.sb([128, NIT + 2], F32, "pow2")
        P.op("ld", lambda e: e.dma_start(out=qiT.t[:], in_=qiT_d.ap().rearrange("(c p) t -> p c t", p=128)), w=[qiT.b])
        P.op("ld", lambda e: e.dma_start(out=kiT.t[:], in_=kiT_d.ap()), w=[kiT.b])
        P.op("ld", lambda e: e.dma_start(out=wtok.t[:], in_=w_d.ap().rearrange("(t p) e -> p t e", p=128)), w=[wtok.b])
        P.op("ld", lambda e: e.dma_start(out=caus.t[:], in_=c_caus.ap()), w=[caus.b])
        P.op("ld", lambda e: e.dma_start(out=pow2.t[:], in_=c_pow2.ap()), w=[pow2.b])
        NSB = 4
        score = [ph.sb([128, S], F32, "score") for _ in range(NSB)]
        mneg = [ph.sb([128, S], BF16, "mneg") for _ in range(2)]
        junk = [ph.sb([128, S], BF16, "junk") for _ in range(2)]
        rr = [ph.sb([128, 512], BF16, "rr") for _ in range(4)]
        dg = [ph.sb([128, 8, 128], BF16, "dg") for _ in range(NSB)]
        pd = [ph.ps([128, 512], F32, "pd") for _ in range(4)]
        psc = [ph.ps([128, 512], F32, "psc") for _ in range(3)]
        sm = [dict((n, ph.sb([128, 1], F32, n)) for n in ("mn", "mx", "w0", "mid", "cnt", "tt", "thr")) for _ in range(NSB)]
        wk = [ph.sb([128, NIT + 2], F32, "wk") for _ in range(NSB)]
        thr_const = ph.sb([128, 1], F32, "thrc")
        P.op("dve", lambda e: e.memset(thr_const.t[:], -1e29), w=[thr_const.b])
        cstage = [ph.sb([128, 4096], BF16, "cst") for _ in range(3)]
        cnt_ = {"ri": 0, "pdi": 0, "sci": 0}

        def prep(qb):
            nk = (qb + 1) * 128
            sc = score[qb % NSB]
            d = dg[qb % NSB]
            for h in range(8):
                P.op("act", lambda e, h=h: e.activation(out=d.t[:, h, :], in_=ident_f.t[:], func=AF.Copy, scale=wtok.t[:, qb, h:h + 1]),
                     r=[ident_f.b, wtok.b], w=[d.b])
            nch = (nk + 511) // 512
            items = []
            for kc in range(nch):
                k0 = kc * 512
                n = min(512, nk - k0)
                pscore = psc[cnt_["sci"] % 3]; cnt_["sci"] += 1
                for h in range(8):
                    items.append((kc, k0, n, h, pscore))
            LAG = 2
            stash = {}
            for idx in range(len(items) + LAG):
                if idx < len(items):
                    kc, k0, n, h, pscore = items[idx]
                    p0 = (h % 2) * 64
                    pdt = pd[cnt_["pdi"] % 4]; cnt_["pdi"] += 1
                    rt = rr[cnt_["ri"] % 4]; cnt_["ri"] += 1
                    stash[idx] = rt
                    P.op("pe", lambda e: e.matmul(
                        pdt.t[:, 0:n], qiT.t[p0:p0 + 64, h // 2, qb * 128:(qb + 1) * 128], kiT.t[p0:p0 + 64, k0:k0 + n],
                        start=True, stop=True), r=[qiT.b, kiT.b], w=[pdt.b])
                    P.op("act", lambda e: e.activation(out=rt.t[:, 0:n], in_=pdt.t[:, 0:n], func=AF.Relu), r=[pdt.b], w=[rt.b])
                j = idx - LAG
                if j >= 0:
                    kc, k0, n, h, pscore = items[j]
                    rt = stash.pop(j)
                    P.op("pe", lambda e: e.matmul(pscore.t[:, 0:n], d.t[:, h, :], rt.t[:, 0:n], start=(h == 0), stop=(h == 7)),
                         r=[d.b, rt.b], w=[pscore.b])
                    if h == 7:
                        P.op("act", lambda e: e.copy(out=sc.t[:, k0:k0 + n], in_=pscore.t[:, 0:n]), r=[pscore.b], w=[sc.b])

        def bis_ops(qb, slot):
            nk = (qb + 1) * 128
            n1 = qb * 128
            sc = score[qb % NSB]
            mg = mneg[slot]
            jk = junk[slot]
            s_ = sm[qb % NSB]
            wkk = wk[qb % NSB]
            ops = []
            ops.append(lambda: P.op("dve", lambda e: e.tensor_tensor(out=sc.t[:, qb * 128:(qb + 1) * 128], in0=sc.t[:, qb * 128:(qb + 1) * 128],
                                                                   in1=caus.t[:], op=ALU.add), r=[sc.b, caus.b], w=[sc.b]))
            if qb >= 2:
                ops.append(lambda: P.op("dve", lambda e: e.tensor_reduce(out=s_["mx"].t[:], in_=sc.t[:, 0:n1], axis=AX.X, op=ALU.max),
                                        r=[sc.b], w=[s_["mx"].b]))
                ops.append(lambda: P.op("dve", lambda e: e.tensor_reduce(out=s_["mn"].t[:], in_=sc.t[:, 0:n1], axis=AX.X, op=ALU.min),
                                        r=[sc.b], w=[s_["mn"].b]))
                ops.append(lambda: P.op("dve", lambda e: e.tensor_tensor(out=s_["w0"].t[:], in0=s_["mx"].t[:], in1=s_["mn"].t[:], op=ALU.subtract),
                                        r=[s_["mx"].b, s_["mn"].b], w=[s_["w0"].b]))
                ops.append(lambda: P.op("dve", lambda e: e.tensor_scalar(out=wkk.t[:], in0=pow2.t[:], scalar1=s_["w0"].t[:, 0:1], scalar2=None,
                                                                       op0=ALU.mult), r=[pow2.b, s_["w0"].b], w=[wkk.b]))
                ops.append(lambda: P.op("dve", lambda e: e.tensor_tensor(out=s_["mid"].t[:], in0=s_["mn"].t[:], in1=wkk.t[:, 1:2], op=ALU.add),
                                        r=[s_["mn"].b, wkk.b], w=[s_["mid"].b]))
                for it in range(1, NIT + 1):
                    ops.append(lambda: P.op("dve", lambda e: e.tensor_scalar(out=jk.t[:, 0:nk], in0=sc.t[:, 0:nk], scalar1=s_["mid"].t[:, 0:1],
                                                                           scalar2=0.0, op0=ALU.is_ge, op1=ALU.add, accum_out=s_["cnt"].t[:, 0:1]),
                                            r=[sc.b, s_["mid"].b], w=[jk.b, s_["cnt"].b]))
                    ops.append(lambda it=it: P.op("dve", lambda e: e.tensor_scalar(out=s_["tt"].t[:], in0=s_["cnt"].t[:], scalar1=255.5,
                                                                                 scalar2=wkk.t[:, it:it + 1], op0=ALU.is_ge, op1=ALU.mult),
                                                  r=[s_["cnt"].b, wkk.b], w=[s_["tt"].b]))
                    ops.append(lambda it=it: P.op("dve", lambda e: e.scalar_tensor_tensor(out=s_["mid"].t[:], in0=s_["tt"].t[:],
                                                                                        scalar=wkk.t[:, it + 1:it + 2], in1=s_["mid"].t[:],
                                                                                        op0=ALU.subtract, op1=ALU.add),
                                                  r=[s_["tt"].b, wkk.b, s_["mid"].b], w=[s_["mid"].b]))
                ops.append(lambda: P.op("dve", lambda e: e.tensor_tensor(out=s_["thr"].t[:], in0=s_["mid"].t[:], in1=wkk.t[:, NIT + 1:NIT + 2],
                                                                       op=ALU.subtract), r=[s_["mid"].b, wkk.b], w=[s_["thr"].b]))
                thr = s_["thr"]
            else:
                thr = thr_const
            ops.append(lambda: P.op("dve", lambda e: e.tensor_scalar(out=mg.t[:, 0:nk], in0=sc.t[:, 0:nk], scalar1=thr.t[:, 0:1],
                                                                   scalar2=NEGM, op0=ALU.is_lt, op1=ALU.mult), r=[sc.b, thr.b], w=[mg.b]))
            ops.append(lambda: P.op("st", lambda e: e.dma_start(out=mask_d.ap()[qb * 128:(qb + 1) * 128, 0:nk], in_=mg.t[:, 0:nk]), r=[mg.b]))
            return ops

        prep(0)
        prep(1)
        for q0 in range(0, NT, 2):
            conv_emit(cstage, 4)
            if q0 + 2 < NT:
                prep(q0 + 2)
                prep(q0 + 3)
            oa = bis_ops(q0, 0)
            ob = bis_ops(q0 + 1, 1)
            for i in range(max(len(oa), len(ob))):
                if i < len(oa):
                    oa[i]()
                if i < len(ob):
                    ob[i]()
        conv_flush_pending()
        ph.close()

    def phase_C():
        ph = Phase(P)
        vaug = ph.sb([128, NT, 520], BF16, "vaug")
        P.op("ld", lambda e: e.dma_start(out=vaug.t[:], in_=v_d.ap().rearrange("(t p) n -> p t n", p=128)), w=[vaug.b])
        qh = [ph.sb([64, S], BF16, "qh") for _ in range(2)]
        kh = [ph.sb([64, S], BF16, "kh") for _ in range(2)]
        gt = [ph.sb([128, 2560], BF16, "gt") for _ in range(2)]
        attT = [ph.sb([64, S], BF16, "attT") for _ in range(2)]
        mk = [[ph.sb([128, S], BF16, "mk") for _ in range(4)] for _ in range(2)]
        pT = [ph.sb([128, 512], BF16, "pT") for _ in range(3)]
        rec = [ph.sb([128, 4], F32, "rec") for _ in range(2)]
        atok = [ph.sb([128, 4, 64], BF16, "atok") for _ in range(2)]
        pss = [ph.ps([128, 512], F32, "pss") for _ in range(3)]
        pacc = [ph.ps([128, 512], F32, "pacc") for _ in range(2)]
        ptr = [ph.ps([128, 1024], BF16, "ptrc") for _ in range(2)]
        si = 0
        ai = 0
        mi = 0
        cstage = [ph.sb([128, 4096], BF16, "cst") for _ in range(3)]
        for h in range(8):
            q_ = qh[h % 2]; k_ = kh[h % 2]; g_ = gt[h % 2]; at_ = attT[h % 2]
            P.op("ld", lambda e, q_=q_, h=h: e.dma_start(out=q_.t[:], in_=qT_d.ap()[h * 64:(h + 1) * 64, :]), w=[q_.b])
            P.op("ld", lambda e, k_=k_, h=h: e.dma_start(out=k_.t[:], in_=kT_d.ap()[h * 64:(h + 1) * 64, :]), w=[k_.b])
            fa = f0_d.ap()
            P.op("ld", lambda e, g_=g_, h=h, fa=fa: e.dma_start(out=g_.t[:], in_=bass.AP(tensor=fa.tensor, offset=h * L0,
                                                                                    ap=[[1, 128], [1, 2560]])), w=[g_.b])
            for Q in range(8):
                conv_emit(cstage, 2)
                mks = mk[mi % 2]; mi += 1
                for j in range(4):
                    qb = 4 * Q + j
                    nk = (qb + 1) * 128
                    P.op("ld", lambda e, m=mks[j], qb=qb, nk=nk: e.dma_start(out=m.t[:, 0:nk], in_=mask_d.ap()[qb * 128:(qb + 1) * 128, 0:nk]),
                         w=[mks[j].b])
                acc = pacc[ai % 2]; ai += 1
                P.op("pe", lambda e, acc=acc: e.matmul(acc.t[:, 0:260], zer_b.t[:, 0:128], zer_b.t[:, 0:260], start=True, stop=False),
                     r=[zer_b.b], w=[acc.b])
                nkb = 4 * Q + 4
                sbase = si
                si += nkb

                def emit_S(kb, Q=Q, q_=q_, k_=k_, g_=g_, mks=mks, sbase=sbase):
                    j0 = max(0, kb - 4 * Q)
                    c0 = j0 * 128
                    ps_ = pss[(sbase + kb) % 3]
                    P.op("pe", lambda e: e.matmul(ps_.t[:, c0:512], k_.t[:, kb * 128:(kb + 1) * 128], q_.t[:, Q * 512 + c0:(Q + 1) * 512],
                                                  start=True, stop=False), r=[k_.b, q_.b], w=[ps_.b])
                    dl = min(4 * Q - kb, 13)
                    off = dl * 128 + 384
                    P.op("pe", lambda e: e.matmul(ps_.t[:, c0:512], anti_b.t[:], g_.t[:, off + c0:off + 512], start=False, stop=False),
                         r=[anti_b.b, g_.b], w=[ps_.b])
                    for j in range(j0, 4):
                        P.op("pe", lambda e, j=j: e.matmul(ps_.t[:, j * 128:(j + 1) * 128], mks[j].t[:, kb * 128:(kb + 1) * 128], ident_b.t[:],
                                                           start=False, stop=True), r=[mks[j].b, ident_b.b], w=[ps_.b])

                emit_S(0)
                for kb in range(nkb):
                    if kb + 1 < nkb:
                        emit_S(kb + 1)
                    j0 = max(0, kb - 4 * Q)
                    c0 = j0 * 128
                    ps_ = pss[(sbase + kb) % 3]
                    pt_ = pT[(sbase + kb) % 3]
                    P.op("act", lambda e, ps_=ps_, pt_=pt_, c0=c0: e.activation(out=pt_.t[:, c0:512], in_=ps_.t[:, c0:512], func=AF.Exp),
                         r=[ps_.b], w=[pt_.b])
                    for j in range(j0, 4):
                        P.op("pe", lambda e, acc=acc, pt_=pt_, kb=kb, j=j, h=h, Q=Q: e.matmul(
                            acc.t[:, j * 65:(j + 1) * 65], pt_.t[:, j * 128:(j + 1) * 128], vaug.t[:, kb, h * 65:(h + 1) * 65],
                            start=False, stop=(kb == 4 * Q + j)), r=[pt_.b, vaug.b], w=[acc.b])
                rc = rec[ai % 2]
                ak = atok[ai % 2]
                P.op("dve", lambda e, acc=acc, rc=rc: e.reciprocal(out=rc.t[:], in_=acc.t[:, 0:260].rearrange("p (j d) -> p j d", d=65)[:, :, 64]),
                     r=[acc.b], w=[rc.b])
                for j in range(4):
                    P.op("dve", lambda e, acc=acc, rc=rc, ak=ak, j=j: e.tensor_scalar(out=ak.t[:, j, :], in0=acc.t[:, j * 65:j * 65 + 64],
                                                                                   scalar1=rc.t[:, j:j + 1], scalar2=None, op0=ALU.mult),
                         r=[acc.b, rc.b], w=[ak.b])
                pt2 = ptr[ai % 2]
                for j in range(4):
                    P.op("pe", lambda e, pt2=pt2, ak=ak, j=j: e.transpose(out=pt2.t[0:64, j * 128:(j + 1) * 128], in_=ak.t[:, j, :],
                                                                        identity=ident_b.t[:]), r=[ak.b, ident_b.b], w=[pt2.b])
                P.op("act", lambda e, pt2=pt2, at_=at_, Q=Q: e.copy(out=at_.t[:, Q * 512:(Q + 1) * 512], in_=pt2.t[0:64, 0:512]),
                     r=[pt2.b], w=[at_.b])
            P.op("st", lambda e, at_=at_, h=h: e.dma_start(out=catT_d.ap()[512 + h * 64:512 + (h + 1) * 64, :], in_=at_.t[:]), r=[at_.b])
        conv_finish(cstage)
        ph.close()

    def phase_outproj(src_kind, wout_d, lng_d, lnb_d, xres_d, xout_d):
        ph = Phase(P)
        wo = ph.sb([128, 8, D], BF16, "wo")
        P.op("gld", lambda e: e.dma_start(out=wo.t[:], in_=wout_d.ap().rearrange("(c p) n -> p c n", p=128)), w=[wo.b])
        gB = ph.sb([128, D], F32, "gB")
        bB = ph.sb([128, D], F32, "bB")
        P.op("ld", lambda e: e.dma_start(out=gB.t[:], in_=bcast_row(lng_d)), w=[gB.b])
        P.op("ld", lambda e: e.dma_start(out=bB.t[:], in_=bcast_row(lnb_d)), w=[bB.b])
        pmx = [ph.ps([128, 512], F32, "pmx") for _ in range(4)]
        xr = [ph.sb([128, D], F32, "xr") for _ in range(2)]
        z = [ph.sb([128, D], F32, "z") for _ in range(2)]
        xo = [ph.sb([128, D], F32, "xo") for _ in range(2)]
        st6 = [ph.sb([128, 2, 6], F32, "st6") for _ in range(2)]
        mv = [ph.sb([128, 2], F32, "mv") for _ in range(2)]
        rstd = [ph.sb([128, 1], F32, "rstd") for _ in range(2)]
        if src_kind == "catT":
            cT = [ph.sb([128, 8, 512], BF16, "cT") for _ in range(2)]
        else:
            ug = [[ph.sb([128, 8, 132], F32, "ug") for _ in range(3)] for _ in range(2)]
            rc8 = [ph.sb([128, 8], F32, "rc8") for _ in range(2)]
            otok = [ph.sb([128, D], BF16, "otok") for _ in range(2)]
            oT = [ph.sb([128, 8, 128], BF16, "oT") for _ in range(2)]
            ptr = [ph.ps([128, 1024], BF16, "ptro") for _ in range(2)]
        catTv = catT_d.ap().rearrange("(c p) t -> p c t", p=128)
        for tt in range(NT):
            i2 = tt % 2
            t0 = tt * 128
            if src_kind == "catT":
                if tt % 4 == 0:
                    ct = cT[(tt // 4) % 2]
                    P.op("ld", lambda e, ct=ct, t0=t0: e.dma_start(out=ct.t[:], in_=catTv[:, :, t0:t0 + 512]), w=[ct.b])
                ct = cT[(tt // 4) % 2]
                lhs = lambda c, ct=ct, tt=tt: ct.t[:, c, (tt % 4) * 128:(tt % 4 + 1) * 128]
                lhs_b = ct.b
            else:
                u3 = ug[i2]
                for g in range(3):
                    P.op("ld", lambda e, g=g, u3=u3, t0=t0: e.dma_start(out=u3[g].t[:], in_=u_d.ap()[g, t0:t0 + 128, :, :]), w=[u3[g].b])
                P.op("dve", lambda e, u3=u3: e.tensor_tensor(out=u3[0].t[:], in0=u3[0].t[:], in1=u3[1].t[:], op=ALU.add),
                     r=[u3[0].b, u3[1].b], w=[u3[0].b])
                P.op("dve", lambda e, u3=u3: e.tensor_tensor(out=u3[0].t[:], in0=u3[0].t[:], in1=u3[2].t[:], op=ALU.add),
                     r=[u3[0].b, u3[2].b], w=[u3[0].b])
                rc = rc8[i2]
                ot = otok[i2]
                P.op("dve", lambda e, u3=u3, rc=rc: e.reciprocal(out=rc.t[:], in_=u3[0].t[:, :, 128]), r=[u3[0].b], w=[rc.b])
                for hh in range(8):
                    P.op("dve", lambda e, u3=u3, rc=rc, ot=ot, hh=hh: e.tensor_scalar(out=ot.t[:, hh * 128:(hh + 1) * 128], in0=u3[0].t[:, hh, 0:128],
                                                                                   scalar1=rc.t[:, hh:hh + 1], scalar2=None, op0=ALU.mult),
                         r=[u3[0].b, rc.b], w=[ot.b])
                o_T = oT[i2]
                for half in range(2):
                    pt = ptr[half]
                    for cc in range(4):
                        c = half * 4 + cc
                        P.op("pe", lambda e, pt=pt, ot=ot, c=c, cc=cc: e.transpose(out=pt.t[:, cc * 128:(cc + 1) * 128], in_=ot.t[:, c * 128:(c + 1) * 128],
                                                                                identity=ident_b.t[:]), r=[ot.b, ident_b.b], w=[pt.b])
                    P.op("act", lambda e, pt=pt, o_T=o_T, half=half: e.copy(out=o_T.t[:, half * 4:(half + 1) * 4, :].rearrange("p c t -> p (c t)"),
                                                                          in_=pt.t[:, 0:512]), r=[pt.b], w=[o_T.b])
                lhs = lambda c, o_T=o_T: o_T.t[:, c, :]
                lhs_b = o_T.b
            x_ = xr[i2]
            P.op("ld", lambda e, x_=x_, t0=t0: e.dma_start(out=x_.t[:], in_=xres_d.ap()[t0:t0 + 128, :]), w=[x_.b])
            z_ = z[i2]
            for half in range(2):
                pm_ = pmx[(tt * 2 + half) % 4]
                for c in range(8):
                    P.op("pe", lambda e, pm_=pm_, c=c, half=half, lhs=lhs: e.matmul(pm_.t[:], lhs(c), wo.t[:, c, half * 512:(half + 1) * 512],
                                                                                  start=(c == 0), stop=(c == 7)), r=[lhs_b, wo.b], w=[pm_.b])
                P.op("dve", lambda e, pm_=pm_, x_=x_, z_=z_, half=half: e.scalar_tensor_tensor(
                    out=z_.t[:, half * 512:(half + 1) * 512], in0=x_.t[:, half * 512:(half + 1) * 512], scalar=ALPHA, in1=pm_.t[:],
                    op0=ALU.mult, op1=ALU.add), r=[pm_.b, x_.b], w=[z_.b])
            layernorm_rows(ph, z_, gB, bB, xo[i2], st6[i2], mv[i2], rstd[i2], None)
            P.op("st", lambda e, i2=i2, t0=t0: e.dma_start(out=xout_d.ap()[t0:t0 + 128, :], in_=xo[i2].t[:]), r=[xo[i2].b])
        ph.close()

    def phase_E():
        ph = Phase(P)
        NF = 22
        wd = ph.sb([128, NF, D], BF16, "wd")
        wdv = ewd.ap().rearrange("(f p) n -> p f n", p=128)
        for f0 in range(0, NF, 6):
            f1 = min(NF, f0 + 6)
            P.op("gld", lambda e, f0=f0, f1=f1: e.dma_start(out=wd.t[:, f0:f1, :], in_=wdv[:, f0:f1, :]), w=[wd.b])
        gB = ph.sb([128, D], F32, "gB")
        bB = ph.sb([128, D], F32, "bB")
        P.op("ld", lambda e: e.dma_start(out=gB.t[:], in_=bcast_row(eln2g)), w=[gB.b])
        P.op("ld", lambda e: e.dma_start(out=bB.t[:], in_=bcast_row(eln2b)), w=[bB.b])
        xtok = [ph.sb([128, D], BF16, "xtok") for _ in range(8)]
        xT = ph.sb([128, 8, 1024], BF16, "xT")
        hT = ph.sb([128, NF, 1024], BF16, "hT")
        wgp = [ph.sb([128, 8, 256], BF16, "wgp") for _ in range(2)]
        wup = [ph.sb([128, 8, 256], BF16, "wup") for _ in range(2)]
        sg = [ph.sb([128, 512], F32, "sg") for _ in range(2)]
        ptr = [ph.ps([128, 1024], BF16, "ptre") for _ in range(2)]
        pg = [ph.ps([128, 512], F32, "pg") for _ in range(2)]
        pu = [ph.ps([128, 512], F32, "pu") for _ in range(2)]
        py = [ph.ps([128, 512], F32, "py") for _ in range(2)]
        xr = [ph.sb([128, D], F32, "xr") for _ in range(2)]
        z = [ph.sb([128, D], F32, "z") for _ in range(2)]
        xo = [ph.sb([128, D], F32, "xo") for _ in range(2)]
        st6 = [ph.sb([128, 2, 6], F32, "st6") for _ in range(2)]
        mv = [ph.sb([128, 2], F32, "mv") for _ in range(2)]
        rstd = [ph.sb([128, 1], F32, "rstd") for _ in range(2)]
        wgv = ewg.ap().rearrange("(c p) n -> p c n", p=128)
        wuv = ewu.ap().rearrange("(c p) n -> p c n", p=128)
        pi = 0
        gi = 0
        for grp in range(4):
            t0 = grp * 1024
            load_tok_T(ph, lambda j, t0=t0: x1_d.ap()[t0 + j * 128:t0 + (j + 1) * 128, :], 8, xtok, xT, ptr, "x1")
            for pc in range(11):
                wg_ = wgp[pi % 2]; wu_ = wup[pi % 2]; pi += 1
                P.op("gld", lambda e, wg_=wg_, pc=pc: e.dma_start(out=wg_.t[:], in_=wgv[:, :, pc * 256:(pc + 1) * 256]), w=[wg_.b])
                P.op("gld", lambda e, wu_=wu_, pc=pc: e.dma_start(out=wu_.t[:], in_=wuv[:, :, pc * 256:(pc + 1) * 256]), w=[wu_.b])
                for fs in range(2):
                    fc = pc * 2 + fs
                    for half in range(2):
                        pg_ = pg[gi % 2]; pu_ = pu[gi % 2]; sg_ = sg[gi % 2]; gi += 1
                        for c in range(8):
                            P.op("pe", lambda e, pg_=pg_, wg_=wg_, c=c, fs=fs, half=half: e.matmul(
                                pg_.t[:], wg_.t[:, c, fs * 128:(fs + 1) * 128], xT.t[:, c, half * 512:(half + 1) * 512],
                                start=(c == 0), stop=(c == 7)), r=[wg_.b, xT.b], w=[pg_.b])
                        for c in range(8):
                            P.op("pe", lambda e, pu_=pu_, wu_=wu_, c=c, fs=fs, half=half: e.matmul(
                                pu_.t[:], wu_.t[:, c, fs * 128:(fs + 1) * 128], xT.t[:, c, half * 512:(half + 1) * 512],
                                start=(c == 0), stop=(c == 7)), r=[wu_.b, xT.b], w=[pu_.b])
                        P.op("act", lambda e, pg_=pg_, sg_=sg_: e.activation(out=sg_.t[:], in_=pg_.t[:], func=AF.Silu), r=[pg_.b], w=[sg_.b])
                        P.op("dve", lambda e, pu_=pu_, sg_=sg_, fc=fc, half=half: e.tensor_tensor(
                            out=hT.t[:, fc, half * 512:(half + 1) * 512], in0=pu_.t[:], in1=sg_.t[:], op=ALU.mult), r=[pu_.b, sg_.b], w=[hT.b])
            for j in range(8):
                tt = grp * 8 + j
                i2 = tt % 2
                x_ = xr[i2]; z_ = z[i2]
                P.op("ld", lambda e, x_=x_, tt=tt: e.dma_start(out=x_.t[:], in_=x1_d.ap()[tt * 128:(tt + 1) * 128, :]), w=[x_.b])
                for half in range(2):
                    py_ = py[half]
                    for fc in range(NF):
                        P.op("pe", lambda e, py_=py_, fc=fc, j=j, half=half: e.matmul(
                            py_.t[:], hT.t[:, fc, j * 128:(j + 1) * 128], wd.t[:, fc, half * 512:(half + 1) * 512],
                            start=(fc == 0), stop=(fc == NF - 1)), r=[hT.b, wd.b], w=[py_.b])
                    P.op("dve", lambda e, py_=py_, x_=x_, z_=z_, half=half: e.scalar_tensor_tensor(
                        out=z_.t[:, half * 512:(half + 1) * 512], in0=x_.t[:, half * 512:(half + 1) * 512], scalar=ALPHA, in1=py_.t[:],
                        op0=ALU.mult, op1=ALU.add), r=[py_.b, x_.b], w=[z_.b])
                layernorm_rows(ph, z_, gB, bB, xo[i2], st6[i2], mv[i2], rstd[i2], None)
                P.op("st", lambda e, i2=i2, tt=tt: e.dma_start(out=x2_d.ap()[tt * 128:(tt + 1) * 128, :], in_=xo[i2].t[:]), r=[xo[i2].b])
        ph.close()

    def phase_F(g):
        r = (1, 4, 16)[g]
        Lc = S // r
        nb = Lc // 128
        ph = Phase(P)
        wq = ph.sb([128, 8, 1024], BF16, "wq")
        wk_ = ph.sb([128, 8, 1024], BF16, "wk")
        wv = ph.sb([128, 8, 1024], BF16, "wv")
        wv_ap = owin.ap().rearrange("(c p) n -> p c n", p=128)
        for j, wt in enumerate((wq, wk_, wv)):
            col = (g * 3 + j) * 1024
            for c0 in range(0, 8, 4):
                P.op("gld", lambda e, wt=wt, col=col, c0=c0: e.dma_start(out=wt.t[:, c0:c0 + 4, :], in_=wv_ap[:, c0:c0 + 4, col:col + 1024]), w=[wt.b])
        g1 = ph.sb([128, 8, 256], BF16, "g1")
        fa = f1_d.ap()
        for h in range(8):
            P.op("ld", lambda e, h=h: e.dma_start(out=g1.t[:, h, :], in_=bass.AP(tensor=fa.tensor, offset=(g * 8 + h) * L1,
                                                                              ap=[[1, 128], [1, 256]])), w=[g1.b])
        xtok = [ph.sb([128, D], BF16, "xtok") for _ in range(4)]
        xT = ph.sb([128, 8, S], BF16, "xTp")
        ptr = [ph.ps([128, 1024], BF16, "ptrf")] * 2
        x2a = x2_d.ap()

        def rows(tt):
            rho = (tt * 128) // Lc
            l0 = (tt * 128) % Lc
            return bass.AP(tensor=x2a.tensor, offset=(l0 * r + rho) * D, ap=[[r * D, 128], [1, D]])
        xTs = [T(xT.t, "x") for _ in range(8)]
        for sbk in range(8):
            xs = xTs[sbk]
            for j in range(4):
                P.op("gld", lambda e, j=j, sbk=sbk: e.dma_start(out=xtok[j].t[:], in_=rows(sbk * 4 + j)), w=[xtok[j].b])
            for c in range(8):
                pt = ptr[c % 2]
                for jj in range(4):
                    P.op("pe", lambda e, pt=pt, jj=jj, c=c: e.transpose(out=pt.t[:, jj * 128:(jj + 1) * 128], in_=xtok[jj].t[:, c * 128:(c + 1) * 128],
                                                                      identity=ident_b.t[:]), r=[xtok[jj].b, ident_b.b], w=[pt.b])
                if c % 2 == 0:
                    P.op("act", lambda e, pt=pt, c=c, sbk=sbk: e.copy(out=xT.t[:, c, sbk * 512:(sbk + 1) * 512], in_=pt.t[:, 0:512]), r=[pt.b], w=[xs.b])
                else:
                    P.op("dve", lambda e, pt=pt, c=c, sbk=sbk: e.tensor_copy(out=xT.t[:, c, sbk * 512:(sbk + 1) * 512], in_=pt.t[:, 0:512]), r=[pt.b], w=[xs.b])
        qh = [ph.sb([128, S], BF16, "qh") for _ in range(2)]
        kh = [ph.sb([128, S], BF16, "kh") for _ in range(2)]
        vh = [ph.sb([128, NT, 129], BF16, "vh") for _ in range(2)]
        ust = [ph.sb([128, NT, 129], F32, "ust")] * 2
        pT = [ph.sb([128, 4, 128], BF16, "pT") for _ in range(3)]
        pq = [ph.ps([128, 512], F32, "pq") for _ in range(2)]
        pss = [ph.ps([128, 512], F32, "pss") for _ in range(3)]
        pacc = [ph.ps([128, 512], F32, "pacc") for _ in range(2)]
        QS = float(128 ** -0.5)
        pqi = 0
        si = 0
        for vv in vh:
            P.op("dve", lambda e, vv=vv: e.memset(vv.t[:, :, 128:129], 1.0), w=[vv.b])
        for h in range(8):
            q_ = qh[h % 2]; k_ = kh[h % 2]; v_ = vh[h % 2]; u_ = ust[h % 2]
            for sbk in range(8):
                for which, wt, dst in ((0, wq, q_), (1, wk_, k_)):
                    pp = pq[pqi % 2]; pqi += 1
                    for c in range(8):
                        P.op("pe", lambda e, pp=pp, wt=wt, c=c, h=h, sbk=sbk: e.matmul(
                            pp.t[:], wt.t[:, c, h * 128:(h + 1) * 128], xT.t[:, c, sbk * 512:(sbk + 1) * 512], start=(c == 0), stop=(c == 7)),
                            r=[wt.b, xTs[sbk].b], w=[pp.b])
                    if which == 0:
                        P.op("act", lambda e, pp=pp, dst=dst, sbk=sbk: e.activation(out=dst.t[:, sbk * 512:(sbk + 1) * 512], in_=pp.t[:], func=AF.Copy, scale=QS),
                             r=[pp.b], w=[dst.b])
                    else:
                        P.op("dve", lambda e, pp=pp, dst=dst, sbk=sbk: e.tensor_copy(out=dst.t[:, sbk * 512:(sbk + 1) * 512], in_=pp.t[:]), r=[pp.b], w=[dst.b])
                pp = pq[pqi % 2]; pqi += 1
                for j in range(4):
                    for c in range(8):
                        P.op("pe", lambda e, pp=pp, c=c, h=h, sbk=sbk, j=j: e.matmul(
                            pp.t[:, j * 128:(j + 1) * 128], xT.t[:, c, sbk * 512 + j * 128:sbk * 512 + (j + 1) * 128], wv.t[:, c, h * 128:(h + 1) * 128],
                            start=(c == 0), stop=(c == 7)), r=[wv.b, xTs[sbk].b], w=[pp.b])
                P.op("act", lambda e, pp=pp, v_=v_, sbk=sbk: e.copy(out=v_.t[:, sbk * 4:(sbk + 1) * 4, 0:128], in_=pp.t[:].rearrange("p (j d) -> p j d", j=4)),
                     r=[pp.b], w=[v_.b])
            sbase = si
            si += NT // 2

            def emit_S(t2, q_=q_, k_=k_, h=h, sbase=sbase):
                ps_ = pss[(sbase + t2 // 2) % 3]
                for bi in range(2):
                    tt = t2 + bi
                    n = tt % nb
                    P.op("pe", lambda e, tt=tt, bi=bi: e.matmul(
                        ps_.t[:, (2 * bi + 1) * 128:(2 * bi + 2) * 128], k_.t[:, tt * 128:(tt + 1) * 128], q_.t[:, tt * 128:(tt + 1) * 128],
                        start=True, stop=False), r=[k_.b, q_.b], w=[ps_.b])
                    P.op("pe", lambda e, bi=bi: e.matmul(
                        ps_.t[:, (2 * bi + 1) * 128:(2 * bi + 2) * 128], anti_b.t[:], g1.t[:, h, 0:128], start=False, stop=True),
                        r=[anti_b.b, g1.b], w=[ps_.b])
                    if n > 0:
                        P.op("pe", lambda e, tt=tt, bi=bi: e.matmul(
                            ps_.t[:, (2 * bi) * 128:(2 * bi + 1) * 128], k_.t[:, (tt - 1) * 128:tt * 128], q_.t[:, tt * 128:(tt + 1) * 128],
                            start=True, stop=False), r=[k_.b, q_.b], w=[ps_.b])
                        P.op("pe", lambda e, bi=bi: e.matmul(
                            ps_.t[:, (2 * bi) * 128:(2 * bi + 1) * 128], anti_b.t[:], g1.t[:, h, 128:256], start=False, stop=True),
                            r=[anti_b.b, g1.b], w=[ps_.b])

            emit_S(0)
            for t2 in range(0, NT, 2):
                if t2 + 2 < NT:
                    emit_S(t2 + 2)
                ps_ = pss[(sbase + t2 // 2) % 3]; pt_ = pT[(sbase + t2 // 2) % 3]; acc = pacc[(t2 // 2) % 2]
                first_has_prev = (t2 % nb) > 0
                c0 = 0 if first_has_prev else 128
                P.op("act", lambda e, ps_=ps_, pt_=pt_, c0=c0: e.activation(out=pt_.t[:].rearrange("p a b -> p (a b)")[:, c0:512], in_=ps_.t[:, c0:512], func=AF.Exp),
                     r=[ps_.b], w=[pt_.b])
                for bi in range(2):
                    tt = t2 + bi
                    n = tt % nb
                    P.op("pe", lambda e, acc=acc, pt_=pt_, v_=v_, tt=tt, bi=bi, n=n: e.matmul(
                        acc.t[:, bi * 129:(bi + 1) * 129], pt_.t[:, 2 * bi + 1, :], v_.t[:, tt, :], start=True, stop=(n == 0)),
                        r=[pt_.b, v_.b], w=[acc.b])
                    if n > 0:
                        P.op("pe", lambda e, acc=acc, pt_=pt_, v_=v_, tt=tt, bi=bi: e.matmul(
                            acc.t[:, bi * 129:(bi + 1) * 129], pt_.t[:, 2 * bi, :], v_.t[:, tt - 1, :], start=False, stop=True),
                            r=[pt_.b, v_.b], w=[acc.b])
                P.op("dve", lambda e, acc=acc, u_=u_, t2=t2: e.tensor_copy(out=u_.t[:, t2:t2 + 2, :], in_=acc.t[:, 0:258].rearrange("p (b d) -> p b d", b=2)),
                     r=[acc.b], w=[u_.b])
            uda = u_d.ap()
            for rho in range(r):
                P.op("st", lambda e, u_=u_, rho=rho, h=h: e.dma_start(
                    out=bass.AP(tensor=uda.tensor, offset=((g * S + rho) * 8 + h) * 132, ap=[[r * 8 * 132, 128], [128 * r * 8 * 132, nb], [1, 129]]),
                    in_=u_.t[:, rho * nb:(rho + 1) * nb, :]), r=[u_.b])
        ph.close()

    conv_jobs = []
    for e_ in range(8):
        for pc in range(7):
            conv_jobs.append(("wg", e_, pc))
            conv_jobs.append(("wu", e_, pc))
    for e_ in range(8):
        for ch in range(2):
            for q_ in range(4):
                conv_jobs.append(("wd", e_, ch * 4 + q_))
    conv_state = {"next": 0, "pending_store": None, "k": 0}

    def conv_emit(stage, n):
        for _ in range(n):
            pend = conv_state["pending_store"]
            if pend is not None:
                stg_, dst_ap, ncol = pend
                P.op("gst", lambda e, stg_=stg_, dst_ap=dst_ap, ncol=ncol: e.dma_start(out=dst_ap, in_=stg_.t[:, 0:ncol]), r=[stg_.b])
                conv_state["pending_store"] = None
            if conv_state["next"] >= len(conv_jobs):
                continue
            kind, e_, i_ = conv_jobs[conv_state["next"]]
            conv_state["next"] += 1
            stg_ = stage[conv_state["k"] % len(stage)]
            conv_state["k"] += 1
            if kind in ("wg", "wu"):
                src = (omwg if kind == "wg" else omwu).ap()[e_].rearrange("(c p) n -> p c n", p=128)[:, :, i_ * 512:(i_ + 1) * 512]
                dst = (wgs_d if kind == "wg" else wus_d).ap()[(e_ * 7 + i_) * 128:(e_ * 7 + i_ + 1) * 128, :]
                P.op("gld", lambda e, stg_=stg_, src=src: e.dma_start(out=stg_.t[:, 0:4096].rearrange("p (c n) -> p c n", c=8), in_=src), w=[stg_.b])
                conv_state["pending_store"] = (stg_, dst, 4096)
            else:
                ch, q_ = i_ // 4, i_ % 4
                src = omwd.ap()[e_].rearrange("(f p) n -> p f n", p=128)[:, q_ * 7:(q_ + 1) * 7, ch * 512:(ch + 1) * 512]
                dst = wds_d.ap()[(e_ * 8 + i_) * 128:(e_ * 8 + i_ + 1) * 128, :]
                P.op("gld", lambda e, stg_=stg_, src=src: e.dma_start(out=stg_.t[:, 0:3584].rearrange("p (f n) -> p f n", f=7), in_=src), w=[stg_.b])
                conv_state["pending_store"] = (stg_, dst, 3584)

    def conv_flush_pending():
        pend = conv_state["pending_store"]
        if pend is not None:
            stg_, dst_ap, ncol = pend
            P.op("gst", lambda e: e.dma_start(out=dst_ap, in_=stg_.t[:, 0:ncol]), r=[stg_.b])
            conv_state["pending_store"] = None

    def conv_finish(stage):
        while conv_state["next"] < len(conv_jobs) or conv_state["pending_store"] is not None:
            conv_emit(stage, 1)

    NTILE = 23
    NS = NTILE * 512
    I32 = mybir.dt.int32

    def phase_H1():
        ph = Phase(P)
        if conv_state["next"] < len(conv_jobs):
            cst_ = [ph.sb([128, 4096], BF16, "cst") for _ in range(3)]
            conv_finish(cst_)
        rt = ph.sb([128, 8, 8], F32, "router")
        P.op("ld", lambda e: e.dma_start(out=rt.t[:], in_=orouter.ap().rearrange("(c p) e -> p c e", p=128)), w=[rt.b])
        ustr = ph.sb([128, 128], BF16, "ustr")
        ones_b = ph.sb([128, 128], BF16, "onesb")
        thr8 = ph.sb([128, 8], F32, "thr8")
        iota23 = ph.sb([128, NTILE], F32, "iota23")
        cwg = ph.sb([128, 7], F32, "cwg")
        cwd = ph.sb([128, 8], F32, "cwd")
        P.op("gld", lambda e: e.dma_start(out=ustr.t[:], in_=c_ustr.ap()), w=[ustr.b])
        P.op("ld", lambda e: e.dma_start(out=thr8.t[:], in_=c_thr8.ap()), w=[thr8.b])
        P.op("ld", lambda e: e.dma_start(out=iota23.t[:], in_=c_iota23.ap()), w=[iota23.b])
        P.op("ld", lambda e: e.dma_start(out=cwg.t[:], in_=c_cwg.ap()), w=[cwg.b])
        P.op("ld", lambda e: e.dma_start(out=cwd.t[:], in_=c_cwd.ap()), w=[cwd.b])
        P.op("pool", lambda e: e.memset(ones_b.t[:], 1.0), w=[ones_b.b])
        xf = [ph.sb([128, D], F32, "xf") for _ in range(2)]
        xTf = [ph.sb([128, 8, 128], F32, "xTf") for _ in range(2)]
        MSK = ph.sb([128, NT, 8], F32, "MSK")
        OHA = ph.sb([128, NT, 8], F32, "OHA")
        lg = [ph.sb([128, 8], F32, "lg") for _ in range(2)]
        mx8 = [ph.sb([128, 8], F32, "mx8") for _ in range(2)]
        ee = [ph.sb([128, 8], F32, "ee") for _ in range(2)]
        nv1 = [ph.sb([128, 1], F32, "nv1") for _ in range(2)]
        den = [ph.sb([128, 1], F32, "den") for _ in range(2)]
        ptf = [ph.ps([128, 512], F32, "ptf") for _ in range(2)]
        plg = ph.ps([128, 512], F32, "plg")
        pcum = ph.ps([128, 512], F32, "pcum")
        ptot = ph.ps([128, 512], F32, "ptot")
        for tt in range(NT):
            x_ = xf[tt % 2]; xt_ = xTf[tt % 2]
            P.op("ld", lambda e, x_=x_, tt=tt: e.dma_start(out=x_.t[:], in_=x3_d.ap()[tt * 128:(tt + 1) * 128, :]), w=[x_.b])
            for half in range(2):
                pt = ptf[half]
                for cc in range(4):
                    c = half * 4 + cc
                    P.op("pe", lambda e, x_=x_, c=c, cc=cc, pt=pt: e.transpose(out=pt.t[:, cc * 128:(cc + 1) * 128], in_=x_.t[:, c * 128:(c + 1) * 128],
                                                                             identity=ident_f.t[:]), r=[x_.b, ident_f.b], w=[pt.b])
                P.op("act", lambda e, xt_=xt_, half=half, pt=pt: e.copy(out=xt_.t[:, half * 4:(half + 1) * 4, :].rearrange("p c t -> p (c t)"), in_=pt.t[:]),
                     r=[pt.b], w=[xt_.b])
            for c in range(8):
                P.op("pe", lambda e, xt_=xt_, c=c: e.matmul(plg.t[:, 0:8], xt_.t[:, c, :], rt.t[:, c, :], start=(c == 0), stop=(c == 7)),
                     r=[xt_.b, rt.b], w=[plg.b])
            l_ = lg[tt % 2]; m_ = mx8[tt % 2]; e_ = ee[tt % 2]; n_ = nv1[tt % 2]; d_ = den[tt % 2]
            P.op("dve", lambda e, l_=l_: e.tensor_copy(out=l_.t[:], in_=plg.t[:, 0:8]), r=[plg.b], w=[l_.b])
            P.op("dve", lambda e, l_=l_, m_=m_: e.max(out=m_.t[:], in_=l_.t[:]), r=[l_.b], w=[m_.b])
            P.op("dve", lambda e, m_=m_, n_=n_: e.tensor_scalar(out=n_.t[:], in0=m_.t[:, 0:1], scalar1=-1.0, scalar2=None, op0=ALU.mult),
                 r=[m_.b], w=[n_.b])
            P.op("act", lambda e, l_=l_, e_=e_, n_=n_: e.activation(out=e_.t[:], in_=l_.t[:], func=AF.Exp, bias=n_.t[:, 0:1], scale=1.0),
                 r=[l_.b, n_.b], w=[e_.b])
            P.op("dve", lambda e, l_=l_, m_=m_, tt=tt: e.tensor_scalar(out=MSK.t[:, tt, :], in0=l_.t[:], scalar1=m_.t[:, 1:2], scalar2=None, op0=ALU.is_ge),
                 r=[l_.b, m_.b], w=[MSK.b])
            P.op("dve", lambda e, l_=l_, m_=m_, tt=tt: e.tensor_scalar(out=OHA.t[:, tt, :], in0=l_.t[:], scalar1=m_.t[:, 0:1], scalar2=None, op0=ALU.is_ge),
                 r=[l_.b, m_.b], w=[OHA.b])
            P.op("dve", lambda e, e_=e_, tt=tt: e.tensor_tensor(out=e_.t[:], in0=e_.t[:], in1=MSK.t[:, tt, :], op=ALU.mult), r=[e_.b, MSK.b], w=[e_.b])
            P.op("dve", lambda e, e_=e_, d_=d_: e.tensor_reduce(out=d_.t[:], in_=e_.t[:], axis=AX.X, op=ALU.add), r=[e_.b], w=[d_.b])
            P.op("dve", lambda e, d_=d_, tt=tt: e.reciprocal(out=GA.t[:, tt:tt + 1], in_=d_.t[:]), r=[d_.b], w=[GA.b])
        P.op("dve", lambda e: e.tensor_scalar(out=GB.t[:], in0=GA.t[:], scalar1=-1.0, scalar2=1.0, op0=ALU.mult, op1=ALU.add), r=[GA.b], w=[GB.b])
        mskb = ph.sb([128, NT * 8], BF16, "mskb")
        tot = ph.sb([128, NT, 8], F32, "tot")
        offs = ph.sb([128, NT, 8], F32, "offs")
        slot = ph.sb([128, NT, 8], F32, "slot")
        tmp3 = ph.sb([128, NT, 8], F32, "tmp3")
        ohb = ph.sb([128, NT, 8], F32, "ohb")
        cnt = ph.sb([128, 8], F32, "cnt")
        ntl = ph.sb([128, 8], F32, "ntl")
        tend = ph.sb([128, 8], F32, "tend")
        st512 = ph.sb([128, 8], F32, "st512")
        slf = ph.sb([128, 2, NT], F32, "slf")
        eidf = ph.sb([128, NTILE], F32, "eidf")
        e896 = ph.sb([128, NTILE], F32, "e896")
        e1024 = ph.sb([128, NTILE], F32, "e1024")
        iwgf = ph.sb([128, NTILE, 7], F32, "iwgf")
        iwdf = ph.sb([128, NTILE, 8], F32, "iwdf")
        P.op("dve", lambda e: e.tensor_copy(out=mskb.t[:], in_=MSK.t[:].rearrange("p t e -> p (t e)")), r=[MSK.b], w=[mskb.b])
        P.op("pe", lambda e: e.matmul(pcum.t[:, 0:256], ustr.t[:], mskb.t[:], start=True, stop=True), r=[ustr.b, mskb.b], w=[pcum.b])
        P.op("pe", lambda e: e.matmul(ptot.t[:, 0:256], ones_b.t[:], mskb.t[:], start=True, stop=True), r=[ones_b.b, mskb.b], w=[ptot.b])
        P.op("dve", lambda e: e.tensor_copy(out=tot.t[:].rearrange("p t e -> p (t e)"), in_=ptot.t[:, 0:256]), r=[ptot.b], w=[tot.b])
        P.op("dve", lambda e: e.memset(offs.t[:, 0, :], 0.0), w=[offs.b])
        for tt in range(1, NT):
            P.op("dve", lambda e, tt=tt: e.tensor_tensor(out=offs.t[:, tt, :], in0=offs.t[:, tt - 1, :], in1=tot.t[:, tt - 1, :], op=ALU.add),
                 r=[offs.b, tot.b], w=[offs.b])
        P.op("dve", lambda e: e.tensor_tensor(out=cnt.t[:], in0=offs.t[:, NT - 1, :], in1=tot.t[:, NT - 1, :], op=ALU.add), r=[offs.b, tot.b], w=[cnt.b])
        P.op("dve", lambda e: e.tensor_scalar(out=ntl.t[:], in0=cnt.t[:], scalar1=0.0, scalar2=None, op0=ALU.is_gt), r=[cnt.b], w=[ntl.b])
        for k in range(1, 8):
            P.op("dve", lambda e, k=k: e.scalar_tensor_tensor(out=ntl.t[:], in0=cnt.t[:], scalar=float(512 * k), in1=ntl.t[:], op0=ALU.is_gt, op1=ALU.add),
                 r=[cnt.b, ntl.b], w=[ntl.b])
        P.op("dve", lambda e: e.tensor_copy(out=tend.t[:, 0:1], in_=ntl.t[:, 0:1]), r=[ntl.b], w=[tend.b])
        for k in range(1, 8):
            P.op("dve", lambda e, k=k: e.tensor_tensor(out=tend.t[:, k:k + 1], in0=tend.t[:, k - 1:k], in1=ntl.t[:, k:k + 1], op=ALU.add),
                 r=[tend.b, ntl.b], w=[tend.b])
        P.op("dve", lambda e: e.tensor_tensor(out=st512.t[:], in0=tend.t[:], in1=ntl.t[:], op=ALU.subtract), r=[tend.b, ntl.b], w=[st512.b])
        P.op("dve", lambda e: e.tensor_scalar(out=st512.t[:], in0=st512.t[:], scalar1=512.0, scalar2=None, op0=ALU.mult), r=[st512.b], w=[st512.b])
        P.op("dve", lambda e: e.tensor_tensor(out=slot.t[:].rearrange("p t e -> p (t e)"), in0=pcum.t[:, 0:256], in1=offs.t[:].rearrange("p t e -> p (t e)"), op=ALU.add),
             r=[pcum.b, offs.b], w=[slot.b])
        for k in range(8):
            P.op("dve", lambda e, k=k: e.tensor_scalar(out=slot.t[:, :, k], in0=slot.t[:, :, k], scalar1=st512.t[:, k:k + 1], scalar2=None, op0=ALU.add),
                 r=[slot.b, st512.b], w=[slot.b])
        P.op("dve", lambda e: e.tensor_tensor(out=ohb.t[:], in0=MSK.t[:], in1=OHA.t[:], op=ALU.subtract), r=[MSK.b, OHA.b], w=[ohb.b])
        P.op("dve", lambda e: e.tensor_tensor(out=tmp3.t[:], in0=slot.t[:], in1=OHA.t[:], op=ALU.mult), r=[slot.b, OHA.b], w=[tmp3.b])
        P.op("dve", lambda e: e.tensor_reduce(out=slf.t[:, 0, :], in_=tmp3.t[:], axis=AX.X, op=ALU.add), r=[tmp3.b], w=[slf.b])
        P.op("dve", lambda e: e.tensor_tensor(out=tmp3.t[:], in0=slot.t[:], in1=ohb.t[:], op=ALU.mult), r=[slot.b, ohb.b, slf.b], w=[tmp3.b])
        P.op("dve", lambda e: e.tensor_reduce(out=slf.t[:, 1, :], in_=tmp3.t[:], axis=AX.X, op=ALU.add), r=[tmp3.b], w=[slf.b])
        P.op("dve", lambda e: e.tensor_copy(out=SLI.t[:], in_=slf.t[:]), r=[slf.b], w=[SLI.b])
        P.op("dve", lambda e: e.memset(eidf.t[:], 0.0), w=[eidf.b])
        for k in range(7):
            P.op("dve", lambda e, k=k: e.scalar_tensor_tensor(out=eidf.t[:], in0=iota23.t[:], scalar=tend.t[:, k:k + 1], in1=eidf.t[:], op0=ALU.is_ge, op1=ALU.add),
                 r=[iota23.b, tend.b, eidf.b], w=[eidf.b])
        P.op("dve", lambda e: e.tensor_scalar(out=e896.t[:], in0=eidf.t[:], scalar1=896.0, scalar2=None, op0=ALU.mult), r=[eidf.b], w=[e896.b])
        P.op("dve", lambda e: e.tensor_scalar(out=e1024.t[:], in0=eidf.t[:], scalar1=1024.0, scalar2=None, op0=ALU.mult), r=[eidf.b], w=[e1024.b])
        for i in range(NTILE):
            P.op("dve", lambda e, i=i: e.tensor_scalar(out=iwgf.t[:, i, :], in0=cwg.t[:], scalar1=e896.t[:, i:i + 1], scalar2=None, op0=ALU.add),
                 r=[cwg.b, e896.b], w=[iwgf.b])
            P.op("dve", lambda e, i=i: e.tensor_scalar(out=iwdf.t[:, i, :], in0=cwd.t[:], scalar1=e1024.t[:, i:i + 1], scalar2=None, op0=ALU.add),
                 r=[cwd.b, e1024.b], w=[iwdf.b])
        P.op("dve", lambda e: e.tensor_copy(out=IWG.t[:], in_=iwgf.t[:]), r=[iwgf.b], w=[IWG.b])
        P.op("dve", lambda e: e.tensor_copy(out=IWD.t[:], in_=iwdf.t[:]), r=[iwdf.b], w=[IWD.b])
        xb = [ph.sb([128, D], BF16, "xb") for _ in range(3)]
        for tt in range(NT):
            b_ = xb[tt % 3]
            P.op("gld", lambda e, b_=b_, tt=tt: e.dma_start(out=b_.t[:], in_=x3_d.ap()[tt * 128:(tt + 1) * 128, :]), w=[b_.b])
            for ab in range(2):
                P.op("gst", lambda e, b_=b_, tt=tt, ab=ab: e.indirect_dma_start(
                    out=xs_d.ap(), out_offset=bass.IndirectOffsetOnAxis(ap=SLI.t[:, ab, tt:tt + 1], axis=0), in_=b_.t[:], in_offset=None),
                    r=[b_.b, SLI.b])
        ph.close()

    def phase_H2():
        ph = Phase(P)
        NF = 28
        xs = [[ph.sb([128, D], BF16, "xs") for _ in range(4)] for _ in range(2)]
        xsT = [ph.sb([128, 8, 512], BF16, "xsT") for _ in range(2)]
        hT = ph.sb([128, NF, 512], BF16, "hT")
        wgp = [ph.sb([128, 4096], BF16, "wgp") for _ in range(3)]
        wup = [ph.sb([128, 4096], BF16, "wup") for _ in range(3)]
        wdp = [ph.sb([128, 7, 512], BF16, "wdp") for _ in range(8)]
        sg = [ph.sb([128, 512], F32, "sg") for _ in range(2)]
        ys = [ph.sb([128, 4, D], F32, "ys") for _ in range(2)]
        ptr = [ph.ps([128, 1024], BF16, "ptrh") for _ in range(2)]
        pg = [ph.ps([128, 512], F32, "pg") for _ in range(2)]
        pu = [ph.ps([128, 512], F32, "pu") for _ in range(2)]
        py = [ph.ps([128, 512], F32, "pyh") for _ in range(2)]
        wi = 0
        gi = 0
        yi = 0
        for i in range(NTILE):
            xs_ = xs[i % 2]; xT_ = xsT[i % 2]; ys_ = ys[i % 2]
            load_tok_T(ph, lambda j, i=i: xs_d.ap()[i * 512 + j * 128:i * 512 + (j + 1) * 128, :], 4, xs_, xT_, ptr, "xs", queue="ld")
            for k in range(8):
                P.op("gld", lambda e, k=k, i=i: e.indirect_dma_start(
                    out=wdp[k].t[:].rearrange("p f n -> p (f n)"), out_offset=None, in_=wds_d.ap(),
                    in_offset=bass.IndirectOffsetOnAxis(ap=IWD.t[:, i, k:k + 1], axis=0)), r=[IWD.b], w=[wdp[k].b])
                if k == 3:
                    pass
            for pc in range(7):
                wg_ = wgp[wi % 3]; wu_ = wup[wi % 3]; wi += 1
                P.op("gld", lambda e, wg_=wg_, pc=pc, i=i: e.indirect_dma_start(
                    out=wg_.t[:], out_offset=None, in_=wgs_d.ap(), in_offset=bass.IndirectOffsetOnAxis(ap=IWG.t[:, i, pc:pc + 1], axis=0)),
                    r=[IWG.b], w=[wg_.b])
                P.op("gld", lambda e, wu_=wu_, pc=pc, i=i: e.indirect_dma_start(
                    out=wu_.t[:], out_offset=None, in_=wus_d.ap(), in_offset=bass.IndirectOffsetOnAxis(ap=IWG.t[:, i, pc:pc + 1], axis=0)),
                    r=[IWG.b], w=[wu_.b])
                for fs in range(4):
                    fc = pc * 4 + fs
                    pg_ = pg[gi % 2]; pu_ = pu[gi % 2]; sg_ = sg[gi % 2]; gi += 1
                    for c in range(8):
                        P.op("pe", lambda e, pg_=pg_, wg_=wg_, c=c, fs=fs, xT_=xT_: e.matmul(
                            pg_.t[:], wg_.t[:, c * 512 + fs * 128:c * 512 + (fs + 1) * 128], xT_.t[:, c, :], start=(c == 0), stop=(c == 7)),
                            r=[wg_.b, xT_.b], w=[pg_.b])
                    P.op("act", lambda e, pg_=pg_, sg_=sg_: e.activation(out=sg_.t[:], in_=pg_.t[:], func=AF.Silu), r=[pg_.b], w=[sg_.b])
                    for c in range(8):
                        P.op("pe", lambda e, pu_=pu_, wu_=wu_, c=c, fs=fs, xT_=xT_: e.matmul(
                            pu_.t[:], wu_.t[:, c * 512 + fs * 128:c * 512 + (fs + 1) * 128], xT_.t[:, c, :], start=(c == 0), stop=(c == 7)),
                            r=[wu_.b, xT_.b], w=[pu_.b])
                    P.op("dve", lambda e, pu_=pu_, sg_=sg_, fc=fc: e.tensor_tensor(out=hT.t[:, fc, :], in0=pu_.t[:], in1=sg_.t[:], op=ALU.mult),
                         r=[pu_.b, sg_.b], w=[hT.b])
            for ch in range(2):
                for j in range(4):
                    py_ = py[yi % 2]; yi += 1
                    for q_ in range(4):
                        wd_ = wdp[ch * 4 + q_]
                        for fl in range(7):
                            fc = q_ * 7 + fl
                            P.op("pe", lambda e, py_=py_, fc=fc, j=j, wd_=wd_, fl=fl: e.matmul(
                                py_.t[:], hT.t[:, fc, j * 128:(j + 1) * 128], wd_.t[:, fl, :], start=(fc == 0), stop=(fc == NF - 1)),
                                r=[hT.b, wd_.b], w=[py_.b])
                    if yi % 2 == 0:
                        P.op("act", lambda e, py_=py_, ys_=ys_, j=j, ch=ch: e.copy(out=ys_.t[:, j, ch * 512:(ch + 1) * 512], in_=py_.t[:]), r=[py_.b], w=[ys_.b])
                    else:
                        P.op("dve", lambda e, py_=py_, ys_=ys_, j=j, ch=ch: e.tensor_copy(out=ys_.t[:, j, ch * 512:(ch + 1) * 512], in_=py_.t[:]), r=[py_.b], w=[ys_.b])
            P.op("st", lambda e, ys_=ys_, i=i: e.dma_start(out=ys_d.ap()[i * 512:(i + 1) * 512, :].rearrange("(j p) n -> p j n", p=128), in_=ys_.t[:]), r=[ys_.b])
        ph.close()

    def phase_H3():
        ph = Phase(P)
        gB_ = ph.sb([128, D], F32, "gB")
        bB_ = ph.sb([128, D], F32, "bB")
        P.op("ld", lambda e: e.dma_start(out=gB_.t[:], in_=bcast_row(oln2g)), w=[gB_.b])
        P.op("ld", lambda e: e.dma_start(out=bB_.t[:], in_=bcast_row(oln2b)), w=[bB_.b])
        xf = [ph.sb([128, D], F32, "xf") for _ in range(2)]
        ya = [ph.sb([128, D], F32, "ya") for _ in range(2)]
        yb = [ph.sb([128, D], F32, "yb") for _ in range(2)]
        z = [ph.sb([128, D], F32, "z") for _ in range(2)]
        xo = [ph.sb([128, D], F32, "xo") for _ in range(2)]
        st6 = [ph.sb([128, 2, 6], F32, "st6") for _ in range(2)]
        mv = [ph.sb([128, 2], F32, "mv") for _ in range(2)]
        rstd = [ph.sb([128, 1], F32, "rstd") for _ in range(2)]
        for tt in range(NT):
            i2 = tt % 2
            x_ = xf[i2]; a_ = ya[i2]; b_ = yb[i2]; z_ = z[i2]
            P.op("ld", lambda e, x_=x_, tt=tt: e.dma_start(out=x_.t[:], in_=x3_d.ap()[tt * 128:(tt + 1) * 128, :]), w=[x_.b])
            P.op("gld", lambda e, a_=a_, tt=tt: e.indirect_dma_start(out=a_.t[:], out_offset=None, in_=ys_d.ap(),
                                                                   in_offset=bass.IndirectOffsetOnAxis(ap=SLI.t[:, 0, tt:tt + 1], axis=0)), r=[SLI.b], w=[a_.b])
            P.op("gld", lambda e, b_=b_, tt=tt: e.indirect_dma_start(out=b_.t[:], out_offset=None, in_=ys_d.ap(),
                                                                   in_offset=bass.IndirectOffsetOnAxis(ap=SLI.t[:, 1, tt:tt + 1], axis=0)), r=[SLI.b], w=[b_.b])
            P.op("dve", lambda e, x_=x_, a_=a_, z_=z_, tt=tt: e.tensor_scalar(out=z_.t[:], in0=a_.t[:], scalar1=GA.t[:, tt:tt + 1], scalar2=None, op0=ALU.mult),
                 r=[a_.b, GA.b], w=[z_.b])
            P.op("dve", lambda e, b_=b_, z_=z_, tt=tt: e.scalar_tensor_tensor(out=z_.t[:], in0=b_.t[:], scalar=GB.t[:, tt:tt + 1], in1=z_.t[:], op0=ALU.mult, op1=ALU.add),
                 r=[b_.b, GB.b, z_.b], w=[z_.b])
            P.op("dve", lambda e, x_=x_, z_=z_: e.scalar_tensor_tensor(out=z_.t[:], in0=x_.t[:], scalar=ALPHA, in1=z_.t[:], op0=ALU.mult, op1=ALU.add),
                 r=[x_.b, z_.b], w=[z_.b])
            layernorm_rows(ph, z_, gB_, bB_, xo[i2], st6[i2], mv[i2], rstd[i2], None)
            P.op("st", lambda e, i2=i2, tt=tt: e.dma_start(out=out_d.ap()[tt * 128:(tt + 1) * 128, :], in_=xo[i2].t[:]), r=[xo[i2].b])
        ph.close()

    phases = [
        ("T", phase_tables),
        ("A", phase_A),
        ("B", phase_B),
        ("C", phase_C),
        ("D", lambda: phase_outproj("catT", ewout, eln1g, eln1b, x_d, x1_d)),
        ("E", phase_E),
        ("F0", lambda: phase_F(0)),
        ("F1", lambda: phase_F(1)),
        ("F2", lambda: phase_F(2)),
        ("G", lambda: phase_outproj("ug", owout, oln1g, oln1b, x2_d, x3_d)),
        ("H1", phase_H1),
        ("H2", phase_H2),
        ("H3", phase_H3),
    ]
    P.flush(barrier=True)
    for name, fn in phases:
        if name in skip:
            continue
        fn()
        if stop_after == name:
            break
    G.close()
    P.barrier(issuers=("sp",))
    return nc


INPUT_NAMES = ["x", "rel_bias", "even_w_in", "even_conv_w", "even_conv_b", "even_conv_ln_g", "even_conv_ln_b", "even_w_out",
               "even_ln1_g", "even_ln1_b", "even_ffn_wg", "even_ffn_wu", "even_ffn_wd", "even_ln2_g", "even_ln2_b",
               "odd_w_in", "odd_w_out", "odd_ln1_g", "odd_ln1_b", "odd_router", "odd_moe_wg", "odd_moe_wu", "odd_moe_wd",
               "odd_ln2_g", "odd_ln2_b"]


def make_in_maps(inputs, n_cores=8):
    cs = host_consts()
    shared = {}
    for k in INPUT_NAMES:
        if k == "x":
            continue
        a = np.ascontiguousarray(np.asarray(inputs[k], dtype=np.float32))
        if k == "rel_bias":
            shared[k] = a
        elif a.ndim >= 2 and a.shape[0] == 1:
            shared[k] = np.ascontiguousarray(a[0]) if a.ndim > 2 else a
        else:
            shared[k] = a
    for k, v in cs.items():
        shared["c_" + k] = v
    x = np.asarray(inputs["x"], dtype=np.float32)
    maps = []
    for i in range(n_cores):
        m = dict(shared)
        m["x"] = np.ascontiguousarray(x[i])
        maps.append(m)
    return maps


def kernel(**inputs):
    nc = build()
    maps = make_in_maps(inputs, 8)
    res = run_bass_kernel_spmd(nc, maps, core_ids=list(range(8)))
    out = np.stack([np.asarray(r["out"], dtype=np.float32) for r in res.results], axis=0)
    return out
```

```python
import math
import bisect
from contextlib import ExitStack

import numpy as np
import concourse.bass as bass
import concourse.mybir as mybir
from concourse.bass_utils import run_bass_kernel_spmd

F32 = mybir.dt.float32
BF16 = mybir.dt.bfloat16
AF = mybir.ActivationFunctionType
ALU = mybir.AluOpType
AX = mybir.AxisListType

S = 4096
D = 1024
NT = S // 128
ALPHA = 4 ** 0.25
EPS = 1e-5
NEGM = -30000.0
NIT = 12
EPOCH = 4000
KDMA = 12


class Buf:
    __slots__ = ("name", "w", "rs")

    def __init__(self, name=""):
        self.name = name
        self.w = []
        self.rs = []


class Stream:
    def __init__(self, name, issuer, is_dma):
        self.name = name
        self.issuer = issuer
        self.is_dma = is_dma
        self.ops = []
        self.n_total = 0
        self.inc_idx = []
        self.n_inc = 0
        self.sems = []


class Op:
    __slots__ = ("stream", "fn", "deps", "idx", "inc")


class _Rec:
    __slots__ = ("call",)

    def __init__(self):
        self.call = None

    def __getattr__(self, name):
        def f(*a, **k):
            self.call = (name, a, k)
        return f


class Prog:
    def __init__(self, nc):
        self.nc = nc
        self.es = ExitStack()
        self.eng = {"pe": nc.tensor, "act": nc.scalar, "dve": nc.vector, "pool": nc.gpsimd, "sp": nc.sync}
        self.streams = {}
        for n in ("pe", "act", "dve", "pool"):
            self.streams[n] = Stream(n, n, False)
        for n, iss in (("ld", "sp"), ("st", "sp"), ("gld", "pool"), ("gst", "pool")):
            self.streams[n] = Stream(n, iss, True)
        self.order = []
        self.waited = {}

    def op(self, sname, fn, r=(), w=(), lazy=False):
        st = self.streams[sname]
        o = Op()
        o.stream = st
        if lazy:
            o.fn = fn
        else:
            rec = _Rec()
            fn(rec)
            assert rec.call is not None
            o.fn = rec.call
        o.idx = st.n_total
        o.inc = False
        st.n_total += 1
        deps = set()
        me = (sname, o.idx)
        for b in r:
            for wr in b.w:
                deps.add(wr + ("raw",))
        for b in w:
            for wr in b.w:
                deps.add(wr + ("waw",))
            for rd in b.rs:
                deps.add(rd + ("war",))
        fd = set()
        best = {}
        for (s, i, kind) in deps:
            if s == sname:
                if sname == "pe":
                    continue
                if kind == "waw":
                    continue
                if not st.is_dma and kind != "raw":
                    continue
            if self.streams[s].is_dma:
                fd.add((s, i))
            else:
                if s not in best or best[s] < i:
                    best[s] = i
        for s, i in best.items():
            fd.add((s, i))
        o.deps = fd
        for b in r:
            b.rs.append(me)
        for b in w:
            if st.is_dma and b.w and not b.rs and all(x[0] == sname for x in b.w):
                b.w.append(me)
            else:
                b.w = [me]
            b.rs = []
        st.ops.append(o)
        self.order.append(o)
        return o

    def _sem_for(self, st, ordinal):
        ep = (ordinal - 1) // EPOCH
        while len(st.sems) <= ep:
            st.sems.append(self.es.enter_context(self.nc.semaphore("s_%s_%d" % (st.name, len(st.sems)))))
        return st.sems[ep], (ordinal - 1) % EPOCH + 1

    def _dma_sem(self, st, idx):
        k = idx % KDMA
        while len(st.sems) <= k:
            st.sems.append(self.es.enter_context(self.nc.semaphore("d_%s_%d" % (st.name, len(st.sems)))))
        return k, st.sems[k], 16 * (idx // KDMA + 1)

    def _wait_dma(self, issuer, st, idx):
        k, sem, val = self._dma_sem(st, idx)
        key = (issuer, st.name, k)
        if self.waited.get(key, 0) >= val:
            return
        self.waited[key] = val
        self.eng[issuer].wait_ge(sem, val)

    def _wait(self, issuer, st, ordinal):
        sem, val = self._sem_for(st, ordinal)
        key = (issuer, st.name)
        if self.waited.get(key, 0) >= ordinal:
            return
        self.waited[key] = ordinal
        self.eng[issuer].wait_ge(sem, val)

    def flush(self, barrier=True):
        need = {}
        for o in self.order:
            for s, i in o.deps:
                need.setdefault(s, set()).add(i)
        for sname, st in self.streams.items():
            if not st.ops or st.is_dma:
                continue
            tg = need.get(sname, set())
            for o in st.ops:
                if o.idx in tg:
                    o.inc = True
            st.ops[-1].inc = True
        ordinal_of = {}
        for sname, st in self.streams.items():
            if st.is_dma:
                continue
            for o in st.ops:
                if o.inc:
                    st.n_inc += 1
                    st.inc_idx.append(o.idx)
                    ordinal_of[(sname, o.idx)] = st.n_inc
        for o in self.order:
            st = o.stream
            issuer = st.issuer
            for s, i in sorted(o.deps):
                ps = self.streams[s]
                if ps.is_dma:
                    self._wait_dma(issuer, ps, i)
                else:
                    k = bisect.bisect_left(ps.inc_idx, i)
                    assert k < len(ps.inc_idx), (s, i)
                    self._wait(issuer, ps, k + 1)
            if st.is_dma and o.idx >= KDMA:
                self._wait_dma(issuer, st, o.idx - KDMA)
            if callable(o.fn):
                inst = o.fn(self.eng[issuer])
            else:
                nm, a_, k_ = o.fn
                inst = getattr(self.eng[issuer], nm)(*a_, **k_)
            if st.is_dma:
                k, sem, val = self._dma_sem(st, o.idx)
                inst.then_inc(sem, 16)
            elif o.inc:
                sem, val = self._sem_for(st, ordinal_of[(st.name, o.idx)])
                inst.then_inc(sem, 1)
        self.order = []
        for st in self.streams.values():
            st.ops = []
        if barrier:
            self.barrier()

    def barrier(self, issuers=("pe", "act", "dve", "pool", "sp")):
        for iss in issuers:
            for st in self.streams.values():
                if st.is_dma:
                    for i in range(max(0, st.n_total - KDMA), st.n_total):
                        self._wait_dma(iss, st, i)
                elif st.n_inc > 0:
                    self._wait(iss, st, st.n_inc)


class T:
    __slots__ = ("t", "b")

    def __init__(self, t, name=""):
        self.t = t
        self.b = Buf(name)


class Phase:
    cnt = 0

    def __init__(self, P):
        self.P = P
        self.nc = P.nc
        self.es = ExitStack()
        self.n = 0

    def sb(self, shape, dt, name="t"):
        Phase.cnt += 1
        nm = "%s_%d" % (name, Phase.cnt)
        return T(self.es.enter_context(self.nc.sbuf_tensor(nm, list(shape), dt)), nm)

    def ps(self, shape, dt, name="p"):
        Phase.cnt += 1
        nm = "%s_%d" % (name, Phase.cnt)
        return T(self.es.enter_context(self.nc.psum_tensor(nm, list(shape), dt)), nm)

    def close(self):
        self.P.flush(barrier=True)
        self.es.close()


def _bucket(n):
    n = np.asarray(n).astype(np.int64)
    nf = np.maximum(n, 1).astype(np.float32)
    large = 16 + (np.log(nf / np.float32(16)) / np.float32(math.log(2048 / 16)) * 16).astype(np.int32)
    large = np.minimum(large, 31)
    return np.where(n < 16, n, large)


L0 = 2688
L1 = 384


def host_consts():
    c = {}
    c["ident"] = np.eye(128, dtype=np.float32)
    c["anti"] = np.eye(128, dtype=np.float32)[::-1].copy()
    qi = np.arange(128)[:, None]
    ki = np.arange(128)[None, :]
    c["causneg"] = np.where(ki <= qi, 0.0, -1e30).astype(np.float32)
    n = np.arange(L0)
    b0 = _bucket(np.maximum(n - 511, 0))
    oh0 = np.zeros((32, L0), np.float32)
    oh0[b0, n] = 1.0
    c["oh0"] = oh0
    oh1 = np.zeros((3, 33, L1), np.float32)
    n1 = np.arange(L1)
    dist = n1 - 127
    valid = (dist >= 0) & (dist <= 128)
    for g, r in enumerate((1, 4, 16)):
        bb = _bucket(np.maximum(dist, 0) * r)
        oh1[g, bb[valid], n1[valid]] = 1.0
        oh1[g, 32, :] = np.where(valid, 0.0, NEGM)
    c["oh1"] = oh1
    c["pow2"] = np.tile((0.5 ** np.arange(NIT + 2)).astype(np.float32)[None, :], (128, 1))
    pp = np.arange(128)
    c["ustr"] = (pp[:, None] < pp[None, :]).astype(np.float32)
    c["thr8"] = np.tile((512.0 * np.arange(8)).astype(np.float32)[None, :], (128, 1))
    c["iota23"] = np.tile(np.arange(23, dtype=np.float32)[None, :], (128, 1))
    c["cwg"] = (np.arange(7)[None, :] * 128 + pp[:, None]).astype(np.float32)
    c["cwd"] = (np.arange(8)[None, :] * 128 + pp[:, None]).astype(np.float32)
    return c


def build(stop_after=None, debug=(), hcfg=(4, 8), skip=(), feed=()):
    nc = bass.Bass("TRN2", target_bir_lowering=False)
    P = Prog(nc)

    def din(name, shape, dt=F32):
        return nc.dram_tensor(name, list(shape), dt, kind="ExternalInput")

    def dscr(name, shape, dt):
        kind = "ExternalOutput" if name in debug else ("ExternalInput" if name in feed else "Internal")
        return nc.dram_tensor(name, list(shape), dt, kind=kind)

    x_d = din("x", [S, D])
    rb_d = din("rel_bias", [32, 8])
    ewin = din("even_w_in", [D, 3144])
    ecw = din("even_conv_w", [31, 512])
    ecb = din("even_conv_b", [1, 512])
    ecg = din("even_conv_ln_g", [1, 512])
    ecbb = din("even_conv_ln_b", [1, 512])
    ewout = din("even_w_out", [D, D])
    eln1g = din("even_ln1_g", [1, D])
    eln1b = din("even_ln1_b", [1, D])
    ewg = din("even_ffn_wg", [D, 2816])
    ewu = din("even_ffn_wu", [D, 2816])
    ewd = din("even_ffn_wd", [2816, D])
    eln2g = din("even_ln2_g", [1, D])
    eln2b = din("even_ln2_b", [1, D])
    owin = din("odd_w_in", [D, 9216])
    owout = din("odd_w_out", [D, D])
    oln1g = din("odd_ln1_g", [1, D])
    oln1b = din("odd_ln1_b", [1, D])
    orouter = din("odd_router", [D, 8])
    omwg = din("odd_moe_wg", [8, D, 3584])
    omwu = din("odd_moe_wu", [8, D, 3584])
    omwd = din("odd_moe_wd", [8, 3584, D])
    oln2g = din("odd_ln2_g", [1, D])
    oln2b = din("odd_ln2_b", [1, D])
    c_ident = din("c_ident", [128, 128])
    c_anti = din("c_anti", [128, 128])
    c_caus = din("c_causneg", [128, 128])
    c_oh0 = din("c_oh0", [32, L0])
    c_oh1 = din("c_oh1", [3, 33, L1])
    c_pow2 = din("c_pow2", [128, NIT + 2])
    c_ustr = din("c_ustr", [128, 128])
    c_thr8 = din("c_thr8", [128, 8])
    c_iota23 = din("c_iota23", [128, 23])
    c_cwg = din("c_cwg", [128, 7])
    c_cwd = din("c_cwd", [128, 8])

    out_d = nc.dram_tensor("out", [S, D], F32, kind="ExternalOutput")

    qT_d = dscr("qT", [512, S], BF16)
    kT_d = dscr("kT", [512, S], BF16)
    qiT_d = dscr("qiT", [512, S], BF16)
    kiT_d = dscr("kiT", [128, S], BF16)
    v_d = dscr("vaug", [S, 520], BF16)
    w_d = dscr("widx", [S, 8], F32)
    catT_d = dscr("catT", [D, S], BF16)
    mask_d = dscr("maskneg", [S, S], BF16)
    f0_d = dscr("f0tab", [8, L0], BF16)
    f1_d = dscr("f1tab", [3, 8, L1], BF16)
    x1_d = dscr("x1", [S, D], F32)
    x2_d = dscr("x2", [S, D], F32)
    u_d = dscr("ug", [3, S, 8, 132], F32)
    x3_d = dscr("x3", [S, D], F32)
    wgs_d = dscr("wgs", [56 * 128, 4096], BF16)
    wus_d = dscr("wus", [56 * 128, 4096], BF16)
    wds_d = dscr("wds", [64 * 128, 3584], BF16)
    xs_d = dscr("xs", [23 * 512, D], BF16)
    ys_d = dscr("ys", [23 * 512, D], F32)

    G = Phase(P)
    ident_f = G.sb([128, 128], F32, "identf")
    ident_b = G.sb([128, 128], BF16, "identb")
    anti_b = G.sb([128, 128], BF16, "antib")
    ones_f = G.sb([128, 128], F32, "onesf")
    P.op("ld", lambda e: e.dma_start(out=ident_f.t[:], in_=c_ident.ap()), w=[ident_f.b])
    P.op("gld", lambda e: e.dma_start(out=ident_b.t[:], in_=c_ident.ap()), w=[ident_b.b])
    P.op("gld", lambda e: e.dma_start(out=anti_b.t[:], in_=c_anti.ap()), w=[anti_b.b])
    P.op("dve", lambda e: e.memset(ones_f.t[:], 1.0), w=[ones_f.b])
    GA = G.sb([128, NT], F32, "GA")
    GB = G.sb([128, NT], F32, "GB")
    SLI = G.sb([128, 2, NT], mybir.dt.int32, "SLI")
    IWG = G.sb([128, 23, 7], mybir.dt.int32, "IWG")
    IWD = G.sb([128, 23, 8], mybir.dt.int32, "IWD")
    eps_t = G.sb([128, 1], F32, "epst")
    P.op("dve", lambda e: e.memset(eps_t.t[:], EPS), w=[eps_t.b])
    zer_b = G.sb([128, 512], BF16, "zerb")
    P.op("dve", lambda e: e.memset(zer_b.t[:], 0.0), w=[zer_b.b])

    def evac(i, fn_act, fn_dve):
        return ("act", fn_act) if i % 2 == 0 else ("dve", fn_dve)

    def load_tok_T(ph, src_rows_ap_fn, ntiles, xtok, xT, pts, tag, f32_copy=None, queue="gld"):
        for j in range(ntiles):
            P.op(queue, lambda e, j=j: e.dma_start(out=xtok[j].t[:], in_=src_rows_ap_fn(j)), w=[xtok[j].b])
        k = 0
        for j0 in range(0, ntiles, 4):
            nj = min(4, ntiles - j0)
            for c in range(8):
                pt = pts[k % len(pts)]
                k += 1
                for jj in range(nj):
                    P.op("pe", lambda e, pt=pt, jj=jj, c=c, j0=j0: e.transpose(
                        out=pt.t[:, jj * 128:(jj + 1) * 128], in_=xtok[j0 + jj].t[:, c * 128:(c + 1) * 128],
                        identity=ident_b.t[:]), r=[xtok[j0 + jj].b, ident_b.b], w=[pt.b])
                if k % 2 == 0:
                    P.op("act", lambda e, pt=pt, c=c, j0=j0, nj=nj: e.copy(
                        out=xT.t[:, c, j0 * 128:(j0 + nj) * 128], in_=pt.t[:, 0:nj * 128]), r=[pt.b], w=[xT.b])
                else:
                    P.op("dve", lambda e, pt=pt, c=c, j0=j0, nj=nj: e.tensor_copy(
                        out=xT.t[:, c, j0 * 128:(j0 + nj) * 128], in_=pt.t[:, 0:nj * 128]), r=[pt.b], w=[xT.b])

    def layernorm_rows(ph, z, gB, bB, outt, st6, mv, rstd, nb, gb_eng="dve"):
        for hh in range(2):
            P.op("dve", lambda e, hh=hh: e.bn_stats(out=st6.t[:, hh, :], in_=z.t[:, hh * 512:(hh + 1) * 512]),
                 r=[z.b], w=[st6.b])
        P.op("dve", lambda e: e.bn_aggr(out=mv.t[:], in_=st6.t[:]), r=[st6.b], w=[mv.b])
        P.op("act", lambda e: e.activation(out=rstd.t[:, 0:1], in_=mv.t[:, 1:2], func=AF.Sqrt, bias=eps_t.t[:, 0:1], scale=1.0),
             r=[mv.b, eps_t.b], w=[rstd.b])
        P.op("dve", lambda e: e.reciprocal(out=rstd.t[:, 0:1], in_=rstd.t[:, 0:1]), r=[rstd.b], w=[rstd.b])
        P.op("dve", lambda e: e.scalar_tensor_tensor(out=rstd.t[:, 1:2], in0=mv.t[:, 0:1], scalar=-1.0, in1=rstd.t[:, 0:1],
                                                     op0=ALU.mult, op1=ALU.mult), r=[mv.b, rstd.b], w=[rstd.b])
        P.op("act", lambda e: e.activation(out=z.t[:], in_=z.t[:], func=AF.Identity, bias=rstd.t[:, 1:2], scale=rstd.t[:, 0:1]),
             r=[z.b, rstd.b], w=[z.b])
        P.op(gb_eng, lambda e: e.tensor_tensor(out=z.t[:], in0=z.t[:], in1=gB.t[:], op=ALU.mult),
             r=[z.b, gB.b], w=[z.b])
        P.op(gb_eng, lambda e: e.tensor_tensor(out=outt.t[:], in0=z.t[:], in1=bB.t[:], op=ALU.add),
             r=[z.b, bB.b], w=[outt.b])

    def bcast_row(dr):
        a = dr.ap()
        n = a.shape[-1]
        return bass.AP(tensor=a.tensor, offset=0, ap=[[0, 128], [1, n]])

    def phase_tables():
        ph = Phase(P)
        rb33 = ph.sb([33, 8], BF16, "rb33")
        rb33f = ph.sb([33, 8], F32, "rb33f")
        oh0 = ph.sb([32, L0], BF16, "oh0")
        oh1 = ph.sb([33, 3, L1], BF16, "oh1")
        f0 = ph.sb([8, L0], BF16, "f0")
        f1 = ph.sb([8, 3, L1], BF16, "f1")
        pp = [ph.ps([128, 512], F32, "pp") for _ in range(2)]
        P.op("dve", lambda e: e.memset(rb33f.t[:], 1.0), w=[rb33f.b])
        P.op("ld", lambda e: e.dma_start(out=rb33f.t[0:32, :], in_=rb_d.ap()), r=[], w=[rb33f.b])
        P.op("dve", lambda e: e.tensor_copy(out=rb33.t[:], in_=rb33f.t[:]), r=[rb33f.b], w=[rb33.b])
        P.op("gld", lambda e: e.dma_start(out=oh0.t[:], in_=c_oh0.ap()), w=[oh0.b])
        P.op("gld", lambda e: e.dma_start(out=oh1.t[:], in_=c_oh1.ap().rearrange("g k n -> k g n")), w=[oh1.b])
        k = 0
        for c0 in range(0, L0, 512):
            n = min(512, L0 - c0)
            p = pp[k % 2]
            k += 1
            P.op("pe", lambda e, p=p, c0=c0, n=n: e.matmul(p.t[0:8, 0:n], rb33.t[0:32, :], oh0.t[:, c0:c0 + n],
                                                          start=True, stop=True), r=[rb33.b, oh0.b], w=[p.b])
            P.op("dve", lambda e, p=p, c0=c0, n=n: e.tensor_copy(out=f0.t[:, c0:c0 + n], in_=p.t[0:8, 0:n]),
                 r=[p.b], w=[f0.b])
        for g in range(3):
            p = pp[k % 2]
            k += 1
            P.op("pe", lambda e, p=p, g=g: e.matmul(p.t[0:8, 0:L1], rb33.t[:, :], oh1.t[:, g, :],
                                                    start=True, stop=True), r=[rb33.b, oh1.b], w=[p.b])
            P.op("dve", lambda e, p=p, g=g: e.tensor_copy(out=f1.t[:, g, :], in_=p.t[0:8, 0:L1]),
                 r=[p.b], w=[f1.b])
        P.op("st", lambda e: e.dma_start(out=f0_d.ap(), in_=f0.t[:]), r=[f0.b])
        P.op("st", lambda e: e.dma_start(out=f1_d.ap().rearrange("g h n -> h g n"), in_=f1.t[:]), r=[f1.b])
        ph.close()

    def phase_A():
        ph = Phase(P)
        win = ph.sb([128, 8, 3144], BF16, "win")
        wki2 = ph.sb([128, 8, 128], BF16, "wki2")
        wv_ap = ewin.ap().rearrange("(c p) n -> p c n", p=128)
        for c in range(8):
            P.op("gld", lambda e, c=c: e.dma_start(out=win.t[:, c, :], in_=wv_ap[:, c, :]), w=[win.b])
        P.op("gld", lambda e: e.dma_start(out=wki2.t[:, :, 0:64], in_=wv_ap[:, :, 3072:3136]), w=[wki2.b])
        P.op("gld", lambda e: e.dma_start(out=wki2.t[:, :, 64:128], in_=wv_ap[:, :, 3072:3136]), w=[wki2.b])
        cw_sb = ph.sb([34, 512], F32, "cwsb")
        cwT = ph.sb([128, 4, 34], F32, "cwT")
        P.op("ld", lambda e: e.dma_start(out=cw_sb.t[0:31, :], in_=ecw.ap()), w=[cw_sb.b])
        P.op("ld", lambda e: e.dma_start(out=cw_sb.t[31:32, :], in_=ecb.ap()), w=[cw_sb.b])
        P.op("ld", lambda e: e.dma_start(out=cw_sb.t[32:33, :], in_=ecg.ap()), w=[cw_sb.b])
        P.op("ld", lambda e: e.dma_start(out=cw_sb.t[33:34, :], in_=ecbb.ap()), w=[cw_sb.b])
        pm = [ph.ps([128, 512], F32, "pm") for _ in range(4)]
        pst = [ph.ps([128, 512], F32, "pst") for _ in range(2)]
        ptr = [ph.ps([128, 1024], BF16, "ptr") for _ in range(2)]
        for c in range(4):
            P.op("pe", lambda e, c=c: e.transpose(out=pm[0].t[:, c * 34:(c + 1) * 34], in_=cw_sb.t[0:34, c * 128:(c + 1) * 128],
                                                  identity=ident_f.t[0:34, 0:34]), r=[cw_sb.b, ident_f.b], w=[pm[0].b])
        P.op("dve", lambda e: e.tensor_copy(out=cwT.t[:].rearrange("p c j -> p (c j)"), in_=pm[0].t[:, 0:136]),
             r=[pm[0].b], w=[cwT.b])
        diag = ph.sb([128, 4, 31, 128], BF16, "diag")
        for c in range(4):
            for j in range(31):
                eng = "dve" if (c * 31 + j) % 2 == 0 else "pool"
                P.op(eng, lambda e, c=c, j=j: e.tensor_scalar(out=diag.t[:, c, j, :], in0=ident_f.t[:],
                                                              scalar1=cwT.t[:, c, j:j + 1], scalar2=None, op0=ALU.mult),
                     r=[ident_f.b, cwT.b], w=[diag.b])
        xtok = [ph.sb([128, 1024], BF16, "xtok") for _ in range(4)]
        xT = [ph.sb([128, 8, 512], BF16, "xT") for _ in range(2)]
        ub = [ph.sb([128, 4, 542], BF16, "ub") for _ in range(2)]
        stg = ph.sb([128, 13, 512], BF16, "stg")
        aout = ph.sb([128, 4, 512], BF16, "aout")
        sg = [ph.sb([128, 512], F32, "sg") for _ in range(2)]
        yv = ph.sb([128, 4, 512], F32, "yv")
        ysq = ph.sb([128, 4, 512], F32, "ysq")
        mean = ph.sb([128, 512], F32, "mean")
        rstd = ph.sb([128, 512], F32, "rstd")
        tmp = ph.sb([128, 512], F32, "tmp")
        vst = ph.sb([128, 4, 8, 65], BF16, "vst")
        wst = ph.sb([128, 4, 8], F32, "wst")
        P.op("dve", lambda e: e.memset(ub[0].t[:, :, 0:30], 0.0), w=[ub[0].b])
        P.op("dve", lambda e: e.memset(vst.t[:], 1.0), w=[vst.b])
        xrows = x_d.ap()
        qTv = qT_d.ap().rearrange("(c p) t -> p c t", p=128)
        kTv = kT_d.ap().rearrange("(c p) t -> p c t", p=128)
        qiTv = qiT_d.ap().rearrange("(c p) t -> p c t", p=128)
        catTv = catT_d.ap().rearrange("(c p) t -> p c t", p=128)
        WSCALE = float(8 ** -0.5 * 64 ** -0.5)
        pmi = 0
        for sbk in range(8):
            t0 = sbk * 512
            xt = xT[sbk % 2]
            u_cur = ub[sbk % 2]
            u_prev = ub[(sbk + 1) % 2]
            load_tok_T(ph, lambda j, t0=t0: xrows[t0 + j * 128:t0 + (j + 1) * 128, :], 4, xtok, xt, ptr, "x")
            if sbk > 0:
                P.op("dve", lambda e, u_cur=u_cur, u_prev=u_prev: e.tensor_copy(out=u_cur.t[:, :, 0:30], in_=u_prev.t[:, :, 512:542]),
                     r=[u_prev.b], w=[u_cur.b])

            def proj_fm(col0, wt, pmt):
                for c in range(8):
                    P.op("pe", lambda e, c=c: e.matmul(pmt.t[:], wt.t[:, c, col0:col0 + 128], xt.t[:, c, :],
                                                       start=(c == 0), stop=(c == 7)), r=[wt.b, xt.b], w=[pmt.b])
            for c in range(4):
                pg = pm[pmi % 4]; pmi += 1
                pv = pm[pmi % 4]; pmi += 1
                sgt = sg[c % 2]
                proj_fm(512 + c * 128, win, pg)
                P.op("act", lambda e, pg=pg, sgt=sgt: e.activation(out=sgt.t[:], in_=pg.t[:], func=AF.Sigmoid),
                     r=[pg.b], w=[sgt.b])
                proj_fm(c * 128, win, pv)
                P.op("dve", lambda e, pv=pv, sgt=sgt, c=c, u_cur=u_cur: e.tensor_tensor(
                    out=u_cur.t[:, c, 30:542], in0=pv.t[:], in1=sgt.t[:], op=ALU.mult), r=[pv.b, sgt.b], w=[u_cur.b])
            for i in range(13):
                if i < 4:
                    col0, wt, scale = 1024 + i * 128, win, 0.125
                elif i < 8:
                    col0, wt, scale = 1536 + (i - 4) * 128, win, 1.0
                elif i < 12:
                    col0, wt, scale = 2560 + (i - 8) * 128, win, 1.0
                else:
                    col0, wt, scale = 0, wki2, 1.0
                pq = pm[pmi % 4]; pmi += 1
                proj_fm(col0, wt, pq)
                if i % 2 == 0:
                    P.op("act", lambda e, pq=pq, i=i, scale=scale: e.activation(out=stg.t[:, i, :], in_=pq.t[:], func=AF.Copy, scale=scale),
                         r=[pq.b], w=[stg.b])
                else:
                    P.op("dve", lambda e, pq=pq, i=i, scale=scale: e.tensor_scalar(out=stg.t[:, i, :], in0=pq.t[:], scalar1=scale,
                                                                               scalar2=None, op0=ALU.mult), r=[pq.b], w=[stg.b])
            P.op("st", lambda e, t0=t0: e.dma_start(out=qTv[:, :, t0:t0 + 512], in_=stg.t[:, 0:4, :]), r=[stg.b])
            P.op("st", lambda e, t0=t0: e.dma_start(out=kTv[:, :, t0:t0 + 512], in_=stg.t[:, 4:8, :]), r=[stg.b])
            P.op("st", lambda e, t0=t0: e.dma_start(out=qiTv[:, :, t0:t0 + 512], in_=stg.t[:, 8:12, :]), r=[stg.b])
            P.op("st", lambda e, t0=t0: e.dma_start(out=kiT_d.ap()[:, t0:t0 + 512], in_=stg.t[:, 12, :]), r=[stg.b])
            for j in range(4):
                pv = pm[pmi % 4]; pmi += 1
                for c in range(8):
                    P.op("pe", lambda e, c=c, j=j, pv=pv: e.matmul(pv.t[:], xt.t[:, c, j * 128:(j + 1) * 128], win.t[:, c, 2048:2560],
                                                                 start=(c == 0), stop=(c == 7)), r=[win.b, xt.b], w=[pv.b])
                P.op("act", lambda e, j=j, pv=pv: e.copy(out=vst.t[:, j, :, 0:64], in_=pv.t[:].rearrange("p (h d) -> p h d", h=8)),
                     r=[pv.b], w=[vst.b])
                pw = pm[pmi % 4]; pmi += 1
                for c in range(8):
                    P.op("pe", lambda e, c=c, j=j, pw=pw: e.matmul(pw.t[:, 0:8], xt.t[:, c, j * 128:(j + 1) * 128], win.t[:, c, 3136:3144],
                                                                 start=(c == 0), stop=(c == 7)), r=[win.b, xt.b], w=[pw.b])
                P.op("dve", lambda e, j=j, pw=pw: e.tensor_scalar(out=wst.t[:, j, :], in0=pw.t[:, 0:8], scalar1=WSCALE, scalar2=None,
                                                               op0=ALU.mult), r=[pw.b], w=[wst.b])
            P.op("st", lambda e, t0=t0: e.dma_start(out=v_d.ap()[t0:t0 + 512, :].rearrange("(j p) n -> p j n", p=128),
                                                    in_=vst.t[:].rearrange("p j h d -> p j (h d)")), r=[vst.b])
            P.op("st", lambda e, t0=t0: e.dma_start(out=w_d.ap()[t0:t0 + 512, :].rearrange("(j p) n -> p j n", p=128),
                                                    in_=wst.t[:]), r=[wst.b])
            for c in range(4):
                pc = pm[pmi % 4]; pmi += 1
                for j in range(31):
                    P.op("pe", lambda e, c=c, j=j, pc=pc, u_cur=u_cur: e.matmul(pc.t[:], diag.t[:, c, j, :], u_cur.t[:, c, j:j + 512],
                                                                             start=(j == 0), stop=(j == 30)), r=[diag.b, u_cur.b], w=[pc.b])
                P.op("act", lambda e, c=c, pc=pc: e.activation(out=yv.t[:, c, :], in_=pc.t[:], func=AF.Identity,
                                                              bias=cwT.t[:, c, 31:32], scale=1.0), r=[pc.b, cwT.b], w=[yv.b])
                P.op("act", lambda e, c=c, pc=pc: e.activation(out=ysq.t[:, c, :], in_=pc.t[:], func=AF.Square,
                                                              bias=cwT.t[:, c, 31:32], scale=1.0), r=[pc.b, cwT.b], w=[ysq.b])
            for c in range(4):
                P.op("pe", lambda e, c=c: e.matmul(pst[0].t[:], ones_f.t[:], yv.t[:, c, :], start=(c == 0), stop=(c == 3)),
                     r=[ones_f.b, yv.b], w=[pst[0].b])
            for c in range(4):
                P.op("pe", lambda e, c=c: e.matmul(pst[1].t[:], ones_f.t[:], ysq.t[:, c, :], start=(c == 0), stop=(c == 3)),
                     r=[ones_f.b, ysq.b], w=[pst[1].b])
            P.op("dve", lambda e: e.tensor_scalar(out=mean.t[:], in0=pst[0].t[:], scalar1=1.0 / 512, scalar2=None, op0=ALU.mult),
                 r=[pst[0].b], w=[mean.b])
            P.op("dve", lambda e: e.tensor_tensor(out=tmp.t[:], in0=mean.t[:], in1=mean.t[:], op=ALU.mult), r=[mean.b], w=[tmp.b])
            P.op("dve", lambda e: e.scalar_tensor_tensor(out=rstd.t[:], in0=pst[1].t[:], scalar=1.0 / 512, in1=tmp.t[:],
                                                         op0=ALU.mult, op1=ALU.subtract), r=[pst[1].b, tmp.b], w=[rstd.b])
            P.op("dve", lambda e: e.tensor_scalar(out=rstd.t[:], in0=rstd.t[:], scalar1=EPS, scalar2=None, op0=ALU.add),
                 r=[rstd.b], w=[rstd.b])
            P.op("act", lambda e: e.activation(out=rstd.t[:], in_=rstd.t[:], func=AF.Sqrt), r=[rstd.b], w=[rstd.b])
            P.op("dve", lambda e: e.reciprocal(out=rstd.t[:], in_=rstd.t[:]), r=[rstd.b], w=[rstd.b])
            for c in range(4):
                P.op("dve", lambda e, c=c: e.tensor_tensor(out=yv.t[:, c, :], in0=yv.t[:, c, :], in1=mean.t[:], op=ALU.subtract),
                     r=[yv.b, mean.b], w=[yv.b])
                P.op("dve", lambda e, c=c: e.tensor_tensor(out=yv.t[:, c, :], in0=yv.t[:, c, :], in1=rstd.t[:], op=ALU.mult),
                     r=[yv.b, rstd.b], w=[yv.b])
                P.op("act", lambda e, c=c: e.activation(out=aout.t[:, c, :], in_=yv.t[:, c, :], func=AF.Silu,
                                                       bias=cwT.t[:, c, 33:34], scale=cwT.t[:, c, 32:33]), r=[yv.b, cwT.b], w=[aout.b])
            P.op("st", lambda e, t0=t0: e.dma_start(out=catTv[:, 0:4, t0:t0 + 512], in_=aout.t[:]), r=[aout.b])
        ph.close()

    def phase_B():
        ph = Phase(P)
        qiT = ph.sb([128, 4, S], BF16, "qiT")
        kiT = ph.sb([128, S], BF16, "kiT")
        wtok = ph.sb([128, NT, 8], F32, "wtok")
        caus = ph.sb([128, 128], F32, "caus")
        pow2 = ph.sb([128, NIT + 2], F32, "pow2")
        P.op("ld", lambda e: e.dma_start(out=qiT.t[:], in_=qiT_d.ap().rearrange("(c p) t -> p c t", p=128)), w=[qiT.b])
        P.op("ld", lambda e: e.dma_start(out=kiT.t[:], in_=kiT_d.ap()), w=[kiT.b])
        P.op("ld", lambda e: e.dma_start(out=wtok.t[:], in_=w_d.ap().rearrange("(t p) e -> p t e", p=128)), w=[wtok.b])
        P.op("ld", lambda e: e.dma_start(out=caus.t[:], in_=c_caus.ap()), w=[caus.b])
        P.op("ld", lambda e: e.dma_start(out=pow2.t[:], in_=c_pow2.ap()), w=[pow2.b])
        NSB = 4
        score = [ph.sb([128, S], F32, "score") for _ in range(NSB)]
        mneg = [ph.sb([128, S], BF16, "mneg") for _ in range(2)]
        junk = [ph.sb([128, S], BF16, "junk") for _ in range(2)]
        rr = [ph.sb([128, 512], BF16, "rr") for _ in range(4)]
        dg = [ph.sb([128, 8, 128], BF16, "dg") for _ in range(NSB)]
        pd = [ph.ps([128, 512], F32, "pd") for _ in range(4)]
        psc = [ph.ps([128, 512], F32, "psc") for _ in range(3)]
        sm = [dict((n, ph.sb([128, 1], F32, n)) for n in ("mn", "mx", "w0", "mid", "cnt", "tt", "thr")) for _ in range(NSB)]
        wk = [ph.sb([128, NIT + 2], F32, "wk") for _ in range(NSB)]
        thr_const = ph.sb([128, 1], F32, "thrc")
        P.op("dve", lambda e: e.memset(thr_const.t[:], -1e29), w=[thr_const.b])
        cstage = [ph.sb([128, 4096], BF16, "cst") for _ in range(3)]
        cnt_ = {"ri": 0, "pdi": 0, "sci": 0}

        def prep(qb):
            nk = (qb + 1) * 128
            sc = score[qb % NSB]
            d = dg[qb % NSB]
            for h in range(8):
                P.op("act", lambda e, h=h: e.activation(out=d.t[:, h, :], in_=ident_f.t[:], func=AF.Copy, scale=wtok.t[:, qb, h:h + 1]),
                     r=[ident_f.b, wtok.b], w=[d.b])
            nch = (nk + 511) // 512
            items = []
            for kc in range(nch):
                k0 = kc * 512
                n = min(512, nk - k0)
                pscore = psc[cnt_["sci"] % 3]; cnt_["sci"] += 1
                for h in range(8):
                    items.append((kc, k0, n, h, pscore))
            LAG = 2
            stash = {}
            for idx in range(len(items) + LAG):
                if idx < len(items):
                    kc, k0, n, h, pscore = items[idx]
                    p0 = (h % 2) * 64
                    pdt = pd[cnt_["pdi"] % 4]; cnt_["pdi"] += 1
                    rt = rr[cnt_["ri"] % 4]; cnt_["ri"] += 1
                    stash[idx] = rt
                    P.op("pe", lambda e: e.matmul(
                        pdt.t[:, 0:n], qiT.t[p0:p0 + 64, h // 2, qb * 128:(qb + 1) * 128], kiT.t[p0:p0 + 64, k0:k0 + n],
                        start=True, stop=True), r=[qiT.b, kiT.b], w=[pdt.b])
                    P.op("act", lambda e: e.activation(out=rt.t[:, 0:n], in_=pdt.t[:, 0:n], func=AF.Relu), r=[pdt.b], w=[rt.b])
                j = idx - LAG
                if j >= 0:
                    kc, k0, n, h, pscore = items[j]
                    rt = stash.pop(j)
                    P.op("pe", lambda e: e.matmul(pscore.t[:, 0:n], d.t[:, h, :], rt.t[:, 0:n], start=(h == 0), stop=(h == 7)),
                         r=[d.b, rt.b], w=[pscore.b])
                    if h == 7:
                        P.op("act", lambda e: e.copy(out=sc.t[:, k0:k0 + n], in_=pscore.t[:, 0:n]), r=[pscore.b], w=[sc.b])

        def bis_ops(qb, slot):
            nk = (qb + 1) * 128
            n1 = qb * 128
            sc = score[qb % NSB]
            mg = mneg[slot]
            jk = junk[slot]
            s_ = sm[qb % NSB]
            wkk = wk[qb % NSB]
            ops = []
            ops.append(lambda: P.op("dve", lambda e: e.tensor_tensor(out=sc.t[:, qb * 128:(qb + 1) * 128], in0=sc.t[:, qb * 128:(qb + 1) * 128],
                                                                   in1=caus.t[:], op=ALU.add), r=[sc.b, caus.b], w=[sc.b]))
            if qb >= 2:
                ops.append(lambda: P.op("dve", lambda e: e.tensor_reduce(out=s_["mx"].t[:], in_=sc.t[:, 0:n1], axis=AX.X, op=ALU.max),
                                        r=[sc.b], w=[s_["mx"].b]))
                ops.append(lambda: P.op("dve", lambda e: e.tensor_reduce(out=s_["mn"].t[:], in_=sc.t[:, 0:n1], axis=AX.X, op=ALU.min),
                                        r=[sc.b], w=[s_["mn"].b]))
                ops.append(lambda: P.op("dve", lambda e: e.tensor_tensor(out=s_["w0"].t[:], in0=s_["mx"].t[:], in1=s_["mn"].t[:], op=ALU.subtract),
                                        r=[s_["mx"].b, s_["mn"].b], w=[s_["w0"].b]))
                ops.append(lambda: P.op("dve", lambda e: e.tensor_scalar(out=wkk.t[:], in0=pow2.t[:], scalar1=s_["w0"].t[:, 0:1], scalar2=None,
                                                                       op0=ALU.mult), r=[pow2.b, s_["w0"].b], w=[wkk.b]))
                ops.append(lambda: P.op("dve", lambda e: e.tensor_tensor(out=s_["mid"].t[:], in0=s_["mn"].t[:], in1=wkk.t[:, 1:2], op=ALU.add),
                                        r=[s_["mn"].b, wkk.b], w=[s_["mid"].b]))
                for it in range(1, NIT + 1):
                    ops.append(lambda: P.op("dve", lambda e: e.tensor_scalar(out=jk.t[:, 0:nk], in0=sc.t[:, 0:nk], scalar1=s_["mid"].t[:, 0:1],
                                                                           scalar2=0.0, op0=ALU.is_ge, op1=ALU.add, accum_out=s_["cnt"].t[:, 0:1]),
                                            r=[sc.b, s_["mid"].b], w=[jk.b, s_["cnt"].b]))
                    ops.append(lambda it=it: P.op("dve", lambda e: e.tensor_scalar(out=s_["tt"].t[:], in0=s_["cnt"].t[:], scalar1=255.5,
                                                                                 scalar2=wkk.t[:, it:it + 1], op0=ALU.is_ge, op1=ALU.mult),
                                                  r=[s_["cnt"].b, wkk.b], w=[s_["tt"].b]))
                    ops.append(lambda it=it: P.op("dve", lambda e: e.scalar_tensor_tensor(out=s_["mid"].t[:], in0=s_["tt"].t[:],
                                                                                        scalar=wkk.t[:, it + 1:it + 2], in1=s_["mid"].t[:],
                                                                                        op0=ALU.subtract, op1=ALU.add),
                                                  r=[s_["tt"].b, wkk.b, s_["mid"].b], w=[s_["mid"].b]))
                ops.append(lambda: P.op("dve", lambda e: e.tensor_tensor(out=s_["thr"].t[:], in0=s_["mid"].t[:], in1=wkk.t[:, NIT + 1:NIT + 2],
                                                                       op=ALU.subtract), r=[s_["mid"].b, wkk.b], w=[s_["thr"].b]))
                thr = s_["thr"]
            else:
                thr = thr_const
            ops.append(lambda: P.op("dve", lambda e: e.tensor_scalar(out=mg.t[:, 0:nk], in0=sc.t[:, 0:nk], scalar1=thr.t[:, 0:1],
                                                                   scalar2=NEGM, op0=ALU.is_lt, op1=ALU.mult), r=[sc.b, thr.b], w=[mg.b]))
            ops.append(lambda: P.op("st", lambda e: e.dma_start(out=mask_d.ap()[qb * 128:(qb + 1) * 128, 0:nk], in_=mg.t[:, 0:nk]), r=[mg.b]))
            return ops

        prep(0)
        prep(1)
        for q0 in range(0, NT, 2):
            conv_emit(cstage, 4)
            if q0 + 2 < NT:
                prep(q0 + 2)
                prep(q0 + 3)
            oa = bis_ops(q0, 0)
            ob = bis_ops(q0 + 1, 1)
            for i in range(max(len(oa), len(ob))):
                if i < len(oa):
                    oa[i]()
                if i < len(ob):
                    ob[i]()
        conv_flush_pending()
        ph.close()

    def phase_C():
        ph = Phase(P)
        vaug = ph.sb([128, NT, 520], BF16, "vaug")
        P.op("ld", lambda e: e.dma_start(out=vaug.t[:], in_=v_d.ap().rearrange("(t p) n -> p t n", p=128)), w=[vaug.b])
        qh = [ph.sb([64, S], BF16, "qh") for _ in range(2)]
        kh = [ph.sb([64, S], BF16, "kh") for _ in range(2)]
        gt = [ph.sb([128, 2560], BF16, "gt") for _ in range(2)]
        attT = [ph.sb([64, S], BF16, "attT") for _ in range(2)]
        mk = [[ph.sb([128, S], BF16, "mk") for _ in range(4)] for _ in range(2)]
        pT = [ph.sb([128, 512], BF16, "pT") for _ in range(3)]
        rec = [ph.sb([128, 4], F32, "rec") for _ in range(2)]
        atok = [ph.sb([128, 4, 64], BF16, "atok") for _ in range(2)]
        pss = [ph.ps([128, 512], F32, "pss") for _ in range(3)]
        pacc = [ph.ps([128, 512], F32, "pacc") for _ in range(2)]
        ptr = [ph.ps([128, 1024], BF16, "ptrc") for _ in range(2)]
        si = 0
        ai = 0
        mi = 0
        cstage = [ph.sb([128, 4096], BF16, "cst") for _ in range(3)]
        pend_c = []
        for h in range(8):
            q_ = qh[h % 2]; k_ = kh[h % 2]; g_ = gt[h % 2]; at_ = attT[h % 2]
            P.op("ld", lambda e, q_=q_, h=h: e.dma_start(out=q_.t[:], in_=qT_d.ap()[h * 64:(h + 1) * 64, :]), w=[q_.b])
            P.op("ld", lambda e, k_=k_, h=h: e.dma_start(out=k_.t[:], in_=kT_d.ap()[h * 64:(h + 1) * 64, :]), w=[k_.b])
            fa = f0_d.ap()
            P.op("ld", lambda e, g_=g_, h=h, fa=fa: e.dma_start(out=g_.t[:], in_=bass.AP(tensor=fa.tensor, offset=h * L0,
                                                                                    ap=[[1, 128], [1, 2560]])), w=[g_.b])
            if pend_c:
                pend_c.pop(0)()
            for Q in range(8):
                conv_emit(cstage, 2)
                mks = mk[mi % 2]; mi += 1
                for j in range(4):
                    qb = 4 * Q + j
                    nk = (qb + 1) * 128
                    P.op("ld", lambda e, m=mks[j], qb=qb, nk=nk: e.dma_start(out=m.t[:, 0:nk], in_=mask_d.ap()[qb * 128:(qb + 1) * 128, 0:nk]),
                         w=[mks[j].b])
                acc = pacc[ai % 2]; ai += 1
                P.op("pe", lambda e, acc=acc: e.matmul(acc.t[:, 0:260], zer_b.t[:, 0:128], zer_b.t[:, 0:260], start=True, stop=False),
                     r=[zer_b.b], w=[acc.b])
                nkb = 4 * Q + 4
                sbase = si
                si += nkb

                def emit_S(kb, Q=Q, q_=q_, k_=k_, g_=g_, mks=mks, sbase=sbase):
                    j0 = max(0, kb - 4 * Q)
                    c0 = j0 * 128
                    ps_ = pss[(sbase + kb) % 3]
                    P.op("pe", lambda e: e.matmul(ps_.t[:, c0:512], k_.t[:, kb * 128:(kb + 1) * 128], q_.t[:, Q * 512 + c0:(Q + 1) * 512],
                                                  start=True, stop=False), r=[k_.b, q_.b], w=[ps_.b])
                    dl = min(4 * Q - kb, 13)
                    off = dl * 128 + 384
                    P.op("pe", lambda e: e.matmul(ps_.t[:, c0:512], anti_b.t[:], g_.t[:, off + c0:off + 512], start=False, stop=False),
                         r=[anti_b.b, g_.b], w=[ps_.b])
                    for j in range(j0, 4):
                        P.op("pe", lambda e, j=j: e.matmul(ps_.t[:, j * 128:(j + 1) * 128], mks[j].t[:, kb * 128:(kb + 1) * 128], ident_b.t[:],
                                                           start=False, stop=True), r=[mks[j].b, ident_b.b], w=[ps_.b])

                emit_S(0)
                for kb in range(nkb):
                    if kb + 1 < nkb:
                        emit_S(kb + 1)
                    j0 = max(0, kb - 4 * Q)
                    c0 = j0 * 128
                    ps_ = pss[(sbase + kb) % 3]
                    pt_ = pT[(sbase + kb) % 3]
                    P.op("act", lambda e, ps_=ps_, pt_=pt_, c0=c0: e.activation(out=pt_.t[:, c0:512], in_=ps_.t[:, c0:512], func=AF.Exp),
                         r=[ps_.b], w=[pt_.b])
                    for j in range(j0, 4):
                        P.op("pe", lambda e, acc=acc, pt_=pt_, kb=kb, j=j, h=h, Q=Q: e.matmul(
                            acc.t[:, j * 65:(j + 1) * 65], pt_.t[:, j * 128:(j + 1) * 128], vaug.t[:, kb, h * 65:(h + 1) * 65],
                            start=False, stop=(kb == 4 * Q + j)), r=[pt_.b, vaug.b], w=[acc.b])
                rc = rec[ai % 2]
                ak = atok[ai % 2]
                P.op("dve", lambda e, acc=acc, rc=rc: e.reciprocal(out=rc.t[:], in_=acc.t[:, 0:260].rearrange("p (j d) -> p j d", d=65)[:, :, 64]),
                     r=[acc.b], w=[rc.b])
                for j in range(4):
                    P.op("dve", lambda e, acc=acc, rc=rc, ak=ak, j=j: e.tensor_scalar(out=ak.t[:, j, :], in0=acc.t[:, j * 65:j * 65 + 64],
                                                                                   scalar1=rc.t[:, j:j + 1], scalar2=None, op0=ALU.mult),
                         r=[acc.b, rc.b], w=[ak.b])
                pt2 = ptr[ai % 2]
                for j in range(4):
                    P.op("pe", lambda e, pt2=pt2, ak=ak, j=j: e.transpose(out=pt2.t[0:64, j * 128:(j + 1) * 128], in_=ak.t[:, j, :],
                                                                        identity=ident_b.t[:]), r=[ak.b, ident_b.b], w=[pt2.b])
                P.op("act", lambda e, pt2=pt2, at_=at_, Q=Q: e.copy(out=at_.t[:, Q * 512:(Q + 1) * 512], in_=pt2.t[0:64, 0:512]),
                     r=[pt2.b], w=[at_.b])
            pend_c.append(lambda at_=at_, h=h: P.op("st", lambda e: e.dma_start(out=catT_d.ap()[512 + h * 64:512 + (h + 1) * 64, :], in_=at_.t[:]), r=[at_.b]))
        while pend_c:
            pend_c.pop(0)()
        conv_finish(cstage)
        ph.close()

    def phase_outproj(src_kind, wout_d, lng_d, lnb_d, xres_d, xout_d):
        ph = Phase(P)
        wo = ph.sb([128, 8, D], BF16, "wo")
        P.op("gld", lambda e: e.dma_start(out=wo.t[:], in_=wout_d.ap().rearrange("(c p) n -> p c n", p=128)), w=[wo.b])
        gB = ph.sb([128, D], F32, "gB")
        bB = ph.sb([128, D], F32, "bB")
        P.op("ld", lambda e: e.dma_start(out=gB.t[:], in_=bcast_row(lng_d)), w=[gB.b])
        P.op("ld", lambda e: e.dma_start(out=bB.t[:], in_=bcast_row(lnb_d)), w=[bB.b])
        pmx = [ph.ps([128, 512], F32, "pmx") for _ in range(4)]
        xr = [ph.sb([128, D], F32, "xr") for _ in range(3)]
        z = [ph.sb([128, D], F32, "z") for _ in range(3)]
        xo = [ph.sb([128, D], F32, "xo") for _ in range(3)]
        st6 = [ph.sb([128, 2, 6], F32, "st6") for _ in range(3)]
        mv = [ph.sb([128, 2], F32, "mv") for _ in range(3)]
        rstd = [ph.sb([128, 2], F32, "rstd") for _ in range(3)]
        if src_kind == "catT":
            cT = [ph.sb([128, 8, 512], BF16, "cT") for _ in range(2)]
        else:
            ug = [[ph.sb([128, 8, 132], F32, "ug") for _ in range(3)] for _ in range(3)]
            rc8 = [ph.sb([128, 8], F32, "rc8") for _ in range(3)]
            otok = [ph.sb([128, D], BF16, "otok") for _ in range(3)]
            oT = [ph.sb([128, 8, 128], BF16, "oT") for _ in range(3)]
            ptr = [ph.ps([128, 1024], BF16, "ptro") for _ in range(2)]
        catTv = catT_d.ap().rearrange("(c p) t -> p c t", p=128)
        NB = len(xr)
        st1 = {}

        def stage1(tt):
            i2 = tt % NB
            t0 = tt * 128
            if src_kind == "catT":
                if tt % 4 == 0:
                    ct = cT[(tt // 4) % 2]
                    P.op("ld", lambda e: e.dma_start(out=ct.t[:], in_=catTv[:, :, t0:t0 + 512]), w=[ct.b])
                ct = cT[(tt // 4) % 2]
                lhs = lambda c: ct.t[:, c, (tt % 4) * 128:(tt % 4 + 1) * 128]
                lhs_b = ct.b
            else:
                u3 = ug[i2]
                for g in range(3):
                    P.op("ld", lambda e, g=g: e.dma_start(out=u3[g].t[:], in_=u_d.ap()[g, t0:t0 + 128, :, :]), w=[u3[g].b])
                P.op("dve", lambda e: e.tensor_tensor(out=u3[0].t[:], in0=u3[0].t[:], in1=u3[1].t[:], op=ALU.add),
                     r=[u3[0].b, u3[1].b], w=[u3[0].b])
                P.op("dve", lambda e: e.tensor_tensor(out=u3[0].t[:], in0=u3[0].t[:], in1=u3[2].t[:], op=ALU.add),
                     r=[u3[0].b, u3[2].b], w=[u3[0].b])
                rc = rc8[i2]
                ot = otok[i2]
                P.op("dve", lambda e: e.reciprocal(out=rc.t[:], in_=u3[0].t[:, :, 128]), r=[u3[0].b], w=[rc.b])
                for hh in range(8):
                    eng = "dve" if hh % 2 == 0 else "pool"
                    P.op(eng, lambda e, hh=hh: e.tensor_scalar(out=ot.t[:, hh * 128:(hh + 1) * 128], in0=u3[0].t[:, hh, 0:128],
                                                              scalar1=rc.t[:, hh:hh + 1], scalar2=None, op0=ALU.mult),
                         r=[u3[0].b, rc.b], w=[ot.b])
                o_T = oT[i2]
                for half in range(2):
                    pt = ptr[half]
                    for cc in range(4):
                        c = half * 4 + cc
                        P.op("pe", lambda e, c=c, cc=cc: e.transpose(out=pt.t[:, cc * 128:(cc + 1) * 128], in_=ot.t[:, c * 128:(c + 1) * 128],
                                                                   identity=ident_b.t[:]), r=[ot.b, ident_b.b], w=[pt.b])
                    P.op("act", lambda e: e.copy(out=o_T.t[:, half * 4:(half + 1) * 4, :].rearrange("p c t -> p (c t)"),
                                                 in_=pt.t[:, 0:512]), r=[pt.b], w=[o_T.b])
                lhs = lambda c: o_T.t[:, c, :]
                lhs_b = o_T.b
            x_ = xr[i2]
            P.op("ld", lambda e: e.dma_start(out=x_.t[:], in_=xres_d.ap()[t0:t0 + 128, :]), w=[x_.b])
            st1[tt] = (lhs, lhs_b)

        def stage2(tt):
            i2 = tt % NB
            t0 = tt * 128
            lhs, lhs_b = st1.pop(tt)
            x_ = xr[i2]
            z_ = z[i2]
            for half in range(2):
                pm_ = pmx[(tt * 2 + half) % 4]
                for c in range(8):
                    P.op("pe", lambda e, c=c: e.matmul(pm_.t[:], lhs(c), wo.t[:, c, half * 512:(half + 1) * 512],
                                                       start=(c == 0), stop=(c == 7)), r=[lhs_b, wo.b], w=[pm_.b])
                P.op("dve", lambda e: e.scalar_tensor_tensor(
                    out=z_.t[:, half * 512:(half + 1) * 512], in0=x_.t[:, half * 512:(half + 1) * 512], scalar=ALPHA, in1=pm_.t[:],
                    op0=ALU.mult, op1=ALU.add), r=[pm_.b, x_.b], w=[z_.b])
            layernorm_rows(ph, z_, gB, bB, xo[i2], st6[i2], mv[i2], rstd[i2], None, gb_eng="pool")
            return lambda: P.op("st", lambda e: e.dma_start(out=xout_d.ap()[t0:t0 + 128, :], in_=xo[i2].t[:]), r=[xo[i2].b])

        stage1(0)
        pend = None
        for tt in range(NT):
            if tt + 1 < NT:
                stage1(tt + 1)
            if pend is not None:
                pend()
            pend = stage2(tt)
        pend()
        ph.close()

    def phase_E():
        ph = Phase(P)
        NF = 22
        wd = ph.sb([128, NF, D], BF16, "wd")
        wdv = ewd.ap().rearrange("(f p) n -> p f n", p=128)
        for f0 in range(0, NF, 6):
            f1 = min(NF, f0 + 6)
            P.op("gld", lambda e, f0=f0, f1=f1: e.dma_start(out=wd.t[:, f0:f1, :], in_=wdv[:, f0:f1, :]), w=[wd.b])
        gB = ph.sb([128, D], F32, "gB")
        bB = ph.sb([128, D], F32, "bB")
        P.op("ld", lambda e: e.dma_start(out=gB.t[:], in_=bcast_row(eln2g)), w=[gB.b])
        P.op("ld", lambda e: e.dma_start(out=bB.t[:], in_=bcast_row(eln2b)), w=[bB.b])
        xtok = [ph.sb([128, D], BF16, "xtok") for _ in range(8)]
        xT = ph.sb([128, 8, 1024], BF16, "xT")
        hT = ph.sb([128, NF, 1024], BF16, "hT")
        wgp = [ph.sb([128, 8, 256], BF16, "wgp") for _ in range(2)]
        wup = [ph.sb([128, 8, 256], BF16, "wup") for _ in range(2)]
        sg = [ph.sb([128, 512], F32, "sg") for _ in range(2)]
        ptr = [ph.ps([128, 1024], BF16, "ptre") for _ in range(2)]
        pg = [ph.ps([128, 512], F32, "pg") for _ in range(2)]
        pu = [ph.ps([128, 512], F32, "pu") for _ in range(2)]
        py = [ph.ps([128, 512], F32, "py") for _ in range(2)]
        xr = [ph.sb([128, D], F32, "xr") for _ in range(2)]
        z = [ph.sb([128, D], F32, "z") for _ in range(2)]
        xo = [ph.sb([128, D], F32, "xo") for _ in range(2)]
        st6 = [ph.sb([128, 2, 6], F32, "st6") for _ in range(2)]
        mv = [ph.sb([128, 2], F32, "mv") for _ in range(2)]
        rstd = [ph.sb([128, 2], F32, "rstd") for _ in range(2)]
        wgv = ewg.ap().rearrange("(c p) n -> p c n", p=128)
        wuv = ewu.ap().rearrange("(c p) n -> p c n", p=128)
        pi = 0
        gi = 0
        pend_e = [None]
        for grp in range(4):
            t0 = grp * 1024
            load_tok_T(ph, lambda j, t0=t0: x1_d.ap()[t0 + j * 128:t0 + (j + 1) * 128, :], 8, xtok, xT, ptr, "x1")
            for pc in range(11):
                wg_ = wgp[pi % 2]; wu_ = wup[pi % 2]; pi += 1
                P.op("gld", lambda e, wg_=wg_, pc=pc: e.dma_start(out=wg_.t[:], in_=wgv[:, :, pc * 256:(pc + 1) * 256]), w=[wg_.b])
                P.op("gld", lambda e, wu_=wu_, pc=pc: e.dma_start(out=wu_.t[:], in_=wuv[:, :, pc * 256:(pc + 1) * 256]), w=[wu_.b])
                for fs in range(2):
                    fc = pc * 2 + fs
                    for half in range(2):
                        pg_ = pg[gi % 2]; pu_ = pu[gi % 2]; sg_ = sg[gi % 2]; gi += 1
                        for c in range(8):
                            P.op("pe", lambda e, pg_=pg_, wg_=wg_, c=c, fs=fs, half=half: e.matmul(
                                pg_.t[:], wg_.t[:, c, fs * 128:(fs + 1) * 128], xT.t[:, c, half * 512:(half + 1) * 512],
                                start=(c == 0), stop=(c == 7)), r=[wg_.b, xT.b], w=[pg_.b])
                        for c in range(8):
                            P.op("pe", lambda e, pu_=pu_, wu_=wu_, c=c, fs=fs, half=half: e.matmul(
                                pu_.t[:], wu_.t[:, c, fs * 128:(fs + 1) * 128], xT.t[:, c, half * 512:(half + 1) * 512],
                                start=(c == 0), stop=(c == 7)), r=[wu_.b, xT.b], w=[pu_.b])
                        P.op("act", lambda e, pg_=pg_, sg_=sg_: e.activation(out=sg_.t[:], in_=pg_.t[:], func=AF.Silu), r=[pg_.b], w=[sg_.b])
                        P.op("dve", lambda e, pu_=pu_, sg_=sg_, fc=fc, half=half: e.tensor_tensor(
                            out=hT.t[:, fc, half * 512:(half + 1) * 512], in0=pu_.t[:], in1=sg_.t[:], op=ALU.mult), r=[pu_.b, sg_.b], w=[hT.b])
            for j in range(8):
                tt = grp * 8 + j
                i2 = tt % 2
                x_ = xr[i2]; z_ = z[i2]
                P.op("ld", lambda e, x_=x_, tt=tt: e.dma_start(out=x_.t[:], in_=x1_d.ap()[tt * 128:(tt + 1) * 128, :]), w=[x_.b])
                for half in range(2):
                    py_ = py[half]
                    for fc in range(NF):
                        P.op("pe", lambda e, py_=py_, fc=fc, j=j, half=half: e.matmul(
                            py_.t[:], hT.t[:, fc, j * 128:(j + 1) * 128], wd.t[:, fc, half * 512:(half + 1) * 512],
                            start=(fc == 0), stop=(fc == NF - 1)), r=[hT.b, wd.b], w=[py_.b])
                    P.op("dve", lambda e, py_=py_, x_=x_, z_=z_, half=half: e.scalar_tensor_tensor(
                        out=z_.t[:, half * 512:(half + 1) * 512], in0=x_.t[:, half * 512:(half + 1) * 512], scalar=ALPHA, in1=py_.t[:],
                        op0=ALU.mult, op1=ALU.add), r=[py_.b, x_.b], w=[z_.b])
                layernorm_rows(ph, z_, gB, bB, xo[i2], st6[i2], mv[i2], rstd[i2], None)
                if pend_e[0] is not None:
                    pend_e[0]()
                pend_e[0] = (lambda i2=i2, tt=tt: P.op("st", lambda e: e.dma_start(out=x2_d.ap()[tt * 128:(tt + 1) * 128, :], in_=xo[i2].t[:]), r=[xo[i2].b]))
        pend_e[0]()
        ph.close()

    def phase_F(g):
        r = (1, 4, 16)[g]
        Lc = S // r
        nb = Lc // 128
        ph = Phase(P)
        wq = ph.sb([128, 8, 1024], BF16, "wq")
        wk_ = ph.sb([128, 8, 1024], BF16, "wk")
        wv = ph.sb([128, 8, 1024], BF16, "wv")
        wv_ap = owin.ap().rearrange("(c p) n -> p c n", p=128)
        for j, wt in enumerate((wq, wk_, wv)):
            col = (g * 3 + j) * 1024
            for c0 in range(0, 8, 4):
                P.op("gld", lambda e, wt=wt, col=col, c0=c0: e.dma_start(out=wt.t[:, c0:c0 + 4, :], in_=wv_ap[:, c0:c0 + 4, col:col + 1024]), w=[wt.b])
        g1 = ph.sb([128, 8, 256], BF16, "g1")
        fa = f1_d.ap()
        for h in range(8):
            P.op("ld", lambda e, h=h: e.dma_start(out=g1.t[:, h, :], in_=bass.AP(tensor=fa.tensor, offset=(g * 8 + h) * L1,
                                                                              ap=[[1, 128], [1, 256]])), w=[g1.b])
        xtok = [ph.sb([128, D], BF16, "xtok") for _ in range(4)]
        xT = ph.sb([128, 8, S], BF16, "xTp")
        ptr = [ph.ps([128, 1024], BF16, "ptrf")] * 2
        x2a = x2_d.ap()

        def rows(tt):
            rho = (tt * 128) // Lc
            l0 = (tt * 128) % Lc
            return bass.AP(tensor=x2a.tensor, offset=(l0 * r + rho) * D, ap=[[r * D, 128], [1, D]])
        xTs = [T(xT.t, "x") for _ in range(8)]
        for sbk in range(8):
            xs = xTs[sbk]
            for j in range(4):
                P.op("gld", lambda e, j=j, sbk=sbk: e.dma_start(out=xtok[j].t[:], in_=rows(sbk * 4 + j)), w=[xtok[j].b])
            for c in range(8):
                pt = ptr[c % 2]
                for jj in range(4):
                    P.op("pe", lambda e, pt=pt, jj=jj, c=c: e.transpose(out=pt.t[:, jj * 128:(jj + 1) * 128], in_=xtok[jj].t[:, c * 128:(c + 1) * 128],
                                                                      identity=ident_b.t[:]), r=[xtok[jj].b, ident_b.b], w=[pt.b])
                if c % 2 == 0:
                    P.op("act", lambda e, pt=pt, c=c, sbk=sbk: e.copy(out=xT.t[:, c, sbk * 512:(sbk + 1) * 512], in_=pt.t[:, 0:512]), r=[pt.b], w=[xs.b])
                else:
                    P.op("dve", lambda e, pt=pt, c=c, sbk=sbk: e.tensor_copy(out=xT.t[:, c, sbk * 512:(sbk + 1) * 512], in_=pt.t[:, 0:512]), r=[pt.b], w=[xs.b])
        qh = [ph.sb([128, S], BF16, "qh") for _ in range(2)]
        kh = [ph.sb([128, S], BF16, "kh") for _ in range(2)]
        vh = [ph.sb([128, NT, 129], BF16, "vh") for _ in range(2)]
        ust = [ph.sb([128, NT, 129], F32, "ust")] * 2
        pT = [ph.sb([128, 4, 128], BF16, "pT") for _ in range(3)]
        pq = [ph.ps([128, 512], F32, "pq") for _ in range(2)]
        pss = [ph.ps([128, 512], F32, "pss") for _ in range(3)]
        pacc = [ph.ps([128, 512], F32, "pacc") for _ in range(2)]
        QS = float(128 ** -0.5)
        pqi = 0
        si = 0
        for vv in vh:
            P.op("dve", lambda e, vv=vv: e.memset(vv.t[:, :, 128:129], 1.0), w=[vv.b])
        for h in range(8):
            q_ = qh[h % 2]; k_ = kh[h % 2]; v_ = vh[h % 2]; u_ = ust[h % 2]
            for sbk in range(8):
                for which, wt, dst in ((0, wq, q_), (1, wk_, k_)):
                    pp = pq[pqi % 2]; pqi += 1
                    for c in range(8):
                        P.op("pe", lambda e, pp=pp, wt=wt, c=c, h=h, sbk=sbk: e.matmul(
                            pp.t[:], wt.t[:, c, h * 128:(h + 1) * 128], xT.t[:, c, sbk * 512:(sbk + 1) * 512], start=(c == 0), stop=(c == 7)),
                            r=[wt.b, xTs[sbk].b], w=[pp.b])
                    if which == 0:
                        P.op("act", lambda e, pp=pp, dst=dst, sbk=sbk: e.activation(out=dst.t[:, sbk * 512:(sbk + 1) * 512], in_=pp.t[:], func=AF.Copy, scale=QS),
                             r=[pp.b], w=[dst.b])
                    else:
                        P.op("dve", lambda e, pp=pp, dst=dst, sbk=sbk: e.tensor_copy(out=dst.t[:, sbk * 512:(sbk + 1) * 512], in_=pp.t[:]), r=[pp.b], w=[dst.b])
                pp = pq[pqi % 2]; pqi += 1
                for j in range(4):
                    for c in range(8):
                        P.op("pe", lambda e, pp=pp, c=c, h=h, sbk=sbk, j=j: e.matmul(
                            pp.t[:, j * 128:(j + 1) * 128], xT.t[:, c, sbk * 512 + j * 128:sbk * 512 + (j + 1) * 128], wv.t[:, c, h * 128:(h + 1) * 128],
                            start=(c == 0), stop=(c == 7)), r=[wv.b, xTs[sbk].b], w=[pp.b])
                P.op("act", lambda e, pp=pp, v_=v_, sbk=sbk: e.copy(out=v_.t[:, sbk * 4:(sbk + 1) * 4, 0:128], in_=pp.t[:].rearrange("p (j d) -> p j d", j=4)),
                     r=[pp.b], w=[v_.b])
            sbase = si
            si += NT // 2

            def emit_S(t2, q_=q_, k_=k_, h=h, sbase=sbase):
                ps_ = pss[(sbase + t2 // 2) % 3]
                for bi in range(2):
                    tt = t2 + bi
                    n = tt % nb
                    P.op("pe", lambda e, tt=tt, bi=bi: e.matmul(
                        ps_.t[:, (2 * bi + 1) * 128:(2 * bi + 2) * 128], k_.t[:, tt * 128:(tt + 1) * 128], q_.t[:, tt * 128:(tt + 1) * 128],
                        start=True, stop=False), r=[k_.b, q_.b], w=[ps_.b])
                    P.op("pe", lambda e, bi=bi: e.matmul(
                        ps_.t[:, (2 * bi + 1) * 128:(2 * bi + 2) * 128], anti_b.t[:], g1.t[:, h, 0:128], start=False, stop=True),
                        r=[anti_b.b, g1.b], w=[ps_.b])
                    if n > 0:
                        P.op("pe", lambda e, tt=tt, bi=bi: e.matmul(
                            ps_.t[:, (2 * bi) * 128:(2 * bi + 1) * 128], k_.t[:, (tt - 1) * 128:tt * 128], q_.t[:, tt * 128:(tt + 1) * 128],
                            start=True, stop=False), r=[k_.b, q_.b], w=[ps_.b])
                        P.op("pe", lambda e, bi=bi: e.matmul(
                            ps_.t[:, (2 * bi) * 128:(2 * bi + 1) * 128], anti_b.t[:], g1.t[:, h, 128:256], start=False, stop=True),
                            r=[anti_b.b, g1.b], w=[ps_.b])

            emit_S(0)
            for t2 in range(0, NT, 2):
                if t2 + 2 < NT:
                    emit_S(t2 + 2)
                ps_ = pss[(sbase + t2 // 2) % 3]; pt_ = pT[(sbase + t2 // 2) % 3]; acc = pacc[(t2 // 2) % 2]
                first_has_prev = (t2 % nb) > 0
                c0 = 0 if first_has_prev else 128
                P.op("act", lambda e, ps_=ps_, pt_=pt_, c0=c0: e.activation(out=pt_.t[:].rearrange("p a b -> p (a b)")[:, c0:512], in_=ps_.t[:, c0:512], func=AF.Exp),
                     r=[ps_.b], w=[pt_.b])
                for bi in range(2):
                    tt = t2 + bi
                    n = tt % nb
                    P.op("pe", lambda e, acc=acc, pt_=pt_, v_=v_, tt=tt, bi=bi, n=n: e.matmul(
                        acc.t[:, bi * 129:(bi + 1) * 129], pt_.t[:, 2 * bi + 1, :], v_.t[:, tt, :], start=True, stop=(n == 0)),
                        r=[pt_.b, v_.b], w=[acc.b])
                    if n > 0:
                        P.op("pe", lambda e, acc=acc, pt_=pt_, v_=v_, tt=tt, bi=bi: e.matmul(
                            acc.t[:, bi * 129:(bi + 1) * 129], pt_.t[:, 2 * bi, :], v_.t[:, tt - 1, :], start=False, stop=True),
                            r=[pt_.b, v_.b], w=[acc.b])
                P.op("dve", lambda e, acc=acc, u_=u_, t2=t2: e.tensor_copy(out=u_.t[:, t2:t2 + 2, :], in_=acc.t[:, 0:258].rearrange("p (b d) -> p b d", b=2)),
                     r=[acc.b], w=[u_.b])
            uda = u_d.ap()
            for rho in range(r):
                P.op("st", lambda e, u_=u_, rho=rho, h=h: e.dma_start(
                    out=bass.AP(tensor=uda.tensor, offset=((g * S + rho) * 8 + h) * 132, ap=[[r * 8 * 132, 128], [128 * r * 8 * 132, nb], [1, 129]]),
                    in_=u_.t[:, rho * nb:(rho + 1) * nb, :]), r=[u_.b])
        ph.close()

    conv_jobs = []
    for e_ in range(8):
        for pc in range(7):
            conv_jobs.append(("wg", e_, pc))
            conv_jobs.append(("wu", e_, pc))
    for e_ in range(8):
        for ch in range(2):
            for q_ in range(4):
                conv_jobs.append(("wd", e_, ch * 4 + q_))
    conv_state = {"next": 0, "pending_store": None, "k": 0}

    def conv_emit(stage, n):
        for _ in range(n):
            pend = conv_state["pending_store"]
            if pend is not None:
                stg_, dst_ap, ncol = pend
                P.op("gst", lambda e, stg_=stg_, dst_ap=dst_ap, ncol=ncol: e.dma_start(out=dst_ap, in_=stg_.t[:, 0:ncol]), r=[stg_.b])
                conv_state["pending_store"] = None
            if conv_state["next"] >= len(conv_jobs):
                continue
            kind, e_, i_ = conv_jobs[conv_state["next"]]
            conv_state["next"] += 1
            stg_ = stage[conv_state["k"] % len(stage)]
            conv_state["k"] += 1
            if kind in ("wg", "wu"):
                src = (omwg if kind == "wg" else omwu).ap()[e_].rearrange("(c p) n -> p c n", p=128)[:, :, i_ * 512:(i_ + 1) * 512]
                dst = (wgs_d if kind == "wg" else wus_d).ap()[(e_ * 7 + i_) * 128:(e_ * 7 + i_ + 1) * 128, :]
                P.op("gld", lambda e, stg_=stg_, src=src: e.dma_start(out=stg_.t[:, 0:4096].rearrange("p (c n) -> p c n", c=8), in_=src), w=[stg_.b])
                conv_state["pending_store"] = (stg_, dst, 4096)
            else:
                ch, q_ = i_ // 4, i_ % 4
                src = omwd.ap()[e_].rearrange("(f p) n -> p f n", p=128)[:, q_ * 7:(q_ + 1) * 7, ch * 512:(ch + 1) * 512]
                dst = wds_d.ap()[(e_ * 8 + i_) * 128:(e_ * 8 + i_ + 1) * 128, :]
                P.op("gld", lambda e, stg_=stg_, src=src: e.dma_start(out=stg_.t[:, 0:3584].rearrange("p (f n) -> p f n", f=7), in_=src), w=[stg_.b])
                conv_state["pending_store"] = (stg_, dst, 3584)

    def conv_flush_pending():
        pend = conv_state["pending_store"]
        if pend is not None:
            stg_, dst_ap, ncol = pend
            P.op("gst", lambda e: e.dma_start(out=dst_ap, in_=stg_.t[:, 0:ncol]), r=[stg_.b])
            conv_state["pending_store"] = None

    def conv_finish(stage):
        while conv_state["next"] < len(conv_jobs) or conv_state["pending_store"] is not None:
            conv_emit(stage, 1)

    NTILE = 23
    NS = NTILE * 512
    I32 = mybir.dt.int32

    def phase_H1():
        ph = Phase(P)
        if conv_state["next"] < len(conv_jobs):
            cst_ = [ph.sb([128, 4096], BF16, "cst") for _ in range(3)]
            conv_finish(cst_)
        rt = ph.sb([128, 8, 8], F32, "router")
        P.op("ld", lambda e: e.dma_start(out=rt.t[:], in_=orouter.ap().rearrange("(c p) e -> p c e", p=128)), w=[rt.b])
        ustr = ph.sb([128, 128], BF16, "ustr")
        ones_b = ph.sb([128, 128], BF16, "onesb")
        thr8 = ph.sb([128, 8], F32, "thr8")
        iota23 = ph.sb([128, NTILE], F32, "iota23")
        cwg = ph.sb([128, 7], F32, "cwg")
        cwd = ph.sb([128, 8], F32, "cwd")
        P.op("gld", lambda e: e.dma_start(out=ustr.t[:], in_=c_ustr.ap()), w=[ustr.b])
        P.op("ld", lambda e: e.dma_start(out=thr8.t[:], in_=c_thr8.ap()), w=[thr8.b])
        P.op("ld", lambda e: e.dma_start(out=iota23.t[:], in_=c_iota23.ap()), w=[iota23.b])
        P.op("ld", lambda e: e.dma_start(out=cwg.t[:], in_=c_cwg.ap()), w=[cwg.b])
        P.op("ld", lambda e: e.dma_start(out=cwd.t[:], in_=c_cwd.ap()), w=[cwd.b])
        P.op("pool", lambda e: e.memset(ones_b.t[:], 1.0), w=[ones_b.b])
        xf = [ph.sb([128, D], F32, "xf") for _ in range(2)]
        xTf = [ph.sb([128, 8, 128], F32, "xTf") for _ in range(2)]
        MSK = ph.sb([128, NT, 8], F32, "MSK")
        OHA = ph.sb([128, NT, 8], F32, "OHA")
        lg = [ph.sb([128, 8], F32, "lg") for _ in range(2)]
        mx8 = [ph.sb([128, 8], F32, "mx8") for _ in range(2)]
        ee = [ph.sb([128, 8], F32, "ee") for _ in range(2)]
        nv1 = [ph.sb([128, 1], F32, "nv1") for _ in range(2)]
        den = [ph.sb([128, 1], F32, "den") for _ in range(2)]
        ptf = [ph.ps([128, 512], F32, "ptf") for _ in range(2)]
        plg = ph.ps([128, 512], F32, "plg")
        pcum = ph.ps([128, 512], F32, "pcum")
        ptot = ph.ps([128, 512], F32, "ptot")
        for tt in range(NT):
            x_ = xf[tt % 2]; xt_ = xTf[tt % 2]
            P.op("ld", lambda e, x_=x_, tt=tt: e.dma_start(out=x_.t[:], in_=x3_d.ap()[tt * 128:(tt + 1) * 128, :]), w=[x_.b])
            for half in range(2):
                pt = ptf[half]
                for cc in range(4):
                    c = half * 4 + cc
                    P.op("pe", lambda e, x_=x_, c=c, cc=cc, pt=pt: e.transpose(out=pt.t[:, cc * 128:(cc + 1) * 128], in_=x_.t[:, c * 128:(c + 1) * 128],
                                                                             identity=ident_f.t[:]), r=[x_.b, ident_f.b], w=[pt.b])
                P.op("act", lambda e, xt_=xt_, half=half, pt=pt: e.copy(out=xt_.t[:, half * 4:(half + 1) * 4, :].rearrange("p c t -> p (c t)"), in_=pt.t[:]),
                     r=[pt.b], w=[xt_.b])
            for c in range(8):
                P.op("pe", lambda e, xt_=xt_, c=c: e.matmul(plg.t[:, 0:8], xt_.t[:, c, :], rt.t[:, c, :], start=(c == 0), stop=(c == 7)),
                     r=[xt_.b, rt.b], w=[plg.b])
            l_ = lg[tt % 2]; m_ = mx8[tt % 2]; e_ = ee[tt % 2]; n_ = nv1[tt % 2]; d_ = den[tt % 2]
            P.op("dve", lambda e, l_=l_: e.tensor_copy(out=l_.t[:], in_=plg.t[:, 0:8]), r=[plg.b], w=[l_.b])
            P.op("dve", lambda e, l_=l_, m_=m_: e.max(out=m_.t[:], in_=l_.t[:]), r=[l_.b], w=[m_.b])
            P.op("dve", lambda e, m_=m_, n_=n_: e.tensor_scalar(out=n_.t[:], in0=m_.t[:, 0:1], scalar1=-1.0, scalar2=None, op0=ALU.mult),
                 r=[m_.b], w=[n_.b])
            P.op("act", lambda e, l_=l_, e_=e_, n_=n_: e.activation(out=e_.t[:], in_=l_.t[:], func=AF.Exp, bias=n_.t[:, 0:1], scale=1.0),
                 r=[l_.b, n_.b], w=[e_.b])
            P.op("dve", lambda e, l_=l_, m_=m_, tt=tt: e.tensor_scalar(out=MSK.t[:, tt, :], in0=l_.t[:], scalar1=m_.t[:, 1:2], scalar2=None, op0=ALU.is_ge),
                 r=[l_.b, m_.b], w=[MSK.b])
            P.op("dve", lambda e, l_=l_, m_=m_, tt=tt: e.tensor_scalar(out=OHA.t[:, tt, :], in0=l_.t[:], scalar1=m_.t[:, 0:1], scalar2=None, op0=ALU.is_ge),
                 r=[l_.b, m_.b], w=[OHA.b])
            P.op("dve", lambda e, e_=e_, tt=tt: e.tensor_tensor(out=e_.t[:], in0=e_.t[:], in1=MSK.t[:, tt, :], op=ALU.mult), r=[e_.b, MSK.b], w=[e_.b])
            P.op("dve", lambda e, e_=e_, d_=d_: e.tensor_reduce(out=d_.t[:], in_=e_.t[:], axis=AX.X, op=ALU.add), r=[e_.b], w=[d_.b])
            P.op("dve", lambda e, d_=d_, tt=tt: e.reciprocal(out=GA.t[:, tt:tt + 1], in_=d_.t[:]), r=[d_.b], w=[GA.b])
        P.op("dve", lambda e: e.tensor_scalar(out=GB.t[:], in0=GA.t[:], scalar1=-1.0, scalar2=1.0, op0=ALU.mult, op1=ALU.add), r=[GA.b], w=[GB.b])
        mskb = ph.sb([128, NT * 8], BF16, "mskb")
        tot = ph.sb([128, NT, 8], F32, "tot")
        offs = ph.sb([128, NT, 8], F32, "offs")
        slot = ph.sb([128, NT, 8], F32, "slot")
        tmp3 = ph.sb([128, NT, 8], F32, "tmp3")
        ohb = ph.sb([128, NT, 8], F32, "ohb")
        cnt = ph.sb([128, 8], F32, "cnt")
        ntl = ph.sb([128, 8], F32, "ntl")
        tend = ph.sb([128, 8], F32, "tend")
        st512 = ph.sb([128, 8], F32, "st512")
        slf = ph.sb([128, 2, NT], F32, "slf")
        eidf = ph.sb([128, NTILE], F32, "eidf")
        e896 = ph.sb([128, NTILE], F32, "e896")
        e1024 = ph.sb([128, NTILE], F32, "e1024")
        iwgf = ph.sb([128, NTILE, 7], F32, "iwgf")
        iwdf = ph.sb([128, NTILE, 8], F32, "iwdf")
        P.op("dve", lambda e: e.tensor_copy(out=mskb.t[:], in_=MSK.t[:].rearrange("p t e -> p (t e)")), r=[MSK.b], w=[mskb.b])
        P.op("pe", lambda e: e.matmul(pcum.t[:, 0:256], ustr.t[:], mskb.t[:], start=True, stop=True), r=[ustr.b, mskb.b], w=[pcum.b])
        P.op("pe", lambda e: e.matmul(ptot.t[:, 0:256], ones_b.t[:], mskb.t[:], start=True, stop=True), r=[ones_b.b, mskb.b], w=[ptot.b])
        P.op("dve", lambda e: e.tensor_copy(out=tot.t[:].rearrange("p t e -> p (t e)"), in_=ptot.t[:, 0:256]), r=[ptot.b], w=[tot.b])
        P.op("dve", lambda e: e.memset(offs.t[:, 0, :], 0.0), w=[offs.b])
        for tt in range(1, NT):
            P.op("dve", lambda e, tt=tt: e.tensor_tensor(out=offs.t[:, tt, :], in0=offs.t[:, tt - 1, :], in1=tot.t[:, tt - 1, :], op=ALU.add),
                 r=[offs.b, tot.b], w=[offs.b])
        P.op("dve", lambda e: e.tensor_tensor(out=cnt.t[:], in0=offs.t[:, NT - 1, :], in1=tot.t[:, NT - 1, :], op=ALU.add), r=[offs.b, tot.b], w=[cnt.b])
        P.op("dve", lambda e: e.tensor_scalar(out=ntl.t[:], in0=cnt.t[:], scalar1=0.0, scalar2=None, op0=ALU.is_gt), r=[cnt.b], w=[ntl.b])
        for k in range(1, 8):
            P.op("dve", lambda e, k=k: e.scalar_tensor_tensor(out=ntl.t[:], in0=cnt.t[:], scalar=float(512 * k), in1=ntl.t[:], op0=ALU.is_gt, op1=ALU.add),
                 r=[cnt.b, ntl.b], w=[ntl.b])
        P.op("dve", lambda e: e.tensor_copy(out=tend.t[:, 0:1], in_=ntl.t[:, 0:1]), r=[ntl.b], w=[tend.b])
        for k in range(1, 8):
            P.op("dve", lambda e, k=k: e.tensor_tensor(out=tend.t[:, k:k + 1], in0=tend.t[:, k - 1:k], in1=ntl.t[:, k:k + 1], op=ALU.add),
                 r=[tend.b, ntl.b], w=[tend.b])
        P.op("dve", lambda e: e.tensor_tensor(out=st512.t[:], in0=tend.t[:], in1=ntl.t[:], op=ALU.subtract), r=[tend.b, ntl.b], w=[st512.b])
        P.op("dve", lambda e: e.tensor_scalar(out=st512.t[:], in0=st512.t[:], scalar1=512.0, scalar2=None, op0=ALU.mult), r=[st512.b], w=[st512.b])
        P.op("dve", lambda e: e.tensor_tensor(out=slot.t[:].rearrange("p t e -> p (t e)"), in0=pcum.t[:, 0:256], in1=offs.t[:].rearrange("p t e -> p (t e)"), op=ALU.add),
             r=[pcum.b, offs.b], w=[slot.b])
        for k in range(8):
            P.op("dve", lambda e, k=k: e.tensor_scalar(out=slot.t[:, :, k], in0=slot.t[:, :, k], scalar1=st512.t[:, k:k + 1], scalar2=None, op0=ALU.add),
                 r=[slot.b, st512.b], w=[slot.b])
        P.op("dve", lambda e: e.tensor_tensor(out=ohb.t[:], in0=MSK.t[:], in1=OHA.t[:], op=ALU.subtract), r=[MSK.b, OHA.b], w=[ohb.b])
        P.op("dve", lambda e: e.tensor_tensor(out=tmp3.t[:], in0=slot.t[:], in1=OHA.t[:], op=ALU.mult), r=[slot.b, OHA.b], w=[tmp3.b])
        P.op("dve", lambda e: e.tensor_reduce(out=slf.t[:, 0, :], in_=tmp3.t[:], axis=AX.X, op=ALU.add), r=[tmp3.b], w=[slf.b])
        P.op("dve", lambda e: e.tensor_tensor(out=tmp3.t[:], in0=slot.t[:], in1=ohb.t[:], op=ALU.mult), r=[slot.b, ohb.b, slf.b], w=[tmp3.b])
        P.op("dve", lambda e: e.tensor_reduce(out=slf.t[:, 1, :], in_=tmp3.t[:], axis=AX.X, op=ALU.add), r=[tmp3.b], w=[slf.b])
        P.op("dve", lambda e: e.tensor_copy(out=SLI.t[:], in_=slf.t[:]), r=[slf.b], w=[SLI.b])
        P.op("dve", lambda e: e.memset(eidf.t[:], 0.0), w=[eidf.b])
        for k in range(7):
            P.op("dve", lambda e, k=k: e.scalar_tensor_tensor(out=eidf.t[:], in0=iota23.t[:], scalar=tend.t[:, k:k + 1], in1=eidf.t[:], op0=ALU.is_ge, op1=ALU.add),
                 r=[iota23.b, tend.b, eidf.b], w=[eidf.b])
        P.op("dve", lambda e: e.tensor_scalar(out=e896.t[:], in0=eidf.t[:], scalar1=896.0, scalar2=None, op0=ALU.mult), r=[eidf.b], w=[e896.b])
        P.op("dve", lambda e: e.tensor_scalar(out=e1024.t[:], in0=eidf.t[:], scalar1=1024.0, scalar2=None, op0=ALU.mult), r=[eidf.b], w=[e1024.b])
        for i in range(NTILE):
            P.op("dve", lambda e, i=i: e.tensor_scalar(out=iwgf.t[:, i, :], in0=cwg.t[:], scalar1=e896.t[:, i:i + 1], scalar2=None, op0=ALU.add),
                 r=[cwg.b, e896.b], w=[iwgf.b])
            P.op("dve", lambda e, i=i: e.tensor_scalar(out=iwdf.t[:, i, :], in0=cwd.t[:], scalar1=e1024.t[:, i:i + 1], scalar2=None, op0=ALU.add),
                 r=[cwd.b, e1024.b], w=[iwdf.b])
        P.op("dve", lambda e: e.tensor_copy(out=IWG.t[:], in_=iwgf.t[:]), r=[iwgf.b], w=[IWG.b])
        P.op("dve", lambda e: e.tensor_copy(out=IWD.t[:], in_=iwdf.t[:]), r=[iwdf.b], w=[IWD.b])
        xb = [ph.sb([128, D], BF16, "xb") for _ in range(3)]
        for tt in range(NT):
            b_ = xb[tt % 3]
            P.op("gld", lambda e, b_=b_, tt=tt: e.dma_start(out=b_.t[:], in_=x3_d.ap()[tt * 128:(tt + 1) * 128, :]), w=[b_.b])
            for ab in range(2):
                P.op("gst", lambda e, b_=b_, tt=tt, ab=ab: e.indirect_dma_start(
                    out=xs_d.ap(), out_offset=bass.IndirectOffsetOnAxis(ap=SLI.t[:, ab, tt:tt + 1], axis=0), in_=b_.t[:], in_offset=None),
                    r=[b_.b, SLI.b])
        ph.close()

    def phase_H2():
        ph = Phase(P)
        NF = 28
        xs = [[ph.sb([128, D], BF16, "xs") for _ in range(4)] for _ in range(2)]
        xsT = [ph.sb([128, 8, 512], BF16, "xsT") for _ in range(2)]
        hT = ph.sb([128, NF, 512], BF16, "hT")
        wgp = [ph.sb([128, 4096], BF16, "wgp") for _ in range(3)]
        wup = [ph.sb([128, 4096], BF16, "wup") for _ in range(3)]
        wdp = [ph.sb([128, 7, 512], BF16, "wdp") for _ in range(8)]
        sg = [ph.sb([128, 512], F32, "sg") for _ in range(2)]
        ys = [ph.sb([128, 4, D], F32, "ys") for _ in range(2)]
        ptr = [ph.ps([128, 1024], BF16, "ptrh") for _ in range(2)]
        pg = [ph.ps([128, 512], F32, "pg") for _ in range(2)]
        pu = [ph.ps([128, 512], F32, "pu") for _ in range(2)]
        py = [ph.ps([128, 512], F32, "pyh") for _ in range(2)]
        wi = 0
        gi = 0
        yi = 0
        def prefetch(i):
            load_tok_T(ph, lambda j, i=i: xs_d.ap()[i * 512 + j * 128:i * 512 + (j + 1) * 128, :], 4, xs[i % 2], xsT[i % 2], ptr, "xs", queue="ld")

        prefetch(0)
        pend2 = [None]
        for i in range(NTILE):
            xs_ = xs[i % 2]; xT_ = xsT[i % 2]; ys_ = ys[i % 2]
            for k in range(8):
                P.op("gld", lambda e, k=k, i=i: e.indirect_dma_start(
                    out=wdp[k].t[:].rearrange("p f n -> p (f n)"), out_offset=None, in_=wds_d.ap(),
                    in_offset=bass.IndirectOffsetOnAxis(ap=IWD.t[:, i, k:k + 1], axis=0)), r=[IWD.b], w=[wdp[k].b])
                if k == 3:
                    pass
            for pc in range(7):
                wg_ = wgp[wi % 3]; wu_ = wup[wi % 3]; wi += 1
                P.op("gld", lambda e, wg_=wg_, pc=pc, i=i: e.indirect_dma_start(
                    out=wg_.t[:], out_offset=None, in_=wgs_d.ap(), in_offset=bass.IndirectOffsetOnAxis(ap=IWG.t[:, i, pc:pc + 1], axis=0)),
                    r=[IWG.b], w=[wg_.b])
                P.op("gld", lambda e, wu_=wu_, pc=pc, i=i: e.indirect_dma_start(
                    out=wu_.t[:], out_offset=None, in_=wus_d.ap(), in_offset=bass.IndirectOffsetOnAxis(ap=IWG.t[:, i, pc:pc + 1], axis=0)),
                    r=[IWG.b], w=[wu_.b])
                for fs in range(4):
                    fc = pc * 4 + fs
                    pg_ = pg[gi % 2]; pu_ = pu[gi % 2]; sg_ = sg[gi % 2]; gi += 1
                    for c in range(8):
                        P.op("pe", lambda e, pg_=pg_, wg_=wg_, c=c, fs=fs, xT_=xT_: e.matmul(
                            pg_.t[:], wg_.t[:, c * 512 + fs * 128:c * 512 + (fs + 1) * 128], xT_.t[:, c, :], start=(c == 0), stop=(c == 7)),
                            r=[wg_.b, xT_.b], w=[pg_.b])
                    P.op("act", lambda e, pg_=pg_, sg_=sg_: e.activation(out=sg_.t[:], in_=pg_.t[:], func=AF.Silu), r=[pg_.b], w=[sg_.b])
                    for c in range(8):
                        P.op("pe", lambda e, pu_=pu_, wu_=wu_, c=c, fs=fs, xT_=xT_: e.matmul(
                            pu_.t[:], wu_.t[:, c * 512 + fs * 128:c * 512 + (fs + 1) * 128], xT_.t[:, c, :], start=(c == 0), stop=(c == 7)),
                            r=[wu_.b, xT_.b], w=[pu_.b])
                    P.op("dve", lambda e, pu_=pu_, sg_=sg_, fc=fc: e.tensor_tensor(out=hT.t[:, fc, :], in0=pu_.t[:], in1=sg_.t[:], op=ALU.mult),
                         r=[pu_.b, sg_.b], w=[hT.b])
            if i + 1 < NTILE:
                prefetch(i + 1)
            if pend2[0] is not None:
                pend2[0]()
                pend2[0] = None
            for ch in range(2):
                for j in range(4):
                    py_ = py[yi % 2]; yi += 1
                    for q_ in range(4):
                        wd_ = wdp[ch * 4 + q_]
                        for fl in range(7):
                            fc = q_ * 7 + fl
                            P.op("pe", lambda e, py_=py_, fc=fc, j=j, wd_=wd_, fl=fl: e.matmul(
                                py_.t[:], hT.t[:, fc, j * 128:(j + 1) * 128], wd_.t[:, fl, :], start=(fc == 0), stop=(fc == NF - 1)),
                                r=[hT.b, wd_.b], w=[py_.b])
                    if yi % 2 == 0:
                        P.op("act", lambda e, py_=py_, ys_=ys_, j=j, ch=ch: e.copy(out=ys_.t[:, j, ch * 512:(ch + 1) * 512], in_=py_.t[:]), r=[py_.b], w=[ys_.b])
                    else:
                        P.op("dve", lambda e, py_=py_, ys_=ys_, j=j, ch=ch: e.tensor_copy(out=ys_.t[:, j, ch * 512:(ch + 1) * 512], in_=py_.t[:]), r=[py_.b], w=[ys_.b])
            pend2[0] = (lambda ys_=ys_, i=i: P.op("st", lambda e: e.dma_start(out=ys_d.ap()[i * 512:(i + 1) * 512, :].rearrange("(j p) n -> p j n", p=128), in_=ys_.t[:]), r=[ys_.b]))
        pend2[0]()
        ph.close()

    def phase_H3():
        ph = Phase(P)
        gB_ = ph.sb([128, D], F32, "gB")
        bB_ = ph.sb([128, D], F32, "bB")
        P.op("ld", lambda e: e.dma_start(out=gB_.t[:], in_=bcast_row(oln2g)), w=[gB_.b])
        P.op("ld", lambda e: e.dma_start(out=bB_.t[:], in_=bcast_row(oln2b)), w=[bB_.b])
        xf = [ph.sb([128, D], F32, "xf") for _ in range(3)]
        ya = [ph.sb([128, D], F32, "ya") for _ in range(3)]
        yb = [ph.sb([128, D], F32, "yb") for _ in range(3)]
        z = [ph.sb([128, D], F32, "z") for _ in range(3)]
        xo = [ph.sb([128, D], F32, "xo") for _ in range(3)]
        st6 = [ph.sb([128, 2, 6], F32, "st6") for _ in range(3)]
        mv = [ph.sb([128, 2], F32, "mv") for _ in range(3)]
        rstd = [ph.sb([128, 2], F32, "rstd") for _ in range(3)]
        pend_h = []
        NB3 = len(xf)
        for tt in range(NT):
            i2 = tt % NB3
            x_ = xf[i2]; a_ = ya[i2]; b_ = yb[i2]; z_ = z[i2]
            P.op("ld", lambda e, x_=x_, tt=tt: e.dma_start(out=x_.t[:], in_=x3_d.ap()[tt * 128:(tt + 1) * 128, :]), w=[x_.b])
            if len(pend_h) > 0:
                pend_h.pop(0)()
            P.op("gld", lambda e, a_=a_, tt=tt: e.indirect_dma_start(out=a_.t[:], out_offset=None, in_=ys_d.ap(),
                                                                   in_offset=bass.IndirectOffsetOnAxis(ap=SLI.t[:, 0, tt:tt + 1], axis=0)), r=[SLI.b], w=[a_.b])
            P.op("gld", lambda e, b_=b_, tt=tt: e.indirect_dma_start(out=b_.t[:], out_offset=None, in_=ys_d.ap(),
                                                                   in_offset=bass.IndirectOffsetOnAxis(ap=SLI.t[:, 1, tt:tt + 1], axis=0)), r=[SLI.b], w=[b_.b])
            P.op("dve", lambda e, x_=x_, a_=a_, z_=z_, tt=tt: e.tensor_scalar(out=z_.t[:], in0=a_.t[:], scalar1=GA.t[:, tt:tt + 1], scalar2=None, op0=ALU.mult),
                 r=[a_.b, GA.b], w=[z_.b])
            P.op("dve", lambda e, b_=b_, z_=z_, tt=tt: e.scalar_tensor_tensor(out=z_.t[:], in0=b_.t[:], scalar=GB.t[:, tt:tt + 1], in1=z_.t[:], op0=ALU.mult, op1=ALU.add),
                 r=[b_.b, GB.b, z_.b], w=[z_.b])
            P.op("dve", lambda e, x_=x_, z_=z_: e.scalar_tensor_tensor(out=z_.t[:], in0=x_.t[:], scalar=ALPHA, in1=z_.t[:], op0=ALU.mult, op1=ALU.add),
                 r=[x_.b, z_.b], w=[z_.b])
            layernorm_rows(ph, z_, gB_, bB_, xo[i2], st6[i2], mv[i2], rstd[i2], None)
            pend_h.append(lambda i2=i2, tt=tt: P.op("st", lambda e: e.dma_start(out=out_d.ap()[tt * 128:(tt + 1) * 128, :], in_=xo[i2].t[:]), r=[xo[i2].b]))
        while pend_h:
            pend_h.pop(0)()
        ph.close()

    phases = [
        ("T", phase_tables),
        ("A", phase_A),
        ("B", phase_B),
        ("C", phase_C),
        ("D", lambda: phase_outproj("catT", ewout, eln1g, eln1b, x_d, x1_d)),
        ("E", phase_E),
        ("F0", lambda: phase_F(0)),
        ("F1", lambda: phase_F(1)),
        ("F2", lambda: phase_F(2)),
        ("G", lambda: phase_outproj("ug", owout, oln1g, oln1b, x2_d, x3_d)),
        ("H1", phase_H1),
        ("H2", phase_H2),
        ("H3", phase_H3),
    ]
    P.flush(barrier=True)
    for name, fn in phases:
        if name in skip:
            continue
        fn()
        if stop_after == name:
            break
    G.close()
    P.barrier(issuers=("sp",))
    return nc


INPUT_NAMES = ["x", "rel_bias", "even_w_in", "even_conv_w", "even_conv_b", "even_conv_ln_g", "even_conv_ln_b", "even_w_out",
               "even_ln1_g", "even_ln1_b", "even_ffn_wg", "even_ffn_wu", "even_ffn_wd", "even_ln2_g", "even_ln2_b",
               "odd_w_in", "odd_w_out", "odd_ln1_g", "odd_ln1_b", "odd_router", "odd_moe_wg", "odd_moe_wu", "odd_moe_wd",
               "odd_ln2_g", "odd_ln2_b"]


def make_in_maps(inputs, n_cores=8):
    cs = host_consts()
    shared = {}
    for k in INPUT_NAMES:
        if k == "x":
            continue
        a = np.ascontiguousarray(np.asarray(inputs[k], dtype=np.float32))
        if k == "rel_bias":
            shared[k] = a
        elif a.ndim >= 2 and a.shape[0] == 1:
            shared[k] = np.ascontiguousarray(a[0]) if a.ndim > 2 else a
        else:
            shared[k] = a
    for k, v in cs.items():
        shared["c_" + k] = v
    x = np.asarray(inputs["x"], dtype=np.float32)
    maps = []
    for i in range(n_cores):
        m = dict(shared)
        m["x"] = np.ascontiguousarray(x[i])
        maps.append(m)
    return maps


def kernel(**inputs):
    nc = build()
    maps = make_in_maps(inputs, 8)
    res = run_bass_kernel_spmd(nc, maps, core_ids=list(range(8)))
    out = np.stack([np.asarray(r["out"], dtype=np.float32) for r in res.results], axis=0)
    return out
```

```python
import math
import bisect
from contextlib import ExitStack

import numpy as np
import concourse.bass as bass
import concourse.mybir as mybir
from concourse.bass_utils import run_bass_kernel_spmd

F32 = mybir.dt.float32
BF16 = mybir.dt.bfloat16
AF = mybir.ActivationFunctionType
ALU = mybir.AluOpType
AX = mybir.AxisListType

S = 4096
D = 1024
NT = S // 128
ALPHA = 4 ** 0.25
EPS = 1e-5
NEGM = -30000.0
NIT = 12
EPOCH = 4000
KDMA = 12


class Buf:
    __slots__ = ("name", "w", "rs")

    def __init__(self, name=""):
        self.name = name
        self.w = []
        self.rs = []


class Stream:
    def __init__(self, name, issuer, is_dma):
        self.name = name
        self.issuer = issuer
        self.is_dma = is_dma
        self.ops = []
        self.n_total = 0
        self.inc_idx = []
        self.n_inc = 0
        self.sems = []


class Op:
    __slots__ = ("stream", "fn", "deps", "idx", "inc")


class _Rec:
    __slots__ = ("call",)

    def __init__(self):
        self.call = None

    def __getattr__(self, name):
        def f(*a, **k):
            self.call = (name, a, k)
        return f


class Prog:
    def __init__(self, nc):
        self.nc = nc
        self.es = ExitStack()
        self.eng = {"pe": nc.tensor, "act": nc.scalar, "dve": nc.vector, "pool": nc.gpsimd, "sp": nc.sync}
        self.streams = {}
        for n in ("pe", "act", "dve", "pool"):
            self.streams[n] = Stream(n, n, False)
        for n, iss in (("ld", "sp"), ("st", "sp"), ("gld", "pool"), ("gst", "pool")):
            self.streams[n] = Stream(n, iss, True)
        self.order = []
        self.waited = {}

    def op(self, sname, fn, r=(), w=(), lazy=False):
        st = self.streams[sname]
        o = Op()
        o.stream = st
        if lazy:
            o.fn = fn
        else:
            rec = _Rec()
            fn(rec)
            assert rec.call is not None
            o.fn = rec.call
        o.idx = st.n_total
        o.inc = False
        st.n_total += 1
        deps = set()
        me = (sname, o.idx)
        for b in r:
            for wr in b.w:
                deps.add(wr + ("raw",))
        for b in w:
            for wr in b.w:
                deps.add(wr + ("waw",))
            for rd in b.rs:
                deps.add(rd + ("war",))
        fd = set()
        best = {}
        for (s, i, kind) in deps:
            if s == sname:
                if sname == "pe":
                    continue
                if kind == "waw":
                    continue
                if not st.is_dma and kind != "raw":
                    continue
            if self.streams[s].is_dma:
                fd.add((s, i))
            else:
                if s not in best or best[s] < i:
                    best[s] = i
        for s, i in best.items():
            fd.add((s, i))
        o.deps = fd
        for b in r:
            b.rs.append(me)
        for b in w:
            if st.is_dma and b.w and not b.rs and all(x[0] == sname for x in b.w):
                b.w.append(me)
            else:
                b.w = [me]
            b.rs = []
        st.ops.append(o)
        self.order.append(o)
        return o

    def _sem_for(self, st, ordinal):
        ep = (ordinal - 1) // EPOCH
        while len(st.sems) <= ep:
            st.sems.append(self.es.enter_context(self.nc.semaphore("s_%s_%d" % (st.name, len(st.sems)))))
        return st.sems[ep], (ordinal - 1) % EPOCH + 1

    def _dma_sem(self, st, idx):
        k = idx % KDMA
        while len(st.sems) <= k:
            st.sems.append(self.es.enter_context(self.nc.semaphore("d_%s_%d" % (st.name, len(st.sems)))))
        return k, st.sems[k], 16 * (idx // KDMA + 1)

    def _wait_dma(self, issuer, st, idx):
        k, sem, val = self._dma_sem(st, idx)
        key = (issuer, st.name, k)
        if self.waited.get(key, 0) >= val:
            return
        self.waited[key] = val
        self.eng[issuer].wait_ge(sem, val)

    def _wait(self, issuer, st, ordinal):
        sem, val = self._sem_for(st, ordinal)
        key = (issuer, st.name)
        if self.waited.get(key, 0) >= ordinal:
            return
        self.waited[key] = ordinal
        self.eng[issuer].wait_ge(sem, val)

    def flush(self, barrier=True):
        need = {}
        for o in self.order:
            for s, i in o.deps:
                need.setdefault(s, set()).add(i)
        for sname, st in self.streams.items():
            if not st.ops or st.is_dma:
                continue
            tg = need.get(sname, set())
            for o in st.ops:
                if o.idx in tg:
                    o.inc = True
            st.ops[-1].inc = True
        ordinal_of = {}
        for sname, st in self.streams.items():
            if st.is_dma:
                continue
            for o in st.ops:
                if o.inc:
                    st.n_inc += 1
                    st.inc_idx.append(o.idx)
                    ordinal_of[(sname, o.idx)] = st.n_inc
        for o in self.order:
            st = o.stream
            issuer = st.issuer
            for s, i in sorted(o.deps):
                ps = self.streams[s]
                if ps.is_dma:
                    self._wait_dma(issuer, ps, i)
                else:
                    k = bisect.bisect_left(ps.inc_idx, i)
                    assert k < len(ps.inc_idx), (s, i)
                    self._wait(issuer, ps, k + 1)
            if st.is_dma and o.idx >= KDMA:
                self._wait_dma(issuer, st, o.idx - KDMA)
            if callable(o.fn):
                inst = o.fn(self.eng[issuer])
            else:
                nm, a_, k_ = o.fn
                inst = getattr(self.eng[issuer], nm)(*a_, **k_)
            if st.is_dma:
                k, sem, val = self._dma_sem(st, o.idx)
                inst.then_inc(sem, 16)
            elif o.inc:
                sem, val = self._sem_for(st, ordinal_of[(st.name, o.idx)])
                inst.then_inc(sem, 1)
        self.order = []
        for st in self.streams.values():
            st.ops = []
        if barrier:
            self.barrier()

    def barrier(self, issuers=("pe", "act", "dve", "pool", "sp")):
        for iss in issuers:
            for st in self.streams.values():
                if st.is_dma:
                    for i in range(max(0, st.n_total - KDMA), st.n_total):
                        self._wait_dma(iss, st, i)
                elif st.n_inc > 0:
                    self._wait(iss, st, st.n_inc)


class T:
    __slots__ = ("t", "b")

    def __init__(self, t, name=""):
        self.t = t
        self.b = Buf(name)


class Phase:
    cnt = 0

    def __init__(self, P):
        self.P = P
        self.nc = P.nc
        self.es = ExitStack()
        self.n = 0

    def sb(self, shape, dt, name="t"):
        Phase.cnt += 1
        nm = "%s_%d" % (name, Phase.cnt)
        return T(self.es.enter_context(self.nc.sbuf_tensor(nm, list(shape), dt)), nm)

    def ps(self, shape, dt, name="p"):
        Phase.cnt += 1
        nm = "%s_%d" % (name, Phase.cnt)
        return T(self.es.enter_context(self.nc.psum_tensor(nm, list(shape), dt)), nm)

    def close(self):
        self.P.flush(barrier=True)
        self.es.close()


def _bucket(n):
    n = np.asarray(n).astype(np.int64)
    nf = np.maximum(n, 1).astype(np.float32)
    large = 16 + (np.log(nf / np.float32(16)) / np.float32(math.log(2048 / 16)) * 16).astype(np.int32)
    large = np.minimum(large, 31)
    return np.where(n < 16, n, large)


L0 = 2688
L1 = 384


def host_consts():
    c = {}
    c["ident"] = np.eye(128, dtype=np.float32)
    c["anti"] = np.eye(128, dtype=np.float32)[::-1].copy()
    qi = np.arange(128)[:, None]
    ki = np.arange(128)[None, :]
    c["causneg"] = np.where(ki <= qi, 0.0, -1e30).astype(np.float32)
    n = np.arange(L0)
    b0 = _bucket(np.maximum(n - 511, 0))
    oh0 = np.zeros((32, L0), np.float32)
    oh0[b0, n] = 1.0
    c["oh0"] = oh0
    oh1 = np.zeros((3, 33, L1), np.float32)
    n1 = np.arange(L1)
    dist = n1 - 127
    valid = (dist >= 0) & (dist <= 128)
    for g, r in enumerate((1, 4, 16)):
        bb = _bucket(np.maximum(dist, 0) * r)
        oh1[g, bb[valid], n1[valid]] = 1.0
        oh1[g, 32, :] = np.where(valid, 0.0, NEGM)
    c["oh1"] = oh1
    c["pow2"] = np.tile((0.5 ** np.arange(NIT + 2)).astype(np.float32)[None, :], (128, 1))
    pp = np.arange(128)
    c["ustr"] = (pp[:, None] < pp[None, :]).astype(np.float32)
    c["thr8"] = np.tile((512.0 * np.arange(8)).astype(np.float32)[None, :], (128, 1))
    c["iota23"] = np.tile(np.arange(23, dtype=np.float32)[None, :], (128, 1))
    c["cwg"] = (np.arange(7)[None, :] * 128 + pp[:, None]).astype(np.float32)
    c["cwd"] = (np.arange(8)[None, :] * 128 + pp[:, None]).astype(np.float32)
    return c


def build(stop_after=None, debug=(), hcfg=(4, 8), skip=(), feed=()):
    nc = bass.Bass("TRN2", target_bir_lowering=False)
    P = Prog(nc)

    def din(name, shape, dt=F32):
        return nc.dram_tensor(name, list(shape), dt, kind="ExternalInput")

    def dscr(name, shape, dt):
        kind = "ExternalOutput" if name in debug else ("ExternalInput" if name in feed else "Internal")
        return nc.dram_tensor(name, list(shape), dt, kind=kind)

    x_d = din("x", [S, D])
    rb_d = din("rel_bias", [32, 8])
    ewin = din("even_w_in", [D, 3144])
    ecw = din("even_conv_w", [31, 512])
    ecb = din("even_conv_b", [1, 512])
    ecg = din("even_conv_ln_g", [1, 512])
    ecbb = din("even_conv_ln_b", [1, 512])
    ewout = din("even_w_out", [D, D])
    eln1g = din("even_ln1_g", [1, D])
    eln1b = din("even_ln1_b", [1, D])
    ewg = din("even_ffn_wg", [D, 2816])
    ewu = din("even_ffn_wu", [D, 2816])
    ewd = din("even_ffn_wd", [2816, D])
    eln2g = din("even_ln2_g", [1, D])
    eln2b = din("even_ln2_b", [1, D])
    owin = din("odd_w_in", [D, 9216])
    owout = din("odd_w_out", [D, D])
    oln1g = din("odd_ln1_g", [1, D])
    oln1b = din("odd_ln1_b", [1, D])
    orouter = din("odd_router", [D, 8])
    omwg = din("odd_moe_wg", [8, D, 3584])
    omwu = din("odd_moe_wu", [8, D, 3584])
    omwd = din("odd_moe_wd", [8, 3584, D])
    oln2g = din("odd_ln2_g", [1, D])
    oln2b = din("odd_ln2_b", [1, D])
    c_ident = din("c_ident", [128, 128])
    c_anti = din("c_anti", [128, 128])
    c_caus = din("c_causneg", [128, 128])
    c_oh0 = din("c_oh0", [32, L0])
    c_oh1 = din("c_oh1", [3, 33, L1])
    c_pow2 = din("c_pow2", [128, NIT + 2])
    c_ustr = din("c_ustr", [128, 128])
    c_thr8 = din("c_thr8", [128, 8])
    c_iota23 = din("c_iota23", [128, 23])
    c_cwg = din("c_cwg", [128, 7])
    c_cwd = din("c_cwd", [128, 8])

    out_d = nc.dram_tensor("out", [S, D], F32, kind="ExternalOutput")

    qT_d = dscr("qT", [512, S], BF16)
    kT_d = dscr("kT", [512, S], BF16)
    qiT_d = dscr("qiT", [512, S], BF16)
    kiT_d = dscr("kiT", [128, S], BF16)
    v_d = dscr("vaug", [S, 520], BF16)
    w_d = dscr("widx", [S, 8], F32)
    catT_d = dscr("catT", [D, S], BF16)
    mask_d = dscr("maskneg", [S, S], BF16)
    f0_d = dscr("f0tab", [8, L0], BF16)
    f1_d = dscr("f1tab", [3, 8, L1], BF16)
    x1_d = dscr("x1", [S, D], F32)
    x2_d = dscr("x2", [S, D], F32)
    u_d = dscr("ug", [3, S, 8, 132], F32)
    x3_d = dscr("x3", [S, D], F32)
    wgs_d = dscr("wgs", [56 * 128, 4096], BF16)
    wus_d = dscr("wus", [56 * 128, 4096], BF16)
    wds_d = dscr("wds", [64 * 128, 3584], BF16)
    xs_d = dscr("xs", [23 * 512, D], BF16)
    ys_d = dscr("ys", [23 * 512, D], F32)

    G = Phase(P)
    ident_f = G.sb([128, 128], F32, "identf")
    ident_b = G.sb([128, 128], BF16, "identb")
    anti_b = G.sb([128, 128], BF16, "antib")
    ones_f = G.sb([128, 128], F32, "onesf")
    P.op("ld", lambda e: e.dma_start(out=ident_f.t[:], in_=c_ident.ap()), w=[ident_f.b])
    P.op("gld", lambda e: e.dma_start(out=ident_b.t[:], in_=c_ident.ap()), w=[ident_b.b])
    P.op("gld", lambda e: e.dma_start(out=anti_b.t[:], in_=c_anti.ap()), w=[anti_b.b])
    P.op("dve", lambda e: e.memset(ones_f.t[:], 1.0), w=[ones_f.b])
    GA = G.sb([128, NT], F32, "GA")
    GB = G.sb([128, NT], F32, "GB")
    SLI = G.sb([128, 2, NT], mybir.dt.int32, "SLI")
    IWG = G.sb([128, 23, 7], mybir.dt.int32, "IWG")
    IWD = G.sb([128, 23, 8], mybir.dt.int32, "IWD")
    eps_t = G.sb([128, 1], F32, "epst")
    P.op("dve", lambda e: e.memset(eps_t.t[:], EPS), w=[eps_t.b])
    zer_b = G.sb([128, 512], BF16, "zerb")
    P.op("dve", lambda e: e.memset(zer_b.t[:], 0.0), w=[zer_b.b])

    def evac(i, fn_act, fn_dve):
        return ("act", fn_act) if i % 2 == 0 else ("dve", fn_dve)

    def load_tok_T(ph, src_rows_ap_fn, ntiles, xtok, xT, pts, tag, f32_copy=None, queue="gld"):
        for j in range(ntiles):
            P.op(queue, lambda e, j=j: e.dma_start(out=xtok[j].t[:], in_=src_rows_ap_fn(j)), w=[xtok[j].b])
        k = 0
        for j0 in range(0, ntiles, 4):
            nj = min(4, ntiles - j0)
            for c in range(8):
                pt = pts[k % len(pts)]
                k += 1
                for jj in range(nj):
                    P.op("pe", lambda e, pt=pt, jj=jj, c=c, j0=j0: e.transpose(
                        out=pt.t[:, jj * 128:(jj + 1) * 128], in_=xtok[j0 + jj].t[:, c * 128:(c + 1) * 128],
                        identity=ident_b.t[:]), r=[xtok[j0 + jj].b, ident_b.b], w=[pt.b])
                if k % 2 == 0:
                    P.op("act", lambda e, pt=pt, c=c, j0=j0, nj=nj: e.copy(
                        out=xT.t[:, c, j0 * 128:(j0 + nj) * 128], in_=pt.t[:, 0:nj * 128]), r=[pt.b], w=[xT.b])
                else:
                    P.op("dve", lambda e, pt=pt, c=c, j0=j0, nj=nj: e.tensor_copy(
                        out=xT.t[:, c, j0 * 128:(j0 + nj) * 128], in_=pt.t[:, 0:nj * 128]), r=[pt.b], w=[xT.b])

    def layernorm_rows(ph, z, gB, bB, outt, st6, mv, rstd, nb, gb_eng="dve"):
        for hh in range(2):
            P.op("dve", lambda e, hh=hh: e.bn_stats(out=st6.t[:, hh, :], in_=z.t[:, hh * 512:(hh + 1) * 512]),
                 r=[z.b], w=[st6.b])
        P.op("dve", lambda e: e.bn_aggr(out=mv.t[:], in_=st6.t[:]), r=[st6.b], w=[mv.b])
        P.op("act", lambda e: e.activation(out=rstd.t[:, 0:1], in_=mv.t[:, 1:2], func=AF.Sqrt, bias=eps_t.t[:, 0:1], scale=1.0),
             r=[mv.b, eps_t.b], w=[rstd.b])
        P.op("dve", lambda e: e.reciprocal(out=rstd.t[:, 0:1], in_=rstd.t[:, 0:1]), r=[rstd.b], w=[rstd.b])
        P.op("dve", lambda e: e.scalar_tensor_tensor(out=rstd.t[:, 1:2], in0=mv.t[:, 0:1], scalar=-1.0, in1=rstd.t[:, 0:1],
                                                     op0=ALU.mult, op1=ALU.mult), r=[mv.b, rstd.b], w=[rstd.b])
        P.op("act", lambda e: e.activation(out=z.t[:], in_=z.t[:], func=AF.Identity, bias=rstd.t[:, 1:2], scale=rstd.t[:, 0:1]),
             r=[z.b, rstd.b], w=[z.b])
        P.op(gb_eng, lambda e: e.tensor_tensor(out=z.t[:], in0=z.t[:], in1=gB.t[:], op=ALU.mult),
             r=[z.b, gB.b], w=[z.b])
        P.op(gb_eng, lambda e: e.tensor_tensor(out=outt.t[:], in0=z.t[:], in1=bB.t[:], op=ALU.add),
             r=[z.b, bB.b], w=[outt.b])

    def bcast_row(dr):
        a = dr.ap()
        n = a.shape[-1]
        return bass.AP(tensor=a.tensor, offset=0, ap=[[0, 128], [1, n]])

    def phase_tables():
        ph = Phase(P)
        rb33 = ph.sb([33, 8], BF16, "rb33")
        rb33f = ph.sb([33, 8], F32, "rb33f")
        oh0 = ph.sb([32, L0], BF16, "oh0")
        oh1 = ph.sb([33, 3, L1], BF16, "oh1")
        f0 = ph.sb([8, L0], BF16, "f0")
        f1 = ph.sb([8, 3, L1], BF16, "f1")
        pp = [ph.ps([128, 512], F32, "pp") for _ in range(2)]
        P.op("dve", lambda e: e.memset(rb33f.t[:], 1.0), w=[rb33f.b])
        P.op("ld", lambda e: e.dma_start(out=rb33f.t[0:32, :], in_=rb_d.ap()), r=[], w=[rb33f.b])
        P.op("dve", lambda e: e.tensor_copy(out=rb33.t[:], in_=rb33f.t[:]), r=[rb33f.b], w=[rb33.b])
        P.op("gld", lambda e: e.dma_start(out=oh0.t[:], in_=c_oh0.ap()), w=[oh0.b])
        P.op("gld", lambda e: e.dma_start(out=oh1.t[:], in_=c_oh1.ap().rearrange("g k n -> k g n")), w=[oh1.b])
        k = 0
        for c0 in range(0, L0, 512):
            n = min(512, L0 - c0)
            p = pp[k % 2]
            k += 1
            P.op("pe", lambda e, p=p, c0=c0, n=n: e.matmul(p.t[0:8, 0:n], rb33.t[0:32, :], oh0.t[:, c0:c0 + n],
                                                          start=True, stop=True), r=[rb33.b, oh0.b], w=[p.b])
            P.op("dve", lambda e, p=p, c0=c0, n=n: e.tensor_copy(out=f0.t[:, c0:c0 + n], in_=p.t[0:8, 0:n]),
                 r=[p.b], w=[f0.b])
        for g in range(3):
            p = pp[k % 2]
            k += 1
            P.op("pe", lambda e, p=p, g=g: e.matmul(p.t[0:8, 0:L1], rb33.t[:, :], oh1.t[:, g, :],
                                                    start=True, stop=True), r=[rb33.b, oh1.b], w=[p.b])
            P.op("dve", lambda e, p=p, g=g: e.tensor_copy(out=f1.t[:, g, :], in_=p.t[0:8, 0:L1]),
                 r=[p.b], w=[f1.b])
        P.op("st", lambda e: e.dma_start(out=f0_d.ap(), in_=f0.t[:]), r=[f0.b])
        P.op("st", lambda e: e.dma_start(out=f1_d.ap().rearrange("g h n -> h g n"), in_=f1.t[:]), r=[f1.b])
        ph.close()

    def phase_A():
        ph = Phase(P)
        win = ph.sb([128, 8, 3144], BF16, "win")
        wki2 = ph.sb([128, 8, 128], BF16, "wki2")
        wv_ap = ewin.ap().rearrange("(c p) n -> p c n", p=128)
        for c in range(8):
            P.op("gld", lambda e, c=c: e.dma_start(out=win.t[:, c, :], in_=wv_ap[:, c, :]), w=[win.b])
        P.op("gld", lambda e: e.dma_start(out=wki2.t[:, :, 0:64], in_=wv_ap[:, :, 3072:3136]), w=[wki2.b])
        P.op("gld", lambda e: e.dma_start(out=wki2.t[:, :, 64:128], in_=wv_ap[:, :, 3072:3136]), w=[wki2.b])
        cw_sb = ph.sb([34, 512], F32, "cwsb")
        cwT = ph.sb([128, 4, 34], F32, "cwT")
        P.op("ld", lambda e: e.dma_start(out=cw_sb.t[0:31, :], in_=ecw.ap()), w=[cw_sb.b])
        P.op("ld", lambda e: e.dma_start(out=cw_sb.t[31:32, :], in_=ecb.ap()), w=[cw_sb.b])
        P.op("ld", lambda e: e.dma_start(out=cw_sb.t[32:33, :], in_=ecg.ap()), w=[cw_sb.b])
        P.op("ld", lambda e: e.dma_start(out=cw_sb.t[33:34, :], in_=ecbb.ap()), w=[cw_sb.b])
        pm = [ph.ps([128, 512], F32, "pm") for _ in range(4)]
        pst = [ph.ps([128, 512], F32, "pst") for _ in range(2)]
        ptr = [ph.ps([128, 1024], BF16, "ptr") for _ in range(2)]
        for c in range(4):
            P.op("pe", lambda e, c=c: e.transpose(out=pm[0].t[:, c * 34:(c + 1) * 34], in_=cw_sb.t[0:34, c * 128:(c + 1) * 128],
                                                  identity=ident_f.t[0:34, 0:34]), r=[cw_sb.b, ident_f.b], w=[pm[0].b])
        P.op("dve", lambda e: e.tensor_copy(out=cwT.t[:].rearrange("p c j -> p (c j)"), in_=pm[0].t[:, 0:136]),
             r=[pm[0].b], w=[cwT.b])
        diag = ph.sb([128, 4, 31, 128], BF16, "diag")
        for c in range(4):
            for j in range(31):
                eng = "dve" if (c * 31 + j) % 2 == 0 else "pool"
                P.op(eng, lambda e, c=c, j=j: e.tensor_scalar(out=diag.t[:, c, j, :], in0=ident_f.t[:],
                                                              scalar1=cwT.t[:, c, j:j + 1], scalar2=None, op0=ALU.mult),
                     r=[ident_f.b, cwT.b], w=[diag.b])
        xtok = [ph.sb([128, 1024], BF16, "xtok") for _ in range(4)]
        xT = [ph.sb([128, 8, 512], BF16, "xT") for _ in range(2)]
        ub = [ph.sb([128, 4, 542], BF16, "ub") for _ in range(2)]
        stg = ph.sb([128, 13, 512], BF16, "stg")
        aout = ph.sb([128, 4, 512], BF16, "aout")
        sg = [ph.sb([128, 512], F32, "sg") for _ in range(2)]
        yv = ph.sb([128, 4, 512], F32, "yv")
        ysq = ph.sb([128, 4, 512], F32, "ysq")
        mean = ph.sb([128, 512], F32, "mean")
        rstd = ph.sb([128, 512], F32, "rstd")
        tmp = ph.sb([128, 512], F32, "tmp")
        vst = ph.sb([128, 4, 8, 65], BF16, "vst")
        wst = ph.sb([128, 4, 8], F32, "wst")
        P.op("dve", lambda e: e.memset(ub[0].t[:, :, 0:30], 0.0), w=[ub[0].b])
        P.op("dve", lambda e: e.memset(vst.t[:], 1.0), w=[vst.b])
        xrows = x_d.ap()
        qTv = qT_d.ap().rearrange("(c p) t -> p c t", p=128)
        kTv = kT_d.ap().rearrange("(c p) t -> p c t", p=128)
        qiTv = qiT_d.ap().rearrange("(c p) t -> p c t", p=128)
        catTv = catT_d.ap().rearrange("(c p) t -> p c t", p=128)
        WSCALE = float(8 ** -0.5 * 64 ** -0.5)
        pmi = 0
        for sbk in range(8):
            t0 = sbk * 512
            xt = xT[sbk % 2]
            u_cur = ub[sbk % 2]
            u_prev = ub[(sbk + 1) % 2]
            load_tok_T(ph, lambda j, t0=t0: xrows[t0 + j * 128:t0 + (j + 1) * 128, :], 4, xtok, xt, ptr, "x")
            if sbk > 0:
                P.op("dve", lambda e, u_cur=u_cur, u_prev=u_prev: e.tensor_copy(out=u_cur.t[:, :, 0:30], in_=u_prev.t[:, :, 512:542]),
                     r=[u_prev.b], w=[u_cur.b])

            def proj_fm(col0, wt, pmt):
                for c in range(8):
                    P.op("pe", lambda e, c=c: e.matmul(pmt.t[:], wt.t[:, c, col0:col0 + 128], xt.t[:, c, :],
                                                       start=(c == 0), stop=(c == 7)), r=[wt.b, xt.b], w=[pmt.b])
            for c in range(4):
                pg = pm[pmi % 4]; pmi += 1
                pv = pm[pmi % 4]; pmi += 1
                sgt = sg[c % 2]
                proj_fm(512 + c * 128, win, pg)
                P.op("act", lambda e, pg=pg, sgt=sgt: e.activation(out=sgt.t[:], in_=pg.t[:], func=AF.Sigmoid),
                     r=[pg.b], w=[sgt.b])
                proj_fm(c * 128, win, pv)
                P.op("dve", lambda e, pv=pv, sgt=sgt, c=c, u_cur=u_cur: e.tensor_tensor(
                    out=u_cur.t[:, c, 30:542], in0=pv.t[:], in1=sgt.t[:], op=ALU.mult), r=[pv.b, sgt.b], w=[u_cur.b])
            for i in range(13):
                if i < 4:
                    col0, wt, scale = 1024 + i * 128, win, 0.125
                elif i < 8:
                    col0, wt, scale = 1536 + (i - 4) * 128, win, 1.0
                elif i < 12:
                    col0, wt, scale = 2560 + (i - 8) * 128, win, 1.0
                else:
                    col0, wt, scale = 0, wki2, 1.0
                pq = pm[pmi % 4]; pmi += 1
                proj_fm(col0, wt, pq)
                if i % 2 == 0:
                    P.op("act", lambda e, pq=pq, i=i, scale=scale: e.activation(out=stg.t[:, i, :], in_=pq.t[:], func=AF.Copy, scale=scale),
                         r=[pq.b], w=[stg.b])
                else:
                    P.op("dve", lambda e, pq=pq, i=i, scale=scale: e.tensor_scalar(out=stg.t[:, i, :], in0=pq.t[:], scalar1=scale,
                                                                               scalar2=None, op0=ALU.mult), r=[pq.b], w=[stg.b])
            P.op("st", lambda e, t0=t0: e.dma_start(out=qTv[:, :, t0:t0 + 512], in_=stg.t[:, 0:4, :]), r=[stg.b])
            P.op("st", lambda e, t0=t0: e.dma_start(out=kTv[:, :, t0:t0 + 512], in_=stg.t[:, 4:8, :]), r=[stg.b])
            P.op("st", lambda e, t0=t0: e.dma_start(out=qiTv[:, :, t0:t0 + 512], in_=stg.t[:, 8:12, :]), r=[stg.b])
            P.op("st", lambda e, t0=t0: e.dma_start(out=kiT_d.ap()[:, t0:t0 + 512], in_=stg.t[:, 12, :]), r=[stg.b])
            for j in range(4):
                pv = pm[pmi % 4]; pmi += 1
                for c in range(8):
                    P.op("pe", lambda e, c=c, j=j, pv=pv: e.matmul(pv.t[:], xt.t[:, c, j * 128:(j + 1) * 128], win.t[:, c, 2048:2560],
                                                                 start=(c == 0), stop=(c == 7)), r=[win.b, xt.b], w=[pv.b])
                P.op("act", lambda e, j=j, pv=pv: e.copy(out=vst.t[:, j, :, 0:64], in_=pv.t[:].rearrange("p (h d) -> p h d", h=8)),
                     r=[pv.b], w=[vst.b])
                pw = pm[pmi % 4]; pmi += 1
                for c in range(8):
                    P.op("pe", lambda e, c=c, j=j, pw=pw: e.matmul(pw.t[:, 0:8], xt.t[:, c, j * 128:(j + 1) * 128], win.t[:, c, 3136:3144],
                                                                 start=(c == 0), stop=(c == 7)), r=[win.b, xt.b], w=[pw.b])
                P.op("dve", lambda e, j=j, pw=pw: e.tensor_scalar(out=wst.t[:, j, :], in0=pw.t[:, 0:8], scalar1=WSCALE, scalar2=None,
                                                               op0=ALU.mult), r=[pw.b], w=[wst.b])
            P.op("st", lambda e, t0=t0: e.dma_start(out=v_d.ap()[t0:t0 + 512, :].rearrange("(j p) n -> p j n", p=128),
                                                    in_=vst.t[:].rearrange("p j h d -> p j (h d)")), r=[vst.b])
            P.op("st", lambda e, t0=t0: e.dma_start(out=w_d.ap()[t0:t0 + 512, :].rearrange("(j p) n -> p j n", p=128),
                                                    in_=wst.t[:]), r=[wst.b])
            for c in range(4):
                pc = pm[pmi % 4]; pmi += 1
                for j in range(31):
                    P.op("pe", lambda e, c=c, j=j, pc=pc, u_cur=u_cur: e.matmul(pc.t[:], diag.t[:, c, j, :], u_cur.t[:, c, j:j + 512],
                                                                             start=(j == 0), stop=(j == 30)), r=[diag.b, u_cur.b], w=[pc.b])
                P.op("act", lambda e, c=c, pc=pc: e.activation(out=yv.t[:, c, :], in_=pc.t[:], func=AF.Identity,
                                                              bias=cwT.t[:, c, 31:32], scale=1.0), r=[pc.b, cwT.b], w=[yv.b])
                P.op("act", lambda e, c=c, pc=pc: e.activation(out=ysq.t[:, c, :], in_=pc.t[:], func=AF.Square,
                                                              bias=cwT.t[:, c, 31:32], scale=1.0), r=[pc.b, cwT.b], w=[ysq.b])
            for c in range(4):
                P.op("pe", lambda e, c=c: e.matmul(pst[0].t[:], ones_f.t[:], yv.t[:, c, :], start=(c == 0), stop=(c == 3)),
                     r=[ones_f.b, yv.b], w=[pst[0].b])
            for c in range(4):
                P.op("pe", lambda e, c=c: e.matmul(pst[1].t[:], ones_f.t[:], ysq.t[:, c, :], start=(c == 0), stop=(c == 3)),
                     r=[ones_f.b, ysq.b], w=[pst[1].b])
            P.op("dve", lambda e: e.tensor_scalar(out=mean.t[:], in0=pst[0].t[:], scalar1=1.0 / 512, scalar2=None, op0=ALU.mult),
                 r=[pst[0].b], w=[mean.b])
            P.op("dve", lambda e: e.tensor_tensor(out=tmp.t[:], in0=mean.t[:], in1=mean.t[:], op=ALU.mult), r=[mean.b], w=[tmp.b])
            P.op("dve", lambda e: e.scalar_tensor_tensor(out=rstd.t[:], in0=pst[1].t[:], scalar=1.0 / 512, in1=tmp.t[:],
                                                         op0=ALU.mult, op1=ALU.subtract), r=[pst[1].b, tmp.b], w=[rstd.b])
            P.op("dve", lambda e: e.tensor_scalar(out=rstd.t[:], in0=rstd.t[:], scalar1=EPS, scalar2=None, op0=ALU.add),
                 r=[rstd.b], w=[rstd.b])
            P.op("act", lambda e: e.activation(out=rstd.t[:], in_=rstd.t[:], func=AF.Sqrt), r=[rstd.b], w=[rstd.b])
            P.op("dve", lambda e: e.reciprocal(out=rstd.t[:], in_=rstd.t[:]), r=[rstd.b], w=[rstd.b])
            for c in range(4):
                P.op("dve", lambda e, c=c: e.tensor_tensor(out=yv.t[:, c, :], in0=yv.t[:, c, :], in1=mean.t[:], op=ALU.subtract),
                     r=[yv.b, mean.b], w=[yv.b])
                P.op("dve", lambda e, c=c: e.tensor_tensor(out=yv.t[:, c, :], in0=yv.t[:, c, :], in1=rstd.t[:], op=ALU.mult),
                     r=[yv.b, rstd.b], w=[yv.b])
                P.op("act", lambda e, c=c: e.activation(out=aout.t[:, c, :], in_=yv.t[:, c, :], func=AF.Silu,
                                                       bias=cwT.t[:, c, 33:34], scale=cwT.t[:, c, 32:33]), r=[yv.b, cwT.b], w=[aout.b])
            P.op("st", lambda e, t0=t0: e.dma_start(out=catTv[:, 0:4, t0:t0 + 512], in_=aout.t[:]), r=[aout.b])
        ph.close()

    def phase_B():
        ph = Phase(P)
        qiT = ph.sb([128, 4, S], BF16, "qiT")
        kiT = ph.sb([128, S], BF16, "kiT")
        wtok = ph.sb([128, NT, 8], F32, "wtok")
        caus = ph.sb([128, 128], F32, "caus")
        pow2 = ph.sb([128, NIT + 2], F32, "pow2")
        P.op("ld", lambda e: e.dma_start(out=qiT.t[:], in_=qiT_d.ap().rearrange("(c p) t -> p c t", p=128)), w=[qiT.b])
        P.op("ld", lambda e: e.dma_start(out=kiT.t[:], in_=kiT_d.ap()), w=[kiT.b])
        P.op("ld", lambda e: e.dma_start(out=wtok.t[:], in_=w_d.ap().rearrange("(t p) e -> p t e", p=128)), w=[wtok.b])
        P.op("ld", lambda e: e.dma_start(out=caus.t[:], in_=c_caus.ap()), w=[caus.b])
        P.op("ld", lambda e: e.dma_start(out=pow2.t[:], in_=c_pow2.ap()), w=[pow2.b])
        NSB = 4
        score = [ph.sb([128, S], F32, "score") for _ in range(NSB)]
        mneg = [ph.sb([128, S], BF16, "mneg") for _ in range(2)]
        junk = [ph.sb([128, S], BF16, "junk") for _ in range(2)]
        rr = [ph.sb([128, 512], BF16, "rr") for _ in range(4)]
        dg = [ph.sb([128, 8, 128], BF16, "dg") for _ in range(NSB)]
        pd = [ph.ps([128, 512], F32, "pd") for _ in range(4)]
        psc = [ph.ps([128, 512], F32, "psc") for _ in range(3)]
        sm = [dict((n, ph.sb([128, 1], F32, n)) for n in ("mn", "mx", "w0", "mid", "cnt", "tt", "thr")) for _ in range(NSB)]
        wk = [ph.sb([128, NIT + 2], F32, "wk") for _ in range(NSB)]
        thr_const = ph.sb([128, 1], F32, "thrc")
        P.op("dve", lambda e: e.memset(thr_const.t[:], -1e29), w=[thr_const.b])
        cstage = [ph.sb([128, 4096], BF16, "cst") for _ in range(3)]
        cnt_ = {"ri": 0, "pdi": 0, "sci": 0}

        def prep(qb):
            nk = (qb + 1) * 128
            sc = score[qb % NSB]
            d = dg[qb % NSB]
            for h in range(8):
                P.op("act", lambda e, h=h: e.activation(out=d.t[:, h, :], in_=ident_f.t[:], func=AF.Copy, scale=wtok.t[:, qb, h:h + 1]),
                     r=[ident_f.b, wtok.b], w=[d.b])
            nch = (nk + 511) // 512
            items = []
            for kc in range(nch):
                k0 = kc * 512
                n = min(512, nk - k0)
                pscore = psc[cnt_["sci"] % 3]; cnt_["sci"] += 1
                for h in range(8):
                    items.append((kc, k0, n, h, pscore))
            LAG = 2
            stash = {}
            for idx in range(len(items) + LAG):
                if idx < len(items):
                    kc, k0, n, h, pscore = items[idx]
                    p0 = (h % 2) * 64
                    pdt = pd[cnt_["pdi"] % 4]; cnt_["pdi"] += 1
                    rt = rr[cnt_["ri"] % 4]; cnt_["ri"] += 1
                    stash[idx] = rt
                    P.op("pe", lambda e: e.matmul(
                        pdt.t[:, 0:n], qiT.t[p0:p0 + 64, h // 2, qb * 128:(qb + 1) * 128], kiT.t[p0:p0 + 64, k0:k0 + n],
                        start=True, stop=True), r=[qiT.b, kiT.b], w=[pdt.b])
                    P.op("act", lambda e: e.activation(out=rt.t[:, 0:n], in_=pdt.t[:, 0:n], func=AF.Relu), r=[pdt.b], w=[rt.b])
                j = idx - LAG
                if j >= 0:
                    kc, k0, n, h, pscore = items[j]
                    rt = stash.pop(j)
                    P.op("pe", lambda e: e.matmul(pscore.t[:, 0:n], d.t[:, h, :], rt.t[:, 0:n], start=(h == 0), stop=(h == 7)),
                         r=[d.b, rt.b], w=[pscore.b])
                    if h == 7:
                        P.op("act", lambda e: e.copy(out=sc.t[:, k0:k0 + n], in_=pscore.t[:, 0:n]), r=[pscore.b], w=[sc.b])

        def bis_ops(qb, slot):
            nk = (qb + 1) * 128
            n1 = qb * 128
            sc = score[qb % NSB]
            mg = mneg[slot]
            jk = junk[slot]
            s_ = sm[qb % NSB]
            wkk = wk[qb % NSB]
            ops = []
            ops.append(lambda: P.op("dve", lambda e: e.tensor_tensor(out=sc.t[:, qb * 128:(qb + 1) * 128], in0=sc.t[:, qb * 128:(qb + 1) * 128],
                                                                   in1=caus.t[:], op=ALU.add), r=[sc.b, caus.b], w=[sc.b]))
            if qb >= 2:
                ops.append(lambda: P.op("dve", lambda e: e.tensor_reduce(out=s_["mx"].t[:], in_=sc.t[:, 0:n1], axis=AX.X, op=ALU.max),
                                        r=[sc.b], w=[s_["mx"].b]))
                ops.append(lambda: P.op("dve", lambda e: e.tensor_reduce(out=s_["mn"].t[:], in_=sc.t[:, 0:n1], axis=AX.X, op=ALU.min),
                                        r=[sc.b], w=[s_["mn"].b]))
                ops.append(lambda: P.op("dve", lambda e: e.tensor_tensor(out=s_["w0"].t[:], in0=s_["mx"].t[:], in1=s_["mn"].t[:], op=ALU.subtract),
                                        r=[s_["mx"].b, s_["mn"].b], w=[s_["w0"].b]))
                ops.append(lambda: P.op("dve", lambda e: e.tensor_scalar(out=wkk.t[:], in0=pow2.t[:], scalar1=s_["w0"].t[:, 0:1], scalar2=None,
                                                                       op0=ALU.mult), r=[pow2.b, s_["w0"].b], w=[wkk.b]))
                ops.append(lambda: P.op("dve", lambda e: e.tensor_tensor(out=s_["mid"].t[:], in0=s_["mn"].t[:], in1=wkk.t[:, 1:2], op=ALU.add),
                                        r=[s_["mn"].b, wkk.b], w=[s_["mid"].b]))
                for it in range(1, NIT + 1):
                    ops.append(lambda: P.op("dve", lambda e: e.tensor_scalar(out=jk.t[:, 0:nk], in0=sc.t[:, 0:nk], scalar1=s_["mid"].t[:, 0:1],
                                                                           scalar2=0.0, op0=ALU.is_ge, op1=ALU.add, accum_out=s_["cnt"].t[:, 0:1]),
                                            r=[sc.b, s_["mid"].b], w=[jk.b, s_["cnt"].b]))
                    ops.append(lambda it=it: P.op("dve", lambda e: e.tensor_scalar(out=s_["tt"].t[:], in0=s_["cnt"].t[:], scalar1=255.5,
                                                                                 scalar2=wkk.t[:, it:it + 1], op0=ALU.is_ge, op1=ALU.mult),
                                                  r=[s_["cnt"].b, wkk.b], w=[s_["tt"].b]))
                    ops.append(lambda it=it: P.op("dve", lambda e: e.scalar_tensor_tensor(out=s_["mid"].t[:], in0=s_["tt"].t[:],
                                                                                        scalar=wkk.t[:, it + 1:it + 2], in1=s_["mid"].t[:],
                                                                                        op0=ALU.subtract, op1=ALU.add),
                                                  r=[s_["tt"].b, wkk.b, s_["mid"].b], w=[s_["mid"].b]))
                ops.append(lambda: P.op("dve", lambda e: e.tensor_tensor(out=s_["thr"].t[:], in0=s_["mid"].t[:], in1=wkk.t[:, NIT + 1:NIT + 2],
                                                                       op=ALU.subtract), r=[s_["mid"].b, wkk.b], w=[s_["thr"].b]))
                thr = s_["thr"]
            else:
                thr = thr_const
            ops.append(lambda: P.op("dve", lambda e: e.tensor_scalar(out=mg.t[:, 0:nk], in0=sc.t[:, 0:nk], scalar1=thr.t[:, 0:1],
                                                                   scalar2=NEGM, op0=ALU.is_lt, op1=ALU.mult), r=[sc.b, thr.b], w=[mg.b]))
            ops.append(lambda: P.op("st", lambda e: e.dma_start(out=mask_d.ap()[qb * 128:(qb + 1) * 128, 0:nk], in_=mg.t[:, 0:nk]), r=[mg.b]))
            return ops

        prep(0)
        prep(1)
        for q0 in range(0, NT, 2):
            conv_emit(cstage, 4)
            if q0 + 2 < NT:
                prep(q0 + 2)
                prep(q0 + 3)
            oa = bis_ops(q0, 0)
            ob = bis_ops(q0 + 1, 1)
            for i in range(max(len(oa), len(ob))):
                if i < len(oa):
                    oa[i]()
                if i < len(ob):
                    ob[i]()
        conv_flush_pending()
        ph.close()

    def phase_C():
        ph = Phase(P)
        vaug = ph.sb([128, NT, 520], BF16, "vaug")
        P.op("ld", lambda e: e.dma_start(out=vaug.t[:], in_=v_d.ap().rearrange("(t p) n -> p t n", p=128)), w=[vaug.b])
        qh = [ph.sb([64, S], BF16, "qh") for _ in range(2)]
        kh = [ph.sb([64, S], BF16, "kh") for _ in range(2)]
        gt = [ph.sb([128, 2560], BF16, "gt") for _ in range(2)]
        attT = [ph.sb([64, S], BF16, "attT") for _ in range(2)]
        eb = [ph.sb([128, 2560], BF16, "eb") for _ in range(2)]
        mk = [[ph.sb([128, S], BF16, "mk") for _ in range(4)] for _ in range(2)]
        pT = [ph.sb([128, 512], BF16, "pT") for _ in range(5)]
        rec = [ph.sb([128, 4], F32, "rec") for _ in range(3)]
        atok = [ph.sb([128, 4, 64], BF16, "atok") for _ in range(3)]
        pss = [ph.ps([128, 512], F32, "pss") for _ in range(4)]
        pacc = [ph.ps([128, 512], F32, "pacc") for _ in range(2)]
        ptr = [ph.ps([128, 1024], BF16, "ptrc") for _ in range(2)]
        si = 0
        ai = 0
        mi = 0
        cstage = [ph.sb([128, 4096], BF16, "cst") for _ in range(3)]
        pend_c = []
        fin_c = []
        for h in range(8):
            q_ = qh[h % 2]; k_ = kh[h % 2]; g_ = gt[h % 2]; at_ = attT[h % 2]
            P.op("ld", lambda e, q_=q_, h=h: e.dma_start(out=q_.t[:], in_=qT_d.ap()[h * 64:(h + 1) * 64, :]), w=[q_.b])
            P.op("ld", lambda e, k_=k_, h=h: e.dma_start(out=k_.t[:], in_=kT_d.ap()[h * 64:(h + 1) * 64, :]), w=[k_.b])
            fa = f0_d.ap()
            P.op("ld", lambda e, g_=g_, h=h, fa=fa: e.dma_start(out=g_.t[:], in_=bass.AP(tensor=fa.tensor, offset=h * L0,
                                                                                    ap=[[1, 128], [1, 2560]])), w=[g_.b])
            if pend_c:
                pend_c.pop(0)()
            eb_ = eb[h % 2]
            for c5 in range(5):
                pe_ = pss[c5 % 4]
                P.op("pe", lambda e, pe_=pe_, c5=c5, g_=g_: e.matmul(pe_.t[:], anti_b.t[:], g_.t[:, c5 * 512:(c5 + 1) * 512], start=True, stop=True),
                     r=[anti_b.b, g_.b], w=[pe_.b])
                P.op("act", lambda e, pe_=pe_, c5=c5, eb_=eb_: e.activation(out=eb_.t[:, c5 * 512:(c5 + 1) * 512], in_=pe_.t[:], func=AF.Exp),
                     r=[pe_.b], w=[eb_.b])
            for Q in range(8):
                conv_emit(cstage, 2)
                mks = mk[mi % 2]; mi += 1
                for j in range(4):
                    qb = 4 * Q + j
                    nk = (qb + 1) * 128
                    P.op("ld", lambda e, m=mks[j], qb=qb, nk=nk: e.dma_start(out=m.t[:, 0:nk], in_=mask_d.ap()[qb * 128:(qb + 1) * 128, 0:nk]),
                         w=[mks[j].b])
                acc = pacc[ai % 2]; ai += 1
                P.op("pe", lambda e, acc=acc: e.matmul(acc.t[:, 0:260], zer_b.t[:, 0:128], zer_b.t[:, 0:260], start=True, stop=False),
                     r=[zer_b.b], w=[acc.b])
                nkb = 4 * Q + 4
                sbase = si
                si += nkb

                def emit_S(kb, Q=Q, q_=q_, k_=k_, g_=g_, mks=mks, sbase=sbase):
                    j0 = max(0, kb - 4 * Q)
                    c0 = j0 * 128
                    ps_ = pss[(sbase + kb) % 4]
                    P.op("pe", lambda e: e.matmul(ps_.t[:, c0:512], k_.t[:, kb * 128:(kb + 1) * 128], q_.t[:, Q * 512 + c0:(Q + 1) * 512],
                                                  start=True, stop=False), r=[k_.b, q_.b], w=[ps_.b])
                    for j in range(j0, 4):
                        P.op("pe", lambda e, j=j: e.matmul(ps_.t[:, j * 128:(j + 1) * 128], mks[j].t[:, kb * 128:(kb + 1) * 128], ident_b.t[:],
                                                           start=False, stop=True), r=[mks[j].b, ident_b.b], w=[ps_.b])

                emit_S(0)
                emit_S(1)
                while fin_c:
                    fin_c.pop(0)()
                for kb in range(nkb):
                    if kb + 2 < nkb:
                        emit_S(kb + 2)
                    j0 = max(0, kb - 4 * Q)
                    c0 = j0 * 128
                    ps_ = pss[(sbase + kb) % 4]
                    pt_ = pT[(sbase + kb) % 5]
                    P.op("act", lambda e, ps_=ps_, pt_=pt_, c0=c0: e.activation(out=pt_.t[:, c0:512], in_=ps_.t[:, c0:512], func=AF.Exp),
                         r=[ps_.b], w=[pt_.b])
                    dl = min(4 * Q - kb, 13)
                    off = dl * 128 + 384
                    P.op("dve", lambda e, pt_=pt_, c0=c0, off=off, eb_=eb_: e.tensor_tensor(out=pt_.t[:, c0:512], in0=pt_.t[:, c0:512],
                                                                                       in1=eb_.t[:, off + c0:off + 512], op=ALU.mult),
                         r=[pt_.b, eb_.b], w=[pt_.b])
                    for j in range(j0, 4):
                        P.op("pe", lambda e, acc=acc, pt_=pt_, kb=kb, j=j, h=h, Q=Q: e.matmul(
                            acc.t[:, j * 65:(j + 1) * 65], pt_.t[:, j * 128:(j + 1) * 128], vaug.t[:, kb, h * 65:(h + 1) * 65],
                            start=False, stop=(kb == 4 * Q + j)), r=[pt_.b, vaug.b], w=[acc.b])
                rc = rec[ai % 3]
                ak = atok[ai % 3]
                P.op("dve", lambda e, acc=acc, rc=rc: e.reciprocal(out=rc.t[:], in_=acc.t[:, 0:260].rearrange("p (j d) -> p j d", d=65)[:, :, 64]),
                     r=[acc.b], w=[rc.b])
                for j in range(4):
                    P.op("dve", lambda e, acc=acc, rc=rc, ak=ak, j=j: e.tensor_scalar(out=ak.t[:, j, :], in0=acc.t[:, j * 65:j * 65 + 64],
                                                                                   scalar1=rc.t[:, j:j + 1], scalar2=None, op0=ALU.mult),
                         r=[acc.b, rc.b], w=[ak.b])
                pt2 = ptr[ai % 2]

                def finish(pt2=pt2, ak=ak, at_=at_, Q=Q):
                    for j in range(4):
                        P.op("pe", lambda e, j=j: e.transpose(out=pt2.t[0:64, j * 128:(j + 1) * 128], in_=ak.t[:, j, :],
                                                              identity=ident_b.t[:]), r=[ak.b, ident_b.b], w=[pt2.b])
                    P.op("act", lambda e: e.copy(out=at_.t[:, Q * 512:(Q + 1) * 512], in_=pt2.t[0:64, 0:512]),
                         r=[pt2.b], w=[at_.b])
                if Q == 7:
                    finish()
                else:
                    fin_c.append(finish)
            pend_c.append(lambda at_=at_, h=h: P.op("st", lambda e: e.dma_start(out=catT_d.ap()[512 + h * 64:512 + (h + 1) * 64, :], in_=at_.t[:]), r=[at_.b]))
        while pend_c:
            pend_c.pop(0)()
        conv_finish(cstage)
        ph.close()

    def phase_outproj(src_kind, wout_d, lng_d, lnb_d, xres_d, xout_d):
        ph = Phase(P)
        wo = ph.sb([128, 8, D], BF16, "wo")
        P.op("gld", lambda e: e.dma_start(out=wo.t[:], in_=wout_d.ap().rearrange("(c p) n -> p c n", p=128)), w=[wo.b])
        gB = ph.sb([128, D], F32, "gB")
        bB = ph.sb([128, D], F32, "bB")
        P.op("ld", lambda e: e.dma_start(out=gB.t[:], in_=bcast_row(lng_d)), w=[gB.b])
        P.op("ld", lambda e: e.dma_start(out=bB.t[:], in_=bcast_row(lnb_d)), w=[bB.b])
        pmx = [ph.ps([128, 512], F32, "pmx") for _ in range(4)]
        xr = [ph.sb([128, D], F32, "xr") for _ in range(3)]
        z = [ph.sb([128, D], F32, "z") for _ in range(3)]
        xo = [ph.sb([128, D], F32, "xo") for _ in range(3)]
        st6 = [ph.sb([128, 2, 6], F32, "st6") for _ in range(3)]
        mv = [ph.sb([128, 2], F32, "mv") for _ in range(3)]
        rstd = [ph.sb([128, 2], F32, "rstd") for _ in range(3)]
        if src_kind == "catT":
            cT = [ph.sb([128, 8, 512], BF16, "cT") for _ in range(2)]
        else:
            ug = [[ph.sb([128, 8, 132], F32, "ug") for _ in range(3)] for _ in range(3)]
            rc8 = [ph.sb([128, 8], F32, "rc8") for _ in range(3)]
            otok = [ph.sb([128, D], BF16, "otok") for _ in range(3)]
            oT = [ph.sb([128, 8, 128], BF16, "oT") for _ in range(3)]
            ptr = [ph.ps([128, 1024], BF16, "ptro") for _ in range(2)]
        catTv = catT_d.ap().rearrange("(c p) t -> p c t", p=128)
        NB = len(xr)
        st1 = {}

        def stage1(tt):
            i2 = tt % NB
            t0 = tt * 128
            if src_kind == "catT":
                if tt % 4 == 0:
                    ct = cT[(tt // 4) % 2]
                    P.op("ld", lambda e: e.dma_start(out=ct.t[:], in_=catTv[:, :, t0:t0 + 512]), w=[ct.b])
                ct = cT[(tt // 4) % 2]
                lhs = lambda c: ct.t[:, c, (tt % 4) * 128:(tt % 4 + 1) * 128]
                lhs_b = ct.b
            else:
                u3 = ug[i2]
                for g in range(3):
                    P.op("ld", lambda e, g=g: e.dma_start(out=u3[g].t[:], in_=u_d.ap()[g, t0:t0 + 128, :, :]), w=[u3[g].b])
                P.op("dve", lambda e: e.tensor_tensor(out=u3[0].t[:], in0=u3[0].t[:], in1=u3[1].t[:], op=ALU.add),
                     r=[u3[0].b, u3[1].b], w=[u3[0].b])
                P.op("dve", lambda e: e.tensor_tensor(out=u3[0].t[:], in0=u3[0].t[:], in1=u3[2].t[:], op=ALU.add),
                     r=[u3[0].b, u3[2].b], w=[u3[0].b])
                rc = rc8[i2]
                ot = otok[i2]
                P.op("dve", lambda e: e.reciprocal(out=rc.t[:], in_=u3[0].t[:, :, 128]), r=[u3[0].b], w=[rc.b])
                for hh in range(8):
                    eng = "dve" if hh % 2 == 0 else "pool"
                    P.op(eng, lambda e, hh=hh: e.tensor_scalar(out=ot.t[:, hh * 128:(hh + 1) * 128], in0=u3[0].t[:, hh, 0:128],
                                                              scalar1=rc.t[:, hh:hh + 1], scalar2=None, op0=ALU.mult),
                         r=[u3[0].b, rc.b], w=[ot.b])
                o_T = oT[i2]
                for half in range(2):
                    pt = ptr[half]
                    for cc in range(4):
                        c = half * 4 + cc
                        P.op("pe", lambda e, c=c, cc=cc: e.transpose(out=pt.t[:, cc * 128:(cc + 1) * 128], in_=ot.t[:, c * 128:(c + 1) * 128],
                                                                   identity=ident_b.t[:]), r=[ot.b, ident_b.b], w=[pt.b])
                    P.op("act", lambda e: e.copy(out=o_T.t[:, half * 4:(half + 1) * 4, :].rearrange("p c t -> p (c t)"),
                                                 in_=pt.t[:, 0:512]), r=[pt.b], w=[o_T.b])
                lhs = lambda c: o_T.t[:, c, :]
                lhs_b = o_T.b
            x_ = xr[i2]
            P.op("ld", lambda e: e.dma_start(out=x_.t[:], in_=xres_d.ap()[t0:t0 + 128, :]), w=[x_.b])
            st1[tt] = (lhs, lhs_b)

        def stage2(tt):
            i2 = tt % NB
            t0 = tt * 128
            lhs, lhs_b = st1.pop(tt)
            x_ = xr[i2]
            z_ = z[i2]
            for half in range(2):
                pm_ = pmx[(tt * 2 + half) % 4]
                for c in range(8):
                    P.op("pe", lambda e, c=c: e.matmul(pm_.t[:], lhs(c), wo.t[:, c, half * 512:(half + 1) * 512],
                                                       start=(c == 0), stop=(c == 7)), r=[lhs_b, wo.b], w=[pm_.b])
                P.op("dve", lambda e: e.scalar_tensor_tensor(
                    out=z_.t[:, half * 512:(half + 1) * 512], in0=x_.t[:, half * 512:(half + 1) * 512], scalar=ALPHA, in1=pm_.t[:],
                    op0=ALU.mult, op1=ALU.add), r=[pm_.b, x_.b], w=[z_.b])
            layernorm_rows(ph, z_, gB, bB, xo[i2], st6[i2], mv[i2], rstd[i2], None, gb_eng="pool")
            return lambda: P.op("st", lambda e: e.dma_start(out=xout_d.ap()[t0:t0 + 128, :], in_=xo[i2].t[:]), r=[xo[i2].b])

        stage1(0)
        pend = None
        for tt in range(NT):
            if tt + 1 < NT:
                stage1(tt + 1)
            if pend is not None:
                pend()
            pend = stage2(tt)
        pend()
        ph.close()

    def phase_E():
        ph = Phase(P)
        NF = 22
        wd = ph.sb([128, NF, D], BF16, "wd")
        wdv = ewd.ap().rearrange("(f p) n -> p f n", p=128)
        for f0 in range(0, NF, 6):
            f1 = min(NF, f0 + 6)
            P.op("gld", lambda e, f0=f0, f1=f1: e.dma_start(out=wd.t[:, f0:f1, :], in_=wdv[:, f0:f1, :]), w=[wd.b])
        gB = ph.sb([128, D], F32, "gB")
        bB = ph.sb([128, D], F32, "bB")
        P.op("ld", lambda e: e.dma_start(out=gB.t[:], in_=bcast_row(eln2g)), w=[gB.b])
        P.op("ld", lambda e: e.dma_start(out=bB.t[:], in_=bcast_row(eln2b)), w=[bB.b])
        xtok = [ph.sb([128, D], BF16, "xtok") for _ in range(8)]
        xT = ph.sb([128, 8, 1024], BF16, "xT")
        hT = ph.sb([128, NF, 1024], BF16, "hT")
        wgp = [ph.sb([128, 8, 256], BF16, "wgp") for _ in range(2)]
        wup = [ph.sb([128, 8, 256], BF16, "wup") for _ in range(2)]
        sg = [ph.sb([128, 512], F32, "sg") for _ in range(2)]
        ptr = [ph.ps([128, 1024], BF16, "ptre") for _ in range(2)]
        pg = [ph.ps([128, 512], F32, "pg") for _ in range(2)]
        pu = [ph.ps([128, 512], F32, "pu") for _ in range(2)]
        py = [ph.ps([128, 512], F32, "py") for _ in range(2)]
        xr = [ph.sb([128, D], F32, "xr") for _ in range(2)]
        z = [ph.sb([128, D], F32, "z") for _ in range(2)]
        xo = [ph.sb([128, D], F32, "xo") for _ in range(2)]
        st6 = [ph.sb([128, 2, 6], F32, "st6") for _ in range(2)]
        mv = [ph.sb([128, 2], F32, "mv") for _ in range(2)]
        rstd = [ph.sb([128, 2], F32, "rstd") for _ in range(2)]
        wgv = ewg.ap().rearrange("(c p) n -> p c n", p=128)
        wuv = ewu.ap().rearrange("(c p) n -> p c n", p=128)
        pi = 0
        gi = 0
        pend_e = [None]
        for grp in range(4):
            t0 = grp * 1024
            load_tok_T(ph, lambda j, t0=t0: x1_d.ap()[t0 + j * 128:t0 + (j + 1) * 128, :], 8, xtok, xT, ptr, "x1")
            for pc in range(11):
                wg_ = wgp[pi % 2]; wu_ = wup[pi % 2]; pi += 1
                P.op("gld", lambda e, wg_=wg_, pc=pc: e.dma_start(out=wg_.t[:], in_=wgv[:, :, pc * 256:(pc + 1) * 256]), w=[wg_.b])
                P.op("gld", lambda e, wu_=wu_, pc=pc: e.dma_start(out=wu_.t[:], in_=wuv[:, :, pc * 256:(pc + 1) * 256]), w=[wu_.b])
                for fs in range(2):
                    fc = pc * 2 + fs
                    for half in range(2):
                        pg_ = pg[gi % 2]; pu_ = pu[gi % 2]; sg_ = sg[gi % 2]; gi += 1
                        for c in range(8):
                            P.op("pe", lambda e, pg_=pg_, wg_=wg_, c=c, fs=fs, half=half: e.matmul(
                                pg_.t[:], wg_.t[:, c, fs * 128:(fs + 1) * 128], xT.t[:, c, half * 512:(half + 1) * 512],
                                start=(c == 0), stop=(c == 7)), r=[wg_.b, xT.b], w=[pg_.b])
                        for c in range(8):
                            P.op("pe", lambda e, pu_=pu_, wu_=wu_, c=c, fs=fs, half=half: e.matmul(
                                pu_.t[:], wu_.t[:, c, fs * 128:(fs + 1) * 128], xT.t[:, c, half * 512:(half + 1) * 512],
                                start=(c == 0), stop=(c == 7)), r=[wu_.b, xT.b], w=[pu_.b])
                        P.op("act", lambda e, pg_=pg_, sg_=sg_: e.activation(out=sg_.t[:], in_=pg_.t[:], func=AF.Silu), r=[pg_.b], w=[sg_.b])
                        P.op("dve", lambda e, pu_=pu_, sg_=sg_, fc=fc, half=half: e.tensor_tensor(
                            out=hT.t[:, fc, half * 512:(half + 1) * 512], in0=pu_.t[:], in1=sg_.t[:], op=ALU.mult), r=[pu_.b, sg_.b], w=[hT.b])
            for j in range(8):
                tt = grp * 8 + j
                i2 = tt % 2
                x_ = xr[i2]; z_ = z[i2]
                P.op("ld", lambda e, x_=x_, tt=tt: e.dma_start(out=x_.t[:], in_=x1_d.ap()[tt * 128:(tt + 1) * 128, :]), w=[x_.b])
                for half in range(2):
                    py_ = py[half]
                    for fc in range(NF):
                        P.op("pe", lambda e, py_=py_, fc=fc, j=j, half=half: e.matmul(
                            py_.t[:], hT.t[:, fc, j * 128:(j + 1) * 128], wd.t[:, fc, half * 512:(half + 1) * 512],
                            start=(fc == 0), stop=(fc == NF - 1)), r=[hT.b, wd.b], w=[py_.b])
                    P.op("dve", lambda e, py_=py_, x_=x_, z_=z_, half=half: e.scalar_tensor_tensor(
                        out=z_.t[:, half * 512:(half + 1) * 512], in0=x_.t[:, half * 512:(half + 1) * 512], scalar=ALPHA, in1=py_.t[:],
                        op0=ALU.mult, op1=ALU.add), r=[py_.b, x_.b], w=[z_.b])
                layernorm_rows(ph, z_, gB, bB, xo[i2], st6[i2], mv[i2], rstd[i2], None)
                if pend_e[0] is not None:
                    pend_e[0]()
                pend_e[0] = (lambda i2=i2, tt=tt: P.op("st", lambda e: e.dma_start(out=x2_d.ap()[tt * 128:(tt + 1) * 128, :], in_=xo[i2].t[:]), r=[xo[i2].b]))
        pend_e[0]()
        ph.close()

    def phase_F(g):
        r = (1, 4, 16)[g]
        Lc = S // r
        nb = Lc // 128
        ph = Phase(P)
        wq = ph.sb([128, 8, 1024], BF16, "wq")
        wk_ = ph.sb([128, 8, 1024], BF16, "wk")
        wv = ph.sb([128, 8, 1024], BF16, "wv")
        wv_ap = owin.ap().rearrange("(c p) n -> p c n", p=128)
        for j, wt in enumerate((wq, wk_, wv)):
            col = (g * 3 + j) * 1024
            for c0 in range(0, 8, 4):
                P.op("gld", lambda e, wt=wt, col=col, c0=c0: e.dma_start(out=wt.t[:, c0:c0 + 4, :], in_=wv_ap[:, c0:c0 + 4, col:col + 1024]), w=[wt.b])
        g1 = ph.sb([128, 8, 256], BF16, "g1")
        fa = f1_d.ap()
        for h in range(8):
            P.op("ld", lambda e, h=h: e.dma_start(out=g1.t[:, h, :], in_=bass.AP(tensor=fa.tensor, offset=(g * 8 + h) * L1,
                                                                              ap=[[1, 128], [1, 256]])), w=[g1.b])
        xtok = [ph.sb([128, D], BF16, "xtok") for _ in range(4)]
        xT = ph.sb([128, 8, S], BF16, "xTp")
        ptr = [ph.ps([128, 1024], BF16, "ptrf")] * 2
        x2a = x2_d.ap()

        def rows(tt):
            rho = (tt * 128) // Lc
            l0 = (tt * 128) % Lc
            return bass.AP(tensor=x2a.tensor, offset=(l0 * r + rho) * D, ap=[[r * D, 128], [1, D]])
        xTs = [T(xT.t, "x") for _ in range(8)]
        for sbk in range(8):
            xs = xTs[sbk]
            for j in range(4):
                P.op("gld", lambda e, j=j, sbk=sbk: e.dma_start(out=xtok[j].t[:], in_=rows(sbk * 4 + j)), w=[xtok[j].b])
            for c in range(8):
                pt = ptr[c % 2]
                for jj in range(4):
                    P.op("pe", lambda e, pt=pt, jj=jj, c=c: e.transpose(out=pt.t[:, jj * 128:(jj + 1) * 128], in_=xtok[jj].t[:, c * 128:(c + 1) * 128],
                                                                      identity=ident_b.t[:]), r=[xtok[jj].b, ident_b.b], w=[pt.b])
                if c % 2 == 0:
                    P.op("act", lambda e, pt=pt, c=c, sbk=sbk: e.copy(out=xT.t[:, c, sbk * 512:(sbk + 1) * 512], in_=pt.t[:, 0:512]), r=[pt.b], w=[xs.b])
                else:
                    P.op("dve", lambda e, pt=pt, c=c, sbk=sbk: e.tensor_copy(out=xT.t[:, c, sbk * 512:(sbk + 1) * 512], in_=pt.t[:, 0:512]), r=[pt.b], w=[xs.b])
        qh = [ph.sb([128, S], BF16, "qh") for _ in range(2)]
        kh = [ph.sb([128, S], BF16, "kh") for _ in range(2)]
        vh = [ph.sb([128, NT, 129], BF16, "vh") for _ in range(2)]
        ust = [ph.sb([128, NT, 129], F32, "ust")] * 2
        pT = [ph.sb([128, 4, 128], BF16, "pT") for _ in range(3)]
        pq = [ph.ps([128, 512], F32, "pq") for _ in range(2)]
        pss = [ph.ps([128, 512], F32, "pss") for _ in range(3)]
        pacc = [ph.ps([128, 512], F32, "pacc") for _ in range(2)]
        QS = float(128 ** -0.5)
        pqi = 0
        si = 0
        for vv in vh:
            P.op("dve", lambda e, vv=vv: e.memset(vv.t[:, :, 128:129], 1.0), w=[vv.b])
        for h in range(8):
            q_ = qh[h % 2]; k_ = kh[h % 2]; v_ = vh[h % 2]; u_ = ust[h % 2]
            for sbk in range(8):
                for which, wt, dst in ((0, wq, q_), (1, wk_, k_)):
                    pp = pq[pqi % 2]; pqi += 1
                    for c in range(8):
                        P.op("pe", lambda e, pp=pp, wt=wt, c=c, h=h, sbk=sbk: e.matmul(
                            pp.t[:], wt.t[:, c, h * 128:(h + 1) * 128], xT.t[:, c, sbk * 512:(sbk + 1) * 512], start=(c == 0), stop=(c == 7)),
                            r=[wt.b, xTs[sbk].b], w=[pp.b])
                    if which == 0:
                        P.op("act", lambda e, pp=pp, dst=dst, sbk=sbk: e.activation(out=dst.t[:, sbk * 512:(sbk + 1) * 512], in_=pp.t[:], func=AF.Copy, scale=QS),
                             r=[pp.b], w=[dst.b])
                    else:
                        P.op("dve", lambda e, pp=pp, dst=dst, sbk=sbk: e.tensor_copy(out=dst.t[:, sbk * 512:(sbk + 1) * 512], in_=pp.t[:]), r=[pp.b], w=[dst.b])
                pp = pq[pqi % 2]; pqi += 1
                for j in range(4):
                    for c in range(8):
                        P.op("pe", lambda e, pp=pp, c=c, h=h, sbk=sbk, j=j: e.matmul(
                            pp.t[:, j * 128:(j + 1) * 128], xT.t[:, c, sbk * 512 + j * 128:sbk * 512 + (j + 1) * 128], wv.t[:, c, h * 128:(h + 1) * 128],
                            start=(c == 0), stop=(c == 7)), r=[wv.b, xTs[sbk].b], w=[pp.b])
                P.op("act", lambda e, pp=pp, v_=v_, sbk=sbk: e.copy(out=v_.t[:, sbk * 4:(sbk + 1) * 4, 0:128], in_=pp.t[:].rearrange("p (j d) -> p j d", j=4)),
                     r=[pp.b], w=[v_.b])
            sbase = si
            si += NT // 2

            def emit_S(t2, q_=q_, k_=k_, h=h, sbase=sbase):
                ps_ = pss[(sbase + t2 // 2) % 3]
                for bi in range(2):
                    tt = t2 + bi
                    n = tt % nb
                    P.op("pe", lambda e, tt=tt, bi=bi: e.matmul(
                        ps_.t[:, (2 * bi + 1) * 128:(2 * bi + 2) * 128], k_.t[:, tt * 128:(tt + 1) * 128], q_.t[:, tt * 128:(tt + 1) * 128],
                        start=True, stop=False), r=[k_.b, q_.b], w=[ps_.b])
                    P.op("pe", lambda e, bi=bi: e.matmul(
                        ps_.t[:, (2 * bi + 1) * 128:(2 * bi + 2) * 128], anti_b.t[:], g1.t[:, h, 0:128], start=False, stop=True),
                        r=[anti_b.b, g1.b], w=[ps_.b])
                    if n > 0:
                        P.op("pe", lambda e, tt=tt, bi=bi: e.matmul(
                            ps_.t[:, (2 * bi) * 128:(2 * bi + 1) * 128], k_.t[:, (tt - 1) * 128:tt * 128], q_.t[:, tt * 128:(tt + 1) * 128],
                            start=True, stop=False), r=[k_.b, q_.b], w=[ps_.b])
                        P.op("pe", lambda e, bi=bi: e.matmul(
                            ps_.t[:, (2 * bi) * 128:(2 * bi + 1) * 128], anti_b.t[:], g1.t[:, h, 128:256], start=False, stop=True),
                            r=[anti_b.b, g1.b], w=[ps_.b])

            emit_S(0)
            for t2 in range(0, NT, 2):
                if t2 + 2 < NT:
                    emit_S(t2 + 2)
                ps_ = pss[(sbase + t2 // 2) % 3]; pt_ = pT[(sbase + t2 // 2) % 3]; acc = pacc[(t2 // 2) % 2]
                first_has_prev = (t2 % nb) > 0
                c0 = 0 if first_has_prev else 128
                P.op("act", lambda e, ps_=ps_, pt_=pt_, c0=c0: e.activation(out=pt_.t[:].rearrange("p a b -> p (a b)")[:, c0:512], in_=ps_.t[:, c0:512], func=AF.Exp),
                     r=[ps_.b], w=[pt_.b])
                for bi in range(2):
                    tt = t2 + bi
                    n = tt % nb
                    P.op("pe", lambda e, acc=acc, pt_=pt_, v_=v_, tt=tt, bi=bi, n=n: e.matmul(
                        acc.t[:, bi * 129:(bi + 1) * 129], pt_.t[:, 2 * bi + 1, :], v_.t[:, tt, :], start=True, stop=(n == 0)),
                        r=[pt_.b, v_.b], w=[acc.b])
                    if n > 0:
                        P.op("pe", lambda e, acc=acc, pt_=pt_, v_=v_, tt=tt, bi=bi: e.matmul(
                            acc.t[:, bi * 129:(bi + 1) * 129], pt_.t[:, 2 * bi, :], v_.t[:, tt - 1, :], start=False, stop=True),
                            r=[pt_.b, v_.b], w=[acc.b])
                P.op("dve", lambda e, acc=acc, u_=u_, t2=t2: e.tensor_copy(out=u_.t[:, t2:t2 + 2, :], in_=acc.t[:, 0:258].rearrange("p (b d) -> p b d", b=2)),
                     r=[acc.b], w=[u_.b])
            uda = u_d.ap()
            for rho in range(r):
                P.op("st", lambda e, u_=u_, rho=rho, h=h: e.dma_start(
                    out=bass.AP(tensor=uda.tensor, offset=((g * S + rho) * 8 + h) * 132, ap=[[r * 8 * 132, 128], [128 * r * 8 * 132, nb], [1, 129]]),
                    in_=u_.t[:, rho * nb:(rho + 1) * nb, :]), r=[u_.b])
        ph.close()

    conv_jobs = []
    for e_ in range(8):
        for pc in range(7):
            conv_jobs.append(("wg", e_, pc))
            conv_jobs.append(("wu", e_, pc))
    for e_ in range(8):
        for ch in range(2):
            for q_ in range(4):
                conv_jobs.append(("wd", e_, ch * 4 + q_))
    conv_state = {"next": 0, "pending_store": None, "k": 0}

    def conv_emit(stage, n):
        for _ in range(n):
            pend = conv_state["pending_store"]
            if pend is not None:
                stg_, dst_ap, ncol = pend
                P.op("gst", lambda e, stg_=stg_, dst_ap=dst_ap, ncol=ncol: e.dma_start(out=dst_ap, in_=stg_.t[:, 0:ncol]), r=[stg_.b])
                conv_state["pending_store"] = None
            if conv_state["next"] >= len(conv_jobs):
                continue
            kind, e_, i_ = conv_jobs[conv_state["next"]]
            conv_state["next"] += 1
            stg_ = stage[conv_state["k"] % len(stage)]
            conv_state["k"] += 1
            if kind in ("wg", "wu"):
                src = (omwg if kind == "wg" else omwu).ap()[e_].rearrange("(c p) n -> p c n", p=128)[:, :, i_ * 512:(i_ + 1) * 512]
                dst = (wgs_d if kind == "wg" else wus_d).ap()[(e_ * 7 + i_) * 128:(e_ * 7 + i_ + 1) * 128, :]
                P.op("gld", lambda e, stg_=stg_, src=src: e.dma_start(out=stg_.t[:, 0:4096].rearrange("p (c n) -> p c n", c=8), in_=src), w=[stg_.b])
                conv_state["pending_store"] = (stg_, dst, 4096)
            else:
                ch, q_ = i_ // 4, i_ % 4
                src = omwd.ap()[e_].rearrange("(f p) n -> p f n", p=128)[:, q_ * 7:(q_ + 1) * 7, ch * 512:(ch + 1) * 512]
                dst = wds_d.ap()[(e_ * 8 + i_) * 128:(e_ * 8 + i_ + 1) * 128, :]
                P.op("gld", lambda e, stg_=stg_, src=src: e.dma_start(out=stg_.t[:, 0:3584].rearrange("p (f n) -> p f n", f=7), in_=src), w=[stg_.b])
                conv_state["pending_store"] = (stg_, dst, 3584)

    def conv_flush_pending():
        pend = conv_state["pending_store"]
        if pend is not None:
            stg_, dst_ap, ncol = pend
            P.op("gst", lambda e: e.dma_start(out=dst_ap, in_=stg_.t[:, 0:ncol]), r=[stg_.b])
            conv_state["pending_store"] = None

    def conv_finish(stage):
        while conv_state["next"] < len(conv_jobs) or conv_state["pending_store"] is not None:
            conv_emit(stage, 1)

    NTILE = 23
    NS = NTILE * 512
    I32 = mybir.dt.int32

    def phase_H1():
        ph = Phase(P)
        if conv_state["next"] < len(conv_jobs):
            cst_ = [ph.sb([128, 4096], BF16, "cst") for _ in range(3)]
            conv_finish(cst_)
        rt = ph.sb([128, 8, 8], F32, "router")
        P.op("ld", lambda e: e.dma_start(out=rt.t[:], in_=orouter.ap().rearrange("(c p) e -> p c e", p=128)), w=[rt.b])
        ustr = ph.sb([128, 128], BF16, "ustr")
        ones_b = ph.sb([128, 128], BF16, "onesb")
        thr8 = ph.sb([128, 8], F32, "thr8")
        iota23 = ph.sb([128, NTILE], F32, "iota23")
        cwg = ph.sb([128, 7], F32, "cwg")
        cwd = ph.sb([128, 8], F32, "cwd")
        P.op("gld", lambda e: e.dma_start(out=ustr.t[:], in_=c_ustr.ap()), w=[ustr.b])
        P.op("ld", lambda e: e.dma_start(out=thr8.t[:], in_=c_thr8.ap()), w=[thr8.b])
        P.op("ld", lambda e: e.dma_start(out=iota23.t[:], in_=c_iota23.ap()), w=[iota23.b])
        P.op("ld", lambda e: e.dma_start(out=cwg.t[:], in_=c_cwg.ap()), w=[cwg.b])
        P.op("ld", lambda e: e.dma_start(out=cwd.t[:], in_=c_cwd.ap()), w=[cwd.b])
        P.op("pool", lambda e: e.memset(ones_b.t[:], 1.0), w=[ones_b.b])
        xf = [ph.sb([128, D], F32, "xf") for _ in range(2)]
        xTf = [ph.sb([128, 8, 128], F32, "xTf") for _ in range(2)]
        MSK = ph.sb([128, NT, 8], F32, "MSK")
        OHA = ph.sb([128, NT, 8], F32, "OHA")
        lg = [ph.sb([128, 8], F32, "lg") for _ in range(2)]
        mx8 = [ph.sb([128, 8], F32, "mx8") for _ in range(2)]
        ee = [ph.sb([128, 8], F32, "ee") for _ in range(2)]
        nv1 = [ph.sb([128, 1], F32, "nv1") for _ in range(2)]
        den = [ph.sb([128, 1], F32, "den") for _ in range(2)]
        ptf = [ph.ps([128, 512], F32, "ptf") for _ in range(2)]
        plg = ph.ps([128, 512], F32, "plg")
        pcum = ph.ps([128, 512], F32, "pcum")
        ptot = ph.ps([128, 512], F32, "ptot")
        for tt in range(NT):
            x_ = xf[tt % 2]; xt_ = xTf[tt % 2]
            P.op("ld", lambda e, x_=x_, tt=tt: e.dma_start(out=x_.t[:], in_=x3_d.ap()[tt * 128:(tt + 1) * 128, :]), w=[x_.b])
            for half in range(2):
                pt = ptf[half]
                for cc in range(4):
                    c = half * 4 + cc
                    P.op("pe", lambda e, x_=x_, c=c, cc=cc, pt=pt: e.transpose(out=pt.t[:, cc * 128:(cc + 1) * 128], in_=x_.t[:, c * 128:(c + 1) * 128],
                                                                             identity=ident_f.t[:]), r=[x_.b, ident_f.b], w=[pt.b])
                P.op("act", lambda e, xt_=xt_, half=half, pt=pt: e.copy(out=xt_.t[:, half * 4:(half + 1) * 4, :].rearrange("p c t -> p (c t)"), in_=pt.t[:]),
                     r=[pt.b], w=[xt_.b])
            for c in range(8):
                P.op("pe", lambda e, xt_=xt_, c=c: e.matmul(plg.t[:, 0:8], xt_.t[:, c, :], rt.t[:, c, :], start=(c == 0), stop=(c == 7)),
                     r=[xt_.b, rt.b], w=[plg.b])
            l_ = lg[tt % 2]; m_ = mx8[tt % 2]; e_ = ee[tt % 2]; n_ = nv1[tt % 2]; d_ = den[tt % 2]
            P.op("dve", lambda e, l_=l_: e.tensor_copy(out=l_.t[:], in_=plg.t[:, 0:8]), r=[plg.b], w=[l_.b])
            P.op("dve", lambda e, l_=l_, m_=m_: e.max(out=m_.t[:], in_=l_.t[:]), r=[l_.b], w=[m_.b])
            P.op("dve", lambda e, m_=m_, n_=n_: e.tensor_scalar(out=n_.t[:], in0=m_.t[:, 0:1], scalar1=-1.0, scalar2=None, op0=ALU.mult),
                 r=[m_.b], w=[n_.b])
            P.op("act", lambda e, l_=l_, e_=e_, n_=n_: e.activation(out=e_.t[:], in_=l_.t[:], func=AF.Exp, bias=n_.t[:, 0:1], scale=1.0),
                 r=[l_.b, n_.b], w=[e_.b])
            P.op("dve", lambda e, l_=l_, m_=m_, tt=tt: e.tensor_scalar(out=MSK.t[:, tt, :], in0=l_.t[:], scalar1=m_.t[:, 1:2], scalar2=None, op0=ALU.is_ge),
                 r=[l_.b, m_.b], w=[MSK.b])
            P.op("dve", lambda e, l_=l_, m_=m_, tt=tt: e.tensor_scalar(out=OHA.t[:, tt, :], in0=l_.t[:], scalar1=m_.t[:, 0:1], scalar2=None, op0=ALU.is_ge),
                 r=[l_.b, m_.b], w=[OHA.b])
            P.op("dve", lambda e, e_=e_, tt=tt: e.tensor_tensor(out=e_.t[:], in0=e_.t[:], in1=MSK.t[:, tt, :], op=ALU.mult), r=[e_.b, MSK.b], w=[e_.b])
            P.op("dve", lambda e, e_=e_, d_=d_: e.tensor_reduce(out=d_.t[:], in_=e_.t[:], axis=AX.X, op=ALU.add), r=[e_.b], w=[d_.b])
            P.op("dve", lambda e, d_=d_, tt=tt: e.reciprocal(out=GA.t[:, tt:tt + 1], in_=d_.t[:]), r=[d_.b], w=[GA.b])
        P.op("dve", lambda e: e.tensor_scalar(out=GB.t[:], in0=GA.t[:], scalar1=-1.0, scalar2=1.0, op0=ALU.mult, op1=ALU.add), r=[GA.b], w=[GB.b])
        mskb = ph.sb([128, NT * 8], BF16, "mskb")
        tot = ph.sb([128, NT, 8], F32, "tot")
        offs = ph.sb([128, NT, 8], F32, "offs")
        slot = ph.sb([128, NT, 8], F32, "slot")
        tmp3 = ph.sb([128, NT, 8], F32, "tmp3")
        ohb = ph.sb([128, NT, 8], F32, "ohb")
        cnt = ph.sb([128, 8], F32, "cnt")
        ntl = ph.sb([128, 8], F32, "ntl")
        tend = ph.sb([128, 8], F32, "tend")
        st512 = ph.sb([128, 8], F32, "st512")
        slf = ph.sb([128, 2, NT], F32, "slf")
        eidf = ph.sb([128, NTILE], F32, "eidf")
        e896 = ph.sb([128, NTILE], F32, "e896")
        e1024 = ph.sb([128, NTILE], F32, "e1024")
        iwgf = ph.sb([128, NTILE, 7], F32, "iwgf")
        iwdf = ph.sb([128, NTILE, 8], F32, "iwdf")
        P.op("dve", lambda e: e.tensor_copy(out=mskb.t[:], in_=MSK.t[:].rearrange("p t e -> p (t e)")), r=[MSK.b], w=[mskb.b])
        P.op("pe", lambda e: e.matmul(pcum.t[:, 0:256], ustr.t[:], mskb.t[:], start=True, stop=True), r=[ustr.b, mskb.b], w=[pcum.b])
        P.op("pe", lambda e: e.matmul(ptot.t[:, 0:256], ones_b.t[:], mskb.t[:], start=True, stop=True), r=[ones_b.b, mskb.b], w=[ptot.b])
        P.op("dve", lambda e: e.tensor_copy(out=tot.t[:].rearrange("p t e -> p (t e)"), in_=ptot.t[:, 0:256]), r=[ptot.b], w=[tot.b])
        P.op("dve", lambda e: e.memset(offs.t[:, 0, :], 0.0), w=[offs.b])
        for tt in range(1, NT):
            P.op("dve", lambda e, tt=tt: e.tensor_tensor(out=offs.t[:, tt, :], in0=offs.t[:, tt - 1, :], in1=tot.t[:, tt - 1, :], op=ALU.add),
                 r=[offs.b, tot.b], w=[offs.b])
        P.op("dve", lambda e: e.tensor_tensor(out=cnt.t[:], in0=offs.t[:, NT - 1, :], in1=tot.t[:, NT - 1, :], op=ALU.add), r=[offs.b, tot.b], w=[cnt.b])
        P.op("dve", lambda e: e.tensor_scalar(out=ntl.t[:], in0=cnt.t[:], scalar1=0.0, scalar2=None, op0=ALU.is_gt), r=[cnt.b], w=[ntl.b])
        for k in range(1, 8):
            P.op("dve", lambda e, k=k: e.scalar_tensor_tensor(out=ntl.t[:], in0=cnt.t[:], scalar=float(512 * k), in1=ntl.t[:], op0=ALU.is_gt, op1=ALU.add),
                 r=[cnt.b, ntl.b], w=[ntl.b])
        P.op("dve", lambda e: e.tensor_copy(out=tend.t[:, 0:1], in_=ntl.t[:, 0:1]), r=[ntl.b], w=[tend.b])
        for k in range(1, 8):
            P.op("dve", lambda e, k=k: e.tensor_tensor(out=tend.t[:, k:k + 1], in0=tend.t[:, k - 1:k], in1=ntl.t[:, k:k + 1], op=ALU.add),
                 r=[tend.b, ntl.b], w=[tend.b])
        P.op("dve", lambda e: e.tensor_tensor(out=st512.t[:], in0=tend.t[:], in1=ntl.t[:], op=ALU.subtract), r=[tend.b, ntl.b], w=[st512.b])
        P.op("dve", lambda e: e.tensor_scalar(out=st512.t[:], in0=st512.t[:], scalar1=512.0, scalar2=None, op0=ALU.mult), r=[st512.b], w=[st512.b])
        P.op("dve", lambda e: e.tensor_tensor(out=slot.t[:].rearrange("p t e -> p (t e)"), in0=pcum.t[:, 0:256], in1=offs.t[:].rearrange("p t e -> p (t e)"), op=ALU.add),
             r=[pcum.b, offs.b], w=[slot.b])
        for k in range(8):
            P.op("dve", lambda e, k=k: e.tensor_scalar(out=slot.t[:, :, k], in0=slot.t[:, :, k], scalar1=st512.t[:, k:k + 1], scalar2=None, op0=ALU.add),
                 r=[slot.b, st512.b], w=[slot.b])
        P.op("dve", lambda e: e.tensor_tensor(out=ohb.t[:], in0=MSK.t[:], in1=OHA.t[:], op=ALU.subtract), r=[MSK.b, OHA.b], w=[ohb.b])
        P.op("dve", lambda e: e.tensor_tensor(out=tmp3.t[:], in0=slot.t[:], in1=OHA.t[:], op=ALU.mult), r=[slot.b, OHA.b], w=[tmp3.b])
        P.op("dve", lambda e: e.tensor_reduce(out=slf.t[:, 0, :], in_=tmp3.t[:], axis=AX.X, op=ALU.add), r=[tmp3.b], w=[slf.b])
        P.op("dve", lambda e: e.tensor_tensor(out=tmp3.t[:], in0=slot.t[:], in1=ohb.t[:], op=ALU.mult), r=[slot.b, ohb.b, slf.b], w=[tmp3.b])
        P.op("dve", lambda e: e.tensor_reduce(out=slf.t[:, 1, :], in_=tmp3.t[:], axis=AX.X, op=ALU.add), r=[tmp3.b], w=[slf.b])
        P.op("dve", lambda e: e.tensor_copy(out=SLI.t[:], in_=slf.t[:]), r=[slf.b], w=[SLI.b])
        P.op("dve", lambda e: e.memset(eidf.t[:], 0.0), w=[eidf.b])
        for k in range(7):
            P.op("dve", lambda e, k=k: e.scalar_tensor_tensor(out=eidf.t[:], in0=iota23.t[:], scalar=tend.t[:, k:k + 1], in1=eidf.t[:], op0=ALU.is_ge, op1=ALU.add),
                 r=[iota23.b, tend.b, eidf.b], w=[eidf.b])
        P.op("dve", lambda e: e.tensor_scalar(out=e896.t[:], in0=eidf.t[:], scalar1=896.0, scalar2=None, op0=ALU.mult), r=[eidf.b], w=[e896.b])
        P.op("dve", lambda e: e.tensor_scalar(out=e1024.t[:], in0=eidf.t[:], scalar1=1024.0, scalar2=None, op0=ALU.mult), r=[eidf.b], w=[e1024.b])
        for i in range(NTILE):
            P.op("dve", lambda e, i=i: e.tensor_scalar(out=iwgf.t[:, i, :], in0=cwg.t[:], scalar1=e896.t[:, i:i + 1], scalar2=None, op0=ALU.add),
                 r=[cwg.b, e896.b], w=[iwgf.b])
            P.op("dve", lambda e, i=i: e.tensor_scalar(out=iwdf.t[:, i, :], in0=cwd.t[:], scalar1=e1024.t[:, i:i + 1], scalar2=None, op0=ALU.add),
                 r=[cwd.b, e1024.b], w=[iwdf.b])
        P.op("dve", lambda e: e.tensor_copy(out=IWG.t[:], in_=iwgf.t[:]), r=[iwgf.b], w=[IWG.b])
        P.op("dve", lambda e: e.tensor_copy(out=IWD.t[:], in_=iwdf.t[:]), r=[iwdf.b], w=[IWD.b])
        xb = [ph.sb([128, D], BF16, "xb") for _ in range(3)]
        for tt in range(NT):
            b_ = xb[tt % 3]
            P.op("gld", lambda e, b_=b_, tt=tt: e.dma_start(out=b_.t[:], in_=x3_d.ap()[tt * 128:(tt + 1) * 128, :]), w=[b_.b])
            for ab in range(2):
                P.op("gst", lambda e, b_=b_, tt=tt, ab=ab: e.indirect_dma_start(
                    out=xs_d.ap(), out_offset=bass.IndirectOffsetOnAxis(ap=SLI.t[:, ab, tt:tt + 1], axis=0), in_=b_.t[:], in_offset=None),
                    r=[b_.b, SLI.b])
        ph.close()

    def phase_H2():
        ph = Phase(P)
        NF = 28
        xs = [[ph.sb([128, D], BF16, "xs") for _ in range(4)] for _ in range(2)]
        xsT = [ph.sb([128, 8, 512], BF16, "xsT") for _ in range(2)]
        hT = ph.sb([128, NF, 512], BF16, "hT")
        wgp = [ph.sb([128, 4096], BF16, "wgp") for _ in range(3)]
        wup = [ph.sb([128, 4096], BF16, "wup") for _ in range(3)]
        wdp = [ph.sb([128, 7, 512], BF16, "wdp") for _ in range(8)]
        sg = [ph.sb([128, 512], F32, "sg") for _ in range(2)]
        ys = [ph.sb([128, 4, D], F32, "ys") for _ in range(2)]
        ptr = [ph.ps([128, 1024], BF16, "ptrh") for _ in range(2)]
        pg = [ph.ps([128, 512], F32, "pg") for _ in range(2)]
        pu = [ph.ps([128, 512], F32, "pu") for _ in range(2)]
        py = [ph.ps([128, 512], F32, "pyh") for _ in range(2)]
        wi = 0
        gi = 0
        yi = 0
        def prefetch(i):
            load_tok_T(ph, lambda j, i=i: xs_d.ap()[i * 512 + j * 128:i * 512 + (j + 1) * 128, :], 4, xs[i % 2], xsT[i % 2], ptr, "xs", queue="ld")

        prefetch(0)
        pend2 = [None]
        for i in range(NTILE):
            xs_ = xs[i % 2]; xT_ = xsT[i % 2]; ys_ = ys[i % 2]
            for k in range(8):
                P.op("gld", lambda e, k=k, i=i: e.indirect_dma_start(
                    out=wdp[k].t[:].rearrange("p f n -> p (f n)"), out_offset=None, in_=wds_d.ap(),
                    in_offset=bass.IndirectOffsetOnAxis(ap=IWD.t[:, i, k:k + 1], axis=0)), r=[IWD.b], w=[wdp[k].b])
                if k == 3:
                    pass
            for pc in range(7):
                wg_ = wgp[wi % 3]; wu_ = wup[wi % 3]; wi += 1
                P.op("gld", lambda e, wg_=wg_, pc=pc, i=i: e.indirect_dma_start(
                    out=wg_.t[:], out_offset=None, in_=wgs_d.ap(), in_offset=bass.IndirectOffsetOnAxis(ap=IWG.t[:, i, pc:pc + 1], axis=0)),
                    r=[IWG.b], w=[wg_.b])
                P.op("gld", lambda e, wu_=wu_, pc=pc, i=i: e.indirect_dma_start(
                    out=wu_.t[:], out_offset=None, in_=wus_d.ap(), in_offset=bass.IndirectOffsetOnAxis(ap=IWG.t[:, i, pc:pc + 1], axis=0)),
                    r=[IWG.b], w=[wu_.b])
                for fs in range(4):
                    fc = pc * 4 + fs
                    pg_ = pg[gi % 2]; pu_ = pu[gi % 2]; sg_ = sg[gi % 2]; gi += 1
                    for c in range(8):
                        P.op("pe", lambda e, pg_=pg_, wg_=wg_, c=c, fs=fs, xT_=xT_: e.matmul(
                            pg_.t[:], wg_.t[:, c * 512 + fs * 128:c * 512 + (fs + 1) * 128], xT_.t[:, c, :], start=(c == 0), stop=(c == 7)),
                            r=[wg_.b, xT_.b], w=[pg_.b])
                    P.op("act", lambda e, pg_=pg_, sg_=sg_: e.activation(out=sg_.t[:], in_=pg_.t[:], func=AF.Silu), r=[pg_.b], w=[sg_.b])
                    for c in range(8):
                        P.op("pe", lambda e, pu_=pu_, wu_=wu_, c=c, fs=fs, xT_=xT_: e.matmul(
                            pu_.t[:], wu_.t[:, c * 512 + fs * 128:c * 512 + (fs + 1) * 128], xT_.t[:, c, :], start=(c == 0), stop=(c == 7)),
                            r=[wu_.b, xT_.b], w=[pu_.b])
                    P.op("dve", lambda e, pu_=pu_, sg_=sg_, fc=fc: e.tensor_tensor(out=hT.t[:, fc, :], in0=pu_.t[:], in1=sg_.t[:], op=ALU.mult),
                         r=[pu_.b, sg_.b], w=[hT.b])
            if i + 1 < NTILE:
                prefetch(i + 1)
            if pend2[0] is not None:
                pend2[0]()
                pend2[0] = None
            for ch in range(2):
                for j in range(4):
                    py_ = py[yi % 2]; yi += 1
                    for q_ in range(4):
                        wd_ = wdp[ch * 4 + q_]
                        for fl in range(7):
                            fc = q_ * 7 + fl
                            P.op("pe", lambda e, py_=py_, fc=fc, j=j, wd_=wd_, fl=fl: e.matmul(
                                py_.t[:], hT.t[:, fc, j * 128:(j + 1) * 128], wd_.t[:, fl, :], start=(fc == 0), stop=(fc == NF - 1)),
                                r=[hT.b, wd_.b], w=[py_.b])
                    if yi % 2 == 0:
                        P.op("act", lambda e, py_=py_, ys_=ys_, j=j, ch=ch: e.copy(out=ys_.t[:, j, ch * 512:(ch + 1) * 512], in_=py_.t[:]), r=[py_.b], w=[ys_.b])
                    else:
                        P.op("dve", lambda e, py_=py_, ys_=ys_, j=j, ch=ch: e.tensor_copy(out=ys_.t[:, j, ch * 512:(ch + 1) * 512], in_=py_.t[:]), r=[py_.b], w=[ys_.b])
            pend2[0] = (lambda ys_=ys_, i=i: P.op("st", lambda e: e.dma_start(out=ys_d.ap()[i * 512:(i + 1) * 512, :].rearrange("(j p) n -> p j n", p=128), in_=ys_.t[:]), r=[ys_.b]))
        pend2[0]()
        ph.close()

    def phase_H3():
        ph = Phase(P)
        gB_ = ph.sb([128, D], F32, "gB")
        bB_ = ph.sb([128, D], F32, "bB")
        P.op("ld", lambda e: e.dma_start(out=gB_.t[:], in_=bcast_row(oln2g)), w=[gB_.b])
        P.op("ld", lambda e: e.dma_start(out=bB_.t[:], in_=bcast_row(oln2b)), w=[bB_.b])
        xf = [ph.sb([128, D], F32, "xf") for _ in range(3)]
        ya = [ph.sb([128, D], F32, "ya") for _ in range(3)]
        yb = [ph.sb([128, D], F32, "yb") for _ in range(3)]
        z = [ph.sb([128, D], F32, "z") for _ in range(3)]
        xo = [ph.sb([128, D], F32, "xo") for _ in range(3)]
        st6 = [ph.sb([128, 2, 6], F32, "st6") for _ in range(3)]
        mv = [ph.sb([128, 2], F32, "mv") for _ in range(3)]
        rstd = [ph.sb([128, 2], F32, "rstd") for _ in range(3)]
        pend_h = []
        NB3 = len(xf)
        for tt in range(NT):
            i2 = tt % NB3
            x_ = xf[i2]; a_ = ya[i2]; b_ = yb[i2]; z_ = z[i2]
            P.op("ld", lambda e, x_=x_, tt=tt: e.dma_start(out=x_.t[:], in_=x3_d.ap()[tt * 128:(tt + 1) * 128, :]), w=[x_.b])
            if len(pend_h) > 0:
                pend_h.pop(0)()
            P.op("gld", lambda e, a_=a_, tt=tt: e.indirect_dma_start(out=a_.t[:], out_offset=None, in_=ys_d.ap(),
                                                                   in_offset=bass.IndirectOffsetOnAxis(ap=SLI.t[:, 0, tt:tt + 1], axis=0)), r=[SLI.b], w=[a_.b])
            P.op("gld", lambda e, b_=b_, tt=tt: e.indirect_dma_start(out=b_.t[:], out_offset=None, in_=ys_d.ap(),
                                                                   in_offset=bass.IndirectOffsetOnAxis(ap=SLI.t[:, 1, tt:tt + 1], axis=0)), r=[SLI.b], w=[b_.b])
            P.op("dve", lambda e, x_=x_, a_=a_, z_=z_, tt=tt: e.tensor_scalar(out=z_.t[:], in0=a_.t[:], scalar1=GA.t[:, tt:tt + 1], scalar2=None, op0=ALU.mult),
                 r=[a_.b, GA.b], w=[z_.b])
            P.op("dve", lambda e, b_=b_, z_=z_, tt=tt: e.scalar_tensor_tensor(out=z_.t[:], in0=b_.t[:], scalar=GB.t[:, tt:tt + 1], in1=z_.t[:], op0=ALU.mult, op1=ALU.add),
                 r=[b_.b, GB.b, z_.b], w=[z_.b])
            P.op("dve", lambda e, x_=x_, z_=z_: e.scalar_tensor_tensor(out=z_.t[:], in0=x_.t[:], scalar=ALPHA, in1=z_.t[:], op0=ALU.mult, op1=ALU.add),
                 r=[x_.b, z_.b], w=[z_.b])
            layernorm_rows(ph, z_, gB_, bB_, xo[i2], st6[i2], mv[i2], rstd[i2], None)
            pend_h.append(lambda i2=i2, tt=tt: P.op("st", lambda e: e.dma_start(out=out_d.ap()[tt * 128:(tt + 1) * 128, :], in_=xo[i2].t[:]), r=[xo[i2].b]))
        while pend_h:
            pend_h.pop(0)()
        ph.close()

    phases = [
        ("T", phase_tables),
        ("A", phase_A),
        ("B", phase_B),
        ("C", phase_C),
        ("D", lambda: phase_outproj("catT", ewout, eln1g, eln1b, x_d, x1_d)),
        ("E", phase_E),
        ("F0", lambda: phase_F(0)),
        ("F1", lambda: phase_F(1)),
        ("F2", lambda: phase_F(2)),
        ("G", lambda: phase_outproj("ug", owout, oln1g, oln1b, x2_d, x3_d)),
        ("H1", phase_H1),
        ("H2", phase_H2),
        ("H3", phase_H3),
    ]
    P.flush(barrier=True)
    for name, fn in phases:
        if name in skip:
            continue
        fn()
        if stop_after == name:
            break
    G.close()
    P.barrier(issuers=("sp",))
    return nc


INPUT_NAMES = ["x", "rel_bias", "even_w_in", "even_conv_w", "even_conv_b", "even_conv_ln_g", "even_conv_ln_b", "even_w_out",
               "even_ln1_g", "even_ln1_b", "even_ffn_wg", "even_ffn_wu", "even_ffn_wd", "even_ln2_g", "even_ln2_b",
               "odd_w_in", "odd_w_out", "odd_ln1_g", "odd_ln1_b", "odd_router", "odd_moe_wg", "odd_moe_wu", "odd_moe_wd",
               "odd_ln2_g", "odd_ln2_b"]


def make_in_maps(inputs, n_cores=8):
    cs = host_consts()
    shared = {}
    for k in INPUT_NAMES:
        if k == "x":
            continue
        a = np.ascontiguousarray(np.asarray(inputs[k], dtype=np.float32))
        if k == "rel_bias":
            shared[k] = a
        elif a.ndim >= 2 and a.shape[0] == 1:
            shared[k] = np.ascontiguousarray(a[0]) if a.ndim > 2 else a
        else:
            shared[k] = a
    for k, v in cs.items():
        shared["c_" + k] = v
    x = np.asarray(inputs["x"], dtype=np.float32)
    maps = []
    for i in range(n_cores):
        m = dict(shared)
        m["x"] = np.ascontiguousarray(x[i])
        maps.append(m)
    return maps


def kernel(**inputs):
    nc = build()
    maps = make_in_maps(inputs, 8)
    res = run_bass_kernel_spmd(nc, maps, core_ids=list(range(8)))
    out = np.stack([np.asarray(r["out"], dtype=np.float32) for r in res.results], axis=0)
    return out
```

```python
import math
import bisect
from contextlib import ExitStack

import numpy as np
import concourse.bass as bass
import concourse.mybir as mybir
from concourse.bass_utils import run_bass_kernel_spmd

F32 = mybir.dt.float32
BF16 = mybir.dt.bfloat16
AF = mybir.ActivationFunctionType
ALU = mybir.AluOpType
AX = mybir.AxisListType

S = 4096
D = 1024
NT = S // 128
ALPHA = 4 ** 0.25
EPS = 1e-5
NEGM = -30000.0
NIT = 12
EPOCH = 4000
KDMA = 12


class Buf:
    __slots__ = ("name", "w", "rs")

    def __init__(self, name=""):
        self.name = name
        self.w = []
        self.rs = []


class Stream:
    def __init__(self, name, issuer, is_dma):
        self.name = name
        self.issuer = issuer
        self.is_dma = is_dma
        self.ops = []
        self.n_total = 0
        self.inc_idx = []
        self.n_inc = 0
        self.sems = []


class Op:
    __slots__ = ("stream", "fn", "deps", "idx", "inc")


class _Rec:
    __slots__ = ("call",)

    def __init__(self):
        self.call = None

    def __getattr__(self, name):
        def f(*a, **k):
            self.call = (name, a, k)
        return f


class Prog:
    def __init__(self, nc):
        self.nc = nc
        self.es = ExitStack()
        self.eng = {"pe": nc.tensor, "act": nc.scalar, "dve": nc.vector, "pool": nc.gpsimd, "sp": nc.sync}
        self.streams = {}
        for n in ("pe", "act", "dve", "pool"):
            self.streams[n] = Stream(n, n, False)
        for n, iss in (("ld", "sp"), ("st", "sp"), ("gld", "pool"), ("gst", "pool")):
            self.streams[n] = Stream(n, iss, True)
        self.order = []
        self.waited = {}

    def op(self, sname, fn, r=(), w=(), lazy=False):
        st = self.streams[sname]
        o = Op()
        o.stream = st
        if lazy:
            o.fn = fn
        else:
            rec = _Rec()
            fn(rec)
            assert rec.call is not None
            o.fn = rec.call
        o.idx = st.n_total
        o.inc = False
        st.n_total += 1
        deps = set()
        me = (sname, o.idx)
        for b in r:
            for wr in b.w:
                deps.add(wr + ("raw",))
        for b in w:
            for wr in b.w:
                deps.add(wr + ("waw",))
            for rd in b.rs:
                deps.add(rd + ("war",))
        fd = set()
        best = {}
        for (s, i, kind) in deps:
            if s == sname:
                if sname == "pe":
                    continue
                if kind == "waw":
                    continue
                if not st.is_dma and kind != "raw":
                    continue
            if self.streams[s].is_dma:
                fd.add((s, i))
            else:
                if s not in best or best[s] < i:
                    best[s] = i
        for s, i in best.items():
            fd.add((s, i))
        o.deps = fd
        for b in r:
            b.rs.append(me)
        for b in w:
            if st.is_dma and b.w and not b.rs and all(x[0] == sname for x in b.w):
                b.w.append(me)
            else:
                b.w = [me]
            b.rs = []
        st.ops.append(o)
        self.order.append(o)
        return o

    def _sem_for(self, st, ordinal):
        ep = (ordinal - 1) // EPOCH
        while len(st.sems) <= ep:
            st.sems.append(self.es.enter_context(self.nc.semaphore("s_%s_%d" % (st.name, len(st.sems)))))
        return st.sems[ep], (ordinal - 1) % EPOCH + 1

    def _dma_sem(self, st, idx):
        k = idx % KDMA
        while len(st.sems) <= k:
            st.sems.append(self.es.enter_context(self.nc.semaphore("d_%s_%d" % (st.name, len(st.sems)))))
        return k, st.sems[k], 16 * (idx // KDMA + 1)

    def _wait_dma(self, issuer, st, idx):
        k, sem, val = self._dma_sem(st, idx)
        key = (issuer, st.name, k)
        if self.waited.get(key, 0) >= val:
            return
        self.waited[key] = val
        self.eng[issuer].wait_ge(sem, val)

    def _wait(self, issuer, st, ordinal):
        sem, val = self._sem_for(st, ordinal)
        key = (issuer, st.name)
        if self.waited.get(key, 0) >= ordinal:
            return
        self.waited[key] = ordinal
        self.eng[issuer].wait_ge(sem, val)

    def flush(self, barrier=True):
        need = {}
        for o in self.order:
            for s, i in o.deps:
                need.setdefault(s, set()).add(i)
        for sname, st in self.streams.items():
            if not st.ops or st.is_dma:
                continue
            tg = need.get(sname, set())
            for o in st.ops:
                if o.idx in tg:
                    o.inc = True
            st.ops[-1].inc = True
        ordinal_of = {}
        for sname, st in self.streams.items():
            if st.is_dma:
                continue
            for o in st.ops:
                if o.inc:
                    st.n_inc += 1
                    st.inc_idx.append(o.idx)
                    ordinal_of[(sname, o.idx)] = st.n_inc
        for o in self.order:
            st = o.stream
            issuer = st.issuer
            for s, i in sorted(o.deps):
                ps = self.streams[s]
                if ps.is_dma:
                    self._wait_dma(issuer, ps, i)
                else:
                    k = bisect.bisect_left(ps.inc_idx, i)
                    assert k < len(ps.inc_idx), (s, i)
                    self._wait(issuer, ps, k + 1)
            if st.is_dma and o.idx >= KDMA:
                self._wait_dma(issuer, st, o.idx - KDMA)
            if callable(o.fn):
                inst = o.fn(self.eng[issuer])
            else:
                nm, a_, k_ = o.fn
                inst = getattr(self.eng[issuer], nm)(*a_, **k_)
            if st.is_dma:
                k, sem, val = self._dma_sem(st, o.idx)
                inst.then_inc(sem, 16)
            elif o.inc:
                sem, val = self._sem_for(st, ordinal_of[(st.name, o.idx)])
                inst.then_inc(sem, 1)
        self.order = []
        for st in self.streams.values():
            st.ops = []
        if barrier:
            self.barrier()

    def barrier(self, issuers=("pe", "act", "dve", "pool", "sp")):
        for iss in issuers:
            for st in self.streams.values():
                if st.is_dma:
                    for i in range(max(0, st.n_total - KDMA), st.n_total):
                        self._wait_dma(iss, st, i)
                elif st.n_inc > 0:
                    self._wait(iss, st, st.n_inc)


class T:
    __slots__ = ("t", "b")

    def __init__(self, t, name=""):
        self.t = t
        self.b = Buf(name)


class Phase:
    cnt = 0

    def __init__(self, P):
        self.P = P
        self.nc = P.nc
        self.es = ExitStack()
        self.n = 0

    def sb(self, shape, dt, name="t"):
        Phase.cnt += 1
        nm = "%s_%d" % (name, Phase.cnt)
        return T(self.es.enter_context(self.nc.sbuf_tensor(nm, list(shape), dt)), nm)

    def ps(self, shape, dt, name="p"):
        Phase.cnt += 1
        nm = "%s_%d" % (name, Phase.cnt)
        return T(self.es.enter_context(self.nc.psum_tensor(nm, list(shape), dt)), nm)

    def close(self):
        self.P.flush(barrier=True)
        self.es.close()


def _bucket(n):
    n = np.asarray(n).astype(np.int64)
    nf = np.maximum(n, 1).astype(np.float32)
    large = 16 + (np.log(nf / np.float32(16)) / np.float32(math.log(2048 / 16)) * 16).astype(np.int32)
    large = np.minimum(large, 31)
    return np.where(n < 16, n, large)


L0 = 2688
L1 = 384


def host_consts():
    c = {}
    c["ident"] = np.eye(128, dtype=np.float32)
    c["anti"] = np.eye(128, dtype=np.float32)[::-1].copy()
    qi = np.arange(128)[:, None]
    ki = np.arange(128)[None, :]
    c["causneg"] = np.where(ki <= qi, 0.0, -1e30).astype(np.float32)
    n = np.arange(L0)
    b0 = _bucket(np.maximum(n - 511, 0))
    oh0 = np.zeros((32, L0), np.float32)
    oh0[b0, n] = 1.0
    c["oh0"] = oh0
    oh1 = np.zeros((3, 33, L1), np.float32)
    n1 = np.arange(L1)
    dist = n1 - 127
    valid = (dist >= 0) & (dist <= 128)
    for g, r in enumerate((1, 4, 16)):
        bb = _bucket(np.maximum(dist, 0) * r)
        oh1[g, bb[valid], n1[valid]] = 1.0
        oh1[g, 32, :] = np.where(valid, 0.0, NEGM)
    c["oh1"] = oh1
    c["pow2"] = np.tile((0.5 ** np.arange(NIT + 2)).astype(np.float32)[None, :], (128, 1))
    pp = np.arange(128)
    c["ustr"] = (pp[:, None] < pp[None, :]).astype(np.float32)
    c["thr8"] = np.tile((512.0 * np.arange(8)).astype(np.float32)[None, :], (128, 1))
    c["iota23"] = np.tile(np.arange(23, dtype=np.float32)[None, :], (128, 1))
    c["cwg"] = (np.arange(7)[None, :] * 128 + pp[:, None]).astype(np.float32)
    c["cwd"] = (np.arange(8)[None, :] * 128 + pp[:, None]).astype(np.float32)
    return c


def build(stop_after=None, debug=(), hcfg=(4, 8), skip=(), feed=()):
    nc = bass.Bass("TRN2", target_bir_lowering=False)
    P = Prog(nc)

    def din(name, shape, dt=F32):
        return nc.dram_tensor(name, list(shape), dt, kind="ExternalInput")

    def dscr(name, shape, dt):
        kind = "ExternalOutput" if name in debug else ("ExternalInput" if name in feed else "Internal")
        return nc.dram_tensor(name, list(shape), dt, kind=kind)

    x_d = din("x", [S, D])
    rb_d = din("rel_bias", [32, 8])
    ewin = din("even_w_in", [D, 3144])
    ecw = din("even_conv_w", [31, 512])
    ecb = din("even_conv_b", [1, 512])
    ecg = din("even_conv_ln_g", [1, 512])
    ecbb = din("even_conv_ln_b", [1, 512])
    ewout = din("even_w_out", [D, D])
    eln1g = din("even_ln1_g", [1, D])
    eln1b = din("even_ln1_b", [1, D])
    ewg = din("even_ffn_wg", [D, 2816])
    ewu = din("even_ffn_wu", [D, 2816])
    ewd = din("even_ffn_wd", [2816, D])
    eln2g = din("even_ln2_g", [1, D])
    eln2b = din("even_ln2_b", [1, D])
    owin = din("odd_w_in", [D, 9216])
    owout = din("odd_w_out", [D, D])
    oln1g = din("odd_ln1_g", [1, D])
    oln1b = din("odd_ln1_b", [1, D])
    orouter = din("odd_router", [D, 8])
    omwg = din("odd_moe_wg", [8, D, 3584])
    omwu = din("odd_moe_wu", [8, D, 3584])
    omwd = din("odd_moe_wd", [8, 3584, D])
    oln2g = din("odd_ln2_g", [1, D])
    oln2b = din("odd_ln2_b", [1, D])
    c_ident = din("c_ident", [128, 128])
    c_anti = din("c_anti", [128, 128])
    c_caus = din("c_causneg", [128, 128])
    c_oh0 = din("c_oh0", [32, L0])
    c_oh1 = din("c_oh1", [3, 33, L1])
    c_pow2 = din("c_pow2", [128, NIT + 2])
    c_ustr = din("c_ustr", [128, 128])
    c_thr8 = din("c_thr8", [128, 8])
    c_iota23 = din("c_iota23", [128, 23])
    c_cwg = din("c_cwg", [128, 7])
    c_cwd = din("c_cwd", [128, 8])

    out_d = nc.dram_tensor("out", [S, D], F32, kind="ExternalOutput")

    qT_d = dscr("qT", [512, S], BF16)
    kT_d = dscr("kT", [512, S], BF16)
    qiT_d = dscr("qiT", [512, S], BF16)
    kiT_d = dscr("kiT", [128, S], BF16)
    v_d = dscr("vaug", [S, 520], BF16)
    w_d = dscr("widx", [S, 8], F32)
    catT_d = dscr("catT", [D, S], BF16)
    mask_d = dscr("maskneg", [S, S], BF16)
    f0_d = dscr("f0tab", [8, L0], BF16)
    f1_d = dscr("f1tab", [3, 8, L1], BF16)
    x1_d = dscr("x1", [S, D], F32)
    x2_d = dscr("x2", [S, D], F32)
    u_d = dscr("ug", [3, S, 8, 132], F32)
    x3_d = dscr("x3", [S, D], F32)
    wgs_d = dscr("wgs", [56 * 128, 4096], BF16)
    wus_d = dscr("wus", [56 * 128, 4096], BF16)
    wds_d = dscr("wds", [64 * 128, 3584], BF16)
    xs_d = dscr("xs", [23 * 512, D], BF16)
    ys_d = dscr("ys", [23 * 512, D], F32)

    G = Phase(P)
    ident_f = G.sb([128, 128], F32, "identf")
    ident_b = G.sb([128, 128], BF16, "identb")
    anti_b = G.sb([128, 128], BF16, "antib")
    ones_f = G.sb([128, 128], F32, "onesf")
    P.op("ld", lambda e: e.dma_start(out=ident_f.t[:], in_=c_ident.ap()), w=[ident_f.b])
    P.op("gld", lambda e: e.dma_start(out=ident_b.t[:], in_=c_ident.ap()), w=[ident_b.b])
    P.op("gld", lambda e: e.dma_start(out=anti_b.t[:], in_=c_anti.ap()), w=[anti_b.b])
    P.op("dve", lambda e: e.memset(ones_f.t[:], 1.0), w=[ones_f.b])
    GA = G.sb([128, NT], F32, "GA")
    GB = G.sb([128, NT], F32, "GB")
    SLI = G.sb([128, 2, NT], mybir.dt.int32, "SLI")
    IWG = G.sb([128, 23, 7], mybir.dt.int32, "IWG")
    IWD = G.sb([128, 23, 8], mybir.dt.int32, "IWD")
    eps_t = G.sb([128, 1], F32, "epst")
    P.op("dve", lambda e: e.memset(eps_t.t[:], EPS), w=[eps_t.b])
    zer_b = G.sb([128, 512], BF16, "zerb")
    P.op("dve", lambda e: e.memset(zer_b.t[:], 0.0), w=[zer_b.b])

    def evac(i, fn_act, fn_dve):
        return ("act", fn_act) if i % 2 == 0 else ("dve", fn_dve)

    def load_tok_T(ph, src_rows_ap_fn, ntiles, xtok, xT, pts, tag, f32_copy=None, queue="gld"):
        for j in range(ntiles):
            P.op(queue, lambda e, j=j: e.dma_start(out=xtok[j].t[:], in_=src_rows_ap_fn(j)), w=[xtok[j].b])
        k = 0
        for j0 in range(0, ntiles, 4):
            nj = min(4, ntiles - j0)
            for c in range(8):
                pt = pts[k % len(pts)]
                k += 1
                for jj in range(nj):
                    P.op("pe", lambda e, pt=pt, jj=jj, c=c, j0=j0: e.transpose(
                        out=pt.t[:, jj * 128:(jj + 1) * 128], in_=xtok[j0 + jj].t[:, c * 128:(c + 1) * 128],
                        identity=ident_b.t[:]), r=[xtok[j0 + jj].b, ident_b.b], w=[pt.b])
                if k % 2 == 0:
                    P.op("act", lambda e, pt=pt, c=c, j0=j0, nj=nj: e.copy(
                        out=xT.t[:, c, j0 * 128:(j0 + nj) * 128], in_=pt.t[:, 0:nj * 128]), r=[pt.b], w=[xT.b])
                else:
                    P.op("dve", lambda e, pt=pt, c=c, j0=j0, nj=nj: e.tensor_copy(
                        out=xT.t[:, c, j0 * 128:(j0 + nj) * 128], in_=pt.t[:, 0:nj * 128]), r=[pt.b], w=[xT.b])

    def layernorm_rows(ph, z, gB, bB, outt, st6, mv, rstd, nb, gb_eng="dve"):
        for hh in range(2):
            P.op("dve", lambda e, hh=hh: e.bn_stats(out=st6.t[:, hh, :], in_=z.t[:, hh * 512:(hh + 1) * 512]),
                 r=[z.b], w=[st6.b])
        P.op("dve", lambda e: e.bn_aggr(out=mv.t[:], in_=st6.t[:]), r=[st6.b], w=[mv.b])
        P.op("act", lambda e: e.activation(out=rstd.t[:, 0:1], in_=mv.t[:, 1:2], func=AF.Sqrt, bias=eps_t.t[:, 0:1], scale=1.0),
             r=[mv.b, eps_t.b], w=[rstd.b])
        P.op("dve", lambda e: e.reciprocal(out=rstd.t[:, 0:1], in_=rstd.t[:, 0:1]), r=[rstd.b], w=[rstd.b])
        P.op("dve", lambda e: e.scalar_tensor_tensor(out=rstd.t[:, 1:2], in0=mv.t[:, 0:1], scalar=-1.0, in1=rstd.t[:, 0:1],
                                                     op0=ALU.mult, op1=ALU.mult), r=[mv.b, rstd.b], w=[rstd.b])
        P.op("act", lambda e: e.activation(out=z.t[:], in_=z.t[:], func=AF.Identity, bias=rstd.t[:, 1:2], scale=rstd.t[:, 0:1]),
             r=[z.b, rstd.b], w=[z.b])
        P.op(gb_eng, lambda e: e.tensor_tensor(out=z.t[:], in0=z.t[:], in1=gB.t[:], op=ALU.mult),
             r=[z.b, gB.b], w=[z.b])
        P.op(gb_eng, lambda e: e.tensor_tensor(out=outt.t[:], in0=z.t[:], in1=bB.t[:], op=ALU.add),
             r=[z.b, bB.b], w=[outt.b])

    def bcast_row(dr):
        a = dr.ap()
        n = a.shape[-1]
        return bass.AP(tensor=a.tensor, offset=0, ap=[[0, 128], [1, n]])

    def phase_tables():
        ph = Phase(P)
        rb33 = ph.sb([33, 8], BF16, "rb33")
        rb33f = ph.sb([33, 8], F32, "rb33f")
        oh0 = ph.sb([32, L0], BF16, "oh0")
        oh1 = ph.sb([33, 3, L1], BF16, "oh1")
        f0 = ph.sb([8, L0], BF16, "f0")
        f1 = ph.sb([8, 3, L1], BF16, "f1")
        pp = [ph.ps([128, 512], F32, "pp") for _ in range(2)]
        P.op("dve", lambda e: e.memset(rb33f.t[:], 1.0), w=[rb33f.b])
        P.op("ld", lambda e: e.dma_start(out=rb33f.t[0:32, :], in_=rb_d.ap()), r=[], w=[rb33f.b])
        P.op("dve", lambda e: e.tensor_copy(out=rb33.t[:], in_=rb33f.t[:]), r=[rb33f.b], w=[rb33.b])
        P.op("gld", lambda e: e.dma_start(out=oh0.t[:], in_=c_oh0.ap()), w=[oh0.b])
        P.op("gld", lambda e: e.dma_start(out=oh1.t[:], in_=c_oh1.ap().rearrange("g k n -> k g n")), w=[oh1.b])
        k = 0
        for c0 in range(0, L0, 512):
            n = min(512, L0 - c0)
            p = pp[k % 2]
            k += 1
            P.op("pe", lambda e, p=p, c0=c0, n=n: e.matmul(p.t[0:8, 0:n], rb33.t[0:32, :], oh0.t[:, c0:c0 + n],
                                                          start=True, stop=True), r=[rb33.b, oh0.b], w=[p.b])
            P.op("dve", lambda e, p=p, c0=c0, n=n: e.tensor_copy(out=f0.t[:, c0:c0 + n], in_=p.t[0:8, 0:n]),
                 r=[p.b], w=[f0.b])
        for g in range(3):
            p = pp[k % 2]
            k += 1
            P.op("pe", lambda e, p=p, g=g: e.matmul(p.t[0:8, 0:L1], rb33.t[:, :], oh1.t[:, g, :],
                                                    start=True, stop=True), r=[rb33.b, oh1.b], w=[p.b])
            P.op("dve", lambda e, p=p, g=g: e.tensor_copy(out=f1.t[:, g, :], in_=p.t[0:8, 0:L1]),
                 r=[p.b], w=[f1.b])
        P.op("st", lambda e: e.dma_start(out=f0_d.ap(), in_=f0.t[:]), r=[f0.b])
        P.op("st", lambda e: e.dma_start(out=f1_d.ap().rearrange("g h n -> h g n"), in_=f1.t[:]), r=[f1.b])
        ph.close()

    def phase_A():
        ph = Phase(P)
        win = ph.sb([128, 8, 3144], BF16, "win")
        wki2 = ph.sb([128, 8, 128], BF16, "wki2")
        wv_ap = ewin.ap().rearrange("(c p) n -> p c n", p=128)
        for c in range(8):
            P.op("gld", lambda e, c=c: e.dma_start(out=win.t[:, c, :], in_=wv_ap[:, c, :]), w=[win.b])
        P.op("gld", lambda e: e.dma_start(out=wki2.t[:, :, 0:64], in_=wv_ap[:, :, 3072:3136]), w=[wki2.b])
        P.op("gld", lambda e: e.dma_start(out=wki2.t[:, :, 64:128], in_=wv_ap[:, :, 3072:3136]), w=[wki2.b])
        cw_sb = ph.sb([34, 512], F32, "cwsb")
        cwT = ph.sb([128, 4, 34], F32, "cwT")
        P.op("ld", lambda e: e.dma_start(out=cw_sb.t[0:31, :], in_=ecw.ap()), w=[cw_sb.b])
        P.op("ld", lambda e: e.dma_start(out=cw_sb.t[31:32, :], in_=ecb.ap()), w=[cw_sb.b])
        P.op("ld", lambda e: e.dma_start(out=cw_sb.t[32:33, :], in_=ecg.ap()), w=[cw_sb.b])
        P.op("ld", lambda e: e.dma_start(out=cw_sb.t[33:34, :], in_=ecbb.ap()), w=[cw_sb.b])
        pm = [ph.ps([128, 512], F32, "pm") for _ in range(4)]
        pst = [ph.ps([128, 512], F32, "pst") for _ in range(2)]
        ptr = [ph.ps([128, 1024], BF16, "ptr") for _ in range(2)]
        for c in range(4):
            P.op("pe", lambda e, c=c: e.transpose(out=pm[0].t[:, c * 34:(c + 1) * 34], in_=cw_sb.t[0:34, c * 128:(c + 1) * 128],
                                                  identity=ident_f.t[0:34, 0:34]), r=[cw_sb.b, ident_f.b], w=[pm[0].b])
        P.op("dve", lambda e: e.tensor_copy(out=cwT.t[:].rearrange("p c j -> p (c j)"), in_=pm[0].t[:, 0:136]),
             r=[pm[0].b], w=[cwT.b])
        diag = ph.sb([128, 4, 31, 128], BF16, "diag")
        for c in range(4):
            for j in range(31):
                eng = "dve" if (c * 31 + j) % 2 == 0 else "pool"
                P.op(eng, lambda e, c=c, j=j: e.tensor_scalar(out=diag.t[:, c, j, :], in0=ident_f.t[:],
                                                              scalar1=cwT.t[:, c, j:j + 1], scalar2=None, op0=ALU.mult),
                     r=[ident_f.b, cwT.b], w=[diag.b])
        xtok = [ph.sb([128, 1024], BF16, "xtok") for _ in range(4)]
        xT = [ph.sb([128, 8, 512], BF16, "xT") for _ in range(2)]
        ub = [ph.sb([128, 4, 542], BF16, "ub") for _ in range(2)]
        stg = ph.sb([128, 13, 512], BF16, "stg")
        aout = ph.sb([128, 4, 512], BF16, "aout")
        sg = [ph.sb([128, 512], F32, "sg") for _ in range(2)]
        yv = ph.sb([128, 4, 512], F32, "yv")
        ysq = ph.sb([128, 4, 512], F32, "ysq")
        mean = ph.sb([128, 512], F32, "mean")
        rstd = ph.sb([128, 512], F32, "rstd")
        tmp = ph.sb([128, 512], F32, "tmp")
        vst = ph.sb([128, 4, 8, 65], BF16, "vst")
        wst = ph.sb([128, 4, 8], F32, "wst")
        P.op("dve", lambda e: e.memset(ub[0].t[:, :, 0:30], 0.0), w=[ub[0].b])
        P.op("dve", lambda e: e.memset(vst.t[:], 1.0), w=[vst.b])
        xrows = x_d.ap()
        qTv = qT_d.ap().rearrange("(c p) t -> p c t", p=128)
        kTv = kT_d.ap().rearrange("(c p) t -> p c t", p=128)
        qiTv = qiT_d.ap().rearrange("(c p) t -> p c t", p=128)
        catTv = catT_d.ap().rearrange("(c p) t -> p c t", p=128)
        WSCALE = float(8 ** -0.5 * 64 ** -0.5)
        pmi = 0
        for sbk in range(8):
            t0 = sbk * 512
            xt = xT[sbk % 2]
            u_cur = ub[sbk % 2]
            u_prev = ub[(sbk + 1) % 2]
            load_tok_T(ph, lambda j, t0=t0: xrows[t0 + j * 128:t0 + (j + 1) * 128, :], 4, xtok, xt, ptr, "x")
            if sbk > 0:
                P.op("dve", lambda e, u_cur=u_cur, u_prev=u_prev: e.tensor_copy(out=u_cur.t[:, :, 0:30], in_=u_prev.t[:, :, 512:542]),
                     r=[u_prev.b], w=[u_cur.b])

            def proj_fm(col0, wt, pmt):
                for c in range(8):
                    P.op("pe", lambda e, c=c: e.matmul(pmt.t[:], wt.t[:, c, col0:col0 + 128], xt.t[:, c, :],
                                                       start=(c == 0), stop=(c == 7)), r=[wt.b, xt.b], w=[pmt.b])
            for c in range(4):
                pg = pm[pmi % 4]; pmi += 1
                pv = pm[pmi % 4]; pmi += 1
                sgt = sg[c % 2]
                proj_fm(512 + c * 128, win, pg)
                P.op("act", lambda e, pg=pg, sgt=sgt: e.activation(out=sgt.t[:], in_=pg.t[:], func=AF.Sigmoid),
                     r=[pg.b], w=[sgt.b])
                proj_fm(c * 128, win, pv)
                P.op("dve", lambda e, pv=pv, sgt=sgt, c=c, u_cur=u_cur: e.tensor_tensor(
                    out=u_cur.t[:, c, 30:542], in0=pv.t[:], in1=sgt.t[:], op=ALU.mult), r=[pv.b, sgt.b], w=[u_cur.b])
            for i in range(13):
                if i < 4:
                    col0, wt, scale = 1024 + i * 128, win, 0.125
                elif i < 8:
                    col0, wt, scale = 1536 + (i - 4) * 128, win, 1.0
                elif i < 12:
                    col0, wt, scale = 2560 + (i - 8) * 128, win, 1.0
                else:
                    col0, wt, scale = 0, wki2, 1.0
                pq = pm[pmi % 4]; pmi += 1
                proj_fm(col0, wt, pq)
                if i % 2 == 0:
                    P.op("act", lambda e, pq=pq, i=i, scale=scale: e.activation(out=stg.t[:, i, :], in_=pq.t[:], func=AF.Copy, scale=scale),
                         r=[pq.b], w=[stg.b])
                else:
                    P.op("dve", lambda e, pq=pq, i=i, scale=scale: e.tensor_scalar(out=stg.t[:, i, :], in0=pq.t[:], scalar1=scale,
                                                                               scalar2=None, op0=ALU.mult), r=[pq.b], w=[stg.b])
            P.op("st", lambda e, t0=t0: e.dma_start(out=qTv[:, :, t0:t0 + 512], in_=stg.t[:, 0:4, :]), r=[stg.b])
            P.op("st", lambda e, t0=t0: e.dma_start(out=kTv[:, :, t0:t0 + 512], in_=stg.t[:, 4:8, :]), r=[stg.b])
            P.op("st", lambda e, t0=t0: e.dma_start(out=qiTv[:, :, t0:t0 + 512], in_=stg.t[:, 8:12, :]), r=[stg.b])
            P.op("st", lambda e, t0=t0: e.dma_start(out=kiT_d.ap()[:, t0:t0 + 512], in_=stg.t[:, 12, :]), r=[stg.b])
            for j in range(4):
                pv = pm[pmi % 4]; pmi += 1
                for c in range(8):
                    P.op("pe", lambda e, c=c, j=j, pv=pv: e.matmul(pv.t[:], xt.t[:, c, j * 128:(j + 1) * 128], win.t[:, c, 2048:2560],
                                                                 start=(c == 0), stop=(c == 7)), r=[win.b, xt.b], w=[pv.b])
                P.op("act", lambda e, j=j, pv=pv: e.copy(out=vst.t[:, j, :, 0:64], in_=pv.t[:].rearrange("p (h d) -> p h d", h=8)),
                     r=[pv.b], w=[vst.b])
                pw = pm[pmi % 4]; pmi += 1
                for c in range(8):
                    P.op("pe", lambda e, c=c, j=j, pw=pw: e.matmul(pw.t[:, 0:8], xt.t[:, c, j * 128:(j + 1) * 128], win.t[:, c, 3136:3144],
                                                                 start=(c == 0), stop=(c == 7)), r=[win.b, xt.b], w=[pw.b])
                P.op("dve", lambda e, j=j, pw=pw: e.tensor_scalar(out=wst.t[:, j, :], in0=pw.t[:, 0:8], scalar1=WSCALE, scalar2=None,
                                                               op0=ALU.mult), r=[pw.b], w=[wst.b])
            P.op("st", lambda e, t0=t0: e.dma_start(out=v_d.ap()[t0:t0 + 512, :].rearrange("(j p) n -> p j n", p=128),
                                                    in_=vst.t[:].rearrange("p j h d -> p j (h d)")), r=[vst.b])
            P.op("st", lambda e, t0=t0: e.dma_start(out=w_d.ap()[t0:t0 + 512, :].rearrange("(j p) n -> p j n", p=128),
                                                    in_=wst.t[:]), r=[wst.b])
            for c in range(4):
                pc = pm[pmi % 4]; pmi += 1
                for j in range(31):
                    P.op("pe", lambda e, c=c, j=j, pc=pc, u_cur=u_cur: e.matmul(pc.t[:], diag.t[:, c, j, :], u_cur.t[:, c, j:j + 512],
                                                                             start=(j == 0), stop=(j == 30)), r=[diag.b, u_cur.b], w=[pc.b])
                P.op("act", lambda e, c=c, pc=pc: e.activation(out=yv.t[:, c, :], in_=pc.t[:], func=AF.Identity,
                                                              bias=cwT.t[:, c, 31:32], scale=1.0), r=[pc.b, cwT.b], w=[yv.b])
                P.op("act", lambda e, c=c, pc=pc: e.activation(out=ysq.t[:, c, :], in_=pc.t[:], func=AF.Square,
                                                              bias=cwT.t[:, c, 31:32], scale=1.0), r=[pc.b, cwT.b], w=[ysq.b])
            for c in range(4):
                P.op("pe", lambda e, c=c: e.matmul(pst[0].t[:], ones_f.t[:], yv.t[:, c, :], start=(c == 0), stop=(c == 3)),
                     r=[ones_f.b, yv.b], w=[pst[0].b])
            for c in range(4):
                P.op("pe", lambda e, c=c: e.matmul(pst[1].t[:], ones_f.t[:], ysq.t[:, c, :], start=(c == 0), stop=(c == 3)),
                     r=[ones_f.b, ysq.b], w=[pst[1].b])
            P.op("dve", lambda e: e.tensor_scalar(out=mean.t[:], in0=pst[0].t[:], scalar1=1.0 / 512, scalar2=None, op0=ALU.mult),
                 r=[pst[0].b], w=[mean.b])
            P.op("dve", lambda e: e.tensor_tensor(out=tmp.t[:], in0=mean.t[:], in1=mean.t[:], op=ALU.mult), r=[mean.b], w=[tmp.b])
            P.op("dve", lambda e: e.scalar_tensor_tensor(out=rstd.t[:], in0=pst[1].t[:], scalar=1.0 / 512, in1=tmp.t[:],
                                                         op0=ALU.mult, op1=ALU.subtract), r=[pst[1].b, tmp.b], w=[rstd.b])
            P.op("dve", lambda e: e.tensor_scalar(out=rstd.t[:], in0=rstd.t[:], scalar1=EPS, scalar2=None, op0=ALU.add),
                 r=[rstd.b], w=[rstd.b])
            P.op("act", lambda e: e.activation(out=rstd.t[:], in_=rstd.t[:], func=AF.Sqrt), r=[rstd.b], w=[rstd.b])
            P.op("dve", lambda e: e.reciprocal(out=rstd.t[:], in_=rstd.t[:]), r=[rstd.b], w=[rstd.b])
            for c in range(4):
                P.op("dve", lambda e, c=c: e.tensor_tensor(out=yv.t[:, c, :], in0=yv.t[:, c, :], in1=mean.t[:], op=ALU.subtract),
                     r=[yv.b, mean.b], w=[yv.b])
                P.op("dve", lambda e, c=c: e.tensor_tensor(out=yv.t[:, c, :], in0=yv.t[:, c, :], in1=rstd.t[:], op=ALU.mult),
                     r=[yv.b, rstd.b], w=[yv.b])
                P.op("act", lambda e, c=c: e.activation(out=aout.t[:, c, :], in_=yv.t[:, c, :], func=AF.Silu,
                                                       bias=cwT.t[:, c, 33:34], scale=cwT.t[:, c, 32:33]), r=[yv.b, cwT.b], w=[aout.b])
            P.op("st", lambda e, t0=t0: e.dma_start(out=catTv[:, 0:4, t0:t0 + 512], in_=aout.t[:]), r=[aout.b])
        ph.close()

    def phase_B():
        ph = Phase(P)
        qiT = ph.sb([128, 4, S], BF16, "qiT")
        kiT = ph.sb([128, S], BF16, "kiT")
        wtok = ph.sb([128, NT, 8], F32, "wtok")
        caus = ph.sb([128, 128], F32, "caus")
        pow2 = ph.sb([128, NIT + 2], F32, "pow2")
        P.op("ld", lambda e: e.dma_start(out=qiT.t[:], in_=qiT_d.ap().rearrange("(c p) t -> p c t", p=128)), w=[qiT.b])
        P.op("ld", lambda e: e.dma_start(out=kiT.t[:], in_=kiT_d.ap()), w=[kiT.b])
        P.op("ld", lambda e: e.dma_start(out=wtok.t[:], in_=w_d.ap().rearrange("(t p) e -> p t e", p=128)), w=[wtok.b])
        P.op("ld", lambda e: e.dma_start(out=caus.t[:], in_=c_caus.ap()), w=[caus.b])
        P.op("ld", lambda e: e.dma_start(out=pow2.t[:], in_=c_pow2.ap()), w=[pow2.b])
        NSB = 4
        score = [ph.sb([128, S], F32, "score") for _ in range(NSB)]
        mneg = [ph.sb([128, S], BF16, "mneg") for _ in range(2)]
        junk = [ph.sb([128, S], BF16, "junk") for _ in range(2)]
        rr = [ph.sb([128, 512], BF16, "rr") for _ in range(4)]
        dg = [ph.sb([128, 8, 128], BF16, "dg") for _ in range(NSB)]
        pd = [ph.ps([128, 512], F32, "pd") for _ in range(4)]
        psc = [ph.ps([128, 512], F32, "psc") for _ in range(3)]
        sm = [dict((n, ph.sb([128, 1], F32, n)) for n in ("mn", "mx", "w0", "mid", "cnt", "tt", "thr")) for _ in range(NSB)]
        wk = [ph.sb([128, NIT + 2], F32, "wk") for _ in range(NSB)]
        thr_const = ph.sb([128, 1], F32, "thrc")
        P.op("dve", lambda e: e.memset(thr_const.t[:], -1e29), w=[thr_const.b])
        cstage = [ph.sb([128, 4096], BF16, "cst") for _ in range(3)]
        cnt_ = {"ri": 0, "pdi": 0, "sci": 0}

        def prep(qb):
            nk = (qb + 1) * 128
            sc = score[qb % NSB]
            d = dg[qb % NSB]
            for h in range(8):
                P.op("act", lambda e, h=h: e.activation(out=d.t[:, h, :], in_=ident_f.t[:], func=AF.Copy, scale=wtok.t[:, qb, h:h + 1]),
                     r=[ident_f.b, wtok.b], w=[d.b])
            nch = (nk + 511) // 512
            items = []
            for kc in range(nch):
                k0 = kc * 512
                n = min(512, nk - k0)
                pscore = psc[cnt_["sci"] % 3]; cnt_["sci"] += 1
                for h in range(8):
                    items.append((kc, k0, n, h, pscore))
            LAG = 2
            stash = {}
            for idx in range(len(items) + LAG):
                if idx < len(items):
                    kc, k0, n, h, pscore = items[idx]
                    p0 = (h % 2) * 64
                    pdt = pd[cnt_["pdi"] % 4]; cnt_["pdi"] += 1
                    rt = rr[cnt_["ri"] % 4]; cnt_["ri"] += 1
                    stash[idx] = rt
                    P.op("pe", lambda e: e.matmul(
                        pdt.t[:, 0:n], qiT.t[p0:p0 + 64, h // 2, qb * 128:(qb + 1) * 128], kiT.t[p0:p0 + 64, k0:k0 + n],
                        start=True, stop=True), r=[qiT.b, kiT.b], w=[pdt.b])
                    P.op("act", lambda e: e.activation(out=rt.t[:, 0:n], in_=pdt.t[:, 0:n], func=AF.Relu), r=[pdt.b], w=[rt.b])
                j = idx - LAG
                if j >= 0:
                    kc, k0, n, h, pscore = items[j]
                    rt = stash.pop(j)
                    P.op("pe", lambda e: e.matmul(pscore.t[:, 0:n], d.t[:, h, :], rt.t[:, 0:n], start=(h == 0), stop=(h == 7)),
                         r=[d.b, rt.b], w=[pscore.b])
                    if h == 7:
                        P.op("act", lambda e: e.copy(out=sc.t[:, k0:k0 + n], in_=pscore.t[:, 0:n]), r=[pscore.b], w=[sc.b])

        def bis_ops(qb, slot):
            nk = (qb + 1) * 128
            n1 = qb * 128
            sc = score[qb % NSB]
            mg = mneg[slot]
            jk = junk[slot]
            s_ = sm[qb % NSB]
            wkk = wk[qb % NSB]
            ops = []
            ops.append(lambda: P.op("dve", lambda e: e.tensor_tensor(out=sc.t[:, qb * 128:(qb + 1) * 128], in0=sc.t[:, qb * 128:(qb + 1) * 128],
                                                                   in1=caus.t[:], op=ALU.add), r=[sc.b, caus.b], w=[sc.b]))
            if qb >= 2:
                ops.append(lambda: P.op("dve", lambda e: e.tensor_reduce(out=s_["mx"].t[:], in_=sc.t[:, 0:n1], axis=AX.X, op=ALU.max),
                                        r=[sc.b], w=[s_["mx"].b]))
                ops.append(lambda: P.op("dve", lambda e: e.tensor_reduce(out=s_["mn"].t[:], in_=sc.t[:, 0:n1], axis=AX.X, op=ALU.min),
                                        r=[sc.b], w=[s_["mn"].b]))
                ops.append(lambda: P.op("dve", lambda e: e.tensor_tensor(out=s_["w0"].t[:], in0=s_["mx"].t[:], in1=s_["mn"].t[:], op=ALU.subtract),
                                        r=[s_["mx"].b, s_["mn"].b], w=[s_["w0"].b]))
                ops.append(lambda: P.op("dve", lambda e: e.tensor_scalar(out=wkk.t[:], in0=pow2.t[:], scalar1=s_["w0"].t[:, 0:1], scalar2=None,
                                                                       op0=ALU.mult), r=[pow2.b, s_["w0"].b], w=[wkk.b]))
                ops.append(lambda: P.op("dve", lambda e: e.tensor_tensor(out=s_["mid"].t[:], in0=s_["mn"].t[:], in1=wkk.t[:, 1:2], op=ALU.add),
                                        r=[s_["mn"].b, wkk.b], w=[s_["mid"].b]))
                for it in range(1, NIT + 1):
                    ops.append(lambda: P.op("dve", lambda e: e.tensor_scalar(out=jk.t[:, 0:nk], in0=sc.t[:, 0:nk], scalar1=s_["mid"].t[:, 0:1],
                                                                           scalar2=0.0, op0=ALU.is_ge, op1=ALU.add, accum_out=s_["cnt"].t[:, 0:1]),
                                            r=[sc.b, s_["mid"].b], w=[jk.b, s_["cnt"].b]))
                    ops.append(lambda it=it: P.op("dve", lambda e: e.tensor_scalar(out=s_["tt"].t[:], in0=s_["cnt"].t[:], scalar1=255.5,
                                                                                 scalar2=wkk.t[:, it:it + 1], op0=ALU.is_ge, op1=ALU.mult),
                                                  r=[s_["cnt"].b, wkk.b], w=[s_["tt"].b]))
                    ops.append(lambda it=it: P.op("dve", lambda e: e.scalar_tensor_tensor(out=s_["mid"].t[:], in0=s_["tt"].t[:],
                                                                                        scalar=wkk.t[:, it + 1:it + 2], in1=s_["mid"].t[:],
                                                                                        op0=ALU.subtract, op1=ALU.add),
                                                  r=[s_["tt"].b, wkk.b, s_["mid"].b], w=[s_["mid"].b]))
                ops.append(lambda: P.op("dve", lambda e: e.tensor_tensor(out=s_["thr"].t[:], in0=s_["mid"].t[:], in1=wkk.t[:, NIT + 1:NIT + 2],
                                                                       op=ALU.subtract), r=[s_["mid"].b, wkk.b], w=[s_["thr"].b]))
                thr = s_["thr"]
            else:
                thr = thr_const
            ops.append(lambda: P.op("dve", lambda e: e.tensor_scalar(out=mg.t[:, 0:nk], in0=sc.t[:, 0:nk], scalar1=thr.t[:, 0:1],
                                                                   scalar2=NEGM, op0=ALU.is_lt, op1=ALU.mult), r=[sc.b, thr.b], w=[mg.b]))
            ops.append(lambda: P.op("st", lambda e: e.dma_start(out=mask_d.ap()[qb * 128:(qb + 1) * 128, 0:nk], in_=mg.t[:, 0:nk]), r=[mg.b]))
            return ops

        prep(0)
        prep(1)
        for q0 in range(0, NT, 2):
            conv_emit(cstage, 4)
            if q0 + 2 < NT:
                prep(q0 + 2)
                prep(q0 + 3)
            oa = bis_ops(q0, 0)
            ob = bis_ops(q0 + 1, 1)
            for i in range(max(len(oa), len(ob))):
                if i < len(oa):
                    oa[i]()
                if i < len(ob):
                    ob[i]()
        conv_flush_pending()
        ph.close()

    def phase_C():
        ph = Phase(P)
        vaug = ph.sb([128, NT, 520], BF16, "vaug")
        P.op("ld", lambda e: e.dma_start(out=vaug.t[:], in_=v_d.ap().rearrange("(t p) n -> p t n", p=128)), w=[vaug.b])
        qh = [ph.sb([64, S], BF16, "qh") for _ in range(2)]
        kh = [ph.sb([64, S], BF16, "kh") for _ in range(2)]
        gt = [ph.sb([128, 2560], BF16, "gt") for _ in range(2)]
        attT = [ph.sb([64, S], BF16, "attT") for _ in range(2)]
        eb = [ph.sb([128, 2560], BF16, "eb") for _ in range(2)]
        mk = [[ph.sb([128, S], BF16, "mk") for _ in range(4)] for _ in range(2)]
        pT = [ph.sb([128, 512], BF16, "pT") for _ in range(5)]
        rec = [ph.sb([128, 4], F32, "rec") for _ in range(3)]
        atok = [ph.sb([128, 4, 64], BF16, "atok") for _ in range(3)]
        pss = [ph.ps([128, 512], F32, "pss") for _ in range(4)]
        pacc = [ph.ps([128, 512], F32, "pacc") for _ in range(2)]
        ptr = [ph.ps([128, 1024], BF16, "ptrc") for _ in range(2)]
        si = 0
        ai = 0
        mi = 0
        cstage = [ph.sb([128, 4096], BF16, "cst") for _ in range(3)]
        pend_c = []
        fin_c = []
        for h in range(8):
            q_ = qh[h % 2]; k_ = kh[h % 2]; g_ = gt[h % 2]; at_ = attT[h % 2]
            P.op("ld", lambda e, q_=q_, h=h: e.dma_start(out=q_.t[:], in_=qT_d.ap()[h * 64:(h + 1) * 64, :]), w=[q_.b])
            P.op("ld", lambda e, k_=k_, h=h: e.dma_start(out=k_.t[:], in_=kT_d.ap()[h * 64:(h + 1) * 64, :]), w=[k_.b])
            fa = f0_d.ap()
            P.op("ld", lambda e, g_=g_, h=h, fa=fa: e.dma_start(out=g_.t[:], in_=bass.AP(tensor=fa.tensor, offset=h * L0,
                                                                                    ap=[[1, 128], [1, 2560]])), w=[g_.b])
            if pend_c:
                pend_c.pop(0)()
            eb_ = eb[h % 2]
            for c5 in range(5):
                pe_ = pss[c5 % 4]
                P.op("pe", lambda e, pe_=pe_, c5=c5, g_=g_: e.matmul(pe_.t[:], anti_b.t[:], g_.t[:, c5 * 512:(c5 + 1) * 512], start=True, stop=True),
                     r=[anti_b.b, g_.b], w=[pe_.b])
                P.op("act", lambda e, pe_=pe_, c5=c5, eb_=eb_: e.activation(out=eb_.t[:, c5 * 512:(c5 + 1) * 512], in_=pe_.t[:], func=AF.Exp),
                     r=[pe_.b], w=[eb_.b])
            for Q in range(8):
                conv_emit(cstage, 2)
                mks = mk[mi % 2]; mi += 1
                for j in range(4):
                    qb = 4 * Q + j
                    nk = (qb + 1) * 128
                    P.op("ld", lambda e, m=mks[j], qb=qb, nk=nk: e.dma_start(out=m.t[:, 0:nk], in_=mask_d.ap()[qb * 128:(qb + 1) * 128, 0:nk]),
                         w=[mks[j].b])
                acc = pacc[ai % 2]; ai += 1
                P.op("pe", lambda e, acc=acc: e.matmul(acc.t[:, 0:260], zer_b.t[:, 0:128], zer_b.t[:, 0:260], start=True, stop=False),
                     r=[zer_b.b], w=[acc.b])
                nkb = 4 * Q + 4
                sbase = si
                si += nkb

                def emit_S(kb, Q=Q, q_=q_, k_=k_, g_=g_, mks=mks, sbase=sbase):
                    j0 = max(0, kb - 4 * Q)
                    c0 = j0 * 128
                    ps_ = pss[(sbase + kb) % 4]
                    P.op("pe", lambda e: e.matmul(ps_.t[:, c0:512], k_.t[:, kb * 128:(kb + 1) * 128], q_.t[:, Q * 512 + c0:(Q + 1) * 512],
                                                  start=True, stop=False), r=[k_.b, q_.b], w=[ps_.b])
                    for j in range(j0, 4):
                        P.op("pe", lambda e, j=j: e.matmul(ps_.t[:, j * 128:(j + 1) * 128], mks[j].t[:, kb * 128:(kb + 1) * 128], ident_b.t[:],
                                                           start=False, stop=True), r=[mks[j].b, ident_b.b], w=[ps_.b])

                emit_S(0)
                emit_S(1)
                while fin_c:
                    fin_c.pop(0)()
                for kb in range(nkb):
                    if kb + 2 < nkb:
                        emit_S(kb + 2)
                    j0 = max(0, kb - 4 * Q)
                    c0 = j0 * 128
                    ps_ = pss[(sbase + kb) % 4]
                    pt_ = pT[(sbase + kb) % 5]
                    P.op("act", lambda e, ps_=ps_, pt_=pt_, c0=c0: e.activation(out=pt_.t[:, c0:512], in_=ps_.t[:, c0:512], func=AF.Exp),
                         r=[ps_.b], w=[pt_.b])
                    dl = min(4 * Q - kb, 13)
                    off = dl * 128 + 384
                    P.op("dve", lambda e, pt_=pt_, c0=c0, off=off, eb_=eb_: e.tensor_tensor(out=pt_.t[:, c0:512], in0=pt_.t[:, c0:512],
                                                                                       in1=eb_.t[:, off + c0:off + 512], op=ALU.mult),
                         r=[pt_.b, eb_.b], w=[pt_.b])
                    for j in range(j0, 4):
                        P.op("pe", lambda e, acc=acc, pt_=pt_, kb=kb, j=j, h=h, Q=Q: e.matmul(
                            acc.t[:, j * 65:(j + 1) * 65], pt_.t[:, j * 128:(j + 1) * 128], vaug.t[:, kb, h * 65:(h + 1) * 65],
                            start=False, stop=(kb == 4 * Q + j)), r=[pt_.b, vaug.b], w=[acc.b])
                rc = rec[ai % 3]
                ak = atok[ai % 3]
                P.op("dve", lambda e, acc=acc, rc=rc: e.reciprocal(out=rc.t[:], in_=acc.t[:, 0:260].rearrange("p (j d) -> p j d", d=65)[:, :, 64]),
                     r=[acc.b], w=[rc.b])
                for j in range(4):
                    P.op("dve", lambda e, acc=acc, rc=rc, ak=ak, j=j: e.tensor_scalar(out=ak.t[:, j, :], in0=acc.t[:, j * 65:j * 65 + 64],
                                                                                   scalar1=rc.t[:, j:j + 1], scalar2=None, op0=ALU.mult),
                         r=[acc.b, rc.b], w=[ak.b])
                pt2 = ptr[ai % 2]

                def finish(pt2=pt2, ak=ak, at_=at_, Q=Q):
                    for j in range(4):
                        P.op("pe", lambda e, j=j: e.transpose(out=pt2.t[0:64, j * 128:(j + 1) * 128], in_=ak.t[:, j, :],
                                                              identity=ident_b.t[:]), r=[ak.b, ident_b.b], w=[pt2.b])
                    P.op("act", lambda e: e.copy(out=at_.t[:, Q * 512:(Q + 1) * 512], in_=pt2.t[0:64, 0:512]),
                         r=[pt2.b], w=[at_.b])
                if Q == 7:
                    finish()
                else:
                    fin_c.append(finish)
            pend_c.append(lambda at_=at_, h=h: P.op("st", lambda e: e.dma_start(out=catT_d.ap()[512 + h * 64:512 + (h + 1) * 64, :], in_=at_.t[:]), r=[at_.b]))
        while pend_c:
            pend_c.pop(0)()
        conv_finish(cstage)
        ph.close()

    def phase_outproj(src_kind, wout_d, lng_d, lnb_d, xres_d, xout_d):
        ph = Phase(P)
        wo = ph.sb([128, 8, D], BF16, "wo")
        P.op("gld", lambda e: e.dma_start(out=wo.t[:], in_=wout_d.ap().rearrange("(c p) n -> p c n", p=128)), w=[wo.b])
        gB = ph.sb([128, D], F32, "gB")
        bB = ph.sb([128, D], F32, "bB")
        P.op("ld", lambda e: e.dma_start(out=gB.t[:], in_=bcast_row(lng_d)), w=[gB.b])
        P.op("ld", lambda e: e.dma_start(out=bB.t[:], in_=bcast_row(lnb_d)), w=[bB.b])
        pmx = [ph.ps([128, 512], F32, "pmx") for _ in range(4)]
        xr = [ph.sb([128, D], F32, "xr") for _ in range(3)]
        z = [ph.sb([128, D], F32, "z") for _ in range(3)]
        xo = [ph.sb([128, D], F32, "xo") for _ in range(3)]
        st6 = [ph.sb([128, 2, 6], F32, "st6") for _ in range(3)]
        mv = [ph.sb([128, 2], F32, "mv") for _ in range(3)]
        rstd = [ph.sb([128, 2], F32, "rstd") for _ in range(3)]
        if src_kind == "catT":
            cT = [ph.sb([128, 8, 512], BF16, "cT") for _ in range(2)]
        else:
            ug = [[ph.sb([128, 8, 132], F32, "ug") for _ in range(3)] for _ in range(3)]
            rc8 = [ph.sb([128, 8], F32, "rc8") for _ in range(3)]
            otok = [ph.sb([128, D], BF16, "otok") for _ in range(3)]
            oT = [ph.sb([128, 8, 128], BF16, "oT") for _ in range(3)]
            ptr = [ph.ps([128, 1024], BF16, "ptro") for _ in range(2)]
        catTv = catT_d.ap().rearrange("(c p) t -> p c t", p=128)
        NB = len(xr)
        st1 = {}

        def stage1(tt):
            i2 = tt % NB
            t0 = tt * 128
            if src_kind == "catT":
                if tt % 4 == 0:
                    ct = cT[(tt // 4) % 2]
                    P.op("ld", lambda e: e.dma_start(out=ct.t[:], in_=catTv[:, :, t0:t0 + 512]), w=[ct.b])
                ct = cT[(tt // 4) % 2]
                lhs = lambda c: ct.t[:, c, (tt % 4) * 128:(tt % 4 + 1) * 128]
                lhs_b = ct.b
            else:
                u3 = ug[i2]
                for g in range(3):
                    P.op("ld", lambda e, g=g: e.dma_start(out=u3[g].t[:], in_=u_d.ap()[g, t0:t0 + 128, :, :]), w=[u3[g].b])
                P.op("dve", lambda e: e.tensor_tensor(out=u3[0].t[:], in0=u3[0].t[:], in1=u3[1].t[:], op=ALU.add),
                     r=[u3[0].b, u3[1].b], w=[u3[0].b])
                P.op("dve", lambda e: e.tensor_tensor(out=u3[0].t[:], in0=u3[0].t[:], in1=u3[2].t[:], op=ALU.add),
                     r=[u3[0].b, u3[2].b], w=[u3[0].b])
                rc = rc8[i2]
                ot = otok[i2]
                P.op("dve", lambda e: e.reciprocal(out=rc.t[:], in_=u3[0].t[:, :, 128]), r=[u3[0].b], w=[rc.b])
                for hh in range(8):
                    eng = "dve" if hh % 2 == 0 else "pool"
                    P.op(eng, lambda e, hh=hh: e.tensor_scalar(out=ot.t[:, hh * 128:(hh + 1) * 128], in0=u3[0].t[:, hh, 0:128],
                                                              scalar1=rc.t[:, hh:hh + 1], scalar2=None, op0=ALU.mult),
                         r=[u3[0].b, rc.b], w=[ot.b])
                o_T = oT[i2]
                for half in range(2):
                    pt = ptr[half]
                    for cc in range(4):
                        c = half * 4 + cc
                        P.op("pe", lambda e, c=c, cc=cc: e.transpose(out=pt.t[:, cc * 128:(cc + 1) * 128], in_=ot.t[:, c * 128:(c + 1) * 128],
                                                                   identity=ident_b.t[:]), r=[ot.b, ident_b.b], w=[pt.b])
                    P.op("act", lambda e: e.copy(out=o_T.t[:, half * 4:(half + 1) * 4, :].rearrange("p c t -> p (c t)"),
                                                 in_=pt.t[:, 0:512]), r=[pt.b], w=[o_T.b])
                lhs = lambda c: o_T.t[:, c, :]
                lhs_b = o_T.b
            x_ = xr[i2]
            P.op("ld", lambda e: e.dma_start(out=x_.t[:], in_=xres_d.ap()[t0:t0 + 128, :]), w=[x_.b])
            st1[tt] = (lhs, lhs_b)

        def stage2(tt):
            i2 = tt % NB
            t0 = tt * 128
            lhs, lhs_b = st1.pop(tt)
            x_ = xr[i2]
            z_ = z[i2]
            for half in range(2):
                pm_ = pmx[(tt * 2 + half) % 4]
                for c in range(8):
                    P.op("pe", lambda e, c=c: e.matmul(pm_.t[:], lhs(c), wo.t[:, c, half * 512:(half + 1) * 512],
                                                       start=(c == 0), stop=(c == 7)), r=[lhs_b, wo.b], w=[pm_.b])
                P.op("dve", lambda e: e.scalar_tensor_tensor(
                    out=z_.t[:, half * 512:(half + 1) * 512], in0=x_.t[:, half * 512:(half + 1) * 512], scalar=ALPHA, in1=pm_.t[:],
                    op0=ALU.mult, op1=ALU.add), r=[pm_.b, x_.b], w=[z_.b])
            layernorm_rows(ph, z_, gB, bB, xo[i2], st6[i2], mv[i2], rstd[i2], None, gb_eng="pool")
            return lambda: P.op("st", lambda e: e.dma_start(out=xout_d.ap()[t0:t0 + 128, :], in_=xo[i2].t[:]), r=[xo[i2].b])

        stage1(0)
        pend = None
        for tt in range(NT):
            if tt + 1 < NT:
                stage1(tt + 1)
            if pend is not None:
                pend()
            pend = stage2(tt)
        pend()
        ph.close()

    def phase_E():
        ph = Phase(P)
        NF = 22
        wd = ph.sb([128, NF, D], BF16, "wd")
        wdv = ewd.ap().rearrange("(f p) n -> p f n", p=128)
        for f0 in range(0, NF, 6):
            f1 = min(NF, f0 + 6)
            P.op("gld", lambda e, f0=f0, f1=f1: e.dma_start(out=wd.t[:, f0:f1, :], in_=wdv[:, f0:f1, :]), w=[wd.b])
        gB = ph.sb([128, D], F32, "gB")
        bB = ph.sb([128, D], F32, "bB")
        P.op("ld", lambda e: e.dma_start(out=gB.t[:], in_=bcast_row(eln2g)), w=[gB.b])
        P.op("ld", lambda e: e.dma_start(out=bB.t[:], in_=bcast_row(eln2b)), w=[bB.b])
        xtok = [ph.sb([128, D], BF16, "xtok") for _ in range(8)]
        xT = ph.sb([128, 8, 1024], BF16, "xT")
        hT = ph.sb([128, NF, 1024], BF16, "hT")
        wgp = [ph.sb([128, 8, 256], BF16, "wgp") for _ in range(2)]
        wup = [ph.sb([128, 8, 256], BF16, "wup") for _ in range(2)]
        sg = [ph.sb([128, 512], F32, "sg") for _ in range(2)]
        ptr = [ph.ps([128, 1024], BF16, "ptre") for _ in range(2)]
        pg = [ph.ps([128, 512], F32, "pg") for _ in range(2)]
        pu = [ph.ps([128, 512], F32, "pu") for _ in range(2)]
        py = [ph.ps([128, 512], F32, "py") for _ in range(2)]
        xr = [ph.sb([128, D], F32, "xr") for _ in range(2)]
        z = [ph.sb([128, D], F32, "z") for _ in range(2)]
        xo = [ph.sb([128, D], F32, "xo") for _ in range(2)]
        st6 = [ph.sb([128, 2, 6], F32, "st6") for _ in range(2)]
        mv = [ph.sb([128, 2], F32, "mv") for _ in range(2)]
        rstd = [ph.sb([128, 2], F32, "rstd") for _ in range(2)]
        wgv = ewg.ap().rearrange("(c p) n -> p c n", p=128)
        wuv = ewu.ap().rearrange("(c p) n -> p c n", p=128)
        pi = 0
        gi = 0
        pend_e = [None]
        for grp in range(4):
            t0 = grp * 1024
            load_tok_T(ph, lambda j, t0=t0: x1_d.ap()[t0 + j * 128:t0 + (j + 1) * 128, :], 8, xtok, xT, ptr, "x1")
            for pc in range(11):
                wg_ = wgp[pi % 2]; wu_ = wup[pi % 2]; pi += 1
                P.op("gld", lambda e, wg_=wg_, pc=pc: e.dma_start(out=wg_.t[:], in_=wgv[:, :, pc * 256:(pc + 1) * 256]), w=[wg_.b])
                P.op("gld", lambda e, wu_=wu_, pc=pc: e.dma_start(out=wu_.t[:], in_=wuv[:, :, pc * 256:(pc + 1) * 256]), w=[wu_.b])
                for fs in range(2):
                    fc = pc * 2 + fs
                    for half in range(2):
                        pg_ = pg[gi % 2]; pu_ = pu[gi % 2]; sg_ = sg[gi % 2]; gi += 1
                        for c in range(8):
                            P.op("pe", lambda e, pg_=pg_, wg_=wg_, c=c, fs=fs, half=half: e.matmul(
                                pg_.t[:], wg_.t[:, c, fs * 128:(fs + 1) * 128], xT.t[:, c, half * 512:(half + 1) * 512],
                                start=(c == 0), stop=(c == 7)), r=[wg_.b, xT.b], w=[pg_.b])
                        for c in range(8):
                            P.op("pe", lambda e, pu_=pu_, wu_=wu_, c=c, fs=fs, half=half: e.matmul(
                                pu_.t[:], wu_.t[:, c, fs * 128:(fs + 1) * 128], xT.t[:, c, half * 512:(half + 1) * 512],
                                start=(c == 0), stop=(c == 7)), r=[wu_.b, xT.b], w=[pu_.b])
                        P.op("act", lambda e, pg_=pg_, sg_=sg_: e.activation(out=sg_.t[:], in_=pg_.t[:], func=AF.Silu), r=[pg_.b], w=[sg_.b])
                        P.op("dve", lambda e, pu_=pu_, sg_=sg_, fc=fc, half=half: e.tensor_tensor(
                            out=hT.t[:, fc, half * 512:(half + 1) * 512], in0=pu_.t[:], in1=sg_.t[:], op=ALU.mult), r=[pu_.b, sg_.b], w=[hT.b])
            for j in range(8):
                tt = grp * 8 + j
                i2 = tt % 2
                x_ = xr[i2]; z_ = z[i2]
                P.op("ld", lambda e, x_=x_, tt=tt: e.dma_start(out=x_.t[:], in_=x1_d.ap()[tt * 128:(tt + 1) * 128, :]), w=[x_.b])
                for half in range(2):
                    py_ = py[half]
                    for fc in range(NF):
                        P.op("pe", lambda e, py_=py_, fc=fc, j=j, half=half: e.matmul(
                            py_.t[:], hT.t[:, fc, j * 128:(j + 1) * 128], wd.t[:, fc, half * 512:(half + 1) * 512],
                            start=(fc == 0), stop=(fc == NF - 1)), r=[hT.b, wd.b], w=[py_.b])
                    P.op("dve", lambda e, py_=py_, x_=x_, z_=z_, half=half: e.scalar_tensor_tensor(
                        out=z_.t[:, half * 512:(half + 1) * 512], in0=x_.t[:, half * 512:(half + 1) * 512], scalar=ALPHA, in1=py_.t[:],
                        op0=ALU.mult, op1=ALU.add), r=[py_.b, x_.b], w=[z_.b])
                layernorm_rows(ph, z_, gB, bB, xo[i2], st6[i2], mv[i2], rstd[i2], None)
                if pend_e[0] is not None:
                    pend_e[0]()
                pend_e[0] = (lambda i2=i2, tt=tt: P.op("st", lambda e: e.dma_start(out=x2_d.ap()[tt * 128:(tt + 1) * 128, :], in_=xo[i2].t[:]), r=[xo[i2].b]))
        pend_e[0]()
        ph.close()

    def phase_F(g):
        r = (1, 4, 16)[g]
        Lc = S // r
        nb = Lc // 128
        ph = Phase(P)
        wq = ph.sb([128, 8, 1024], BF16, "wq")
        wk_ = ph.sb([128, 8, 1024], BF16, "wk")
        wv = ph.sb([128, 8, 1024], BF16, "wv")
        wv_ap = owin.ap().rearrange("(c p) n -> p c n", p=128)
        for j, wt in enumerate((wq, wk_, wv)):
            col = (g * 3 + j) * 1024
            for c0 in range(0, 8, 4):
                P.op("gld", lambda e, wt=wt, col=col, c0=c0: e.dma_start(out=wt.t[:, c0:c0 + 4, :], in_=wv_ap[:, c0:c0 + 4, col:col + 1024]), w=[wt.b])
        g1 = ph.sb([128, 8, 256], BF16, "g1")
        fa = f1_d.ap()
        for h in range(8):
            P.op("ld", lambda e, h=h: e.dma_start(out=g1.t[:, h, :], in_=bass.AP(tensor=fa.tensor, offset=(g * 8 + h) * L1,
                                                                              ap=[[1, 128], [1, 256]])), w=[g1.b])
        xtok = [ph.sb([128, D], BF16, "xtok") for _ in range(4)]
        xT = ph.sb([128, 8, S], BF16, "xTp")
        ptr = [ph.ps([128, 1024], BF16, "ptrf")] * 2
        x2a = x2_d.ap()

        def rows(tt):
            rho = (tt * 128) // Lc
            l0 = (tt * 128) % Lc
            return bass.AP(tensor=x2a.tensor, offset=(l0 * r + rho) * D, ap=[[r * D, 128], [1, D]])
        xTs = [T(xT.t, "x") for _ in range(8)]
        for sbk in range(8):
            xs = xTs[sbk]
            for j in range(4):
                P.op("gld", lambda e, j=j, sbk=sbk: e.dma_start(out=xtok[j].t[:], in_=rows(sbk * 4 + j)), w=[xtok[j].b])
            for c in range(8):
                pt = ptr[c % 2]
                for jj in range(4):
                    P.op("pe", lambda e, pt=pt, jj=jj, c=c: e.transpose(out=pt.t[:, jj * 128:(jj + 1) * 128], in_=xtok[jj].t[:, c * 128:(c + 1) * 128],
                                                                      identity=ident_b.t[:]), r=[xtok[jj].b, ident_b.b], w=[pt.b])
                if c % 2 == 0:
                    P.op("act", lambda e, pt=pt, c=c, sbk=sbk: e.copy(out=xT.t[:, c, sbk * 512:(sbk + 1) * 512], in_=pt.t[:, 0:512]), r=[pt.b], w=[xs.b])
                else:
                    P.op("dve", lambda e, pt=pt, c=c, sbk=sbk: e.tensor_copy(out=xT.t[:, c, sbk * 512:(sbk + 1) * 512], in_=pt.t[:, 0:512]), r=[pt.b], w=[xs.b])
        qh = [ph.sb([128, S], BF16, "qh") for _ in range(2)]
        kh = [ph.sb([128, S], BF16, "kh") for _ in range(2)]
        vh = [ph.sb([128, NT, 129], BF16, "vh") for _ in range(2)]
        ust = [ph.sb([128, NT, 129], F32, "ust")] * 2
        pT = [ph.sb([128, 4, 128], BF16, "pT") for _ in range(3)]
        pq = [ph.ps([128, 512], F32, "pq") for _ in range(2)]
        pss = [ph.ps([128, 512], F32, "pss") for _ in range(3)]
        pacc = [ph.ps([128, 512], F32, "pacc") for _ in range(2)]
        QS = float(128 ** -0.5)
        pqi = 0
        si = 0
        for vv in vh:
            P.op("dve", lambda e, vv=vv: e.memset(vv.t[:, :, 128:129], 1.0), w=[vv.b])
        for h in range(8):
            q_ = qh[h % 2]; k_ = kh[h % 2]; v_ = vh[h % 2]; u_ = ust[h % 2]
            for sbk in range(8):
                for which, wt, dst in ((0, wq, q_), (1, wk_, k_)):
                    pp = pq[pqi % 2]; pqi += 1
                    for c in range(8):
                        P.op("pe", lambda e, pp=pp, wt=wt, c=c, h=h, sbk=sbk: e.matmul(
                            pp.t[:], wt.t[:, c, h * 128:(h + 1) * 128], xT.t[:, c, sbk * 512:(sbk + 1) * 512], start=(c == 0), stop=(c == 7)),
                            r=[wt.b, xTs[sbk].b], w=[pp.b])
                    if which == 0:
                        P.op("act", lambda e, pp=pp, dst=dst, sbk=sbk: e.activation(out=dst.t[:, sbk * 512:(sbk + 1) * 512], in_=pp.t[:], func=AF.Copy, scale=QS),
                             r=[pp.b], w=[dst.b])
                    else:
                        P.op("dve", lambda e, pp=pp, dst=dst, sbk=sbk: e.tensor_copy(out=dst.t[:, sbk * 512:(sbk + 1) * 512], in_=pp.t[:]), r=[pp.b], w=[dst.b])
                pp = pq[pqi % 2]; pqi += 1
                for j in range(4):
                    for c in range(8):
                        P.op("pe", lambda e, pp=pp, c=c, h=h, sbk=sbk, j=j: e.matmul(
                            pp.t[:, j * 128:(j + 1) * 128], xT.t[:, c, sbk * 512 + j * 128:sbk * 512 + (j + 1) * 128], wv.t[:, c, h * 128:(h + 1) * 128],
                            start=(c == 0), stop=(c == 7)), r=[wv.b, xTs[sbk].b], w=[pp.b])
                P.op("act", lambda e, pp=pp, v_=v_, sbk=sbk: e.copy(out=v_.t[:, sbk * 4:(sbk + 1) * 4, 0:128], in_=pp.t[:].rearrange("p (j d) -> p j d", j=4)),
                     r=[pp.b], w=[v_.b])
            sbase = si
            si += NT // 2

            def emit_S(t2, q_=q_, k_=k_, h=h, sbase=sbase):
                ps_ = pss[(sbase + t2 // 2) % 3]
                for bi in range(2):
                    tt = t2 + bi
                    n = tt % nb
                    P.op("pe", lambda e, tt=tt, bi=bi: e.matmul(
                        ps_.t[:, (2 * bi + 1) * 128:(2 * bi + 2) * 128], k_.t[:, tt * 128:(tt + 1) * 128], q_.t[:, tt * 128:(tt + 1) * 128],
                        start=True, stop=False), r=[k_.b, q_.b], w=[ps_.b])
                    P.op("pe", lambda e, bi=bi: e.matmul(
                        ps_.t[:, (2 * bi + 1) * 128:(2 * bi + 2) * 128], anti_b.t[:], g1.t[:, h, 0:128], start=False, stop=True),
                        r=[anti_b.b, g1.b], w=[ps_.b])
                    if n > 0:
                        P.op("pe", lambda e, tt=tt, bi=bi: e.matmul(
                            ps_.t[:, (2 * bi) * 128:(2 * bi + 1) * 128], k_.t[:, (tt - 1) * 128:tt * 128], q_.t[:, tt * 128:(tt + 1) * 128],
                            start=True, stop=False), r=[k_.b, q_.b], w=[ps_.b])
                        P.op("pe", lambda e, bi=bi: e.matmul(
                            ps_.t[:, (2 * bi) * 128:(2 * bi + 1) * 128], anti_b.t[:], g1.t[:, h, 128:256], start=False, stop=True),
                            r=[anti_b.b, g1.b], w=[ps_.b])

            emit_S(0)
            for t2 in range(0, NT, 2):
                if t2 + 2 < NT:
                    emit_S(t2 + 2)
                ps_ = pss[(sbase + t2 // 2) % 3]; pt_ = pT[(sbase + t2 // 2) % 3]; acc = pacc[(t2 // 2) % 2]
                first_has_prev = (t2 % nb) > 0
                c0 = 0 if first_has_prev else 128
                P.op("act", lambda e, ps_=ps_, pt_=pt_, c0=c0: e.activation(out=pt_.t[:].rearrange("p a b -> p (a b)")[:, c0:512], in_=ps_.t[:, c0:512], func=AF.Exp),
                     r=[ps_.b], w=[pt_.b])
                for bi in range(2):
                    tt = t2 + bi
                    n = tt % nb
                    P.op("pe", lambda e, acc=acc, pt_=pt_, v_=v_, tt=tt, bi=bi, n=n: e.matmul(
                        acc.t[:, bi * 129:(bi + 1) * 129], pt_.t[:, 2 * bi + 1, :], v_.t[:, tt, :], start=True, stop=(n == 0)),
                        r=[pt_.b, v_.b], w=[acc.b])
                    if n > 0:
                        P.op("pe", lambda e, acc=acc, pt_=pt_, v_=v_, tt=tt, bi=bi: e.matmul(
                            acc.t[:, bi * 129:(bi + 1) * 129], pt_.t[:, 2 * bi, :], v_.t[:, tt - 1, :], start=False, stop=True),
                            r=[pt_.b, v_.b], w=[acc.b])
                P.op("dve", lambda e, acc=acc, u_=u_, t2=t2: e.tensor_copy(out=u_.t[:, t2:t2 + 2, :], in_=acc.t[:, 0:258].rearrange("p (b d) -> p b d", b=2)),
                     r=[acc.b], w=[u_.b])
            uda = u_d.ap()
            for rho in range(r):
                P.op("st", lambda e, u_=u_, rho=rho, h=h: e.dma_start(
                    out=bass.AP(tensor=uda.tensor, offset=((g * S + rho) * 8 + h) * 132, ap=[[r * 8 * 132, 128], [128 * r * 8 * 132, nb], [1, 129]]),
                    in_=u_.t[:, rho * nb:(rho + 1) * nb, :]), r=[u_.b])
        ph.close()

    conv_jobs = []
    for e_ in range(8):
        for pc in range(7):
            conv_jobs.append(("wg", e_, pc))
            conv_jobs.append(("wu", e_, pc))
    for e_ in range(8):
        for ch in range(2):
            for q_ in range(4):
                conv_jobs.append(("wd", e_, ch * 4 + q_))
    conv_state = {"next": 0, "pending_store": None, "k": 0}

    def conv_emit(stage, n):
        for _ in range(n):
            pend = conv_state["pending_store"]
            if pend is not None:
                stg_, dst_ap, ncol = pend
                P.op("gst", lambda e, stg_=stg_, dst_ap=dst_ap, ncol=ncol: e.dma_start(out=dst_ap, in_=stg_.t[:, 0:ncol]), r=[stg_.b])
                conv_state["pending_store"] = None
            if conv_state["next"] >= len(conv_jobs):
                continue
            kind, e_, i_ = conv_jobs[conv_state["next"]]
            conv_state["next"] += 1
            stg_ = stage[conv_state["k"] % len(stage)]
            conv_state["k"] += 1
            if kind in ("wg", "wu"):
                src = (omwg if kind == "wg" else omwu).ap()[e_].rearrange("(c p) n -> p c n", p=128)[:, :, i_ * 512:(i_ + 1) * 512]
                dst = (wgs_d if kind == "wg" else wus_d).ap()[(e_ * 7 + i_) * 128:(e_ * 7 + i_ + 1) * 128, :]
                P.op("gld", lambda e, stg_=stg_, src=src: e.dma_start(out=stg_.t[:, 0:4096].rearrange("p (c n) -> p c n", c=8), in_=src), w=[stg_.b])
                conv_state["pending_store"] = (stg_, dst, 4096)
            else:
                ch, q_ = i_ // 4, i_ % 4
                src = omwd.ap()[e_].rearrange("(f p) n -> p f n", p=128)[:, q_ * 7:(q_ + 1) * 7, ch * 512:(ch + 1) * 512]
                dst = wds_d.ap()[(e_ * 8 + i_) * 128:(e_ * 8 + i_ + 1) * 128, :]
                P.op("gld", lambda e, stg_=stg_, src=src: e.dma_start(out=stg_.t[:, 0:3584].rearrange("p (f n) -> p f n", f=7), in_=src), w=[stg_.b])
                conv_state["pending_store"] = (stg_, dst, 3584)

    def conv_flush_pending():
        pend = conv_state["pending_store"]
        if pend is not None:
            stg_, dst_ap, ncol = pend
            P.op("gst", lambda e: e.dma_start(out=dst_ap, in_=stg_.t[:, 0:ncol]), r=[stg_.b])
            conv_state["pending_store"] = None

    def conv_finish(stage):
        while conv_state["next"] < len(conv_jobs) or conv_state["pending_store"] is not None:
            conv_emit(stage, 1)

    NTILE = 23
    NS = NTILE * 512
    I32 = mybir.dt.int32

    def phase_H1():
        ph = Phase(P)
        if conv_state["next"] < len(conv_jobs):
            cst_ = [ph.sb([128, 4096], BF16, "cst") for _ in range(3)]
            conv_finish(cst_)
        rt = ph.sb([128, 8, 8], F32, "router")
        P.op("ld", lambda e: e.dma_start(out=rt.t[:], in_=orouter.ap().rearrange("(c p) e -> p c e", p=128)), w=[rt.b])
        ustr = ph.sb([128, 128], BF16, "ustr")
        ones_b = ph.sb([128, 128], BF16, "onesb")
        thr8 = ph.sb([128, 8], F32, "thr8")
        iota23 = ph.sb([128, NTILE], F32, "iota23")
        cwg = ph.sb([128, 7], F32, "cwg")
        cwd = ph.sb([128, 8], F32, "cwd")
        P.op("gld", lambda e: e.dma_start(out=ustr.t[:], in_=c_ustr.ap()), w=[ustr.b])
        P.op("ld", lambda e: e.dma_start(out=thr8.t[:], in_=c_thr8.ap()), w=[thr8.b])
        P.op("ld", lambda e: e.dma_start(out=iota23.t[:], in_=c_iota23.ap()), w=[iota23.b])
        P.op("ld", lambda e: e.dma_start(out=cwg.t[:], in_=c_cwg.ap()), w=[cwg.b])
        P.op("ld", lambda e: e.dma_start(out=cwd.t[:], in_=c_cwd.ap()), w=[cwd.b])
        P.op("pool", lambda e: e.memset(ones_b.t[:], 1.0), w=[ones_b.b])
        xf = [ph.sb([128, D], F32, "xf") for _ in range(2)]
        xTf = [ph.sb([128, 8, 128], F32, "xTf") for _ in range(2)]
        MSK = ph.sb([128, NT, 8], F32, "MSK")
        OHA = ph.sb([128, NT, 8], F32, "OHA")
        lg = [ph.sb([128, 8], F32, "lg") for _ in range(2)]
        mx8 = [ph.sb([128, 8], F32, "mx8") for _ in range(2)]
        ee = [ph.sb([128, 8], F32, "ee") for _ in range(2)]
        nv1 = [ph.sb([128, 1], F32, "nv1") for _ in range(2)]
        den = [ph.sb([128, 1], F32, "den") for _ in range(2)]
        ptf = [ph.ps([128, 512], F32, "ptf") for _ in range(2)]
        plg = ph.ps([128, 512], F32, "plg")
        pcum = ph.ps([128, 512], F32, "pcum")
        ptot = ph.ps([128, 512], F32, "ptot")
        for tt in range(NT):
            x_ = xf[tt % 2]; xt_ = xTf[tt % 2]
            P.op("ld", lambda e, x_=x_, tt=tt: e.dma_start(out=x_.t[:], in_=x3_d.ap()[tt * 128:(tt + 1) * 128, :]), w=[x_.b])
            for half in range(2):
                pt = ptf[half]
                for cc in range(4):
                    c = half * 4 + cc
                    P.op("pe", lambda e, x_=x_, c=c, cc=cc, pt=pt: e.transpose(out=pt.t[:, cc * 128:(cc + 1) * 128], in_=x_.t[:, c * 128:(c + 1) * 128],
                                                                             identity=ident_f.t[:]), r=[x_.b, ident_f.b], w=[pt.b])
                P.op("act", lambda e, xt_=xt_, half=half, pt=pt: e.copy(out=xt_.t[:, half * 4:(half + 1) * 4, :].rearrange("p c t -> p (c t)"), in_=pt.t[:]),
                     r=[pt.b], w=[xt_.b])
            for c in range(8):
                P.op("pe", lambda e, xt_=xt_, c=c: e.matmul(plg.t[:, 0:8], xt_.t[:, c, :], rt.t[:, c, :], start=(c == 0), stop=(c == 7)),
                     r=[xt_.b, rt.b], w=[plg.b])
            l_ = lg[tt % 2]; m_ = mx8[tt % 2]; e_ = ee[tt % 2]; n_ = nv1[tt % 2]; d_ = den[tt % 2]
            P.op("dve", lambda e, l_=l_: e.tensor_copy(out=l_.t[:], in_=plg.t[:, 0:8]), r=[plg.b], w=[l_.b])
            P.op("dve", lambda e, l_=l_, m_=m_: e.max(out=m_.t[:], in_=l_.t[:]), r=[l_.b], w=[m_.b])
            P.op("dve", lambda e, m_=m_, n_=n_: e.tensor_scalar(out=n_.t[:], in0=m_.t[:, 0:1], scalar1=-1.0, scalar2=None, op0=ALU.mult),
                 r=[m_.b], w=[n_.b])
            P.op("act", lambda e, l_=l_, e_=e_, n_=n_: e.activation(out=e_.t[:], in_=l_.t[:], func=AF.Exp, bias=n_.t[:, 0:1], scale=1.0),
                 r=[l_.b, n_.b], w=[e_.b])
            P.op("dve", lambda e, l_=l_, m_=m_, tt=tt: e.tensor_scalar(out=MSK.t[:, tt, :], in0=l_.t[:], scalar1=m_.t[:, 1:2], scalar2=None, op0=ALU.is_ge),
                 r=[l_.b, m_.b], w=[MSK.b])
            P.op("dve", lambda e, l_=l_, m_=m_, tt=tt: e.tensor_scalar(out=OHA.t[:, tt, :], in0=l_.t[:], scalar1=m_.t[:, 0:1], scalar2=None, op0=ALU.is_ge),
                 r=[l_.b, m_.b], w=[OHA.b])
            P.op("dve", lambda e, e_=e_, tt=tt: e.tensor_tensor(out=e_.t[:], in0=e_.t[:], in1=MSK.t[:, tt, :], op=ALU.mult), r=[e_.b, MSK.b], w=[e_.b])
            P.op("dve", lambda e, e_=e_, d_=d_: e.tensor_reduce(out=d_.t[:], in_=e_.t[:], axis=AX.X, op=ALU.add), r=[e_.b], w=[d_.b])
            P.op("dve", lambda e, d_=d_, tt=tt: e.reciprocal(out=GA.t[:, tt:tt + 1], in_=d_.t[:]), r=[d_.b], w=[GA.b])
        P.op("dve", lambda e: e.tensor_scalar(out=GB.t[:], in0=GA.t[:], scalar1=-1.0, scalar2=1.0, op0=ALU.mult, op1=ALU.add), r=[GA.b], w=[GB.b])
        mskb = ph.sb([128, NT * 8], BF16, "mskb")
        tot = ph.sb([128, NT, 8], F32, "tot")
        offs = ph.sb([128, NT, 8], F32, "offs")
        slot = ph.sb([128, NT, 8], F32, "slot")
        tmp3 = ph.sb([128, NT, 8], F32, "tmp3")
        ohb = ph.sb([128, NT, 8], F32, "ohb")
        cnt = ph.sb([128, 8], F32, "cnt")
        ntl = ph.sb([128, 8], F32, "ntl")
        tend = ph.sb([128, 8], F32, "tend")
        st512 = ph.sb([128, 8], F32, "st512")
        slf = ph.sb([128, 2, NT], F32, "slf")
        eidf = ph.sb([128, NTILE], F32, "eidf")
        e896 = ph.sb([128, NTILE], F32, "e896")
        e1024 = ph.sb([128, NTILE], F32, "e1024")
        iwgf = ph.sb([128, NTILE, 7], F32, "iwgf")
        iwdf = ph.sb([128, NTILE, 8], F32, "iwdf")
        P.op("dve", lambda e: e.tensor_copy(out=mskb.t[:], in_=MSK.t[:].rearrange("p t e -> p (t e)")), r=[MSK.b], w=[mskb.b])
        P.op("pe", lambda e: e.matmul(pcum.t[:, 0:256], ustr.t[:], mskb.t[:], start=True, stop=True), r=[ustr.b, mskb.b], w=[pcum.b])
        P.op("pe", lambda e: e.matmul(ptot.t[:, 0:256], ones_b.t[:], mskb.t[:], start=True, stop=True), r=[ones_b.b, mskb.b], w=[ptot.b])
        P.op("dve", lambda e: e.tensor_copy(out=tot.t[:].rearrange("p t e -> p (t e)"), in_=ptot.t[:, 0:256]), r=[ptot.b], w=[tot.b])
        P.op("dve", lambda e: e.memset(offs.t[:, 0, :], 0.0), w=[offs.b])
        for tt in range(1, NT):
            P.op("dve", lambda e, tt=tt: e.tensor_tensor(out=offs.t[:, tt, :], in0=offs.t[:, tt - 1, :], in1=tot.t[:, tt - 1, :], op=ALU.add),
                 r=[offs.b, tot.b], w=[offs.b])
        P.op("dve", lambda e: e.tensor_tensor(out=cnt.t[:], in0=offs.t[:, NT - 1, :], in1=tot.t[:, NT - 1, :], op=ALU.add), r=[offs.b, tot.b], w=[cnt.b])
        P.op("dve", lambda e: e.tensor_scalar(out=ntl.t[:], in0=cnt.t[:], scalar1=0.0, scalar2=None, op0=ALU.is_gt), r=[cnt.b], w=[ntl.b])
        for k in range(1, 8):
            P.op("dve", lambda e, k=k: e.scalar_tensor_tensor(out=ntl.t[:], in0=cnt.t[:], scalar=float(512 * k), in1=ntl.t[:], op0=ALU.is_gt, op1=ALU.add),
                 r=[cnt.b, ntl.b], w=[ntl.b])
        P.op("dve", lambda e: e.tensor_copy(out=tend.t[:, 0:1], in_=ntl.t[:, 0:1]), r=[ntl.b], w=[tend.b])
        for k in range(1, 8):
            P.op("dve", lambda e, k=k: e.tensor_tensor(out=tend.t[:, k:k + 1], in0=tend.t[:, k - 1:k], in1=ntl.t[:, k:k + 1], op=ALU.add),
                 r=[tend.b, ntl.b], w=[tend.b])
        P.op("dve", lambda e: e.tensor_tensor(out=st512.t[:], in0=tend.t[:], in1=ntl.t[:], op=ALU.subtract), r=[tend.b, ntl.b], w=[st512.b])
        P.op("dve", lambda e: e.tensor_scalar(out=st512.t[:], in0=st512.t[:], scalar1=512.0, scalar2=None, op0=ALU.mult), r=[st512.b], w=[st512.b])
        P.op("dve", lambda e: e.tensor_tensor(out=slot.t[:].rearrange("p t e -> p (t e)"), in0=pcum.t[:, 0:256], in1=offs.t[:].rearrange("p t e -> p (t e)"), op=ALU.add),
             r=[pcum.b, offs.b], w=[slot.b])
        for k in range(8):
            P.op("dve", lambda e, k=k: e.tensor_scalar(out=slot.t[:, :, k], in0=slot.t[:, :, k], scalar1=st512.t[:, k:k + 1], scalar2=None, op0=ALU.add),
                 r=[slot.b, st512.b], w=[slot.b])
        P.op("dve", lambda e: e.tensor_tensor(out=ohb.t[:], in0=MSK.t[:], in1=OHA.t[:], op=ALU.subtract), r=[MSK.b, OHA.b], w=[ohb.b])
        P.op("dve", lambda e: e.tensor_tensor(out=tmp3.t[:], in0=slot.t[:], in1=OHA.t[:], op=ALU.mult), r=[slot.b, OHA.b], w=[tmp3.b])
        P.op("dve", lambda e: e.tensor_reduce(out=slf.t[:, 0, :], in_=tmp3.t[:], axis=AX.X, op=ALU.add), r=[tmp3.b], w=[slf.b])
        P.op("dve", lambda e: e.tensor_tensor(out=tmp3.t[:], in0=slot.t[:], in1=ohb.t[:], op=ALU.mult), r=[slot.b, ohb.b, slf.b], w=[tmp3.b])
        P.op("dve", lambda e: e.tensor_reduce(out=slf.t[:, 1, :], in_=tmp3.t[:], axis=AX.X, op=ALU.add), r=[tmp3.b], w=[slf.b])
        P.op("dve", lambda e: e.tensor_copy(out=SLI.t[:], in_=slf.t[:]), r=[slf.b], w=[SLI.b])
        P.op("dve", lambda e: e.memset(eidf.t[:], 0.0), w=[eidf.b])
        for k in range(7):
            P.op("dve", lambda e, k=k: e.scalar_tensor_tensor(out=eidf.t[:], in0=iota23.t[:], scalar=tend.t[:, k:k + 1], in1=eidf.t[:], op0=ALU.is_ge, op1=ALU.add),
                 r=[iota23.b, tend.b, eidf.b], w=[eidf.b])
        P.op("dve", lambda e: e.tensor_scalar(out=e896.t[:], in0=eidf.t[:], scalar1=896.0, scalar2=None, op0=ALU.mult), r=[eidf.b], w=[e896.b])
        P.op("dve", lambda e: e.tensor_scalar(out=e1024.t[:], in0=eidf.t[:], scalar1=1024.0, scalar2=None, op0=ALU.mult), r=[eidf.b], w=[e1024.b])
        for i in range(NTILE):
            P.op("dve", lambda e, i=i: e.tensor_scalar(out=iwgf.t[:, i, :], in0=cwg.t[:], scalar1=e896.t[:, i:i + 1], scalar2=None, op0=ALU.add),
                 r=[cwg.b, e896.b], w=[iwgf.b])
            P.op("dve", lambda e, i=i: e.tensor_scalar(out=iwdf.t[:, i, :], in0=cwd.t[:], scalar1=e1024.t[:, i:i + 1], scalar2=None, op0=ALU.add),
                 r=[cwd.b, e1024.b], w=[iwdf.b])
        P.op("dve", lambda e: e.tensor_copy(out=IWG.t[:], in_=iwgf.t[:]), r=[iwgf.b], w=[IWG.b])
        P.op("dve", lambda e: e.tensor_copy(out=IWD.t[:], in_=iwdf.t[:]), r=[iwdf.b], w=[IWD.b])
        xb = [ph.sb([128, D], BF16, "xb") for _ in range(3)]
        for tt in range(NT):
            b_ = xb[tt % 3]
            P.op("gld", lambda e, b_=b_, tt=tt: e.dma_start(out=b_.t[:], in_=x3_d.ap()[tt * 128:(tt + 1) * 128, :]), w=[b_.b])
            for ab in range(2):
                P.op("gst", lambda e, b_=b_, tt=tt, ab=ab: e.indirect_dma_start(
                    out=xs_d.ap(), out_offset=bass.IndirectOffsetOnAxis(ap=SLI.t[:, ab, tt:tt + 1], axis=0), in_=b_.t[:], in_offset=None),
                    r=[b_.b, SLI.b])
        ph.close()

    def phase_H2():
        ph = Phase(P)
        NF = 28
        xs = [[ph.sb([128, D], BF16, "xs") for _ in range(4)] for _ in range(2)]
        xsT = [ph.sb([128, 8, 512], BF16, "xsT") for _ in range(2)]
        hT = ph.sb([128, NF, 512], BF16, "hT")
        wgp = [ph.sb([128, 4096], BF16, "wgp") for _ in range(3)]
        wup = [ph.sb([128, 4096], BF16, "wup") for _ in range(3)]
        wdp = [ph.sb([128, 7, 512], BF16, "wdp") for _ in range(8)]
        sg = [ph.sb([128, 512], F32, "sg") for _ in range(2)]
        ys = [ph.sb([128, 4, D], F32, "ys") for _ in range(2)]
        ptr = [ph.ps([128, 1024], BF16, "ptrh") for _ in range(2)]
        pg = [ph.ps([128, 512], F32, "pg") for _ in range(2)]
        pu = [ph.ps([128, 512], F32, "pu") for _ in range(2)]
        py = [ph.ps([128, 512], F32, "pyh") for _ in range(2)]
        wi = 0
        gi = 0
        yi = 0
        def prefetch_loads(i):
            for j in range(4):
                P.op("ld", lambda e, j=j: e.dma_start(out=xs[i % 2][j].t[:], in_=xs_d.ap()[i * 512 + j * 128:i * 512 + (j + 1) * 128, :]),
                     w=[xs[i % 2][j].b])

        def prefetch_T(i):
            xT_ = xsT[i % 2]
            for c in range(8):
                pt = ptr[c % 2]
                for jj in range(4):
                    P.op("pe", lambda e, jj=jj: e.transpose(out=pt.t[:, jj * 128:(jj + 1) * 128], in_=xs[i % 2][jj].t[:, c * 128:(c + 1) * 128],
                                                            identity=ident_b.t[:]), r=[xs[i % 2][jj].b, ident_b.b], w=[pt.b])
                if c % 2 == 0:
                    P.op("act", lambda e: e.copy(out=xT_.t[:, c, :], in_=pt.t[:, 0:512]), r=[pt.b], w=[xT_.b])
                else:
                    P.op("dve", lambda e: e.tensor_copy(out=xT_.t[:, c, :], in_=pt.t[:, 0:512]), r=[pt.b], w=[xT_.b])

        prefetch_loads(0)
        prefetch_T(0)
        pend2 = [None]
        for i in range(NTILE):
            xs_ = xs[i % 2]; xT_ = xsT[i % 2]; ys_ = ys[i % 2]
            if i + 1 < NTILE:
                prefetch_loads(i + 1)
            if pend2[0] is not None:
                pend2[0]()
                pend2[0] = None
            for pc in range(7):
                wg_ = wgp[wi % 3]; wu_ = wup[wi % 3]; wi += 1
                P.op("gld", lambda e, wg_=wg_, pc=pc, i=i: e.indirect_dma_start(
                    out=wg_.t[:], out_offset=None, in_=wgs_d.ap(), in_offset=bass.IndirectOffsetOnAxis(ap=IWG.t[:, i, pc:pc + 1], axis=0)),
                    r=[IWG.b], w=[wg_.b])
                P.op("gld", lambda e, wu_=wu_, pc=pc, i=i: e.indirect_dma_start(
                    out=wu_.t[:], out_offset=None, in_=wus_d.ap(), in_offset=bass.IndirectOffsetOnAxis(ap=IWG.t[:, i, pc:pc + 1], axis=0)),
                    r=[IWG.b], w=[wu_.b])
                if pc == 2:
                    for k in range(8):
                        P.op("gld", lambda e, k=k, i=i: e.indirect_dma_start(
                            out=wdp[k].t[:].rearrange("p f n -> p (f n)"), out_offset=None, in_=wds_d.ap(),
                            in_offset=bass.IndirectOffsetOnAxis(ap=IWD.t[:, i, k:k + 1], axis=0)), r=[IWD.b], w=[wdp[k].b])
                for fs in range(4):
                    fc = pc * 4 + fs
                    pg_ = pg[gi % 2]; pu_ = pu[gi % 2]; sg_ = sg[gi % 2]; gi += 1
                    for c in range(8):
                        P.op("pe", lambda e, pg_=pg_, wg_=wg_, c=c, fs=fs, xT_=xT_: e.matmul(
                            pg_.t[:], wg_.t[:, c * 512 + fs * 128:c * 512 + (fs + 1) * 128], xT_.t[:, c, :], start=(c == 0), stop=(c == 7)),
                            r=[wg_.b, xT_.b], w=[pg_.b])
                    P.op("act", lambda e, pg_=pg_, sg_=sg_: e.activation(out=sg_.t[:], in_=pg_.t[:], func=AF.Silu), r=[pg_.b], w=[sg_.b])
                    for c in range(8):
                        P.op("pe", lambda e, pu_=pu_, wu_=wu_, c=c, fs=fs, xT_=xT_: e.matmul(
                            pu_.t[:], wu_.t[:, c * 512 + fs * 128:c * 512 + (fs + 1) * 128], xT_.t[:, c, :], start=(c == 0), stop=(c == 7)),
                            r=[wu_.b, xT_.b], w=[pu_.b])
                    P.op("dve", lambda e, pu_=pu_, sg_=sg_, fc=fc: e.tensor_tensor(out=hT.t[:, fc, :], in0=pu_.t[:], in1=sg_.t[:], op=ALU.mult),
                         r=[pu_.b, sg_.b], w=[hT.b])
            if i + 1 < NTILE:
                prefetch_T(i + 1)
            for ch in range(2):
                for j in range(4):
                    py_ = py[yi % 2]; yi += 1
                    for q_ in range(4):
                        wd_ = wdp[ch * 4 + q_]
                        for fl in range(7):
                            fc = q_ * 7 + fl
                            P.op("pe", lambda e, py_=py_, fc=fc, j=j, wd_=wd_, fl=fl: e.matmul(
                                py_.t[:], hT.t[:, fc, j * 128:(j + 1) * 128], wd_.t[:, fl, :], start=(fc == 0), stop=(fc == NF - 1)),
                                r=[hT.b, wd_.b], w=[py_.b])
                    if yi % 2 == 0:
                        P.op("act", lambda e, py_=py_, ys_=ys_, j=j, ch=ch: e.copy(out=ys_.t[:, j, ch * 512:(ch + 1) * 512], in_=py_.t[:]), r=[py_.b], w=[ys_.b])
                    else:
                        P.op("dve", lambda e, py_=py_, ys_=ys_, j=j, ch=ch: e.tensor_copy(out=ys_.t[:, j, ch * 512:(ch + 1) * 512], in_=py_.t[:]), r=[py_.b], w=[ys_.b])
            pend2[0] = (lambda ys_=ys_, i=i: P.op("st", lambda e: e.dma_start(out=ys_d.ap()[i * 512:(i + 1) * 512, :].rearrange("(j p) n -> p j n", p=128), in_=ys_.t[:]), r=[ys_.b]))
        pend2[0]()
        ph.close()

    def phase_H3():
        ph = Phase(P)
        gB_ = ph.sb([128, D], F32, "gB")
        bB_ = ph.sb([128, D], F32, "bB")
        P.op("ld", lambda e: e.dma_start(out=gB_.t[:], in_=bcast_row(oln2g)), w=[gB_.b])
        P.op("ld", lambda e: e.dma_start(out=bB_.t[:], in_=bcast_row(oln2b)), w=[bB_.b])
        xf = [ph.sb([128, D], F32, "xf") for _ in range(3)]
        ya = [ph.sb([128, D], F32, "ya") for _ in range(3)]
        yb = [ph.sb([128, D], F32, "yb") for _ in range(3)]
        z = [ph.sb([128, D], F32, "z") for _ in range(3)]
        xo = [ph.sb([128, D], F32, "xo") for _ in range(3)]
        st6 = [ph.sb([128, 2, 6], F32, "st6") for _ in range(3)]
        mv = [ph.sb([128, 2], F32, "mv") for _ in range(3)]
        rstd = [ph.sb([128, 2], F32, "rstd") for _ in range(3)]
        pend_h = []
        NB3 = len(xf)
        for tt in range(NT):
            i2 = tt % NB3
            x_ = xf[i2]; a_ = ya[i2]; b_ = yb[i2]; z_ = z[i2]
            P.op("ld", lambda e, x_=x_, tt=tt: e.dma_start(out=x_.t[:], in_=x3_d.ap()[tt * 128:(tt + 1) * 128, :]), w=[x_.b])
            if len(pend_h) > 0:
                pend_h.pop(0)()
            P.op("gld", lambda e, a_=a_, tt=tt: e.indirect_dma_start(out=a_.t[:], out_offset=None, in_=ys_d.ap(),
                                                                   in_offset=bass.IndirectOffsetOnAxis(ap=SLI.t[:, 0, tt:tt + 1], axis=0)), r=[SLI.b], w=[a_.b])
            P.op("gld", lambda e, b_=b_, tt=tt: e.indirect_dma_start(out=b_.t[:], out_offset=None, in_=ys_d.ap(),
                                                                   in_offset=bass.IndirectOffsetOnAxis(ap=SLI.t[:, 1, tt:tt + 1], axis=0)), r=[SLI.b], w=[b_.b])
            P.op("dve", lambda e, x_=x_, a_=a_, z_=z_, tt=tt: e.tensor_scalar(out=z_.t[:], in0=a_.t[:], scalar1=GA.t[:, tt:tt + 1], scalar2=None, op0=ALU.mult),
                 r=[a_.b, GA.b], w=[z_.b])
            P.op("dve", lambda e, b_=b_, z_=z_, tt=tt: e.scalar_tensor_tensor(out=z_.t[:], in0=b_.t[:], scalar=GB.t[:, tt:tt + 1], in1=z_.t[:], op0=ALU.mult, op1=ALU.add),
                 r=[b_.b, GB.b, z_.b], w=[z_.b])
            P.op("dve", lambda e, x_=x_, z_=z_: e.scalar_tensor_tensor(out=z_.t[:], in0=x_.t[:], scalar=ALPHA, in1=z_.t[:], op0=ALU.mult, op1=ALU.add),
                 r=[x_.b, z_.b], w=[z_.b])
            layernorm_rows(ph, z_, gB_, bB_, xo[i2], st6[i2], mv[i2], rstd[i2], None)
            pend_h.append(lambda i2=i2, tt=tt: P.op("st", lambda e: e.dma_start(out=out_d.ap()[tt * 128:(tt + 1) * 128, :], in_=xo[i2].t[:]), r=[xo[i2].b]))
        while pend_h:
            pend_h.pop(0)()
        ph.close()

    phases = [
        ("T", phase_tables),
        ("A", phase_A),
        ("B", phase_B),
        ("C", phase_C),
        ("D", lambda: phase_outproj("catT", ewout, eln1g, eln1b, x_d, x1_d)),
        ("E", phase_E),
        ("F0", lambda: phase_F(0)),
        ("F1", lambda: phase_F(1)),
        ("F2", lambda: phase_F(2)),
        ("G", lambda: phase_outproj("ug", owout, oln1g, oln1b, x2_d, x3_d)),
        ("H1", phase_H1),
        ("H2", phase_H2),
        ("H3", phase_H3),
    ]
    P.flush(barrier=True)
    for name, fn in phases:
        if name in skip:
            continue
        fn()
        if stop_after == name:
            break
    G.close()
    P.barrier(issuers=("sp",))
    return nc


INPUT_NAMES = ["x", "rel_bias", "even_w_in", "even_conv_w", "even_conv_b", "even_conv_ln_g", "even_conv_ln_b", "even_w_out",
               "even_ln1_g", "even_ln1_b", "even_ffn_wg", "even_ffn_wu", "even_ffn_wd", "even_ln2_g", "even_ln2_b",
               "odd_w_in", "odd_w_out", "odd_ln1_g", "odd_ln1_b", "odd_router", "odd_moe_wg", "odd_moe_wu", "odd_moe_wd",
               "odd_ln2_g", "odd_ln2_b"]


def make_in_maps(inputs, n_cores=8):
    cs = host_consts()
    shared = {}
    for k in INPUT_NAMES:
        if k == "x":
            continue
        a = np.ascontiguousarray(np.asarray(inputs[k], dtype=np.float32))
        if k == "rel_bias":
            shared[k] = a
        elif a.ndim >= 2 and a.shape[0] == 1:
            shared[k] = np.ascontiguousarray(a[0]) if a.ndim > 2 else a
        else:
            shared[k] = a
    for k, v in cs.items():
        shared["c_" + k] = v
    x = np.asarray(inputs["x"], dtype=np.float32)
    maps = []
    for i in range(n_cores):
        m = dict(shared)
        m["x"] = np.ascontiguousarray(x[i])
        maps.append(m)
    return maps


def kernel(**inputs):
    nc = build()
    maps = make_in_maps(inputs, 8)
    res = run_bass_kernel_spmd(nc, maps, core_ids=list(range(8)))
    out = np.stack([np.asarray(r["out"], dtype=np.float32) for r in res.results], axis=0)
    return out
```
